# Optimizing a Trainium2 kernel written in Bass

```python
import math
import jax
import jax.numpy as jnp
from jax import lax
import numpy as np

D_MODEL = 1024
BATCH = 8
SEQ = 4096
DEPTH = 2

HEAD_DIM = 64
Q_BLOCK = 128

A_HEADS = 4
A_PATTERNS = ((128, 1), (512, 4), (2048, 16))
A_GROUPS = 3
A_WIDTH = A_HEADS * HEAD_DIM

B_HEADS = 4
B_WIDTH = B_HEADS * HEAD_DIM
IDX_HEADS = 4
IDX_DIM = 64
TOPK_MAX = 256

C_HEADS = 4
C_WIDTH = C_HEADS * HEAD_DIM
C_LORA_W = 32
C_LORA_A = 32
C_LORA_G = 64
C_LORA_V = 16
C_IN = 3 * C_WIDTH + C_LORA_W + C_LORA_A + C_LORA_G
C_GN_EPS = 64e-5

D_HEADS = 4
D_INNER = D_HEADS * HEAD_DIM
D_GROUPS = 2
D_STATE = 64
D_CONV = 4
D_CHUNK = 128
D_XBC = D_INNER + 2 * D_GROUPS * D_STATE
D_NORM_EPS = 1e-5

N_BRANCH = 4
BRANCH_WIDTH = 256

N_EXPERT_GROUPS = 4
EXPERTS_PER_GROUP = 8
N_EXPERTS = N_EXPERT_GROUPS * EXPERTS_PER_GROUP
TOP_K_EXPERTS = 2
D_EXPERT = 512
MOE_BLOCK = 128

LN_EPS = 1e-5
DEEPNORM_ALPHA = (2 * DEPTH) ** 0.25
DEEPNORM_BETA = (8 * DEPTH) ** -0.25

IN_SEGMENTS = (
    ('a_qkv', 3 * A_GROUPS * A_WIDTH),
    ('b_qkv', 3 * B_WIDTH),
    ('b_idx_q', IDX_HEADS * IDX_DIM),
    ('b_idx_k', IDX_DIM),
    ('b_idx_w', IDX_HEADS),
    ('c_in', C_IN),
    ('d_z', D_INNER),
    ('d_xbc', D_XBC),
    ('d_dt', D_HEADS),
    ('gates', N_BRANCH * D_MODEL),
)
IN_WIDTH = sum(size for _, size in IN_SEGMENTS)

kernel_name = 'hybrid_gated_dilated_dsa_rwkv7_ssd_hiermoe'


def _split(x, sizes):
    out, off = [], 0
    for s in sizes:
        out.append(x[..., off:off + s])
        off += s
    return out


def layer_norm(x, g, b):
    xf = x.astype(jnp.float32)
    mu = jnp.mean(xf, axis=-1, keepdims=True)
    var = jnp.mean(jnp.square(xf - mu), axis=-1, keepdims=True)
    return ((xf - mu) * lax.rsqrt(var + LN_EPS) * g + b).astype(x.dtype)


def dilated_window_attention(qs, ks, vs):
    b, T = qs[0].shape[:2]
    scale = HEAD_DIM ** -0.5
    kps = [jnp.pad(k, ((0, 0), (w, 0), (0, 0), (0, 0))) for k, (w, _) in zip(ks, A_PATTERNS)]
    vps = [jnp.pad(v, ((0, 0), (w, 0), (0, 0), (0, 0))) for v, (w, _) in zip(vs, A_PATTERNS)]

    def block(i):
        start = i * Q_BLOCK
        outs, lses = [], []
        for q, kp, vp, (win, dil) in zip(qs, kps, vps, A_PATTERNS):
            nq, nk, span = Q_BLOCK // dil, win // dil, (win + Q_BLOCK) // dil
            qb = lax.dynamic_slice_in_dim(q, start, Q_BLOCK, axis=1).reshape(b, nq, dil, A_HEADS, HEAD_DIM)
            kb = lax.dynamic_slice_in_dim(kp, start, win + Q_BLOCK, axis=1).reshape(b, span, dil, A_HEADS, HEAD_DIM)
            vb = lax.dynamic_slice_in_dim(vp, start, win + Q_BLOCK, axis=1).reshape(b, span, dil, A_HEADS, HEAD_DIM)
            s = jnp.einsum('barhe,bcrhe->brhac', qb, kb).astype(jnp.float32) * scale
            a_idx = jnp.arange(nq)[:, None]
            c_idx = jnp.arange(span)[None, :]
            band = (c_idx >= a_idx) & (c_idx <= a_idx + nk)
            key_pos = start - win + jnp.arange(span)[None, :] * dil + jnp.arange(dil)[:, None]
            ok = band[None, :, :] & (key_pos >= 0)[:, None, :]
            s = jnp.where(ok[None, :, None], s, -jnp.inf)
            lse = jax.nn.logsumexp(s, axis=-1)
            p = jnp.exp(s - lse[..., None]).astype(vb.dtype)
            o = jnp.einsum('brhac,bcrhe->barhe', p, vb).reshape(b, Q_BLOCK, A_HEADS, HEAD_DIM)
            outs.append(o)
            lses.append(lse.transpose(0, 3, 1, 2).reshape(b, Q_BLOCK, A_HEADS))
        mix = jax.nn.softmax(jnp.stack(lses), axis=0)
        out = mix[0][..., None].astype(outs[0].dtype) * outs[0]
        for g in range(1, A_GROUPS):
            out = out + mix[g][..., None].astype(outs[g].dtype) * outs[g]
        return out

    out = lax.map(block, jnp.arange(T // Q_BLOCK))
    return out.transpose(1, 0, 2, 3, 4).reshape(b, T, A_WIDTH)


def indexer_sparse_attention(q, k, v, iq, ik, iw):
    b, T = q.shape[:2]
    top_k = min(TOPK_MAX, T // 4)
    scale = HEAD_DIM ** -0.5
    key_pos = jnp.arange(T)
    gather = jax.vmap(lambda arr, idx: arr[idx])

    def block(i):
        start = i * Q_BLOCK
        t = start + jnp.arange(Q_BLOCK)
        qi = lax.dynamic_slice_in_dim(iq, start, Q_BLOCK, axis=1)
        wi = lax.dynamic_slice_in_dim(iw, start, Q_BLOCK, axis=1).astype(jnp.float32)
        logits = jnp.einsum('bqhd,bsd->bqhs', qi, ik).astype(jnp.float32) * IDX_DIM ** -0.5
        score = jnp.einsum('bqhs,bqh->bqs', jax.nn.relu(logits), wi) * IDX_HEADS ** -0.5
        score = jnp.where((key_pos[None, :] <= t[:, None])[None], score, -jnp.inf)
        _, sel = lax.top_k(score, top_k)
        valid = sel <= t[None, :, None]
        kg = gather(k, sel)
        vg = gather(v, sel)
        qb = lax.dynamic_slice_in_dim(q, start, Q_BLOCK, axis=1)
        s = jnp.einsum('bqhe,bqjhe->bhqj', qb, kg).astype(jnp.float32) * scale
        s = jnp.where(valid[:, None], s, -jnp.inf)
        p = jax.nn.softmax(s, axis=-1).astype(vg.dtype)
        return jnp.einsum('bhqj,bqjhe->bqhe', p, vg)

    out = lax.map(block, jnp.arange(T // Q_BLOCK))
    return out.transpose(1, 0, 2, 3, 4).reshape(b, T, B_WIDTH)


def token_shift(x):
    return jnp.pad(x, ((0, 0), (1, 0), (0, 0)))[:, :-1]


def wkv7_scan(r, w, k, v, a, bb):
    b, T, H, N = r.shape

    def step(state, inp):
        rt, wt, kt, vt, at, bt = inp
        sa = jnp.einsum('bhij,bhj->bhi', state, at)
        state = state * wt[:, :, None, :] + sa[..., None] * bt[:, :, None, :] + vt[..., None] * kt[:, :, None, :]
        return state, jnp.einsum('bhij,bhj->bhi', state, rt)

    seq_major = tuple(jnp.moveaxis(t.astype(jnp.float32), 1, 0) for t in (r, w, k, v, a, bb))
    _, y = lax.scan(step, jnp.zeros((b, H, N, N), jnp.float32), seq_major)
    return jnp.moveaxis(y, 0, 1)


def rwkv7_time_mix(pc, mu, w0, w2, a0, a2, g2, k_k, k_a, r_k, gn_w, gn_b, v_first, v_lora):
    b, T, _ = pc.shape
    pc = pc + (token_shift(pc) - pc) * mu
    r, k, v, xw, xa, xg = _split(pc, (C_WIDTH, C_WIDTH, C_WIDTH, C_LORA_W, C_LORA_A, C_LORA_G))
    w = -jax.nn.softplus(-(w0 + jnp.tanh(xw) @ w2)) - 0.5
    a = jax.nn.sigmoid(a0 + xa @ a2)
    g = jax.nn.sigmoid(xg) @ g2
    if v_lora is not None:
        v0, v1, v2 = v_lora
        v = v + (v_first - v) * jax.nn.sigmoid(v0 + (v @ v1) @ v2)
    heads = lambda t: t.astype(jnp.float32).reshape(b, T, C_HEADS, HEAD_DIM)
    kk = heads(k * k_k)
    kk = kk / jnp.maximum(jnp.linalg.norm(kk, axis=-1, keepdims=True), 1e-12)
    k_mod = heads(k * (1.0 + (a - 1.0) * k_a))
    r_h, v_h, a_h = heads(r), heads(v), heads(a)
    decay = jnp.exp(-jnp.exp(heads(w)))
    y = wkv7_scan(r_h, decay, k_mod, v_h, -kk, kk * a_h)
    y_mu = jnp.mean(y, axis=-1, keepdims=True)
    y_var = jnp.mean(jnp.square(y - y_mu), axis=-1, keepdims=True)
    y = ((y - y_mu) * lax.rsqrt(y_var + C_GN_EPS)).reshape(b, T, C_WIDTH) * gn_w + gn_b
    bonus = jnp.sum(r_h * k_mod * r_k, axis=-1, keepdims=True) * v_h
    o = (y + bonus.reshape(b, T, C_WIDTH)) * g
    return o.astype(pc.dtype), v


def ssd_chunked(x, a, bm, cm):
    b, T, H, P = x.shape
    nc = T // D_CHUNK
    x = x.reshape(b, nc, D_CHUNK, H, P)
    bm = bm.reshape(b, nc, D_CHUNK, H, -1)
    cm = cm.reshape(b, nc, D_CHUNK, H, -1)
    a_cs = jnp.cumsum(a.reshape(b, nc, D_CHUNK, H).transpose(0, 3, 1, 2), axis=-1)
    seg = a_cs[..., :, None] - a_cs[..., None, :]
    causal = jnp.tril(jnp.ones((D_CHUNK, D_CHUNK), dtype=bool))
    decay_ls = jnp.exp(jnp.where(causal, seg, -jnp.inf))
    scores = jnp.einsum('bclhn,bcshn->bhcls', cm, bm) * decay_ls
    y_diag = jnp.einsum('bhcls,bcshp->bclhp', scores, x)
    decay_to_end = jnp.exp(a_cs[..., -1:] - a_cs)
    chunk_states = jnp.einsum('bclhn,bhcl,bclhp->bchpn', bm, decay_to_end, x)
    chunk_decay = jnp.exp(a_cs[..., -1])

    def step(state, inp):
        st, dc = inp
        return state * dc[..., None, None] + st, state

    _, states_in = lax.scan(step, jnp.zeros((b, H, P, bm.shape[-1]), jnp.float32),
                            (jnp.moveaxis(chunk_states, 1, 0), jnp.moveaxis(chunk_decay, 2, 0)))
    states_in = jnp.moveaxis(states_in, 0, 1)
    y_off = jnp.einsum('bclhn,bchpn,bhcl->bclhp', cm, states_in, jnp.exp(a_cs))
    return (y_diag + y_off).reshape(b, T, H, P)


def mamba2_mixer(z, xbc, dt_raw, conv_w, conv_b, dt_bias, a_log, d_skip, norm_w):
    b, T, _ = z.shape
    xbc = lax.conv_general_dilated(xbc, conv_w[:, None, :], window_strides=(1,),
                                   padding=[(D_CONV - 1, 0)],
                                   dimension_numbers=('NWC', 'WIO', 'NWC'),
                                   feature_group_count=D_XBC)
    xbc = jax.nn.silu(xbc + conv_b).astype(jnp.float32)
    heads_per_group = D_HEADS // D_GROUPS
    xs = xbc[..., :D_INNER].reshape(b, T, D_HEADS, HEAD_DIM)
    bm = xbc[..., D_INNER:D_INNER + D_GROUPS * D_STATE].reshape(b, T, D_GROUPS, D_STATE)
    cm = xbc[..., D_INNER + D_GROUPS * D_STATE:].reshape(b, T, D_GROUPS, D_STATE)
    bm = jnp.repeat(bm, heads_per_group, axis=2)
    cm = jnp.repeat(cm, heads_per_group, axis=2)
    dt = jax.nn.softplus(dt_raw.astype(jnp.float32) + dt_bias.astype(jnp.float32))
    a = -jnp.exp(a_log.astype(jnp.float32))
    y = ssd_chunked(xs * dt[..., None], dt * a, bm, cm) + d_skip.astype(jnp.float32)[:, None] * xs
    y = y.reshape(b, T, D_INNER) * jax.nn.silu(z.astype(jnp.float32))
    y = y.reshape(b, T, D_GROUPS, D_INNER // D_GROUPS)
    y = y * lax.rsqrt(jnp.mean(jnp.square(y), axis=-1, keepdims=True) + D_NORM_EPS)
    return (y.reshape(b, T, D_INNER) * norm_w).astype(z.dtype)


def grouped_expert_ffn(x2d, expert, gate, w_gate, w_up, w_down):
    m, dm = x2d.shape
    n_assign = m * TOP_K_EXPERTS
    flat_e = expert.reshape(-1)
    flat_tok = jnp.arange(n_assign) // TOP_K_EXPERTS
    flat_gate = gate.reshape(-1)
    order = jnp.argsort(flat_e)
    se, stok, sgate = flat_e[order], flat_tok[order], flat_gate[order]
    counts = jnp.bincount(flat_e, length=N_EXPERTS)
    padded = (counts + MOE_BLOCK - 1) // MOE_BLOCK * MOE_BLOCK
    pad_end = jnp.cumsum(padded)
    pad_start = pad_end - padded
    start = jnp.cumsum(counts) - counts
    dest = pad_start[se] + jnp.arange(n_assign) - start[se]
    cap = (n_assign + N_EXPERTS * (MOE_BLOCK - 1) + MOE_BLOCK - 1) // MOE_BLOCK * MOE_BLOCK
    n_blocks = cap // MOE_BLOCK
    buf_tok = jnp.full((cap,), m, dtype=jnp.int32).at[dest].set(stok)
    x_ext = jnp.concatenate([x2d, jnp.zeros((1, dm), x2d.dtype)], axis=0)
    xb = x_ext[buf_tok].reshape(n_blocks, MOE_BLOCK, dm)
    blk_expert = jnp.minimum(jnp.searchsorted(pad_end, jnp.arange(n_blocks) * MOE_BLOCK, side='right'),
                             N_EXPERTS - 1)

    def expert_block(args):
        xblk, e = args
        h = jax.nn.silu(xblk @ w_gate[e]) * (xblk @ w_up[e])
        return h @ w_down[e]

    yb = lax.map(expert_block, (xb, blk_expert)).reshape(cap, dm)
    y = yb[dest] * sgate[:, None]
    return jnp.zeros_like(x2d).at[stok].add(y)


def hierarchical_moe(x2d, wg, bg, we, be, w_gate, w_up, w_down):
    m = x2d.shape[0]
    xf = x2d.astype(jnp.float32)
    p_group = jax.nn.softmax(xf @ wg.astype(jnp.float32) + bg.astype(jnp.float32), axis=-1)
    pg_top, g_sel = lax.top_k(p_group, 1)
    logits_e = (xf @ we.astype(jnp.float32) + be.astype(jnp.float32)).reshape(m, N_EXPERT_GROUPS, EXPERTS_PER_GROUP)
    logits_e = logits_e[jnp.arange(m), g_sel[:, 0]]
    pe_top, e_sel = lax.top_k(jax.nn.softmax(logits_e, axis=-1), TOP_K_EXPERTS)
    gate = pg_top * pe_top / jnp.sum(pe_top, axis=-1, keepdims=True)
    expert = g_sel * EXPERTS_PER_GROUP + e_sel
    return grouped_expert_ffn(x2d, expert, gate.astype(x2d.dtype), w_gate, w_up, w_down)


def setup_inputs(seed: int = 0) -> dict:
    key = jax.random.key(seed)
    ks = iter(jax.random.split(key, 48))
    L = DEPTH

    def nrm(shape, scale):
        return jax.random.normal(next(ks), shape, jnp.float32) * scale

    def unif(shape, lo, hi):
        return jax.random.uniform(next(ks), shape, jnp.float32, minval=lo, maxval=hi)

    dt = jnp.exp(unif((L, D_HEADS), math.log(1e-3), math.log(1e-1)))
    return {
        'x': nrm((BATCH, SEQ, D_MODEL), 1.0),
        'w_in': nrm((L, D_MODEL, IN_WIDTH), D_MODEL ** -0.5),
        'c_mu': unif((L, C_IN), 0.0, 1.0),
        'c_w0': unif((L, C_WIDTH), -6.0, -1.0),
        'c_w2': nrm((L, C_LORA_W, C_WIDTH), 0.1),
        'c_a0': nrm((L, C_WIDTH), 0.1),
        'c_a2': nrm((L, C_LORA_A, C_WIDTH), C_LORA_A ** -0.5),
        'c_g2': nrm((L, C_LORA_G, C_WIDTH), C_LORA_G ** -0.5),
        'c_kk': 0.85 + nrm((L, C_WIDTH), 0.02),
        'c_ka': 1.0 + nrm((L, C_WIDTH), 0.02),
        'c_rk': nrm((L, C_HEADS, HEAD_DIM), 0.1),
        'c_gn_w': 1.0 + nrm((L, C_WIDTH), 0.02),
        'c_gn_b': nrm((L, C_WIDTH), 0.02),
        'c_v0': nrm((L - 1, C_WIDTH), 0.1),
        'c_v1': nrm((L - 1, C_WIDTH, C_LORA_V), C_WIDTH ** -0.5),
        'c_v2': nrm((L - 1, C_LORA_V, C_WIDTH), C_LORA_V ** -0.5),
        'd_conv_w': nrm((L, D_CONV, D_XBC), D_CONV ** -0.5),
        'd_conv_b': nrm((L, D_XBC), 0.02),
        'd_dt_bias': dt + jnp.log(-jnp.expm1(-dt)),
        'd_a_log': jnp.log(unif((L, D_HEADS), 1.0, 16.0)),
        'd_skip': 1.0 + nrm((L, D_HEADS), 0.02),
        'd_norm_w': 1.0 + nrm((L, D_INNER), 0.02),
        'w_branch': nrm((L, N_BRANCH, BRANCH_WIDTH, D_MODEL), BRANCH_WIDTH ** -0.5),
        'w_out': nrm((L, D_MODEL, D_MODEL), D_MODEL ** -0.5 * DEEPNORM_BETA),
        'ln1_g': 1.0 + nrm((L, D_MODEL), 0.02),
        'ln1_b': nrm((L, D_MODEL), 0.02),
        'r_group': nrm((L, D_MODEL, N_EXPERT_GROUPS), D_MODEL ** -0.5),
        'r_group_b': nrm((L, N_EXPERT_GROUPS), 0.01),
        'r_expert': nrm((L, D_MODEL, N_EXPERTS), D_MODEL ** -0.5),
        'r_expert_b': nrm((L, N_EXPERTS), 0.01),
        'e_gate': nrm((L, N_EXPERTS, D_MODEL, D_EXPERT), D_MODEL ** -0.5),
        'e_up': nrm((L, N_EXPERTS, D_MODEL, D_EXPERT), D_MODEL ** -0.5),
        'e_down': nrm((L, N_EXPERTS, D_EXPERT, D_MODEL), D_EXPERT ** -0.5 * DEEPNORM_BETA),
        'ln2_g': 1.0 + nrm((L, D_MODEL), 0.02),
        'ln2_b': nrm((L, D_MODEL), 0.02),
    }


def reference(x, w_in, c_mu, c_w0, c_w2, c_a0, c_a2, c_g2, c_kk, c_ka, c_rk, c_gn_w, c_gn_b,
              c_v0, c_v1, c_v2, d_conv_w, d_conv_b, d_dt_bias, d_a_log, d_skip, d_norm_w,
              w_branch, w_out, ln1_g, ln1_b, r_group, r_group_b, r_expert, r_expert_b,
              e_gate, e_up, e_down, ln2_g, ln2_b):
    b, T, _ = x.shape
    v_first = None
    for l in range(DEPTH):
        seg, off = {}, 0
        for name, size in IN_SEGMENTS:
            seg[name] = x @ w_in[l, :, off:off + size]
            off += size

        a_qkv = seg['a_qkv'].reshape(b, T, 3, A_GROUPS, A_HEADS, HEAD_DIM)
        o_a = dilated_window_attention([a_qkv[:, :, 0, g] for g in range(A_GROUPS)],
                                       [a_qkv[:, :, 1, g] for g in range(A_GROUPS)],
                                       [a_qkv[:, :, 2, g] for g in range(A_GROUPS)])

        b_qkv = seg['b_qkv'].reshape(b, T, 3, B_HEADS, HEAD_DIM)
        o_b = indexer_sparse_attention(b_qkv[:, :, 0], b_qkv[:, :, 1], b_qkv[:, :, 2],
                                       seg['b_idx_q'].reshape(b, T, IDX_HEADS, IDX_DIM),
                                       seg['b_idx_k'], seg['b_idx_w'])

        v_lora = None if l == 0 else (c_v0[l - 1], c_v1[l - 1], c_v2[l - 1])
        o_c, v_c = rwkv7_time_mix(seg['c_in'], c_mu[l], c_w0[l], c_w2[l], c_a0[l], c_a2[l], c_g2[l],
                                  c_kk[l], c_ka[l], c_rk[l], c_gn_w[l], c_gn_b[l], v_first, v_lora)
        if l == 0:
            v_first = v_c

        o_d = mamba2_mixer(seg['d_z'], seg['d_xbc'], seg['d_dt'], d_conv_w[l], d_conv_b[l],
                           d_dt_bias[l], d_a_log[l], d_skip[l], d_norm_w[l])

        gates = jax.nn.sigmoid(seg['gates'].reshape(b, T, N_BRANCH, D_MODEL))
        merged = jnp.zeros_like(x)
        for n, o_n in enumerate((o_a, o_b, o_c, o_d)):
            merged = merged + gates[:, :, n] * (o_n @ w_branch[l, n])
        x = layer_norm(DEEPNORM_ALPHA * x + merged @ w_out[l], ln1_g[l], ln1_b[l])

        y = hierarchical_moe(x.reshape(b * T, D_MODEL), r_group[l], r_group_b[l], r_expert[l],
                             r_expert_b[l], e_gate[l], e_up[l], e_down[l])
        x = layer_norm(DEEPNORM_ALPHA * x + y.reshape(b, T, D_MODEL), ln2_g[l], ln2_b[l])
    return x
```

```python
import numpy as np
from contextlib import ExitStack
import concourse.bass as bass
import concourse.mybir as mybir
from concourse.bass_utils import run_bass_kernel_spmd

F32 = mybir.dt.float32
BF16 = mybir.dt.bfloat16
AF = mybir.ActivationFunctionType
ALU = mybir.AluOpType
AX = mybir.AxisListType

ENG = ('pe', 'act', 'dve', 'pool', 'sp')
SAME_ENG_SYNC = True


_UID = [0]


def _sbt(nc, name, shape, dtype):
    _UID[0] += 1
    return nc.sbuf_tensor('%s_u%d' % (name, _UID[0]), shape, dtype)


class K:
    def __init__(self, nc):
        self.nc = nc
        self.eng = {'pe': nc.tensor, 'act': nc.scalar, 'dve': nc.vector,
                    'pool': nc.gpsimd, 'sp': nc.sync}
        self.sem = {e: nc.alloc_semaphore('s_' + e) for e in ENG}
        self.cnt = {e: 0 for e in ENG}
        self.known = {e: {} for e in ENG}
        self.res = {}
        self.NDS = 8
        self.dq = ('sp', 'act', 'pool')
        self.dsem = {q: [nc.alloc_semaphore('d_%s_%d' % (q, i)) for i in range(self.NDS)]
                     for q in self.dq}
        self.dcnt = {q: 0 for q in self.dq}
        self.semobj = {}
        for e in ENG:
            self.semobj['E' + e] = self.sem[e]
        for q in self.dq:
            for i in range(self.NDS):
                self.semobj['D%s%d' % (q, i)] = self.dsem[q][i]
        self.ninst = 0

    def _collect(self, reads, writes):
        need = {}
        for r in reads:
            st = self.res.get(r)
            if st is not None and st[0] is not None:
                k, v = st[0]
                if need.get(k, 0) < v:
                    need[k] = v
        for w in writes:
            st = self.res.get(w)
            if st is not None:
                if st[0] is not None:
                    k, v = st[0]
                    if need.get(k, 0) < v:
                        need[k] = v
                for k, v in st[1].items():
                    if need.get(k, 0) < v:
                        need[k] = v
        return need

    def _wait(self, e, need):
        kn = self.known[e]
        for k, v in need.items():
            if k == 'E' + e and (e == 'pe' or not SAME_ENG_SYNC):
                continue
            if kn.get(k, 0) < v:
                self.eng[e].wait_ge(self.semobj[k], v)
                kn[k] = v
                self.ninst += 1

    def _record(self, ev, reads, writes):
        for w in writes:
            self.res[w] = [ev, {}]
        for r in reads:
            st = self.res.get(r)
            if st is None:
                st = [None, {}]
                self.res[r] = st
            k, v = ev
            if st[1].get(k, 0) < v:
                st[1][k] = v

    def op(self, e, fn, reads=(), writes=()):
        self._wait(e, self._collect(reads, writes))
        inst = fn(self.eng[e])
        self.cnt[e] += 1
        inst.then_inc(self.sem[e], 1)
        self.ninst += 1
        self._record(('E' + e, self.cnt[e]), reads, writes)

    def dma(self, q, out, in_, reads=(), writes=(), **kw):
        need = self._collect(reads, writes)
        n = self.dcnt[q]
        slot, rnd = n % self.NDS, n // self.NDS
        key = 'D%s%d' % (q, slot)
        if rnd > 0 and need.get(key, 0) < 16 * rnd:
            need[key] = 16 * rnd
        self._wait(q, need)
        inst = self.eng[q].dma_start(out=out, in_=in_, **kw)
        inst.then_inc(self.dsem[q][slot], 16)
        self.dcnt[q] = n + 1
        self.ninst += 1
        self._record((key, 16 * (rnd + 1)), reads, writes)

    def all_events(self):
        need = {}
        for e in ENG:
            if self.cnt[e] > 0:
                need['E' + e] = self.cnt[e]
        for q in self.dq:
            n = self.dcnt[q]
            for slot in range(self.NDS):
                uses = (n - slot + self.NDS - 1) // self.NDS if n > slot else 0
                if uses > 0:
                    need['D%s%d' % (q, slot)] = 16 * uses
        return need

    def barrier(self, engines=ENG):
        need = self.all_events()
        for e in engines:
            self._wait(e, dict(need))
        self.res = {}


D_MODEL = 1024
IN_W = 9160
OFF = dict(a_qkv=0, b_qkv=2304, biq=3072, bik=3328, biw=3392, c_in=3396, d_z=4292, d_xbc=4548,
           d_dt=5060, gates=5064)
A_PAT = ((128, 1), (512, 4), (2048, 16))


class Ctx:
    pass


def phase_x(k, c, x_dram, T, xT=None):
    nc = k.nc
    NB = T // 128
    if xT is None:
        xT = c.xT
    with ExitStack() as st:
        xs = [st.enter_context(_sbt(nc, 'xs%d' % i, [128, 1024], F32)) for i in range(2)]
        for b in range(NB):
            s = xs[b % 2]
            k.dma('sp' if b % 2 == 0 else 'act', s[:, :], x_dram[b * 128:(b + 1) * 128, :],
                  writes=[('xs', b % 2)])
            for half in range(2):
                bank = (2 * b + half) % 8
                ps = c.ps[bank]
                for j in range(4):
                    ch = half * 4 + j
                    k.op('pe', lambda e, ps=ps, j=j, ch=ch, s=s: e.transpose(
                        out=ps[:, j * 128:(j + 1) * 128], in_=s[:, ch * 128:(ch + 1) * 128],
                        identity=c.ident[:, :]),
                        reads=[('xs', b % 2)], writes=[('ps', bank)])
                dst = xT[:, half * 4:(half + 1) * 4, b * 128:(b + 1) * 128]
                src_ = ps[:, :].rearrange('p (j n) -> p j n', j=4)
                if half == 0:
                    k.op('act', lambda e, dst=dst, src_=src_: e.copy(out=dst, in_=src_),
                         reads=[('ps', bank)], writes=[('xT', b)])
                else:
                    k.op('dve', lambda e, dst=dst, src_=src_: e.tensor_copy(out=dst, in_=src_),
                         reads=[('ps', bank)], writes=[('xT', b)])
        k.barrier()


class WLoader:
    def __init__(self, k, st, name, width=512, nbuf=2):
        nc = k.nc
        self.k = k
        self.name = name
        self.nbuf = nbuf
        self.f = [st.enter_context(_sbt(nc, '%s_f%d' % (name, i), [128, 8, width], F32)) for i in range(nbuf)]
        self.b = [st.enter_context(_sbt(nc, '%s_b%d' % (name, i), [128, 8, width], BF16)) for i in range(nbuf)]
        self.n = 0

    def load(self, w_dram, col0, ncols, q='sp', cast='pool'):
        k = self.k
        i = self.n % self.nbuf
        self.n += 1
        src = w_dram[:, col0:col0 + ncols].rearrange('(ko ki) n -> ki ko n', ki=128)
        k.dma(q, self.f[i][:, :, 0:ncols], src, writes=[(self.name + 'f', i)])
        fi, bi = self.f[i], self.b[i]
        if cast == 'pool':
            k.op('pool', lambda e: e.tensor_copy(out=bi[:, :, 0:ncols], in_=fi[:, :, 0:ncols]),
                 reads=[(self.name + 'f', i)], writes=[(self.name + 'b', i)])
        else:
            k.op('dve', lambda e: e.tensor_copy(out=bi[:, :, 0:ncols], in_=fi[:, :, 0:ncols]),
                 reads=[(self.name + 'f', i)], writes=[(self.name + 'b', i)])
        return bi, (self.name + 'b', i)


def phase_p(k, c, w_in, T, only=None):
    nc = k.nc
    NB = T // 128
    NG = T // 512
    d = c.d
    with ExitStack() as st:
        wl = WLoader(k, st, 'wl')
        stg = [st.enter_context(_sbt(nc, 'pstg%d' % i, [128, T], F32)) for i in range(2)]
        stgb = [st.enter_context(_sbt(nc, 'pstgb%d' % i, [128, T], BF16)) for i in range(2)]
        tst = [st.enter_context(_sbt(nc, 'ptst%d' % i, [128, 512], F32)) for i in range(2)]
        tstb = [st.enter_context(_sbt(nc, 'ptstb%d' % i, [128, 512], BF16)) for i in range(2)]
        cnt = {'bank': 0, 'fm': 0, 'tm': 0, 'ev': 0}

        def evac(dst, src, bank, wkey, func=None, scale=1.0):
            cnt['ev'] += 1
            if func is not None or cnt['ev'] % 2 == 0:
                f = func if func is not None else AF.Copy
                k.op('act', lambda e: e.activation(out=dst, in_=src, func=f, scale=scale),
                     reads=[('ps', bank)], writes=[wkey])
            else:
                k.op('dve', lambda e: e.tensor_scalar(out=dst, in0=src, scalar1=float(scale), scalar2=None,
                                                      op0=ALU.mult),
                     reads=[('ps', bank)], writes=[wkey])

        def fm_group(col0, total, cw, dst_fn, bf, pad=0, scale=1.0):
            for s0 in range(0, total, 512):
                sw = min(512, total - s0)
                wt, wkey = wl.load(w_in, col0 + s0, sw)
                for j0 in range(0, sw, cw):
                    i = cnt['fm'] % 2
                    cnt['fm'] += 1
                    sg = stgb[i] if bf else stg[i]
                    skey = ('pstgb' if bf else 'pstg', i)
                    for g in range(NG):
                        bank = cnt['bank'] % 8
                        cnt['bank'] += 1
                        ps = c.ps[bank]
                        for kk in range(8):
                            k.op('pe', lambda e, ps=ps, kk=kk, j0=j0, g=g: e.matmul(
                                ps[0:cw, :], lhsT=wt[:, kk, j0:j0 + cw], rhs=c.xT[:, kk, g * 512:(g + 1) * 512],
                                start=(kk == 0), stop=(kk == 7)),
                                reads=[wkey, ('xT', 0)], writes=[('ps', bank)])
                        evac(sg[0:cw, g * 512:(g + 1) * 512], ps[0:cw, :], bank, skey, scale=scale)
                    k.dma('sp' if cnt['fm'] % 2 else 'act', dst_fn((s0 + j0) // cw), sg[0:cw, :], reads=[skey])

        def tm_group(col0, ncols, dst_fn, bf, func=None, tokens=None, nblk=None):
            wt, wkey = wl.load(w_in, col0, ncols)
            for b in range(nblk if nblk is not None else NB):
                tok = tokens(b) if tokens is not None else slice(b * 128, (b + 1) * 128)
                bank = cnt['bank'] % 8
                cnt['bank'] += 1
                ps = c.ps[bank]
                for kk in range(8):
                    k.op('pe', lambda e, ps=ps, kk=kk, tok=tok: e.matmul(
                        ps[:, 0:ncols], lhsT=c.xT[:, kk, tok], rhs=wt[:, kk, 0:ncols],
                        start=(kk == 0), stop=(kk == 7)),
                        reads=[wkey, ('xT', 0)], writes=[('ps', bank)])
                i = cnt['tm'] % 2
                cnt['tm'] += 1
                sg = tstb[i] if bf else tst[i]
                skey = ('ptstb' if bf else 'ptst', i)
                evac(sg[:, 0:ncols], ps[:, 0:ncols], bank, skey, func=func)
                k.dma('sp' if cnt['tm'] % 2 else 'act', dst_fn(b), sg[:, 0:ncols], reads=[skey])

        def want(n):
            return only is None or n in only

        if want('a'):
            fm_group(OFF['a_qkv'], 768, 64, lambda j: d['aq'][j], True)
            fm_group(OFF['a_qkv'] + 768, 768, 64, lambda j: d['ak'][j], True)
            for g, (win, dil) in enumerate(A_PAT):
                nbc = T // (128 * dil)

                def toks(b, dil=dil, nbc=nbc):
                    r, bi = b // nbc, b % nbc
                    s0 = r + dil * 128 * bi
                    return slice(s0, s0 + dil * 127 + 1, dil)
                tm_group(OFF['a_qkv'] + 1536 + g * 256, 256, lambda b, g=g: d['av'][g, b], True, tokens=toks)
        if want('b'):
            fm_group(OFF['b_qkv'], 256, 64, lambda j: d['bq'][j], True)
            fm_group(OFF['b_qkv'] + 256, 256, 64, lambda j: d['bk'][j], True)
            tm_group(OFF['b_qkv'] + 512, 256, lambda b: d['bv'][b], True)
            fm_group(OFF['biq'], 256, 64, lambda j: d['biq'][j], True)
            fm_group(OFF['bik'], 64, 64, lambda j: d['bik'][j], True)
            tm_group(OFF['biw'], 4, lambda b: d['biw'][b], False)
        if want('c'):
            fm_group(OFF['c_in'], 896, 64, lambda j: d['cpc'][j, :, 1:T + 1], False)
        if want('d'):
            fm_group(OFF['d_z'], 256, 64, lambda j: d['dz'][j], False)
            fm_group(OFF['d_xbc'], 512, 64, lambda j: d['dxbc'][j, :, 3:T + 3], False)
            tm_group(OFF['d_dt'], 4, lambda b: d['ddt'][b], False)
        if want('g'):
            for s in range(8):
                tm_group(OFF['gates'] + s * 512, 512, lambda b, s=s: d['gsig'][b, :, s * 512:(s + 1) * 512], True,
                         func=AF.Sigmoid)
        k.barrier()


def alloc_scratch(nc, c, T, debug=False):
    NB = T // 128
    kind = 'ExternalOutput' if debug else 'Internal'
    d = {}

    def dt(name, shape, dtype):
        d[name] = nc.dram_tensor(name, shape, dtype, kind=kind).ap()
    dt('aq', [12, 64, T], BF16)
    dt('ak', [12, 64, T], BF16)
    dt('av', [3, NB, 128, 256], BF16)
    dt('bq', [4, 64, T], BF16)
    dt('bk', [4, 64, T], BF16)
    dt('bv', [NB, 128, 256], BF16)
    dt('biq', [4, 64, T], BF16)
    dt('bik', [1, 64, T], BF16)
    dt('biw', [NB, 128, 4], F32)
    dt('cpc', [14, 64, T + 1], F32)
    dt('dz', [4, 64, T], F32)
    dt('dxbc', [8, 64, T + 3], F32)
    dt('ddt', [NB, 128, 4], F32)
    dt('gsig', [NB, 128, 4096], BF16)
    dt('dxs', [4, 64, T], F32)
    dt('vfirst', [4, 64, T], F32)
    dt('obr', [4, 4, 64, T], BF16)
    c.d = d


def setup_common(nc, k, c, T, st):
    c.ps = [nc.alloc_psum_tensor('ps%d' % i, [128, 512], F32) for i in range(8)]
    c.ident = st.enter_context(_sbt(nc, 'ident_sb', [128, 128], F32))
    k.dma('sp', c.ident[:, :], c.cin['ident'], writes=[('ident', 0)])
    tmp = st.enter_context(_sbt(nc, 'cst_tmp', [128, 256], F32))
    c.maskA = st.enter_context(_sbt(nc, 'maskA_sb', [128, 256], BF16))
    k.dma('sp', tmp[:, :], c.cin['maskA'], writes=[('cst_tmp', 0)])
    k.op('dve', lambda e: e.tensor_copy(out=c.maskA[:, :], in_=tmp[:, :]), reads=[('cst_tmp', 0)],
         writes=[('maskA', 0)])
    c.negmask = st.enter_context(_sbt(nc, 'negmask_sb', [128, 128], F32))
    k.dma('act', c.negmask[:, :], c.cin['negmask'], writes=[('negmask', 0)])
    c.triu = st.enter_context(_sbt(nc, 'triu_sb', [128, 128], F32))
    k.dma('pool', c.triu[:, :], c.cin['triu'], writes=[('triu', 0)])
    c.ones_f = st.enter_context(_sbt(nc, 'ones_f_sb', [128, 128], F32))
    k.dma('sp', c.ones_f[:, :], c.cin['ones_f'], writes=[('ones_f', 0)])
    c.eps5 = st.enter_context(_sbt(nc, 'eps5_sb', [128, 1], F32))
    k.dma('act', c.eps5[:, :], c.cin['eps5'], writes=[('eps', 0)])
    c.mhalf = st.enter_context(_sbt(nc, 'mhalf_sb', [128, 1], F32))
    k.dma('pool', c.mhalf[:, :], c.cin['mhalf'], writes=[('mhalf', 0)])
    c.epsgn = st.enter_context(_sbt(nc, 'epsgn_sb', [128, 1], F32))
    k.dma('sp', c.epsgn[:, :], c.cin['epsgn'], writes=[('epsgn', 0)])
    c.ones_bf = st.enter_context(_sbt(nc, 'ones_bf', [128, 128], BF16))
    k.op('dve', lambda e: e.memset(c.ones_bf[:, :], 1.0), writes=[('ones_bf', 0)])
    k.barrier()


def mixer_a(k, c, T):
    nc = k.nc
    NB = T // 128
    d = c.d
    with ExitStack() as st:
        vall = st.enter_context(_sbt(nc, 'a_v', [128, 3, NB, 256], BF16))
        for g in range(3):
            k.dma(('sp', 'act', 'pool')[g], vall[:, g, :, :], d['av'][g].rearrange('b p c -> p b c'),
                  writes=[('a_v', g)])
        qk = [[st.enter_context(_sbt(nc, 'a_qk%d%d' % (g, s), [64, T], BF16)) for s in range(2)]
              for g in range(3)]
        uz = st.enter_context(_sbt(nc, 'a_uz', [64, 2, T], F32))
        rz = st.enter_context(_sbt(nc, 'a_rz', [64, T], F32))
        ob = st.enter_context(_sbt(nc, 'a_ob', [64, T], BF16))
        Eb = [st.enter_context(_sbt(nc, 'a_E%d' % i, [128, 256], BF16)) for i in range(2)]
        Pb = [st.enter_context(_sbt(nc, 'a_P%d' % i, [128, 256], BF16)) for i in range(2)]
        it = 0
        for h in range(4):
            for g in range(3):
                k.dma('sp', qk[g][0][:, :], d['aq'][g * 4 + h], writes=[('a_q', g)])
                k.dma('act', qk[g][1][:, :], d['ak'][g * 4 + h], writes=[('a_k', g)])
            for g, (win, dil) in enumerate(A_PAT):
                nbc = T // (128 * dil)
                qT, kT = qk[g]
                for blk in range(NB):
                    r, bi = blk // nbc, blk % nbc
                    s0 = r + dil * 128 * bi
                    qs = slice(s0, s0 + dil * 127 + 1, dil)
                    bs, bu = it % 4, 4 + it % 4
                    ps_s, ps_u = c.ps[bs], c.ps[bu]
                    i2 = it % 2
                    it += 1
                    lo = 128 if bi == 0 else 0
                    tiles = ([] if bi == 0 else [(0, blk - 1, s0 - dil * 128)]) + [(1, blk, s0)]
                    for slot, kb, ks0 in tiles:
                        ks = slice(ks0, ks0 + dil * 127 + 1, dil)
                        k.op('pe', lambda e, slot=slot, ks=ks: e.matmul(
                            ps_s[:, slot * 128:(slot + 1) * 128], lhsT=kT[:, ks], rhs=qT[:, qs],
                            start=True, stop=True),
                            reads=[('a_q', g), ('a_k', g)], writes=[('ps', bs)])
                    E, P = Eb[i2], Pb[i2]
                    k.op('act', lambda e: e.activation(out=E[:, lo:256], in_=ps_s[:, lo:256], func=AF.Exp,
                                                       scale=0.125),
                         reads=[('ps', bs)], writes=[('a_E', i2)])
                    k.op('dve', lambda e: e.tensor_tensor(out=P[:, lo:256], in0=E[:, lo:256],
                                                          in1=c.maskA[:, lo:256], op=ALU.mult),
                         reads=[('a_E', i2), ('maskA', 0)], writes=[('a_P', i2)])
                    for ti, (slot, kb, ks0) in enumerate(tiles):
                        first, last = ti == 0, ti == len(tiles) - 1
                        k.op('pe', lambda e, slot=slot, kb=kb, first=first, last=last: e.matmul(
                            ps_u[0:64, 0:128], lhsT=vall[:, g, kb, h * 64:(h + 1) * 64],
                            rhs=P[:, slot * 128:(slot + 1) * 128], start=first, stop=last,
                            skip_group_check=True),
                            reads=[('a_P', i2), ('a_v', g)], writes=[('ps', bu)])
                        k.op('pe', lambda e, slot=slot, first=first, last=last: e.matmul(
                            ps_u[0:64, 128:256], lhsT=c.ones_bf[:, 0:64],
                            rhs=P[:, slot * 128:(slot + 1) * 128], start=False, stop=last,
                            skip_group_check=True),
                            reads=[('a_P', i2), ('ones_bf', 0)], writes=[('ps', bu)])
                    dst = uz[:, :, qs]
                    src = ps_u[0:64, 0:256].rearrange('p (a n) -> p a n', a=2)
                    if g == 0:
                        k.op('act', lambda e, dst=dst, src=src: e.copy(out=dst, in_=src),
                             reads=[('ps', bu)], writes=[('a_uz', 0)])
                    else:
                        k.op('dve', lambda e, dst=dst, src=src: e.tensor_tensor(out=dst, in0=dst, in1=src,
                                                                                op=ALU.add),
                             reads=[('ps', bu), ('a_uz', 0)], writes=[('a_uz', 0)])
            k.op('dve', lambda e: e.reciprocal(out=rz[:, :], in_=uz[:, 1, :]), reads=[('a_uz', 0)],
                 writes=[('a_rz', 0)])
            k.op('dve', lambda e: e.tensor_tensor(out=ob[:, :], in0=uz[:, 0, :], in1=rz[:, :], op=ALU.mult),
                 reads=[('a_uz', 0), ('a_rz', 0)], writes=[('a_ob', 0)])
            k.dma('sp', d['obr'][0, h], ob[:, :], reads=[('a_ob', 0)])
        k.barrier()


def make_consts():
    j = np.arange(128)[:, None]
    i = np.arange(128)[None, :]
    cs = {}
    cs['ident'] = np.eye(128, dtype=np.float32)
    cs['maskA'] = np.concatenate([(j >= i), (j <= i)], axis=1).astype(np.float32)
    cs['triu'] = (j <= i).astype(np.float32)
    cs['ones_f'] = np.ones((128, 128), np.float32)
    cs['eps5'] = np.full((128, 1), 1e-5, np.float32)
    cs['mhalf'] = np.full((128, 1), -0.5, np.float32)
    cs['epsgn'] = np.full((128, 1), 64e-5, np.float32)
    s_ = np.arange(64)[:, None]
    t_ = np.arange(64)[None, :]
    cs['cmask'] = np.stack([(s_ < t_), (s_ <= t_), (s_ > t_)], axis=1).astype(np.float32)
    cs['negmask'] = np.where(i <= j, 0.0, -1.0e30).astype(np.float32)
    return cs


NEG = -1.0e30


def mixer_b(k, c, T):
    nc = k.nc
    NB = T // 128
    d = c.d
    with ExitStack() as st:
        def sb(name, shape, dt_):
            return st.enter_context(_sbt(nc, name, shape, dt_))
        bqs = [sb('b_q%d' % i, [64, 4, 128], BF16) for i in range(2)]
        bk = sb('b_k', [64, 4, T], BF16)
        biqs = [sb('b_iq%d' % i, [64, 4, 128], BF16) for i in range(2)]
        bik = sb('b_ik', [64, T], BF16)
        bv = sb('b_v', [128, NB, 256], BF16)
        biw = sb('b_iw', [128, NB, 4], F32)
        score = sb('b_score', [128, T], F32)
        work = sb('b_work', [128, T], F32)
        cum = sb('b_cum', [128, T], F32)
        ngt = sb('b_ngt', [128, 2], F32)
        selT = sb('b_selT', [128, NB, 128], BF16)
        rt = [sb('b_rt%d' % i, [128, 512], F32) for i in range(2)]
        m8 = [sb('b_m8%d' % i, [128, 8], F32) for i in range(2)]
        Eb = [sb('b_E%d' % i, [128, 512], BF16) for i in range(2)]
        Pb = [sb('b_P%d' % i, [128, 512], BF16) for i in range(2)]
        rz = sb('b_rz', [64, 128], F32)
        ob = [sb('b_ob%d' % i, [64, 128], BF16) for i in range(2)]
        k.dma('act', bk[:, :, :], d['bk'].rearrange('h p t -> p h t'), writes=[('b_k', 0)])
        k.dma('sp', bik[:, :], d['bik'][0], writes=[('b_ik', 0)])
        k.dma('act', bv[:, :, :], d['bv'].rearrange('b p c -> p b c'), writes=[('b_v', 0)])
        k.dma('pool', biw[:, :, :], d['biw'].rearrange('b p c -> p b c'), writes=[('b_iw', 0)])
        cn = {'bank': 0, 'rt': 0, 'ep': 0, 'ob': 0}

        def nbank():
            b = cn['bank'] % 8
            cn['bank'] += 1
            return b
        for qb in range(NB):
            L = 128 * (qb + 1)
            qs = slice(qb * 128, (qb + 1) * 128)
            bq, biq = bqs[qb % 2], biqs[qb % 2]
            k.dma('sp', bq[:, :, :], d['bq'][:, :, qs].rearrange('h p t -> p h t'), writes=[('b_q', qb % 2)])
            k.dma('act', biq[:, :, :], d['biq'][:, :, qs].rearrange('h p t -> p h t'), writes=[('b_iq', qb % 2)])
            for kg in range((L + 511) // 512):
                n = min(512, L - kg * 512)
                seg = slice(kg * 512, kg * 512 + n)
                for ih in range(4):
                    bank = nbank()
                    ps = c.ps[bank]
                    k.op('pe', lambda e: e.matmul(ps[:, 0:n], lhsT=biq[:, ih, :], rhs=bik[:, seg],
                                                  start=True, stop=True),
                         reads=[('b_iq', qb % 2), ('b_ik', 0)], writes=[('ps', bank)])
                    ri = cn['rt'] % 2
                    cn['rt'] += 1
                    r_ = rt[ri]
                    k.op('act', lambda e: e.activation(out=r_[:, 0:n], in_=ps[:, 0:n], func=AF.Relu),
                         reads=[('ps', bank)], writes=[('b_rt', ri)])
                    if ih == 0:
                        k.op('dve', lambda e: e.tensor_scalar(out=score[:, seg], in0=r_[:, 0:n],
                                                              scalar1=biw[:, qb, 0:1], scalar2=None, op0=ALU.mult),
                             reads=[('b_rt', ri), ('b_iw', 0)], writes=[('b_score', 0)])
                    else:
                        k.op('dve', lambda e: e.scalar_tensor_tensor(
                            out=score[:, seg], in0=r_[:, 0:n], scalar=biw[:, qb, ih:ih + 1], in1=score[:, seg],
                            op0=ALU.mult, op1=ALU.add),
                            reads=[('b_rt', ri), ('b_iw', 0), ('b_score', 0)], writes=[('b_score', 0)])
            k.op('dve', lambda e: e.tensor_tensor(out=score[:, qs], in0=score[:, qs], in1=c.negmask[:, :],
                                                  op=ALU.add),
                 reads=[('b_score', 0), ('negmask', 0)], writes=[('b_score', 0)])
            if qb >= 2:
                for r in range(32):
                    m = m8[r % 2]
                    src = score if r == 0 else work
                    k.op('dve', lambda e: e.max(out=m[:, :], in_=src[:, 0:L]),
                         reads=[('b_score', 0), ('b_work', 0)], writes=[('b_m8', r % 2)])
                    if r < 31:
                        k.op('dve', lambda e: e.match_replace(out=work[:, 0:L], in_to_replace=m[:, :],
                                                              in_values=src[:, 0:L], imm_value=NEG),
                             reads=[('b_score', 0), ('b_m8', r % 2)], writes=[('b_work', 0)])
                thr = m8[1][:, 7:8]
                k.op('dve', lambda e: e.tensor_scalar(out=work[:, 0:L], in0=score[:, 0:L], scalar1=thr,
                                                      scalar2=0.0, op0=ALU.is_gt, op1=ALU.add,
                                                      accum_out=ngt[:, 0:1]),
                     reads=[('b_score', 0), ('b_m8', 1)], writes=[('b_work', 0), ('b_ngt', 0)])
                k.op('dve', lambda e: e.tensor_scalar(out=ngt[:, 1:2], in0=ngt[:, 0:1], scalar1=-1.0,
                                                      scalar2=256.0, op0=ALU.mult, op1=ALU.add),
                     reads=[('b_ngt', 0)], writes=[('b_ngt', 1)])
                k.op('dve', lambda e: e.tensor_scalar(out=work[:, 0:L], in0=score[:, 0:L], scalar1=thr,
                                                      scalar2=None, op0=ALU.is_equal),
                     reads=[('b_score', 0), ('b_m8', 1), ('b_ngt', 0)], writes=[('b_work', 0)])
                k.op('dve', lambda e: e.tensor_tensor_scan(out=cum[:, 0:L], data0=work[:, 0:L],
                                                           data1=work[:, 0:L], initial=0.0,
                                                           op0=ALU.add, op1=ALU.max),
                     reads=[('b_work', 0)], writes=[('b_cum', 0)])
                k.op('dve', lambda e: e.scalar_tensor_tensor(out=cum[:, 0:L], in0=cum[:, 0:L],
                                                             scalar=ngt[:, 1:2], in1=work[:, 0:L],
                                                             op0=ALU.is_le, op1=ALU.mult),
                     reads=[('b_work', 0), ('b_cum', 0), ('b_ngt', 1)], writes=[('b_cum', 0)])
                k.op('dve', lambda e: e.scalar_tensor_tensor(out=work[:, 0:L], in0=score[:, 0:L],
                                                             scalar=thr, in1=cum[:, 0:L],
                                                             op0=ALU.is_gt, op1=ALU.add),
                     reads=[('b_score', 0), ('b_cum', 0), ('b_m8', 1)], writes=[('b_work', 0)])
            else:
                k.op('dve', lambda e: e.tensor_scalar(out=work[:, 0:L], in0=score[:, 0:L],
                                                      scalar1=-1.0e29, scalar2=None, op0=ALU.is_ge),
                     reads=[('b_score', 0)], writes=[('b_work', 0)])
            for kb0 in range(0, qb + 1, 4):
                nk = min(4, qb + 1 - kb0)
                bank = nbank()
                ps = c.ps[bank]
                for j in range(nk):
                    kb = kb0 + j
                    k.op('pe', lambda e: e.transpose(out=ps[:, j * 128:(j + 1) * 128],
                                                     in_=work[:, kb * 128:(kb + 1) * 128], identity=c.ident[:, :]),
                         reads=[('b_work', 0)], writes=[('ps', bank)])
                k.op('act', lambda e: e.copy(out=selT[:, kb0:kb0 + nk, :],
                                             in_=ps[:, 0:nk * 128].rearrange('p (a n) -> p a n', a=nk)),
                     reads=[('ps', bank)], writes=[('b_selT', 0)])
            for h in range(4):
                bu = nbank()
                ps_u = c.ps[bu]
                for kb0 in range(0, qb + 1, 4):
                    nk = min(4, qb + 1 - kb0)
                    bs = nbank()
                    if bs == bu:
                        bs = nbank()
                    ps_s = c.ps[bs]
                    for j in range(nk):
                        kb = kb0 + j
                        k.op('pe', lambda e: e.matmul(ps_s[:, j * 128:(j + 1) * 128],
                                                      lhsT=bk[:, h, kb * 128:(kb + 1) * 128], rhs=bq[:, h, :],
                                                      start=True, stop=True),
                             reads=[('b_q', qb % 2), ('b_k', 0)], writes=[('ps', bs)])
                    ei = cn['ep'] % 2
                    cn['ep'] += 1
                    E, P = Eb[ei], Pb[ei]
                    k.op('act', lambda e: e.activation(out=E[:, 0:nk * 128], in_=ps_s[:, 0:nk * 128], func=AF.Exp,
                                                       scale=0.125),
                         reads=[('ps', bs)], writes=[('b_E', ei)])
                    k.op('dve', lambda e: e.tensor_tensor(
                        out=P[:, 0:nk * 128], in0=E[:, 0:nk * 128],
                        in1=selT[:, kb0:kb0 + nk, :].rearrange('p a n -> p (a n)'), op=ALU.mult),
                        reads=[('b_E', ei), ('b_selT', 0)], writes=[('b_P', ei)])
                    for j in range(nk):
                        kb = kb0 + j
                        first = (kb == 0)
                        last = (kb == qb)
                        k.op('pe', lambda e: e.matmul(ps_u[0:64, 0:128], lhsT=bv[:, kb, h * 64:(h + 1) * 64],
                                                      rhs=P[:, j * 128:(j + 1) * 128], start=first, stop=last,
                                                      skip_group_check=True),
                             reads=[('b_P', ei), ('b_v', 0)], writes=[('ps', bu)])
                        k.op('pe', lambda e: e.matmul(ps_u[0:64, 128:256], lhsT=c.ones_bf[:, 0:64],
                                                      rhs=P[:, j * 128:(j + 1) * 128], start=False, stop=last,
                                                      skip_group_check=True),
                             reads=[('b_P', ei), ('ones_bf', 0)], writes=[('ps', bu)])
                k.op('dve', lambda e: e.reciprocal(out=rz[:, :], in_=ps_u[0:64, 128:256]),
                     reads=[('ps', bu)], writes=[('b_rz', 0)])
                oi = cn['ob'] % 2
                cn['ob'] += 1
                o_ = ob[oi]
                k.op('dve', lambda e: e.tensor_tensor(out=o_[:, :], in0=ps_u[0:64, 0:128], in1=rz[:, :],
                                                      op=ALU.mult),
                     reads=[('ps', bu), ('b_rz', 0)], writes=[('b_ob', oi)])
                k.dma('sp' if oi == 0 else 'act', d['obr'][1, h, :, qs], o_[:, :], reads=[('b_ob', oi)])
        k.barrier()


def mixer_d(k, c, T, lp):
    nc = k.nc
    NB = T // 128
    d = c.d
    with ExitStack() as st:
        def sb(name, shape, dt_):
            return st.enter_context(_sbt(nc, 'sb_' + name, shape, dt_))
        BT = sb('d_BT', [64, 2, T], BF16)
        CT = sb('d_CT', [64, 2, T], BF16)
        xbar = sb('d_xbar', [128, NB, 256], BF16)
        Btok = sb('d_Btok', [128, NB, 2, 64], BF16)
        dte = sb('d_dte', [128, NB, 4], F32)
        etot = sb('d_etot', [128, NB, 4], F32)
        cw = sb('d_cw', [64, 8, 4], F32)
        cb = sb('d_cb', [64, 8], F32)
        nw = sb('d_nw', [64, 4], F32)
        dvec = sb('d_vec', [128, 12], F32)
        dt_ = sb('d_dt', [128, NB, 4], F32)
        a_tok = sb('d_atok', [128, NB, 4], F32)
        acs = sb('d_acs', [128, NB, 4], F32)
        tot = sb('d_tot', [128, NB, 4], F32)
        pre = sb('d_pre', [128, NB, 4], F32)
        Abc = sb('d_Abc', [128, 4], F32)
        k.dma('sp', cw[:, :, :], lp['d_cw'], writes=[('d_cw', 0)])
        k.dma('act', cb[:, :], lp['d_cb'], writes=[('d_cb', 0)])
        k.dma('pool', nw[:, :], lp['d_nw'], writes=[('d_nw', 0)])
        k.dma('sp', dvec[:, :], lp['d_vec'].partition_broadcast(128), writes=[('d_vec', 0)])
        k.dma('act', dt_[:, :, :], d['ddt'].rearrange('b p c -> p b c'), writes=[('d_dt', 0)])
        k.op('dve', lambda e: e.tensor_tensor(out=dt_[:, :, :], in0=dt_[:, :, :],
                                              in1=dvec[:, 0:4].unsqueeze(1).to_broadcast([128, NB, 4]), op=ALU.add),
             reads=[('d_dt', 0), ('d_vec', 0)], writes=[('d_dt', 0)])
        k.op('act', lambda e: e.activation(out=dt_[:, :, :], in_=dt_[:, :, :], func=AF.Exp),
             reads=[('d_dt', 0)], writes=[('d_dt', 0)])
        k.op('act', lambda e: e.activation(out=dt_[:, :, :], in_=dt_[:, :, :], func=AF.Ln, bias=1.0),
             reads=[('d_dt', 0)], writes=[('d_dt', 0)])
        k.op('act', lambda e: e.activation(out=Abc[:, :], in_=dvec[:, 4:8], func=AF.Exp),
             reads=[('d_vec', 0)], writes=[('d_Abc', 0)])
        k.op('dve', lambda e: e.scalar_tensor_tensor(out=a_tok[:, :, :], in0=dt_[:, :, :], scalar=-1.0,
                                                     in1=Abc[:, :].unsqueeze(1).to_broadcast([128, NB, 4]),
                                                     op0=ALU.mult, op1=ALU.mult),
             reads=[('d_dt', 0), ('d_Abc', 0)], writes=[('d_atok', 0)])
        bank = 0
        ps = c.ps[bank]
        k.op('pe', lambda e: e.matmul(ps[:, 0:NB * 4], lhsT=c.triu[:, :], rhs=a_tok[:, :, :].rearrange('p b c -> p (b c)'),
                                      start=True, stop=True),
             reads=[('d_atok', 0), ('triu', 0)], writes=[('ps', bank)])
        k.op('dve', lambda e: e.tensor_copy(out=acs[:, :, :].rearrange('p b c -> p (b c)'), in_=ps[:, 0:NB * 4]),
             reads=[('ps', bank)], writes=[('d_acs', 0)])
        bank = 1
        ps1 = c.ps[bank]
        k.op('pe', lambda e: e.matmul(ps1[:, 0:NB * 4], lhsT=c.ones_f[:, :], rhs=a_tok[:, :, :].rearrange('p b c -> p (b c)'),
                                      start=True, stop=True),
             reads=[('d_atok', 0), ('ones_f', 0)], writes=[('ps', bank)])
        k.op('dve', lambda e: e.tensor_copy(out=tot[:, :, :].rearrange('p b c -> p (b c)'), in_=ps1[:, 0:NB * 4]),
             reads=[('ps', bank)], writes=[('d_tot', 0)])
        k.op('dve', lambda e: e.tensor_tensor(out=dte[:, :, :], in0=tot[:, :, :], in1=acs[:, :, :], op=ALU.subtract),
             reads=[('d_acs', 0), ('d_tot', 0)], writes=[('d_dte', 0)])
        k.op('act', lambda e: e.activation(out=dte[:, :, :], in_=dte[:, :, :], func=AF.Exp),
             reads=[('d_dte', 0)], writes=[('d_dte', 0)])
        k.op('act', lambda e: e.activation(out=etot[:, :, :], in_=tot[:, :, :], func=AF.Exp),
             reads=[('d_tot', 0)], writes=[('d_etot', 0)])
        with ExitStack() as st2:
            xin = [st2.enter_context(_sbt(nc, 'd_xin%d' % i, [64, T + 3], F32)) for i in range(2)]
            acc = [st2.enter_context(_sbt(nc, 'd_acc%d' % i, [64, T], F32)) for i in range(2)]
            for ch in range(8):
                i = ch % 2
                xi, ac = xin[i], acc[i]
                k.op('pool', lambda e: e.memset(xi[:, 0:3], 0.0), writes=[('d_xin', i)])
                k.dma('sp' if i == 0 else 'act', xi[:, 3:T + 3], d['dxbc'][ch, :, 3:T + 3], writes=[('d_xin', i)])
                k.op('dve', lambda e: e.tensor_scalar(out=ac[:, :], in0=xi[:, 0:T], scalar1=cw[:, ch, 0:1],
                                                      scalar2=cb[:, ch:ch + 1], op0=ALU.mult, op1=ALU.add),
                     reads=[('d_xin', i), ('d_cw', 0), ('d_cb', 0)], writes=[('d_acc', i)])
                for tap in range(1, 4):
                    k.op('dve', lambda e: e.scalar_tensor_tensor(out=ac[:, :], in0=xi[:, tap:T + tap],
                                                                 scalar=cw[:, ch, tap:tap + 1], in1=ac[:, :],
                                                                 op0=ALU.mult, op1=ALU.add),
                         reads=[('d_xin', i), ('d_cw', 0), ('d_acc', i)], writes=[('d_acc', i)])
                if ch < 4:
                    k.op('act', lambda e: e.activation(out=ac[:, :], in_=ac[:, :], func=AF.Silu),
                         reads=[('d_acc', i)], writes=[('d_acc', i)])
                    k.dma('pool', d['dxs'][ch], ac[:, :], reads=[('d_acc', i)])
                    for b in range(NB):
                        bank = (b % 4) + 2
                        psx = c.ps[bank]
                        k.op('pe', lambda e: e.transpose(out=psx[:, 0:64], in_=ac[:, b * 128:(b + 1) * 128],
                                                         identity=c.ident[0:64, 0:64]),
                             reads=[('d_acc', i)], writes=[('ps', bank)])
                        k.op('dve', lambda e: e.tensor_scalar(out=xbar[:, b, ch * 64:(ch + 1) * 64], in0=psx[:, 0:64],
                                                              scalar1=dt_[:, b, ch:ch + 1], scalar2=None, op0=ALU.mult),
                             reads=[('ps', bank), ('d_dt', 0)], writes=[('d_xbar', ch)])
                elif ch < 6:
                    k.op('act', lambda e: e.activation(out=ac[:, :], in_=ac[:, :], func=AF.Silu),
                         reads=[('d_acc', i)], writes=[('d_acc', i)])
                    k.op('pool', lambda e: e.tensor_copy(out=BT[:, ch - 4, :], in_=ac[:, :]),
                         reads=[('d_acc', i)], writes=[('d_BC', ch)])
                    for b in range(NB):
                        bank = (b % 4) + 2
                        psx = c.ps[bank]
                        k.op('pe', lambda e: e.transpose(out=psx[:, 0:64], in_=ac[:, b * 128:(b + 1) * 128],
                                                         identity=c.ident[0:64, 0:64]),
                             reads=[('d_acc', i)], writes=[('ps', bank)])
                        k.op('act', lambda e: e.copy(out=Btok[:, b, ch - 4, :], in_=psx[:, 0:64]),
                             reads=[('ps', bank)], writes=[('d_Btok', ch)])
                else:
                    k.op('act', lambda e: e.activation(out=CT[:, ch - 6, :], in_=ac[:, :], func=AF.Silu),
                         reads=[('d_acc', i)], writes=[('d_BC', ch)])
            k.barrier()
        with ExitStack() as st3:
            def sb3(name, shape, dt2):
                return st3.enter_context(_sbt(nc, 'sb_' + name, shape, dt2))
            arg = [sb3('d_arg%d' % i, [128, 4, 128], F32) for i in range(2)]
            Dm = [sb3('d_Dm%d' % i, [128, 4, 128], F32) for i in range(2)]
            MT = [sb3('d_MT%d' % i, [128, 4, 128], BF16) for i in range(2)]
            ecs = [sb3('d_ecs%d' % i, [64, 4, 128], F32) for i in range(2)]
            Cd = [sb3('d_Cd%d' % i, [64, 4, 128], F32) for i in range(2)]
            Bd = [sb3('d_Bd%d' % i, [128, 4, 64], BF16) for i in range(2)]
            zts = [sb3('d_zt%d' % i, [64, 4, 128], F32) for i in range(2)]
            xsts = [sb3('d_xst%d' % i, [64, 4, 128], F32) for i in range(2)]
            state = sb3('d_state', [64, 4, 64], F32)
            stmp = sb3('d_stmp', [64, 4, 64], F32)
            CTf = sb3('d_CTf', [64, 2, 128], F32)
            y = sb3('d_y', [64, 4, 128], F32)
            ysq = sb3('d_ysq', [64, 4, 128], F32)
            ss = sb3('d_ss', [64, 2, 128], F32)
            obs = [sb3('d_ob%d' % i, [64, 4, 128], BF16) for i in range(2)]
            k.op('dve', lambda e: e.memset(state[:, :, :], 0.0), writes=[('d_state', 0)])
            bcn = [0]

            def nbank():
                b = bcn[0] % 8
                bcn[0] += 1
                return b
            for lb in range(NB):
                ls = slice(lb * 128, (lb + 1) * 128)
                i2 = lb % 2
                zt, xst, ob = zts[i2], xsts[i2], obs[i2]
                k.dma('sp', zt[:, :, :], d['dz'][:, :, ls].rearrange('h p t -> p h t'), writes=[('d_zt', i2)])
                k.dma('act', xst[:, :, :], d['dxs'][:, :, ls].rearrange('h p t -> p h t'), writes=[('d_xst', i2)])
                bank = nbank()
                psb = c.ps[bank]
                for h in range(4):
                    k.op('pe', lambda e: e.matmul(psb[:, h * 128:(h + 1) * 128],
                                                  lhsT=a_tok[:, lb, h:h + 1].to_broadcast([128, 128]),
                                                  rhs=c.triu[:, :], start=True, stop=True),
                         reads=[('d_atok', 0), ('triu', 0)], writes=[('ps', bank)])
                a_, D_, M_, ec_, Cd_, Bd_ = arg[i2], Dm[i2], MT[i2], ecs[i2], Cd[i2], Bd[i2]
                k.op('dve', lambda e: e.tensor_tensor(
                    out=a_[:, :, :], in0=psb[:, :].rearrange('p (h l) -> p h l', h=4),
                    in1=acs[:, lb, :].unsqueeze(2).to_broadcast([128, 4, 128]), op=ALU.subtract),
                    reads=[('ps', bank), ('d_acs', 0)], writes=[('d_arg', i2)])
                k.op('act', lambda e: e.activation(out=ec_[:, :, :],
                                                   in_=psb[0:64, :].rearrange('p (h l) -> p h l', h=4), func=AF.Exp),
                     reads=[('ps', bank)], writes=[('d_ecs', i2)])
                k.op('pool', lambda e: e.tensor_scalar_min(out=a_[:, :, :], in0=a_[:, :, :], scalar1=0.0),
                     reads=[('d_arg', i2)], writes=[('d_arg', i2)])
                k.op('act', lambda e: e.activation(out=D_[:, :, :], in_=a_[:, :, :], func=AF.Exp),
                     reads=[('d_arg', i2)], writes=[('d_Dm', i2)])
                k.op('pool', lambda e: e.tensor_tensor(
                    out=D_[:, :, :], in0=D_[:, :, :],
                    in1=c.triu[:, :].unsqueeze(1).to_broadcast([128, 4, 128]), op=ALU.mult),
                    reads=[('d_Dm', i2), ('triu', 0)], writes=[('d_Dm', i2)])
                bg = nbank()
                ps_g = c.ps[bg]
                for g in range(2):
                    k.op('pe', lambda e: e.matmul(ps_g[:, g * 128:(g + 1) * 128], lhsT=BT[:, g, ls],
                                                  rhs=CT[:, g, ls], start=True, stop=True),
                         reads=[('d_BC', 0)], writes=[('ps', bg)])
                for g in range(2):
                    k.op('dve', lambda e: e.tensor_tensor(
                        out=M_[:, 2 * g:2 * g + 2, :], in0=D_[:, 2 * g:2 * g + 2, :],
                        in1=ps_g[:, g * 128:(g + 1) * 128].unsqueeze(1).to_broadcast([128, 2, 128]),
                        op=ALU.mult),
                        reads=[('d_Dm', i2), ('ps', bg)], writes=[('d_MT', i2)])
                k.op('pool', lambda e: e.tensor_copy(out=CTf[:, :, :], in_=CT[:, :, ls]),
                     reads=[('d_BC', 0)], writes=[('d_CTf', 0)])
                for g in range(2):
                    k.op('pool', lambda e: e.tensor_tensor(
                        out=Cd_[:, 2 * g:2 * g + 2, :], in0=ec_[:, 2 * g:2 * g + 2, :],
                        in1=CTf[:, g, :].unsqueeze(1).to_broadcast([64, 2, 128]), op=ALU.mult),
                        reads=[('d_ecs', i2), ('d_CTf', 0)], writes=[('d_Cd', i2)])
                for g in range(2):
                    k.op('dve', lambda e: e.tensor_tensor(
                        out=Bd_[:, 2 * g:2 * g + 2, :],
                        in0=Btok[:, lb, g, :].unsqueeze(1).to_broadcast([128, 2, 64]),
                        in1=dte[:, lb, 2 * g:2 * g + 2].unsqueeze(2).to_broadcast([128, 2, 64]), op=ALU.mult),
                        reads=[('d_Btok', 0), ('d_dte', 0)], writes=[('d_Bd', i2)])
                bu = nbank()
                ps_y = c.ps[bu]
                for h in range(4):
                    k.op('pe', lambda e: e.matmul(ps_y[0:64, h * 128:(h + 1) * 128],
                                                  lhsT=xbar[:, lb, h * 64:(h + 1) * 64], rhs=M_[:, h, :],
                                                  start=(h == 0), stop=(lb == 0), skip_group_check=True),
                         reads=[('d_MT', i2), ('d_xbar', 0)], writes=[('ps', bu)])
                    if lb > 0:
                        k.op('pe', lambda e: e.matmul(ps_y[0:64, h * 128:(h + 1) * 128],
                                                      lhsT=state[:, h, :], rhs=Cd_[:, h, :],
                                                      start=False, stop=True, skip_group_check=True),
                             reads=[('d_Cd', i2), ('d_state', 0)], writes=[('ps', bu)])
                if lb < NB - 1:
                    bs_ = nbank()
                    ps_s = c.ps[bs_]
                    for h in range(4):
                        k.op('pe', lambda e: e.matmul(ps_s[0:64, h * 64:(h + 1) * 64], lhsT=Bd_[:, h, :],
                                                      rhs=xbar[:, lb, h * 64:(h + 1) * 64], start=True, stop=True),
                             reads=[('d_Bd', i2), ('d_xbar', 0)], writes=[('ps', bs_)])
                    k.op('dve', lambda e: e.tensor_tensor(
                        out=stmp[:, :, :], in0=state[:, :, :],
                        in1=etot[0:64, lb, :].unsqueeze(2).to_broadcast([64, 4, 64]), op=ALU.mult),
                        reads=[('d_state', 0), ('d_etot', 0)], writes=[('d_stmp', 0)])
                    k.op('dve', lambda e: e.tensor_tensor(
                        out=state[:, :, :], in0=stmp[:, :, :],
                        in1=ps_s[0:64, 0:256].rearrange('p (h q) -> p h q', h=4), op=ALU.add),
                        reads=[('d_stmp', 0), ('ps', bs_)], writes=[('d_state', 0)])
                for h in range(4):
                    k.op('dve', lambda e: e.scalar_tensor_tensor(out=y[:, h, :], in0=xst[:, h, :],
                                                                 scalar=dvec[0:64, 8 + h:9 + h],
                                                                 in1=ps_y[0:64, h * 128:(h + 1) * 128],
                                                                 op0=ALU.mult, op1=ALU.add),
                         reads=[('d_xst', i2), ('d_vec', 0), ('ps', bu)], writes=[('d_y', 0)])
                k.op('act', lambda e: e.activation(out=zt[:, :, :], in_=zt[:, :, :], func=AF.Silu),
                     reads=[('d_zt', i2)], writes=[('d_zt', i2)])
                k.op('dve', lambda e: e.tensor_tensor(out=y[:, :, :], in0=y[:, :, :], in1=zt[:, :, :], op=ALU.mult),
                     reads=[('d_y', 0), ('d_zt', i2)], writes=[('d_y', 0)])
                k.op('act', lambda e: e.activation(out=ysq[:, :, :], in_=y[:, :, :], func=AF.Square),
                     reads=[('d_y', 0)], writes=[('d_ysq', 0)])
                bank = nbank()
                psq = c.ps[bank]
                k.op('pe', lambda e: e.matmul(psq[0:64, :], lhsT=c.ones_f[0:64, 0:64],
                                              rhs=ysq[:, :, :].rearrange('p h l -> p (h l)'), start=True, stop=True),
                     reads=[('d_ysq', 0), ('ones_f', 0)], writes=[('ps', bank)])
                k.op('act', lambda e: e.copy(out=ysq[:, :, :].rearrange('p h l -> p (h l)'), in_=psq[0:64, :]),
                     reads=[('ps', bank)], writes=[('d_ysq', 0)])
                psv = ysq[:, :, :].rearrange('p (g a) l -> p g a l', g=2, a=2)
                k.op('dve', lambda e: e.tensor_tensor(out=ss[:, :, :], in0=psv[:, :, 0, :], in1=psv[:, :, 1, :],
                                                      op=ALU.add),
                     reads=[('d_ysq', 0)], writes=[('d_ss', 0)])
                k.op('act', lambda e: e.activation(out=ss[:, :, :], in_=ss[:, :, :], func=AF.Ln, scale=1.0 / 128,
                                                   bias=c.eps5[0:64, 0:1]),
                     reads=[('d_ss', 0), ('eps', 0)], writes=[('d_ss', 0)])
                k.op('act', lambda e: e.activation(out=ss[:, :, :], in_=ss[:, :, :], func=AF.Exp, scale=-0.5),
                     reads=[('d_ss', 0)], writes=[('d_ss', 0)])
                for g in range(2):
                    k.op('dve', lambda e: e.tensor_tensor(
                        out=y[:, 2 * g:2 * g + 2, :], in0=y[:, 2 * g:2 * g + 2, :],
                        in1=ss[:, g, :].unsqueeze(1).to_broadcast([64, 2, 128]), op=ALU.mult),
                        reads=[('d_y', 0), ('d_ss', 0)], writes=[('d_y', 0)])
                k.op('dve', lambda e: e.tensor_tensor(out=ob[:, :, :], in0=y[:, :, :],
                                                      in1=nw[:, :].unsqueeze(2).to_broadcast([64, 4, 128]), op=ALU.mult),
                     reads=[('d_y', 0), ('d_nw', 0)], writes=[('d_ob', i2)])
                k.dma('pool', d['obr'][3, :, :, ls].rearrange('h p t -> p h t'), ob[:, :, :], reads=[('d_ob', i2)])
            k.barrier()


def mixer_c(k, c, T, lp, layer):
    nc = k.nc
    d = c.d
    N = 256
    NCH = N // 64
    NG = T // N
    with ExitStack() as st:
        def sb(name, shape, dt_=F32):
            return st.enter_context(_sbt(nc, 'c_' + name, shape, dt_))
        p64 = sb('p64', [64, 46])
        wa2 = sb('wa2', [64, 256])
        g2 = sb('g2', [64, 256])
        k.dma('sp', p64[:, :], lp['c_p64'], writes=[('c_par', 0)])
        k.dma('act', wa2[:, :], lp['c_wa2'], writes=[('c_par', 1)])
        k.dma('pool', g2[:, :], lp['c_g2'], writes=[('c_par', 2)])
        if layer > 0:
            v1t = sb('v1t', [64, 4, 16])
            v2 = sb('v2', [16, 256])
            k.dma('sp', v1t[:, :, :], lp['c_v1'], writes=[('c_par', 3)])
            k.dma('act', v2[:, :], lp['c_v2'], writes=[('c_par', 4)])
        mu, w0, a0, kkp, kap, rkp, gnw, gnb, v0 = (p64[:, 0:14], p64[:, 14:18], p64[:, 18:22], p64[:, 22:26],
                                                   p64[:, 26:30], p64[:, 30:34], p64[:, 34:38], p64[:, 38:42],
                                                   p64[:, 42:46])
        prm = sb('prm', [64, 8])
        k.barrier()
        k.op('dve', lambda e: e.tensor_scalar(out=prm[:, 0:4], in0=w0, scalar1=-1.0, scalar2=None, op0=ALU.mult),
             writes=[('c_prm', 0)])
        k.op('dve', lambda e: e.tensor_scalar(out=prm[:, 4:8], in0=kap, scalar1=-1.0, scalar2=1.0, op0=ALU.mult,
                                              op1=ALU.add), writes=[('c_prm', 0)])
        k.barrier()
        negw0, omka = prm[:, 0:4], prm[:, 4:8]
        msk = sb('msk', [64, 3, 64])
        k.dma('sp', msk[:, :, :], c.cin['cmask'], writes=[('c_msk', 0)])
        H = sb('H', [64, 4, 64])
        k.op('dve', lambda e: e.memset(H[:, :, :], 0.0), writes=[('c_H', 0)])
        pc = sb('pc', [64, 14, N + 1])
        pcs = sb('pcs', [64, 14, N])
        tmp = sb('tmp', [64, 14, N])
        th = sb('th', [64, N])
        sg = sb('sg', [64, N])
        e2 = sb('e2', [64, 4, N])
        lw = sb('lw', [64, 4, N])
        base = sb('base', [64, 4, NCH])
        P_ = sb('P', [64, 4, N])
        Pm1 = sb('Pm1', [64, 4, N])
        iP = sb('iP', [64, 4, N])
        a_s = sb('a_s', [64, 4, N])
        g_s = sb('g_s', [64, 4, N])
        kk = sb('kk', [64, 4, N])
        t1 = sb('t1', [64, 4, N])
        kmod = sb('kmod', [64, 4, N])
        bon = sb('bon', [64, 4, N])
        ar = sb('ar', [64, 4, NCH, 2, 64])
        bT = sb('bT', [64, 4, N])
        kT = sb('kT', [64, 4, N])
        vT = sb('vT', [64, 4, N])
        vf = sb('vf', [64, 4, N])
        vl = sb('vl', [16, N])
        tok = sb('tok', [64, NCH, 3, 4, 64])
        MA = sb('MA', [64, 4, 2, 64])
        MB = sb('MB', [64, 4, 2, 64])
        XX = [sb('XX%d' % i, [64, 4, 2, 64]) for i in range(2)]
        TT = sb('TT', [64, 4, 64])
        Xs = sb('Xs', [64, 4, 64])
        Us = sb('Us', [64, 4, 64])
        yT = sb('yT', [64, 4, N])
        dd = sb('dd', [64, 4, N])
        ob = sb('ob', [64, 4, N], BF16)
        identb = c.ident[0:64, 0:64].unsqueeze(1).to_broadcast([64, 4, 64])
        ones64 = c.ones_f[0:64, 0:64]
        bc = [0]

        def nbank():
            b = bc[0] % 8
            bc[0] += 1
            return b

        def ph(t_):
            return t_[:, :, :].rearrange('p h n -> p (h n)')

        def bcast4(col):
            return col.unsqueeze(2).to_broadcast([64, 4, N])

        def headsum(src, dst_fn):
            for half in range(2):
                bank = nbank()
                ps = c.ps[bank]
                k.op('pe', lambda e: e.matmul(ps[0:64, :], lhsT=ones64,
                                              rhs=src[:, 2 * half:2 * half + 2, :].rearrange('p h n -> p (h n)'),
                                              start=True, stop=True),
                     reads=[('c_w', id(src))], writes=[('ps', bank)])
                dst_fn(half, ps[0:64, :].rearrange('p (h n) -> p h n', h=2), bank)

        def W(t_):
            return [('c_w', id(t_))]

        for gi in range(NG):
            t0 = gi * N
            ts = slice(t0, t0 + N)
            if gi == 0:
                k.dma('sp', pc[:, :, 1:N + 1], d['cpc'][:, :, 1:N + 1].rearrange('g p t -> p g t'), writes=W(pc))
                k.op('dve', lambda e: e.memset(pc[:, :, 0:1], 0.0), writes=W(pc))
            else:
                k.dma('sp', pc[:, :, :], d['cpc'][:, :, t0:t0 + N + 1].rearrange('g p t -> p g t'), writes=W(pc))
            k.op('dve', lambda e: e.tensor_tensor(out=tmp[:, :, :], in0=pc[:, :, 0:N], in1=pc[:, :, 1:N + 1],
                                                  op=ALU.subtract), reads=W(pc), writes=W(tmp))
            k.op('pool', lambda e: e.tensor_tensor(out=tmp[:, :, :], in0=tmp[:, :, :],
                                                   in1=mu.unsqueeze(2).to_broadcast([64, 14, N]), op=ALU.mult),
                 reads=W(tmp), writes=W(tmp))
            k.op('dve', lambda e: e.tensor_tensor(out=pcs[:, :, :], in0=tmp[:, :, :], in1=pc[:, :, 1:N + 1],
                                                  op=ALU.add), reads=W(tmp) + W(pc), writes=W(pcs))
            r_, k_, v_ = pcs[:, 0:4, :], pcs[:, 4:8, :], pcs[:, 8:12, :]
            k.op('act', lambda e: e.activation(out=th[0:32, :], in_=pcs[0:32, 12, :], func=AF.Tanh),
                 reads=W(pcs), writes=W(th))
            k.op('act', lambda e: e.activation(out=sg[:, :], in_=pcs[:, 13, :], func=AF.Sigmoid),
                 reads=W(pcs), writes=W(sg))
            for half in range(2):
                bank = nbank()
                ps = c.ps[bank]
                for hh in range(2):
                    h = 2 * half + hh
                    k.op('pe', lambda e: e.matmul(ps[0:64, hh * N:(hh + 1) * N], lhsT=wa2[0:32, h * 64:(h + 1) * 64],
                                                  rhs=th[0:32, :], start=True, stop=True),
                         reads=W(th), writes=[('ps', bank)])
                    k.op('act', lambda e: e.activation(out=e2[:, h, :], in_=ps[0:64, hh * N:(hh + 1) * N],
                                                       func=AF.Exp, scale=-1.0, bias=negw0[:, h:h + 1]),
                         reads=[('ps', bank)], writes=W(e2))
            k.op('act', lambda e: e.activation(out=e2[:, :, :], in_=e2[:, :, :], func=AF.Ln, bias=1.0),
                 reads=W(e2), writes=W(e2))
            k.op('act', lambda e: e.activation(out=e2[:, :, :], in_=e2[:, :, :], func=AF.Exp, scale=-1.0,
                                               bias=c.mhalf[0:64, 0:1]),
                 reads=W(e2), writes=W(e2))
            for half in range(2):
                bank = nbank()
                ps = c.ps[bank]
                for hh in range(2):
                    h = 2 * half + hh
                    k.op('pe', lambda e: e.matmul(ps[0:64, hh * N:(hh + 1) * N], lhsT=wa2[32:64, h * 64:(h + 1) * 64],
                                                  rhs=pcs[32:64, 12, :], start=True, stop=True),
                         reads=W(pcs), writes=[('ps', bank)])
                    k.op('act', lambda e: e.activation(out=a_s[:, h, :], in_=ps[0:64, hh * N:(hh + 1) * N],
                                                       func=AF.Sigmoid, bias=a0[:, h:h + 1]),
                         reads=[('ps', bank)], writes=W(a_s))
            for half in range(2):
                bank = nbank()
                ps = c.ps[bank]
                for hh in range(2):
                    h = 2 * half + hh
                    k.op('pe', lambda e: e.matmul(ps[0:64, hh * N:(hh + 1) * N], lhsT=g2[:, h * 64:(h + 1) * 64],
                                                  rhs=sg[:, :], start=True, stop=True),
                         reads=W(sg), writes=[('ps', bank)])
                k.op('act', lambda e: e.copy(out=g_s[:, 2 * half:2 * half + 2, :],
                                             in_=ps[0:64, :].rearrange('p (h n) -> p h n', h=2)),
                     reads=[('ps', bank)], writes=W(g_s))
            if layer == 0:
                k.op('pool', lambda e: e.tensor_copy(out=vT[:, :, :], in_=v_), reads=W(pcs), writes=W(vT))
                k.dma('act', d['vfirst'][:, :, ts].rearrange('h p t -> p h t'), vT[:, :, :], reads=W(vT))
            else:
                k.dma('act', vf[:, :, :], d['vfirst'][:, :, ts].rearrange('h p t -> p h t'), writes=W(vf))
                bank = nbank()
                ps = c.ps[bank]
                for h in range(4):
                    k.op('pe', lambda e: e.matmul(ps[0:16, 0:N], lhsT=v1t[:, h, :], rhs=pcs[:, 8 + h, :],
                                                  start=(h == 0), stop=(h == 3)),
                         reads=W(pcs), writes=[('ps', bank)])
                k.op('act', lambda e: e.copy(out=vl[:, :], in_=ps[0:16, 0:N]), reads=[('ps', bank)], writes=W(vl))
                for half in range(2):
                    bank = nbank()
                    ps = c.ps[bank]
                    for hh in range(2):
                        h = 2 * half + hh
                        k.op('pe', lambda e: e.matmul(ps[0:64, hh * N:(hh + 1) * N], lhsT=v2[0:16, h * 64:(h + 1) * 64],
                                                      rhs=vl[:, :], start=True, stop=True),
                             reads=W(vl), writes=[('ps', bank)])
                        k.op('act', lambda e: e.activation(out=t1[:, h, :], in_=ps[0:64, hh * N:(hh + 1) * N],
                                                           func=AF.Sigmoid, bias=v0[:, h:h + 1]),
                             reads=[('ps', bank)], writes=W(t1))
                k.op('dve', lambda e: e.tensor_tensor(out=vf[:, :, :], in0=vf[:, :, :], in1=v_, op=ALU.subtract),
                     reads=W(vf) + W(pcs), writes=W(vf))
                k.op('dve', lambda e: e.tensor_tensor(out=vf[:, :, :], in0=vf[:, :, :], in1=t1[:, :, :], op=ALU.mult),
                     reads=W(vf) + W(t1), writes=W(vf))
                k.op('dve', lambda e: e.tensor_tensor(out=vT[:, :, :], in0=vf[:, :, :], in1=v_, op=ALU.add),
                     reads=W(vf) + W(pcs), writes=W(vT))
            k.op('dve', lambda e: e.tensor_tensor(out=kk[:, :, :], in0=k_, in1=bcast4(kkp), op=ALU.mult),
                 reads=W(pcs), writes=W(kk))
            k.op('act', lambda e: e.activation(out=t1[:, :, :], in_=kk[:, :, :], func=AF.Square),
                 reads=W(kk), writes=W(t1))

            def kk_norm(half, psv, bank):
                k.op('act', lambda e: e.activation(out=dd[:, 2 * half:2 * half + 2, :], in_=psv, func=AF.Sqrt),
                     reads=[('ps', bank)], writes=W(dd))
            headsum(t1, kk_norm)
            k.op('dve', lambda e: e.tensor_scalar_max(out=dd[:, :, :], in0=dd[:, :, :], scalar1=1e-12),
                 reads=W(dd), writes=W(dd))
            k.op('dve', lambda e: e.reciprocal(out=dd[:, :, :], in_=dd[:, :, :]), reads=W(dd), writes=W(dd))
            k.op('dve', lambda e: e.tensor_tensor(out=kk[:, :, :], in0=kk[:, :, :], in1=dd[:, :, :], op=ALU.mult),
                 reads=W(kk) + W(dd), writes=W(kk))
            k.op('pool', lambda e: e.tensor_tensor(out=t1[:, :, :], in0=a_s[:, :, :], in1=bcast4(kap), op=ALU.mult),
                 reads=W(a_s), writes=W(t1))
            k.op('pool', lambda e: e.tensor_tensor(out=t1[:, :, :], in0=t1[:, :, :], in1=bcast4(omka), op=ALU.add),
                 reads=W(t1), writes=W(t1))
            k.op('dve', lambda e: e.tensor_tensor(out=kmod[:, :, :], in0=k_, in1=t1[:, :, :], op=ALU.mult),
                 reads=W(pcs) + W(t1), writes=W(kmod))
            k.op('dve', lambda e: e.tensor_tensor(out=t1[:, :, :], in0=r_, in1=kmod[:, :, :], op=ALU.mult),
                 reads=W(pcs) + W(kmod), writes=W(t1))
            k.op('pool', lambda e: e.tensor_tensor(out=t1[:, :, :], in0=t1[:, :, :], in1=bcast4(rkp), op=ALU.mult),
                 reads=W(t1), writes=W(t1))

            def bon_fn(half, psv, bank):
                k.op('dve', lambda e: e.tensor_tensor(out=bon[:, 2 * half:2 * half + 2, :], in0=psv,
                                                      in1=vT[:, 2 * half:2 * half + 2, :], op=ALU.mult),
                     reads=[('ps', bank)] + W(vT), writes=W(bon))
            headsum(t1, bon_fn)
            for h in range(4):
                k.op('dve', lambda e: e.tensor_tensor_scan(out=lw[:, h, :], data0=e2[:, h, :], data1=e2[:, h, :],
                                                           initial=0.0, op0=ALU.add, op1=ALU.max),
                     reads=W(e2), writes=W(lw))
            k.op('dve', lambda e: e.memset(base[:, :, 0:1], 0.0), writes=W(base))
            k.op('dve', lambda e: e.tensor_copy(out=base[:, :, 1:NCH], in_=lw[:, :, 63:N - 1:64]),
                 reads=W(lw), writes=W(base))
            lw4 = lw[:, :, :].rearrange('p h (c s) -> p h c s', s=64)
            k.op('dve', lambda e: e.tensor_tensor(out=lw4, in0=lw4,
                                                  in1=base[:, :, :].unsqueeze(3).to_broadcast([64, 4, NCH, 64]),
                                                  op=ALU.subtract),
                 reads=W(lw) + W(base), writes=W(lw))
            k.op('act', lambda e: e.activation(out=P_[:, :, :], in_=lw[:, :, :], func=AF.Exp, scale=-1.0),
                 reads=W(lw), writes=W(P_))
            k.op('act', lambda e: e.activation(out=iP[:, :, :], in_=lw[:, :, :], func=AF.Exp),
                 reads=W(lw), writes=W(iP))
            k.op('dve', lambda e: e.tensor_tensor(out=t1[:, :, :], in0=lw[:, :, :], in1=e2[:, :, :], op=ALU.subtract),
                 reads=W(lw) + W(e2), writes=W(t1))
            k.op('act', lambda e: e.activation(out=Pm1[:, :, :], in_=t1[:, :, :], func=AF.Exp, scale=-1.0),
                 reads=W(t1), writes=W(Pm1))
            ar5 = ar[:, :, :, :, :]
            k.op('dve', lambda e: e.scalar_tensor_tensor(
                out=ar5[:, :, :, 0, :], in0=kk[:, :, :].rearrange('p h (c s) -> p h c s', s=64), scalar=-1.0,
                in1=Pm1[:, :, :].rearrange('p h (c s) -> p h c s', s=64), op0=ALU.mult, op1=ALU.mult),
                reads=W(kk) + W(Pm1), writes=W(ar))
            k.op('dve', lambda e: e.tensor_tensor(
                out=ar5[:, :, :, 1, :], in0=r_.rearrange('p h (c s) -> p h c s', s=64),
                in1=P_[:, :, :].rearrange('p h (c s) -> p h c s', s=64), op=ALU.mult),
                reads=W(pcs) + W(P_), writes=W(ar))
            k.op('dve', lambda e: e.tensor_tensor(out=bT[:, :, :], in0=kk[:, :, :], in1=a_s[:, :, :], op=ALU.mult),
                 reads=W(kk) + W(a_s), writes=W(bT))
            k.op('dve', lambda e: e.tensor_tensor(out=bT[:, :, :], in0=bT[:, :, :], in1=iP[:, :, :], op=ALU.mult),
                 reads=W(bT) + W(iP), writes=W(bT))
            k.op('dve', lambda e: e.tensor_tensor(out=kT[:, :, :], in0=kmod[:, :, :], in1=iP[:, :, :], op=ALU.mult),
                 reads=W(kmod) + W(iP), writes=W(kT))
            for ci in range(NCH):
                cs = slice(ci * 64, (ci + 1) * 64)
                for qi, src in enumerate((bT, kT, vT)):
                    bank = nbank()
                    ps = c.ps[bank]
                    for h in range(4):
                        k.op('pe', lambda e: e.transpose(out=ps[0:64, h * 64:(h + 1) * 64], in_=src[:, h, cs],
                                                         identity=c.ident[0:64, 0:64]),
                             reads=W(src), writes=[('ps', bank)])
                    eng = 'act' if qi % 2 == 0 else 'dve'
                    dst = tok[:, ci, qi, :, :]
                    srcp = ps[0:64, 0:256].rearrange('p (h n) -> p h n', h=4)
                    if eng == 'act':
                        k.op('act', lambda e: e.copy(out=dst, in_=srcp), reads=[('ps', bank)], writes=W(tok))
                    else:
                        k.op('dve', lambda e: e.tensor_copy(out=dst, in_=srcp), reads=[('ps', bank)], writes=W(tok))
            for ci in range(NCH):
                cs = slice(ci * 64, (ci + 1) * 64)
                bA, bB, bC = nbank(), nbank(), nbank()
                psA, psB, psC = c.ps[bA], c.ps[bB], c.ps[bC]
                for h in range(4):
                    arh = ar[:, h, ci, :, :].rearrange('p a s -> p (a s)')
                    k.op('pe', lambda e: e.matmul(psA[0:64, h * 128:(h + 1) * 128], lhsT=bT[:, h, cs], rhs=arh,
                                                  start=True, stop=True),
                         reads=W(bT) + W(ar), writes=[('ps', bA)])
                    k.op('pe', lambda e: e.matmul(psB[0:64, h * 128:(h + 1) * 128], lhsT=kT[:, h, cs], rhs=arh,
                                                  start=True, stop=True),
                         reads=W(kT) + W(ar), writes=[('ps', bB)])
                    k.op('pe', lambda e: e.matmul(psC[0:64, h * 64:(h + 1) * 64], lhsT=ar[:, h, ci, 0, :],
                                                  rhs=bT[:, h, cs], start=True, stop=True),
                         reads=W(bT) + W(ar), writes=[('ps', bC)])
                mUU = msk[:, 0:2, :].unsqueeze(1).to_broadcast([64, 4, 2, 64])
                k.op('dve', lambda e: e.tensor_tensor(out=MA[:, :, :, :],
                                                      in0=psA[0:64, :].rearrange('p (h a s) -> p h a s', h=4, a=2),
                                                      in1=mUU, op=ALU.mult),
                     reads=[('ps', bA), ('c_msk', 0)], writes=W(MA))
                k.op('dve', lambda e: e.tensor_tensor(out=MB[:, :, :, :],
                                                      in0=psB[0:64, :].rearrange('p (h a s) -> p h a s', h=4, a=2),
                                                      in1=mUU, op=ALU.mult),
                     reads=[('ps', bB), ('c_msk', 0)], writes=W(MB))
                X = XX[0]
                k.op('pool', lambda e: e.tensor_copy(out=X[:, :, 0, :], in_=MA[:, :, 0, :]), reads=W(MA), writes=W(X))
                k.op('dve', lambda e: e.tensor_tensor(out=X[:, :, 1, :],
                                                      in0=psC[0:64, 0:256].rearrange('p (h s) -> p h s', h=4),
                                                      in1=msk[:, 2, :].unsqueeze(1).to_broadcast([64, 4, 64]),
                                                      op=ALU.mult),
                     reads=[('ps', bC), ('c_msk', 0)], writes=W(X))
                k.op('pool', lambda e: e.tensor_tensor(out=TT[:, :, :], in0=MA[:, :, 0, :], in1=identb, op=ALU.add),
                     reads=W(MA), writes=W(TT))
                for it_ in range(5):
                    Xo, Xn = XX[it_ % 2], XX[(it_ + 1) % 2]
                    bank = nbank()
                    ps = c.ps[bank]
                    for h in range(4):
                        k.op('pe', lambda e: e.matmul(ps[0:64, h * 128:h * 128 + 64], lhsT=Xo[:, h, 1, :],
                                                      rhs=Xo[:, h, 0, :], start=True, stop=True),
                             reads=W(Xo), writes=[('ps', bank)])
                        k.op('pe', lambda e: e.matmul(ps[0:64, h * 128 + 64:(h + 1) * 128], lhsT=Xo[:, h, 0, :],
                                                      rhs=Xo[:, h, 1, :], start=True, stop=True),
                             reads=W(Xo), writes=[('ps', bank)])
                    k.op('act', lambda e: e.copy(out=Xn[:, :, :, :],
                                                 in_=ps[0:64, :].rearrange('p (h a s) -> p h a s', h=4, a=2)),
                         reads=[('ps', bank)], writes=W(Xn))
                    bank2 = nbank()
                    ps2 = c.ps[bank2]
                    for h in range(4):
                        k.op('pe', lambda e: e.matmul(ps2[0:64, h * 64:(h + 1) * 64], lhsT=Xn[:, h, 1, :],
                                                      rhs=TT[:, h, :], start=True, stop=True),
                             reads=W(Xn) + W(TT), writes=[('ps', bank2)])
                    k.op('dve', lambda e: e.tensor_tensor(out=TT[:, :, :], in0=TT[:, :, :],
                                                          in1=ps2[0:64, 0:256].rearrange('p (h s) -> p h s', h=4),
                                                          op=ALU.add),
                         reads=[('ps', bank2)] + W(TT), writes=W(TT))
                bank = nbank()
                ps = c.ps[bank]
                for h in range(4):
                    k.op('pe', lambda e: e.matmul(ps[0:64, h * 64:(h + 1) * 64], lhsT=ar[:, h, ci, 0, :], rhs=H[:, h, :],
                                                  start=(h == 0), stop=False, skip_group_check=True),
                         reads=W(ar) + [('c_H', 0)], writes=[('ps', bank)])
                    k.op('pe', lambda e: e.matmul(ps[0:64, h * 64:(h + 1) * 64], lhsT=MB[:, h, 0, :],
                                                  rhs=tok[:, ci, 2, h, :], start=False, stop=True,
                                                  skip_group_check=True),
                         reads=W(MB) + W(tok), writes=[('ps', bank)])
                k.op('act', lambda e: e.copy(out=Xs[:, :, :], in_=ps[0:64, 0:256].rearrange('p (h s) -> p h s', h=4)),
                     reads=[('ps', bank)], writes=W(Xs))
                bank = nbank()
                ps = c.ps[bank]
                for h in range(4):
                    k.op('pe', lambda e: e.matmul(ps[0:64, h * 64:(h + 1) * 64], lhsT=TT[:, h, :], rhs=Xs[:, h, :],
                                                  start=True, stop=True),
                         reads=W(TT) + W(Xs), writes=[('ps', bank)])
                k.op('dve', lambda e: e.tensor_copy(out=Us[:, :, :],
                                                    in_=ps[0:64, 0:256].rearrange('p (h s) -> p h s', h=4)),
                     reads=[('ps', bank)], writes=W(Us))
                bank = nbank()
                ps = c.ps[bank]
                for h in range(4):
                    o_ = ps[0:64, h * 64:(h + 1) * 64]
                    k.op('pe', lambda e: e.matmul(o_, lhsT=H[:, h, :], rhs=ar[:, h, ci, 1, :], start=(h == 0),
                                                  stop=False, skip_group_check=True),
                         reads=W(ar) + [('c_H', 0)], writes=[('ps', bank)])
                    k.op('pe', lambda e: e.matmul(o_, lhsT=Us[:, h, :], rhs=MA[:, h, 1, :], start=False, stop=False,
                                                  skip_group_check=True),
                         reads=W(MA) + W(Us), writes=[('ps', bank)])
                    k.op('pe', lambda e: e.matmul(o_, lhsT=tok[:, ci, 2, h, :], rhs=MB[:, h, 1, :], start=False,
                                                  stop=True, skip_group_check=True),
                         reads=W(MB) + W(tok), writes=[('ps', bank)])
                k.op('act', lambda e: e.copy(out=yT[:, :, cs], in_=ps[0:64, 0:256].rearrange('p (h s) -> p h s', h=4)),
                     reads=[('ps', bank)], writes=W(yT))
                bank = nbank()
                ps = c.ps[bank]
                for h in range(4):
                    o_ = ps[0:64, h * 64:(h + 1) * 64]
                    k.op('pe', lambda e: e.matmul(o_, lhsT=tok[:, ci, 0, h, :], rhs=Us[:, h, :], start=(h == 0),
                                                  stop=False, skip_group_check=True),
                         reads=W(tok) + W(Us), writes=[('ps', bank)])
                    k.op('pe', lambda e: e.matmul(o_, lhsT=tok[:, ci, 1, h, :], rhs=tok[:, ci, 2, h, :], start=False,
                                                  stop=True, skip_group_check=True),
                         reads=W(tok), writes=[('ps', bank)])
                k.op('dve', lambda e: e.tensor_tensor(out=H[:, :, :], in0=H[:, :, :],
                                                      in1=ps[0:64, 0:256].rearrange('p (h s) -> p h s', h=4),
                                                      op=ALU.add),
                     reads=[('ps', bank), ('c_H', 0)], writes=[('c_H', 0)])
                pcl = P_[:, :, ci * 64 + 63:ci * 64 + 64].to_broadcast([64, 4, 64])
                k.op('dve', lambda e: e.tensor_tensor(out=H[:, :, :], in0=H[:, :, :], in1=pcl, op=ALU.mult),
                     reads=[('c_H', 0)] + W(P_), writes=[('c_H', 0)])
            def mean_fn(half, psv, bank):
                k.op('dve', lambda e: e.scalar_tensor_tensor(out=dd[:, 2 * half:2 * half + 2, :], in0=psv,
                                                             scalar=-1.0 / 64, in1=yT[:, 2 * half:2 * half + 2, :],
                                                             op0=ALU.mult, op1=ALU.add),
                     reads=[('ps', bank)] + W(yT), writes=W(dd))
            headsum(yT, mean_fn)
            k.op('act', lambda e: e.activation(out=t1[:, :, :], in_=dd[:, :, :], func=AF.Square),
                 reads=W(dd), writes=W(t1))

            def var_fn(half, psv, bank):
                k.op('act', lambda e: e.activation(out=kmod[:, 2 * half:2 * half + 2, :], in_=psv, func=AF.Ln,
                                                   scale=1.0 / 64, bias=c.epsgn[0:64, 0:1]),
                     reads=[('ps', bank)], writes=W(kmod))
            headsum(t1, var_fn)
            k.op('act', lambda e: e.activation(out=kmod[:, :, :], in_=kmod[:, :, :], func=AF.Exp, scale=-0.5),
                 reads=W(kmod), writes=W(kmod))
            k.op('dve', lambda e: e.tensor_tensor(out=dd[:, :, :], in0=dd[:, :, :], in1=kmod[:, :, :], op=ALU.mult),
                 reads=W(dd) + W(kmod), writes=W(dd))
            k.op('pool', lambda e: e.tensor_tensor(out=dd[:, :, :], in0=dd[:, :, :], in1=bcast4(gnw), op=ALU.mult),
                 reads=W(dd), writes=W(dd))
            k.op('pool', lambda e: e.tensor_tensor(out=dd[:, :, :], in0=dd[:, :, :], in1=bcast4(gnb), op=ALU.add),
                 reads=W(dd), writes=W(dd))
            k.op('dve', lambda e: e.tensor_tensor(out=dd[:, :, :], in0=dd[:, :, :], in1=bon[:, :, :], op=ALU.add),
                 reads=W(dd) + W(bon), writes=W(dd))
            k.op('dve', lambda e: e.tensor_tensor(out=ob[:, :, :], in0=dd[:, :, :], in1=g_s[:, :, :], op=ALU.mult),
                 reads=W(dd) + W(g_s), writes=W(ob))
            k.dma('pool', d['obr'][2, :, :, ts].rearrange('h p t -> p h t'), ob[:, :, :], reads=W(ob))
        k.barrier()


ALPHA = (2 * 2) ** 0.25


def ln_block(k, c, src, skey, dst, dkey, gb, gkey, tmp):
    st6, mv = tmp
    for i in range(2):
        k.op('dve', lambda e: e.bn_stats(out=st6[:, i, :], in_=src[:, i * 512:(i + 1) * 512]),
             reads=[skey], writes=[('ln_st', 0)])
    k.op('dve', lambda e: e.bn_aggr(out=mv[:, 0:2], in_=st6[:, :, :].rearrange('p a b -> p (a b)')),
         reads=[('ln_st', 0)], writes=[('ln_mv', 0)])
    k.op('act', lambda e: e.activation(out=mv[:, 2:3], in_=mv[:, 1:2], func=AF.Ln, bias=c.eps5[:, 0:1]),
         reads=[('ln_mv', 0)], writes=[('ln_mv', 1)])
    k.op('act', lambda e: e.activation(out=mv[:, 3:4], in_=mv[:, 2:3], func=AF.Exp, scale=-0.5),
         reads=[('ln_mv', 1)], writes=[('ln_mv', 2)])
    k.op('dve', lambda e: e.tensor_scalar(out=dst, in0=src[:, :], scalar1=mv[:, 0:1], scalar2=mv[:, 3:4],
                                          op0=ALU.subtract, op1=ALU.mult),
         reads=[skey, ('ln_mv', 0), ('ln_mv', 2)], writes=[dkey])
    k.op('pool', lambda e: e.tensor_tensor(out=dst, in0=dst, in1=gb[:, 0, :], op=ALU.mult),
         reads=[dkey, gkey], writes=[dkey])
    k.op('pool', lambda e: e.tensor_tensor(out=dst, in0=dst, in1=gb[:, 1, :], op=ALU.add),
         reads=[dkey, gkey], writes=[dkey])


def phase_merge(k, c, T, lp, x_dram, x1_dram):
    nc = k.nc
    NB = T // 128
    d = c.d
    with ExitStack() as st:
        def sb(name, shape, dt_=F32):
            return st.enter_context(_sbt(nc, 'm_' + name, shape, dt_))
        wb = sb('wb', [64, 16, 1024], BF16)
        wo = sb('wo', [128, 8, 1024], BF16)
        stg = [sb('stg%d' % i, [128, 4096]) for i in range(2)]
        gb = sb('gb', [128, 2, 1024])
        k.dma('sp', gb[:, 0, :], lp['ln1_g'].partition_broadcast(128), writes=[('m_gb', 0)])
        k.dma('act', gb[:, 1, :], lp['ln1_b'].partition_broadcast(128), writes=[('m_gb', 0)])
        for n in range(4):
            s_ = stg[n % 2]
            k.dma('sp' if n % 2 == 0 else 'act', s_[0:64, :].rearrange('p (a c) -> p a c', a=4),
                  lp['w_branch'][n].rearrange('(a p) c -> p a c', p=64), writes=[('m_stg', n % 2)])
            k.op('dve' if n % 2 == 0 else 'pool', lambda e: e.tensor_copy(
                out=wb[:, 4 * n:4 * n + 4, :], in_=s_[0:64, :].rearrange('p (a c) -> p a c', a=4)),
                reads=[('m_stg', n % 2)], writes=[('m_wb', 0)])
        for hf in range(2):
            s_ = stg[hf]
            k.dma('sp' if hf == 0 else 'act', s_[:, :].rearrange('p (a c) -> p a c', a=4),
                  lp['w_out'][hf * 512:(hf + 1) * 512, :].rearrange('(a p) c -> p a c', p=128),
                  writes=[('m_stg', hf)])
            k.op('dve' if hf == 0 else 'pool', lambda e: e.tensor_copy(
                out=wo[:, 4 * hf:4 * hf + 4, :], in_=s_[:, :].rearrange('p (a c) -> p a c', a=4)),
                reads=[('m_stg', hf)], writes=[('m_wo', 0)])
        ob = [sb('ob%d' % i, [64, 16, 128], BF16) for i in range(2)]
        gs = [sb('gs%d' % i, [128, 4096], BF16) for i in range(2)]
        xs = [sb('xs%d' % i, [128, 1024]) for i in range(2)]
        mg = sb('mg', [128, 1024])
        tm = sb('tm', [128, 512])
        mT = sb('mT', [128, 8, 128], BF16)
        h1 = sb('h1', [128, 1024])
        xo = [sb('xo%d' % i, [128, 1024]) for i in range(2)]
        st6 = sb('st6', [128, 2, 6])
        mv = sb('mv', [128, 4])
        bc = [0]

        def nbank():
            b = bc[0] % 8
            bc[0] += 1
            return b
        for b in range(NB):
            i2 = b % 2
            bs = slice(b * 128, (b + 1) * 128)
            k.dma('sp', ob[i2][:, :, :], d['obr'][:, :, :, bs].rearrange('n h p t -> p (n h) t'),
                  writes=[('m_ob', i2)])
            k.dma('act', gs[i2][:, :], d['gsig'][b], writes=[('m_gs', i2)])
            k.dma('pool', xs[i2][:, :], x_dram[bs, :], writes=[('m_xs', i2)])
            for n in range(4):
                for hc in range(2):
                    bank = nbank()
                    ps = c.ps[bank]
                    for h in range(4):
                        k.op('pe', lambda e: e.matmul(ps[:, :], lhsT=ob[i2][:, 4 * n + h, :],
                                                      rhs=wb[:, 4 * n + h, hc * 512:(hc + 1) * 512],
                                                      start=(h == 0), stop=(h == 3)),
                             reads=[('m_ob', i2), ('m_wb', 0)], writes=[('ps', bank)])
                    gsl = gs[i2][:, n * 1024 + hc * 512:n * 1024 + (hc + 1) * 512]
                    msl = mg[:, hc * 512:(hc + 1) * 512]
                    if n == 0:
                        k.op('dve', lambda e: e.tensor_tensor(out=msl, in0=ps[:, :], in1=gsl, op=ALU.mult),
                             reads=[('ps', bank), ('m_gs', i2)], writes=[('m_mg', hc)])
                    else:
                        k.op('dve', lambda e: e.tensor_tensor(out=tm[:, :], in0=ps[:, :], in1=gsl, op=ALU.mult),
                             reads=[('ps', bank), ('m_gs', i2)], writes=[('m_tm', 0)])
                        k.op('pool', lambda e: e.tensor_tensor(out=msl, in0=msl, in1=tm[:, :], op=ALU.add),
                             reads=[('m_tm', 0), ('m_mg', hc)], writes=[('m_mg', hc)])
            for hc in range(2):
                bank = nbank()
                ps = c.ps[bank]
                for j in range(4):
                    ch = hc * 4 + j
                    k.op('pe', lambda e: e.transpose(out=ps[:, j * 128:(j + 1) * 128],
                                                     in_=mg[:, ch * 128:(ch + 1) * 128], identity=c.ident[:, :]),
                         reads=[('m_mg', hc)], writes=[('ps', bank)])
                k.op('act', lambda e: e.copy(out=mT[:, hc * 4:(hc + 1) * 4, :],
                                             in_=ps[:, :].rearrange('p (j n) -> p j n', j=4)),
                     reads=[('ps', bank)], writes=[('m_mT', 0)])
            for hc in range(2):
                bank = nbank()
                ps = c.ps[bank]
                for kc in range(8):
                    k.op('pe', lambda e: e.matmul(ps[:, :], lhsT=mT[:, kc, :], rhs=wo[:, kc, hc * 512:(hc + 1) * 512],
                                                  start=(kc == 0), stop=(kc == 7)),
                         reads=[('m_mT', 0), ('m_wo', 0)], writes=[('ps', bank)])
                k.op('dve', lambda e: e.scalar_tensor_tensor(out=h1[:, hc * 512:(hc + 1) * 512],
                                                             in0=xs[i2][:, hc * 512:(hc + 1) * 512], scalar=ALPHA,
                                                             in1=ps[:, :], op0=ALU.mult, op1=ALU.add),
                     reads=[('ps', bank), ('m_xs', i2)], writes=[('m_h1', 0)])
            ln_block(k, c, h1, ('m_h1', 0), xo[i2][:, :], ('m_xo', i2), gb, ('m_gb', 0), (st6, mv))
            k.dma('sp', x1_dram[bs, :], xo[i2][:, :], reads=[('m_xo', i2)])
        k.barrier()


def phase_moe(k, c, T, lp, x1_dram, out_dram):
    nc = k.nc
    NB = T // 128
    HT = min(T, 1024)
    NH = T // HT
    NBH = HT // 128
    NTG = HT // 512
    with ExitStack() as st:
        def sb(name, shape, dt_=F32):
            return st.enter_context(_sbt(nc, 'e_' + name, shape, dt_))
        gate = sb('gate', [128, NBH, 32])
        xTp = sb('xTp', [128, 8, HT], BF16)
        wrl = WLoader(k, st, 'e_wr', width=36, nbuf=1)
        wr, wrkey = wrl.load(lp['r_w'], 0, 36)
        lg = sb('lg', [128, 36])
        sm = sb('sm', [128, 16])
        w8 = [sb('w8%d' % i, [128, 4, 8]) for i in range(4)]
        oh = sb('oh', [128, 4])
        gb = sb('gb', [128, 2, 1024])
        rb = sb('rb', [128, 36])
        k.dma('sp', gb[:, 0, :], lp['ln2_g'].partition_broadcast(128), writes=[('e_gb', 0)])
        k.dma('act', gb[:, 1, :], lp['ln2_b'].partition_broadcast(128), writes=[('e_gb', 0)])
        k.dma('pool', rb[:, :], lp['r_bias'].partition_broadcast(128), writes=[('e_rb', 0)])
        def router():
            for b in range(NBH):
                bank = b % 8
                ps = c.ps[bank]
                for kc in range(8):
                    k.op('pe', lambda e: e.matmul(ps[:, 0:36], lhsT=xTp[:, kc, b * 128:(b + 1) * 128], rhs=wr[:, kc, 0:36],
                                                  start=(kc == 0), stop=(kc == 7)),
                         reads=[wrkey], writes=[('ps', bank)])
                R_ = [('e_r', 0)]
                k.op('dve', lambda e: e.tensor_tensor(out=lg[:, :], in0=ps[:, 0:36], in1=rb[:, :], op=ALU.add),
                     reads=[('ps', bank), ('e_rb', 0)] + R_, writes=R_)
                le = lg[:, 4:36].rearrange('p (g j) -> p g j', g=4)
                k.op('dve', lambda e: e.tensor_reduce(out=sm[:, 0:1], in_=lg[:, 0:4], axis=AX.X, op=ALU.max),
                     reads=R_, writes=R_)
                k.op('dve', lambda e: e.tensor_scalar(out=oh[:, :], in0=lg[:, 0:4], scalar1=sm[:, 0:1], scalar2=None,
                                                      op0=ALU.is_equal), reads=R_, writes=R_)
                k.op('dve', lambda e: e.tensor_scalar(out=sm[:, 1:2], in0=sm[:, 0:1], scalar1=-1.0, scalar2=None,
                                                      op0=ALU.mult), reads=R_, writes=R_)
                k.op('act', lambda e: e.activation(out=sm[:, 4:8], in_=lg[:, 0:4], func=AF.Exp, bias=sm[:, 1:2],
                                                   accum_out=sm[:, 2:3]), reads=R_, writes=R_)
                k.op('dve', lambda e: e.reciprocal(out=sm[:, 3:4], in_=sm[:, 2:3]), reads=R_, writes=R_)
                k.op('dve', lambda e: e.tensor_reduce(out=sm[:, 8:12], in_=le, axis=AX.X, op=ALU.max),
                     reads=R_, writes=R_)
                m1b = sm[:, 8:12].unsqueeze(2).to_broadcast([128, 4, 8])
                k.op('dve', lambda e: e.tensor_tensor(out=w8[0][:, :, :], in0=le, in1=m1b, op=ALU.is_equal),
                     reads=R_, writes=R_)
                k.op('dve', lambda e: e.scalar_tensor_tensor(out=w8[1][:, :, :].rearrange('p g j -> p (g j)'),
                                                             in0=w8[0][:, :, :].rearrange('p g j -> p (g j)'),
                                                             scalar=-1.0e30, in1=lg[:, 4:36],
                                                             op0=ALU.mult, op1=ALU.add), reads=R_, writes=R_)
                k.op('dve', lambda e: e.tensor_reduce(out=sm[:, 12:16], in_=w8[1][:, :, :], axis=AX.X, op=ALU.max),
                     reads=R_, writes=R_)
                m2b = sm[:, 12:16].unsqueeze(2).to_broadcast([128, 4, 8])
                k.op('dve', lambda e: e.tensor_tensor(out=w8[0][:, :, :], in0=le, in1=m2b, op=ALU.is_ge),
                     reads=R_, writes=R_)
                k.op('dve', lambda e: e.tensor_tensor(out=w8[1][:, :, :], in0=le, in1=m1b, op=ALU.subtract),
                     reads=R_, writes=R_)
                k.op('act', lambda e: e.activation(out=w8[1][:, :, :], in_=w8[1][:, :, :], func=AF.Exp),
                     reads=R_, writes=R_)
                k.op('dve', lambda e: e.tensor_tensor(out=oh[:, :], in0=oh[:, :],
                                                      in1=sm[:, 3:4].to_broadcast([128, 4]), op=ALU.mult),
                     reads=R_, writes=R_)
                k.op('dve', lambda e: e.tensor_tensor(out=sm[:, 4:8], in0=sm[:, 12:16], in1=sm[:, 8:12],
                                                      op=ALU.subtract), reads=R_, writes=R_)
                k.op('act', lambda e: e.activation(out=sm[:, 4:8], in_=sm[:, 4:8], func=AF.Exp), reads=R_, writes=R_)
                k.op('dve', lambda e: e.tensor_scalar(out=sm[:, 4:8], in0=sm[:, 4:8], scalar1=1.0, scalar2=None,
                                                      op0=ALU.add), reads=R_, writes=R_)
                k.op('dve', lambda e: e.reciprocal(out=sm[:, 4:8], in_=sm[:, 4:8]), reads=R_, writes=R_)
                k.op('dve', lambda e: e.tensor_tensor(out=sm[:, 4:8], in0=sm[:, 4:8], in1=oh[:, :], op=ALU.mult),
                     reads=R_, writes=R_)
                k.op('dve', lambda e: e.tensor_tensor(out=w8[0][:, :, :], in0=w8[0][:, :, :], in1=w8[1][:, :, :],
                                                      op=ALU.mult), reads=R_, writes=R_)
                k.op('dve', lambda e: e.tensor_tensor(out=gate[:, b, :].rearrange('p (g j) -> p g j', g=4),
                                                      in0=w8[0][:, :, :],
                                                      in1=sm[:, 4:8].unsqueeze(2).to_broadcast([128, 4, 8]),
                                                      op=ALU.mult), reads=R_, writes=R_ + [('e_gate', 0)])
            k.barrier()
        wstg = [sb('wstg%d' % i, [128, 4096]) for i in range(2)]
        wset = [[sb('wb%d_%d' % (i, j), [128, 4096], BF16) for j in range(3)] for i in range(2)]
        wcnt = [0]

        def wload(src3, seti, j, q):
            si = wcnt[0] % 2
            wcnt[0] += 1
            stg_ = wstg[si]
            a = src3.shape[1]
            k.dma(q, stg_[:, :].rearrange('p (a c) -> p a c', a=a), src3, writes=[('e_wstg', si)])
            k.op('pool', lambda e: e.tensor_copy(out=wset[seti][j][:, :], in_=stg_[:, :]),
                 reads=[('e_wstg', si)], writes=[('e_wset', seti, j)])
        yacc = sb('yacc', [128, NBH, 1024])
        sl = [sb('sl%d' % i, [128, 512]) for i in range(2)]
        hT = [sb('hT%d' % i, [128, 4, 512], BF16) for i in range(2)]
        xs = [sb('xs%d' % i, [128, 1024]) for i in range(2)]
        st6 = sb('st6', [128, 2, 6])
        mv = sb('mv', [128, 4])
        bc = [0]

        def nbank():
            b = bc[0] % 8
            bc[0] += 1
            return b
        for hp in range(NH):
            tb0 = hp * HT
            phase_x(k, c, x1_dram[tb0:tb0 + HT, :], HT, xT=xTp)
            router()
            for ex in range(32):
                seti = (hp * 32 + ex) % 2
                wload(lp['e_gate'][ex].rearrange('(a p) c -> p a c', p=128), seti, 0, 'sp')
                wload(lp['e_up'][ex].rearrange('(a p) c -> p a c', p=128), seti, 1, 'act')
                wload(lp['e_down'][ex].rearrange('(a p) c -> p a c', p=128), seti, 2, 'sp')
                wg = wset[seti][0][:, :].rearrange('p (a c) -> p a c', a=8)
                wu = wset[seti][1][:, :].rearrange('p (a c) -> p a c', a=8)
                wd_b = wset[seti][2][:, :].rearrange('p (a c) -> p a c', a=4)
                wgkey, wukey, wdkey = ('e_wset', seti, 0), ('e_wset', seti, 1), ('e_wset', seti, 2)
                for tg in range(NTG):
                    ts0 = tg * 512
                    hh = hT[(ex * NTG + tg) % 2]
                    hkey = ('e_hT', (ex * NTG + tg) % 2)
                    for cc in range(4):
                        bg, bu = nbank(), nbank()
                        psg, psu = c.ps[bg], c.ps[bu]
                        for kc in range(8):
                            k.op('pe', lambda e: e.matmul(psg[:, :], lhsT=wg[:, kc, cc * 128:(cc + 1) * 128],
                                                          rhs=xTp[:, kc, ts0:ts0 + 512], start=(kc == 0),
                                                          stop=(kc == 7)),
                                 reads=[wgkey], writes=[('ps', bg)])
                        for kc in range(8):
                            k.op('pe', lambda e: e.matmul(psu[:, :], lhsT=wu[:, kc, cc * 128:(cc + 1) * 128],
                                                          rhs=xTp[:, kc, ts0:ts0 + 512], start=(kc == 0),
                                                          stop=(kc == 7)),
                                 reads=[wukey], writes=[('ps', bu)])
                        s_ = sl[cc % 2]
                        k.op('act', lambda e: e.activation(out=s_[:, :], in_=psg[:, :], func=AF.Silu),
                             reads=[('ps', bg)], writes=[('e_sl', cc % 2)])
                        k.op('dve', lambda e: e.tensor_tensor(out=hh[:, cc, :], in0=s_[:, :], in1=psu[:, :],
                                                              op=ALU.mult),
                             reads=[('e_sl', cc % 2), ('ps', bu)], writes=[hkey])
                    for bl in range(4):
                        bloc = tg * 4 + bl
                        bglob = tb0 // 128 + bloc
                        for hc in range(2):
                            bank = nbank()
                            ps = c.ps[bank]
                            for cc in range(4):
                                k.op('pe', lambda e: e.matmul(ps[:, :], lhsT=hh[:, cc, bl * 128:(bl + 1) * 128],
                                                              rhs=wd_b[:, cc, hc * 512:(hc + 1) * 512],
                                                              start=(cc == 0), stop=(cc == 3)),
                                     reads=[hkey, wdkey], writes=[('ps', bank)])
                            ya = yacc[:, bloc, hc * 512:(hc + 1) * 512]
                            if ex == 0:
                                k.op('dve', lambda e: e.tensor_scalar(out=ya, in0=ps[:, :],
                                                                      scalar1=gate[:, bloc, ex:ex + 1], scalar2=None,
                                                                      op0=ALU.mult),
                                     reads=[('ps', bank), ('e_gate', 0)], writes=[('e_y', bloc)])
                            else:
                                k.op('dve', lambda e: e.scalar_tensor_tensor(out=ya, in0=ps[:, :],
                                                                             scalar=gate[:, bloc, ex:ex + 1], in1=ya,
                                                                             op0=ALU.mult, op1=ALU.add),
                                     reads=[('ps', bank), ('e_gate', 0), ('e_y', bloc)], writes=[('e_y', bloc)])
            for bloc in range(NBH):
                bglob = tb0 // 128 + bloc
                i2 = bloc % 2
                bs = slice(bglob * 128, (bglob + 1) * 128)
                k.dma('sp', xs[i2][:, :], x1_dram[bs, :], writes=[('e_xs', i2)])
                k.op('dve', lambda e: e.scalar_tensor_tensor(out=yacc[:, bloc, :], in0=xs[i2][:, :], scalar=ALPHA,
                                                             in1=yacc[:, bloc, :], op0=ALU.mult, op1=ALU.add),
                     reads=[('e_xs', i2), ('e_y', bloc)], writes=[('e_y', bloc)])
                ln_block(k, c, yacc[:, bloc, :], ('e_y', bloc), xs[i2][:, :], ('e_xs', i2), gb, ('e_gb', 0), (st6, mv))
                k.dma('act', out_dram[bs, :], xs[i2][:, :], reads=[('e_xs', i2)])
        k.barrier()


PARAM_SHAPES = None


def pack_params(inp):
    L = inp['w_in'].shape[0]
    f = lambda a: np.ascontiguousarray(np.asarray(a, dtype=np.float32))
    hp = lambda v: v.reshape(4, 64).T
    p = {}
    p['w_in'] = f(inp['w_in'])
    p['w_branch'] = f(inp['w_branch'])
    p['w_out'] = f(inp['w_out'])
    for n in ('ln1_g', 'ln1_b', 'ln2_g', 'ln2_b'):
        p[n] = f(inp[n]).reshape(L, 1, 1024)
    p['r_w'] = f(np.concatenate([inp['r_group'], inp['r_expert']], axis=2))
    p['r_bias'] = f(np.concatenate([inp['r_group_b'], inp['r_expert_b']], axis=1)).reshape(L, 1, 36)
    p['e_gate'] = f(inp['e_gate'])
    p['e_up'] = f(inp['e_up'])
    p['e_down'] = f(inp['e_down'])
    p64 = []
    for l in range(L):
        v0 = inp['c_v0'][max(l - 1, 0)]
        p64.append(np.concatenate([np.asarray(inp['c_mu'][l]).reshape(14, 64).T, hp(np.asarray(inp['c_w0'][l])),
                                   hp(np.asarray(inp['c_a0'][l])), hp(np.asarray(inp['c_kk'][l])),
                                   hp(np.asarray(inp['c_ka'][l])), hp(np.asarray(inp['c_rk'][l]).reshape(-1)),
                                   hp(np.asarray(inp['c_gn_w'][l])), hp(np.asarray(inp['c_gn_b'][l])),
                                   hp(np.asarray(v0))], axis=1))
    p['c_p64'] = f(np.stack(p64))
    p['c_wa2'] = f(np.concatenate([inp['c_w2'], inp['c_a2']], axis=1))
    p['c_g2'] = f(inp['c_g2'])
    p['c_v1'] = f(np.asarray(inp['c_v1']).reshape(L - 1, 4, 64, 16).transpose(0, 2, 1, 3))
    p['c_v2'] = f(inp['c_v2'])
    p['d_cw'] = f(np.asarray(inp['d_conv_w']).transpose(0, 2, 1).reshape(L, 8, 64, 4).transpose(0, 2, 1, 3))
    p['d_cb'] = f(np.asarray(inp['d_conv_b']).reshape(L, 8, 64).transpose(0, 2, 1))
    p['d_nw'] = f(np.asarray(inp['d_norm_w']).reshape(L, 4, 64).transpose(0, 2, 1))
    p['d_vec'] = f(np.concatenate([inp['d_dt_bias'], inp['d_a_log'], inp['d_skip']], axis=1)).reshape(L, 1, 12)
    return p


def build_full(pshapes, T=4096, depth=2, debug=False, phases=None):
    nc = bass.Bass("TRN2", target_bir_lowering=False)
    k = K(nc)
    c = Ctx()
    x = nc.dram_tensor("x", [T, 1024], F32, kind="ExternalInput").ap()
    y = nc.dram_tensor("y", [T, 1024], F32, kind="ExternalOutput").ap()
    P = {n: nc.dram_tensor(n, list(shp), F32, kind="ExternalInput").ap() for n, shp in pshapes.items()}
    c.cin = {n: nc.dram_tensor(n, list(v.shape), F32, kind="ExternalInput").ap() for n, v in make_consts().items()}
    alloc_scratch(nc, c, T, debug=debug)
    kind = 'ExternalOutput' if debug else 'Internal'
    x1 = nc.dram_tensor('x1s', [T, 1024], F32, kind=kind).ap()
    xmid = nc.dram_tensor('xmid', [T, 1024], F32, kind=kind).ap()
    with ExitStack() as st:
        setup_common(nc, k, c, T, st)
        for l in range(depth):
            lp = {n: P[n][l] for n in P if n not in ('c_v1', 'c_v2')}
            if l > 0:
                lp['c_v1'] = P['c_v1'][l - 1]
                lp['c_v2'] = P['c_v2'][l - 1]
            x_in = x if l == 0 else xmid
            x_out = y if l == depth - 1 else xmid
            on = lambda n: phases is None or n in phases
            if on('p'):
                with ExitStack() as st2:
                    c.xT = st2.enter_context(_sbt(nc, 'xT', [128, 8, T], BF16))
                    phase_x(k, c, x_in, T)
                    phase_p(k, c, lp['w_in'], T)
            if on('a'):
                mixer_a(k, c, T)
            if on('b'):
                mixer_b(k, c, T)
            if on('c'):
                mixer_c(k, c, T, lp, l)
            if on('d'):
                mixer_d(k, c, T, lp)
            if on('m'):
                phase_merge(k, c, T, lp, x_in, x1)
            if on('e'):
                phase_moe(k, c, T, lp, x1, x_out)
        k.barrier()
    return nc, k


def kernel(**inputs):
    x = np.asarray(inputs['x'], dtype=np.float32)
    B, T, _ = x.shape
    p = pack_params(inputs)
    pshapes = {n: v.shape for n, v in p.items()}
    nc, _k = build_full(pshapes, T=T, depth=p['w_in'].shape[0])
    cs = make_consts()
    in_maps = []
    for b in range(B):
        m = {'x': np.ascontiguousarray(x[b])}
        m.update(p)
        m.update(cs)
        in_maps.append(m)
    res = run_bass_kernel_spmd(nc, in_maps, core_ids=list(range(B)))
    return np.stack([np.asarray(r['y'], dtype=np.float32) for r in res.results], axis=0)
```

```python
import numpy as np
from contextlib import ExitStack
import concourse.bass as bass
import concourse.mybir as mybir
from concourse.bass_utils import run_bass_kernel_spmd

F32 = mybir.dt.float32
BF16 = mybir.dt.bfloat16
AF = mybir.ActivationFunctionType
ALU = mybir.AluOpType
AX = mybir.AxisListType

ENG = ('pe', 'act', 'dve', 'pool', 'sp')
SAME_ENG_SYNC = True


_UID = [0]


def _sbt(nc, name, shape, dtype):
    _UID[0] += 1
    return nc.sbuf_tensor('%s_u%d' % (name, _UID[0]), shape, dtype)


class K:
    def __init__(self, nc):
        self.nc = nc
        self.eng = {'pe': nc.tensor, 'act': nc.scalar, 'dve': nc.vector,
                    'pool': nc.gpsimd, 'sp': nc.sync}
        self.sem = {e: nc.alloc_semaphore('s_' + e) for e in ENG}
        self.cnt = {e: 0 for e in ENG}
        self.known = {e: {} for e in ENG}
        self.res = {}
        self.NDS = 8
        self.dq = ('sp', 'act', 'pool')
        self.dsem = {q: [nc.alloc_semaphore('d_%s_%d' % (q, i)) for i in range(self.NDS)]
                     for q in self.dq}
        self.dcnt = {q: 0 for q in self.dq}
        self.semobj = {}
        for e in ENG:
            self.semobj['E' + e] = self.sem[e]
        for q in self.dq:
            for i in range(self.NDS):
                self.semobj['D%s%d' % (q, i)] = self.dsem[q][i]
        self.ninst = 0

    def _collect(self, reads, writes):
        need = {}
        for r in reads:
            st = self.res.get(r)
            if st is not None and st[0] is not None:
                k, v = st[0]
                if need.get(k, 0) < v:
                    need[k] = v
        for w in writes:
            st = self.res.get(w)
            if st is not None:
                if st[0] is not None:
                    k, v = st[0]
                    if need.get(k, 0) < v:
                        need[k] = v
                for k, v in st[1].items():
                    if need.get(k, 0) < v:
                        need[k] = v
        return need

    def _wait(self, e, need):
        kn = self.known[e]
        for k, v in need.items():
            if k == 'E' + e and (e == 'pe' or not SAME_ENG_SYNC):
                continue
            if kn.get(k, 0) < v:
                self.eng[e].wait_ge(self.semobj[k], v)
                kn[k] = v
                self.ninst += 1

    def _record(self, ev, reads, writes):
        for w in writes:
            self.res[w] = [ev, {}]
        for r in reads:
            st = self.res.get(r)
            if st is None:
                st = [None, {}]
                self.res[r] = st
            k, v = ev
            if st[1].get(k, 0) < v:
                st[1][k] = v

    def op(self, e, fn, reads=(), writes=()):
        if any(r[0] == 'ps' for r in reads):
            writes = list(writes) + [r for r in reads if r[0] == 'ps']
            reads = [r for r in reads if r[0] != 'ps']
        self._wait(e, self._collect(reads, writes))
        inst = fn(self.eng[e])
        self.cnt[e] += 1
        inst.then_inc(self.sem[e], 1)
        self.ninst += 1
        self._record(('E' + e, self.cnt[e]), reads, writes)

    def dma(self, q, out, in_, reads=(), writes=(), **kw):
        need = self._collect(reads, writes)
        n = self.dcnt[q]
        slot, rnd = n % self.NDS, n // self.NDS
        key = 'D%s%d' % (q, slot)
        if rnd > 0 and need.get(key, 0) < 16 * rnd:
            need[key] = 16 * rnd
        self._wait(q, need)
        inst = self.eng[q].dma_start(out=out, in_=in_, **kw)
        inst.then_inc(self.dsem[q][slot], 16)
        self.dcnt[q] = n + 1
        self.ninst += 1
        self._record((key, 16 * (rnd + 1)), reads, writes)

    def all_events(self):
        need = {}
        for e in ENG:
            if self.cnt[e] > 0:
                need['E' + e] = self.cnt[e]
        for q in self.dq:
            n = self.dcnt[q]
            for slot in range(self.NDS):
                uses = (n - slot + self.NDS - 1) // self.NDS if n > slot else 0
                if uses > 0:
                    need['D%s%d' % (q, slot)] = 16 * uses
        return need

    def barrier(self, engines=ENG):
        need = self.all_events()
        for e in engines:
            self._wait(e, dict(need))
        self.res = {}


D_MODEL = 1024
IN_W = 9160
OFF = dict(a_qkv=0, b_qkv=2304, biq=3072, bik=3328, biw=3392, c_in=3396, d_z=4292, d_xbc=4548,
           d_dt=5060, gates=5064)
A_PAT = ((128, 1), (512, 4), (2048, 16))


class Ctx:
    pass


def phase_x(k, c, x_dram, T, xT=None):
    nc = k.nc
    NB = T // 128
    if xT is None:
        xT = c.xT
    with ExitStack() as st:
        xs = [st.enter_context(_sbt(nc, 'xs%d' % i, [128, 1024], F32)) for i in range(2)]
        for b in range(NB):
            s = xs[b % 2]
            k.dma('sp' if b % 2 == 0 else 'act', s[:, :], x_dram[b * 128:(b + 1) * 128, :],
                  writes=[('xs', b % 2)])
            for half in range(2):
                bank = (2 * b + half) % 8
                ps = c.ps[bank]
                for j in range(4):
                    ch = half * 4 + j
                    k.op('pe', lambda e, ps=ps, j=j, ch=ch, s=s: e.transpose(
                        out=ps[:, j * 128:(j + 1) * 128], in_=s[:, ch * 128:(ch + 1) * 128],
                        identity=c.ident[:, :]),
                        reads=[('xs', b % 2)], writes=[('ps', bank)])
                dst = xT[:, half * 4:(half + 1) * 4, b * 128:(b + 1) * 128]
                src_ = ps[:, :].rearrange('p (j n) -> p j n', j=4)
                if half == 0:
                    k.op('act', lambda e, dst=dst, src_=src_: e.copy(out=dst, in_=src_),
                         reads=[('ps', bank)], writes=[('xT', b)])
                else:
                    k.op('dve', lambda e, dst=dst, src_=src_: e.tensor_copy(out=dst, in_=src_),
                         reads=[('ps', bank)], writes=[('xT', b)])
        k.barrier()


class WLoader:
    def __init__(self, k, st, name, width=512, nbuf=2):
        nc = k.nc
        self.k = k
        self.name = name
        self.nbuf = nbuf
        self.f = [st.enter_context(_sbt(nc, '%s_f%d' % (name, i), [128, 8, width], F32)) for i in range(nbuf)]
        self.b = [st.enter_context(_sbt(nc, '%s_b%d' % (name, i), [128, 8, width], BF16)) for i in range(nbuf)]
        self.n = 0

    def load(self, w_dram, col0, ncols, q='sp', cast='pool'):
        k = self.k
        i = self.n % self.nbuf
        self.n += 1
        src = w_dram[:, col0:col0 + ncols].rearrange('(ko ki) n -> ki ko n', ki=128)
        k.dma(q, self.f[i][:, :, 0:ncols], src, writes=[(self.name + 'f', i)])
        fi, bi = self.f[i], self.b[i]
        if cast == 'pool':
            k.op('pool', lambda e: e.tensor_copy(out=bi[:, :, 0:ncols], in_=fi[:, :, 0:ncols]),
                 reads=[(self.name + 'f', i)], writes=[(self.name + 'b', i)])
        else:
            k.op('dve', lambda e: e.tensor_copy(out=bi[:, :, 0:ncols], in_=fi[:, :, 0:ncols]),
                 reads=[(self.name + 'f', i)], writes=[(self.name + 'b', i)])
        return bi, (self.name + 'b', i)


def phase_p(k, c, w_in, T, only=None):
    nc = k.nc
    NB = T // 128
    NG = T // 512
    d = c.d
    with ExitStack() as st:
        wl = WLoader(k, st, 'wl')
        stg = [st.enter_context(_sbt(nc, 'pstg%d' % i, [128, T], F32)) for i in range(2)]
        stgb = [st.enter_context(_sbt(nc, 'pstgb%d' % i, [128, T], BF16)) for i in range(2)]
        tst = [st.enter_context(_sbt(nc, 'ptst%d' % i, [128, 512], F32)) for i in range(2)]
        tstb = [st.enter_context(_sbt(nc, 'ptstb%d' % i, [128, 512], BF16)) for i in range(2)]
        cnt = {'bank': 0, 'fm': 0, 'tm': 0, 'ev': 0}

        def evac(dst, src, bank, wkey, func=None, scale=1.0):
            cnt['ev'] += 1
            if func is not None or cnt['ev'] % 2 == 0:
                f = func if func is not None else AF.Copy
                k.op('act', lambda e: e.activation(out=dst, in_=src, func=f, scale=scale),
                     reads=[('ps', bank)], writes=[wkey])
            else:
                k.op('dve', lambda e: e.tensor_scalar(out=dst, in0=src, scalar1=float(scale), scalar2=None,
                                                      op0=ALU.mult),
                     reads=[('ps', bank)], writes=[wkey])

        def fm_group(col0, total, cw, dst_fn, bf, pad=0, scale=1.0):
            for s0 in range(0, total, 512):
                sw = min(512, total - s0)
                wt, wkey = wl.load(w_in, col0 + s0, sw)
                for j0 in range(0, sw, cw):
                    i = cnt['fm'] % 2
                    cnt['fm'] += 1
                    sg = stgb[i] if bf else stg[i]
                    skey = ('pstgb' if bf else 'pstg', i)
                    for g in range(NG):
                        bank = cnt['bank'] % 8
                        cnt['bank'] += 1
                        ps = c.ps[bank]
                        for kk in range(8):
                            k.op('pe', lambda e, ps=ps, kk=kk, j0=j0, g=g: e.matmul(
                                ps[0:cw, :], lhsT=wt[:, kk, j0:j0 + cw], rhs=c.xT[:, kk, g * 512:(g + 1) * 512],
                                start=(kk == 0), stop=(kk == 7)),
                                reads=[wkey, ('xT', 0)], writes=[('ps', bank)])
                        evac(sg[0:cw, g * 512:(g + 1) * 512], ps[0:cw, :], bank, skey, scale=scale)
                    k.dma('sp' if cnt['fm'] % 2 else 'act', dst_fn((s0 + j0) // cw), sg[0:cw, :], reads=[skey])

        def tm_group(col0, ncols, dst_fn, bf, func=None, tokens=None, nblk=None):
            wt, wkey = wl.load(w_in, col0, ncols)
            for b in range(nblk if nblk is not None else NB):
                tok = tokens(b) if tokens is not None else slice(b * 128, (b + 1) * 128)
                bank = cnt['bank'] % 8
                cnt['bank'] += 1
                ps = c.ps[bank]
                for kk in range(8):
                    k.op('pe', lambda e, ps=ps, kk=kk, tok=tok: e.matmul(
                        ps[:, 0:ncols], lhsT=c.xT[:, kk, tok], rhs=wt[:, kk, 0:ncols],
                        start=(kk == 0), stop=(kk == 7)),
                        reads=[wkey, ('xT', 0)], writes=[('ps', bank)])
                i = cnt['tm'] % 2
                cnt['tm'] += 1
                sg = tstb[i] if bf else tst[i]
                skey = ('ptstb' if bf else 'ptst', i)
                evac(sg[:, 0:ncols], ps[:, 0:ncols], bank, skey, func=func)
                k.dma('sp' if cnt['tm'] % 2 else 'act', dst_fn(b), sg[:, 0:ncols], reads=[skey])

        def want(n):
            return only is None or n in only

        if want('a'):
            fm_group(OFF['a_qkv'], 768, 64, lambda j: d['aq'][j], True)
            fm_group(OFF['a_qkv'] + 768, 768, 64, lambda j: d['ak'][j], True)
            for g, (win, dil) in enumerate(A_PAT):
                nbc = T // (128 * dil)

                def toks(b, dil=dil, nbc=nbc):
                    r, bi = b // nbc, b % nbc
                    s0 = r + dil * 128 * bi
                    return slice(s0, s0 + dil * 127 + 1, dil)
                tm_group(OFF['a_qkv'] + 1536 + g * 256, 256, lambda b, g=g: d['av'][g, b], True, tokens=toks)
        if want('b'):
            fm_group(OFF['b_qkv'], 256, 64, lambda j: d['bq'][j], True)
            fm_group(OFF['b_qkv'] + 256, 256, 64, lambda j: d['bk'][j], True)
            tm_group(OFF['b_qkv'] + 512, 256, lambda b: d['bv'][b], True)
            fm_group(OFF['biq'], 256, 64, lambda j: d['biq'][j], True)
            fm_group(OFF['bik'], 64, 64, lambda j: d['bik'][j], True)
            tm_group(OFF['biw'], 4, lambda b: d['biw'][b], False)
        if want('c'):
            fm_group(OFF['c_in'], 896, 64, lambda j: d['cpc'][j, :, 1:T + 1], False)
        if want('d'):
            fm_group(OFF['d_z'], 256, 64, lambda j: d['dz'][j], False)
            fm_group(OFF['d_xbc'], 512, 64, lambda j: d['dxbc'][j, :, 3:T + 3], False)
            tm_group(OFF['d_dt'], 4, lambda b: d['ddt'][b], False)
        if want('g'):
            for s in range(8):
                tm_group(OFF['gates'] + s * 512, 512, lambda b, s=s: d['gsig'][b, :, s * 512:(s + 1) * 512], True,
                         func=AF.Sigmoid)
        k.barrier()


def alloc_scratch(nc, c, T, debug=False):
    NB = T // 128
    kind = 'ExternalOutput' if debug else 'Internal'
    d = {}

    def dt(name, shape, dtype):
        d[name] = nc.dram_tensor(name, shape, dtype, kind=kind).ap()
    dt('aq', [12, 64, T], BF16)
    dt('ak', [12, 64, T], BF16)
    dt('av', [3, NB, 128, 256], BF16)
    dt('bq', [4, 64, T], BF16)
    dt('bk', [4, 64, T], BF16)
    dt('bv', [NB, 128, 256], BF16)
    dt('biq', [4, 64, T], BF16)
    dt('bik', [1, 64, T], BF16)
    dt('biw', [NB, 128, 4], F32)
    dt('cpc', [14, 64, T + 1], F32)
    dt('dz', [4, 64, T], F32)
    dt('dxbc', [8, 64, T + 3], F32)
    dt('ddt', [NB, 128, 4], F32)
    dt('gsig', [NB, 128, 4096], BF16)
    dt('dxs', [4, 64, T], F32)
    dt('vfirst', [4, 64, T], F32)
    dt('obr', [4, 4, 64, T], BF16)
    c.d = d


def setup_common(nc, k, c, T, st):
    c.ps = [nc.alloc_psum_tensor('ps%d' % i, [128, 512], F32) for i in range(8)]
    c.ident = st.enter_context(_sbt(nc, 'ident_sb', [128, 128], F32))
    k.dma('sp', c.ident[:, :], c.cin['ident'], writes=[('ident', 0)])
    tmp = st.enter_context(_sbt(nc, 'cst_tmp', [128, 256], F32))
    c.maskA = st.enter_context(_sbt(nc, 'maskA_sb', [128, 256], BF16))
    k.dma('sp', tmp[:, :], c.cin['maskA'], writes=[('cst_tmp', 0)])
    k.op('dve', lambda e: e.tensor_copy(out=c.maskA[:, :], in_=tmp[:, :]), reads=[('cst_tmp', 0)],
         writes=[('maskA', 0)])
    c.negmask = st.enter_context(_sbt(nc, 'negmask_sb', [128, 128], F32))
    k.dma('act', c.negmask[:, :], c.cin['negmask'], writes=[('negmask', 0)])
    c.triu = st.enter_context(_sbt(nc, 'triu_sb', [128, 128], F32))
    k.dma('pool', c.triu[:, :], c.cin['triu'], writes=[('triu', 0)])
    c.ones_f = st.enter_context(_sbt(nc, 'ones_f_sb', [128, 128], F32))
    k.dma('sp', c.ones_f[:, :], c.cin['ones_f'], writes=[('ones_f', 0)])
    c.eps5 = st.enter_context(_sbt(nc, 'eps5_sb', [128, 1], F32))
    k.dma('act', c.eps5[:, :], c.cin['eps5'], writes=[('eps', 0)])
    c.mhalf = st.enter_context(_sbt(nc, 'mhalf_sb', [128, 1], F32))
    k.dma('pool', c.mhalf[:, :], c.cin['mhalf'], writes=[('mhalf', 0)])
    c.epsgn = st.enter_context(_sbt(nc, 'epsgn_sb', [128, 1], F32))
    k.dma('sp', c.epsgn[:, :], c.cin['epsgn'], writes=[('epsgn', 0)])
    c.ones_bf = st.enter_context(_sbt(nc, 'ones_bf', [128, 128], BF16))
    k.op('dve', lambda e: e.memset(c.ones_bf[:, :], 1.0), writes=[('ones_bf', 0)])
    k.barrier()


def mixer_a(k, c, T):
    nc = k.nc
    NB = T // 128
    d = c.d
    with ExitStack() as st:
        vall = st.enter_context(_sbt(nc, 'a_v', [128, 3, NB, 256], BF16))
        for g in range(3):
            k.dma(('sp', 'act', 'pool')[g], vall[:, g, :, :], d['av'][g].rearrange('b p c -> p b c'),
                  writes=[('a_v', g)])
        qk = [[st.enter_context(_sbt(nc, 'a_qk%d%d' % (g, s), [64, T], BF16)) for s in range(2)]
              for g in range(3)]
        uz = st.enter_context(_sbt(nc, 'a_uz', [64, 2, T], F32))
        rz = st.enter_context(_sbt(nc, 'a_rz', [64, T], F32))
        ob = st.enter_context(_sbt(nc, 'a_ob', [64, T], BF16))
        Eb = [st.enter_context(_sbt(nc, 'a_E%d' % i, [128, 256], BF16)) for i in range(2)]
        Pb = [st.enter_context(_sbt(nc, 'a_P%d' % i, [128, 256], BF16)) for i in range(2)]
        it = 0
        for h in range(4):
            for g in range(3):
                k.dma('sp', qk[g][0][:, :], d['aq'][g * 4 + h], writes=[('a_q', g)])
                k.dma('act', qk[g][1][:, :], d['ak'][g * 4 + h], writes=[('a_k', g)])
            for g, (win, dil) in enumerate(A_PAT):
                nbc = T // (128 * dil)
                qT, kT = qk[g]
                for blk in range(NB):
                    r, bi = blk // nbc, blk % nbc
                    s0 = r + dil * 128 * bi
                    qs = slice(s0, s0 + dil * 127 + 1, dil)
                    bs, bu = it % 4, 4 + it % 4
                    ps_s, ps_u = c.ps[bs], c.ps[bu]
                    i2 = it % 2
                    it += 1
                    lo = 128 if bi == 0 else 0
                    tiles = ([] if bi == 0 else [(0, blk - 1, s0 - dil * 128)]) + [(1, blk, s0)]
                    for slot, kb, ks0 in tiles:
                        ks = slice(ks0, ks0 + dil * 127 + 1, dil)
                        k.op('pe', lambda e, slot=slot, ks=ks: e.matmul(
                            ps_s[:, slot * 128:(slot + 1) * 128], lhsT=kT[:, ks], rhs=qT[:, qs],
                            start=True, stop=True),
                            reads=[('a_q', g), ('a_k', g)], writes=[('ps', bs)])
                    E, P = Eb[i2], Pb[i2]
                    k.op('act', lambda e: e.activation(out=E[:, lo:256], in_=ps_s[:, lo:256], func=AF.Exp,
                                                       scale=0.125),
                         reads=[('ps', bs)], writes=[('a_E', i2)])
                    k.op('dve', lambda e: e.tensor_tensor(out=P[:, lo:256], in0=E[:, lo:256],
                                                          in1=c.maskA[:, lo:256], op=ALU.mult),
                         reads=[('a_E', i2), ('maskA', 0)], writes=[('a_P', i2)])
                    for ti, (slot, kb, ks0) in enumerate(tiles):
                        first, last = ti == 0, ti == len(tiles) - 1
                        k.op('pe', lambda e, slot=slot, kb=kb, first=first, last=last: e.matmul(
                            ps_u[0:64, 0:128], lhsT=vall[:, g, kb, h * 64:(h + 1) * 64],
                            rhs=P[:, slot * 128:(slot + 1) * 128], start=first, stop=last,
                            skip_group_check=True),
                            reads=[('a_P', i2), ('a_v', g)], writes=[('ps', bu)])
                        k.op('pe', lambda e, slot=slot, first=first, last=last: e.matmul(
                            ps_u[0:64, 128:256], lhsT=c.ones_bf[:, 0:64],
                            rhs=P[:, slot * 128:(slot + 1) * 128], start=False, stop=last,
                            skip_group_check=True),
                            reads=[('a_P', i2), ('ones_bf', 0)], writes=[('ps', bu)])
                    dst = uz[:, :, qs]
                    src = ps_u[0:64, 0:256].rearrange('p (a n) -> p a n', a=2)
                    if g == 0:
                        k.op('act', lambda e, dst=dst, src=src: e.copy(out=dst, in_=src),
                             reads=[('ps', bu)], writes=[('a_uz', 0)])
                    else:
                        k.op('dve', lambda e, dst=dst, src=src: e.tensor_tensor(out=dst, in0=dst, in1=src,
                                                                                op=ALU.add),
                             reads=[('ps', bu), ('a_uz', 0)], writes=[('a_uz', 0)])
            k.op('dve', lambda e: e.reciprocal(out=rz[:, :], in_=uz[:, 1, :]), reads=[('a_uz', 0)],
                 writes=[('a_rz', 0)])
            k.op('dve', lambda e: e.tensor_tensor(out=ob[:, :], in0=uz[:, 0, :], in1=rz[:, :], op=ALU.mult),
                 reads=[('a_uz', 0), ('a_rz', 0)], writes=[('a_ob', 0)])
            k.dma('sp', d['obr'][0, h], ob[:, :], reads=[('a_ob', 0)])
        k.barrier()


def make_consts():
    j = np.arange(128)[:, None]
    i = np.arange(128)[None, :]
    cs = {}
    cs['ident'] = np.eye(128, dtype=np.float32)
    cs['maskA'] = np.concatenate([(j >= i), (j <= i)], axis=1).astype(np.float32)
    cs['triu'] = (j <= i).astype(np.float32)
    cs['ones_f'] = np.ones((128, 128), np.float32)
    cs['eps5'] = np.full((128, 1), 1e-5, np.float32)
    cs['mhalf'] = np.full((128, 1), -0.5, np.float32)
    cs['epsgn'] = np.full((128, 1), 64e-5, np.float32)
    s_ = np.arange(64)[:, None]
    t_ = np.arange(64)[None, :]
    cs['cmask'] = np.stack([(s_ < t_), (s_ <= t_), (s_ > t_)], axis=1).astype(np.float32)
    cs['negmask'] = np.where(i <= j, 0.0, -1.0e30).astype(np.float32)
    return cs


NEG = -1.0e30


def mixer_b(k, c, T):
    nc = k.nc
    NB = T // 128
    d = c.d
    with ExitStack() as st:
        def sb(name, shape, dt_):
            return st.enter_context(_sbt(nc, name, shape, dt_))
        bqs = [sb('b_q%d' % i, [64, 4, 128], BF16) for i in range(2)]
        bk = sb('b_k', [64, 4, T], BF16)
        biqs = [sb('b_iq%d' % i, [64, 4, 128], BF16) for i in range(2)]
        bik = sb('b_ik', [64, T], BF16)
        bv = sb('b_v', [128, NB, 256], BF16)
        biw = sb('b_iw', [128, NB, 4], F32)
        score = sb('b_score', [128, T], F32)
        work = sb('b_work', [128, T], F32)
        cum = sb('b_cum', [128, T], F32)
        ngt = sb('b_ngt', [128, 2], F32)
        selT = sb('b_selT', [128, NB, 128], BF16)
        rt = [sb('b_rt%d' % i, [128, 512], F32) for i in range(2)]
        m8 = [sb('b_m8%d' % i, [128, 8], F32) for i in range(2)]
        Eb = [sb('b_E%d' % i, [128, 512], BF16) for i in range(2)]
        Pb = [sb('b_P%d' % i, [128, 512], BF16) for i in range(2)]
        rz = sb('b_rz', [64, 128], F32)
        ob = [sb('b_ob%d' % i, [64, 128], BF16) for i in range(2)]
        k.dma('act', bk[:, :, :], d['bk'].rearrange('h p t -> p h t'), writes=[('b_k', 0)])
        k.dma('sp', bik[:, :], d['bik'][0], writes=[('b_ik', 0)])
        k.dma('act', bv[:, :, :], d['bv'].rearrange('b p c -> p b c'), writes=[('b_v', 0)])
        k.dma('pool', biw[:, :, :], d['biw'].rearrange('b p c -> p b c'), writes=[('b_iw', 0)])
        cn = {'bank': 0, 'rt': 0, 'ep': 0, 'ob': 0}

        def nbank():
            b = cn['bank'] % 8
            cn['bank'] += 1
            return b
        for qb in range(NB):
            L = 128 * (qb + 1)
            qs = slice(qb * 128, (qb + 1) * 128)
            bq, biq = bqs[qb % 2], biqs[qb % 2]
            k.dma('sp', bq[:, :, :], d['bq'][:, :, qs].rearrange('h p t -> p h t'), writes=[('b_q', qb % 2)])
            k.dma('act', biq[:, :, :], d['biq'][:, :, qs].rearrange('h p t -> p h t'), writes=[('b_iq', qb % 2)])
            for kg in range((L + 511) // 512):
                n = min(512, L - kg * 512)
                seg = slice(kg * 512, kg * 512 + n)
                for ih in range(4):
                    bank = nbank()
                    ps = c.ps[bank]
                    k.op('pe', lambda e: e.matmul(ps[:, 0:n], lhsT=biq[:, ih, :], rhs=bik[:, seg],
                                                  start=True, stop=True),
                         reads=[('b_iq', qb % 2), ('b_ik', 0)], writes=[('ps', bank)])
                    ri = cn['rt'] % 2
                    cn['rt'] += 1
                    r_ = rt[ri]
                    k.op('act', lambda e: e.activation(out=r_[:, 0:n], in_=ps[:, 0:n], func=AF.Relu),
                         reads=[('ps', bank)], writes=[('b_rt', ri)])
                    if ih == 0:
                        k.op('dve', lambda e: e.tensor_scalar(out=score[:, seg], in0=r_[:, 0:n],
                                                              scalar1=biw[:, qb, 0:1], scalar2=None, op0=ALU.mult),
                             reads=[('b_rt', ri), ('b_iw', 0)], writes=[('b_score', 0)])
                    else:
                        k.op('dve', lambda e: e.scalar_tensor_tensor(
                            out=score[:, seg], in0=r_[:, 0:n], scalar=biw[:, qb, ih:ih + 1], in1=score[:, seg],
                            op0=ALU.mult, op1=ALU.add),
                            reads=[('b_rt', ri), ('b_iw', 0), ('b_score', 0)], writes=[('b_score', 0)])
            k.op('dve', lambda e: e.tensor_tensor(out=score[:, qs], in0=score[:, qs], in1=c.negmask[:, :],
                                                  op=ALU.add),
                 reads=[('b_score', 0), ('negmask', 0)], writes=[('b_score', 0)])
            if qb >= 2:
                for r in range(32):
                    m = m8[r % 2]
                    src = score if r == 0 else work
                    k.op('dve', lambda e: e.max(out=m[:, :], in_=src[:, 0:L]),
                         reads=[('b_score', 0), ('b_work', 0)], writes=[('b_m8', r % 2)])
                    if r < 31:
                        k.op('dve', lambda e: e.match_replace(out=work[:, 0:L], in_to_replace=m[:, :],
                                                              in_values=src[:, 0:L], imm_value=NEG),
                             reads=[('b_score', 0), ('b_m8', r % 2)], writes=[('b_work', 0)])
                thr = m8[1][:, 7:8]
                k.op('dve', lambda e: e.tensor_scalar(out=work[:, 0:L], in0=score[:, 0:L], scalar1=thr,
                                                      scalar2=0.0, op0=ALU.is_gt, op1=ALU.add,
                                                      accum_out=ngt[:, 0:1]),
                     reads=[('b_score', 0), ('b_m8', 1)], writes=[('b_work', 0), ('b_ngt', 0)])
                k.op('dve', lambda e: e.tensor_scalar(out=ngt[:, 1:2], in0=ngt[:, 0:1], scalar1=-1.0,
                                                      scalar2=256.0, op0=ALU.mult, op1=ALU.add),
                     reads=[('b_ngt', 0)], writes=[('b_ngt', 1)])
                k.op('dve', lambda e: e.tensor_scalar(out=work[:, 0:L], in0=score[:, 0:L], scalar1=thr,
                                                      scalar2=None, op0=ALU.is_equal),
                     reads=[('b_score', 0), ('b_m8', 1), ('b_ngt', 0)], writes=[('b_work', 0)])
                k.op('dve', lambda e: e.tensor_tensor_scan(out=cum[:, 0:L], data0=work[:, 0:L],
                                                           data1=work[:, 0:L], initial=0.0,
                                                           op0=ALU.add, op1=ALU.max),
                     reads=[('b_work', 0)], writes=[('b_cum', 0)])
                k.op('dve', lambda e: e.scalar_tensor_tensor(out=cum[:, 0:L], in0=cum[:, 0:L],
                                                             scalar=ngt[:, 1:2], in1=work[:, 0:L],
                                                             op0=ALU.is_le, op1=ALU.mult),
                     reads=[('b_work', 0), ('b_cum', 0), ('b_ngt', 1)], writes=[('b_cum', 0)])
                k.op('dve', lambda e: e.scalar_tensor_tensor(out=work[:, 0:L], in0=score[:, 0:L],
                                                             scalar=thr, in1=cum[:, 0:L],
                                                             op0=ALU.is_gt, op1=ALU.add),
                     reads=[('b_score', 0), ('b_cum', 0), ('b_m8', 1)], writes=[('b_work', 0)])
            else:
                k.op('dve', lambda e: e.tensor_scalar(out=work[:, 0:L], in0=score[:, 0:L],
                                                      scalar1=-1.0e29, scalar2=None, op0=ALU.is_ge),
                     reads=[('b_score', 0)], writes=[('b_work', 0)])
            for kb0 in range(0, qb + 1, 4):
                nk = min(4, qb + 1 - kb0)
                bank = nbank()
                ps = c.ps[bank]
                for j in range(nk):
                    kb = kb0 + j
                    k.op('pe', lambda e: e.transpose(out=ps[:, j * 128:(j + 1) * 128],
                                                     in_=work[:, kb * 128:(kb + 1) * 128], identity=c.ident[:, :]),
                         reads=[('b_work', 0)], writes=[('ps', bank)])
                k.op('act', lambda e: e.copy(out=selT[:, kb0:kb0 + nk, :],
                                             in_=ps[:, 0:nk * 128].rearrange('p (a n) -> p a n', a=nk)),
                     reads=[('ps', bank)], writes=[('b_selT', 0)])
            for h in range(4):
                bu = nbank()
                ps_u = c.ps[bu]
                for kb0 in range(0, qb + 1, 4):
                    nk = min(4, qb + 1 - kb0)
                    bs = nbank()
                    if bs == bu:
                        bs = nbank()
                    ps_s = c.ps[bs]
                    for j in range(nk):
                        kb = kb0 + j
                        k.op('pe', lambda e: e.matmul(ps_s[:, j * 128:(j + 1) * 128],
                                                      lhsT=bk[:, h, kb * 128:(kb + 1) * 128], rhs=bq[:, h, :],
                                                      start=True, stop=True),
                             reads=[('b_q', qb % 2), ('b_k', 0)], writes=[('ps', bs)])
                    ei = cn['ep'] % 2
                    cn['ep'] += 1
                    E, P = Eb[ei], Pb[ei]
                    k.op('act', lambda e: e.activation(out=E[:, 0:nk * 128], in_=ps_s[:, 0:nk * 128], func=AF.Exp,
                                                       scale=0.125),
                         reads=[('ps', bs)], writes=[('b_E', ei)])
                    k.op('dve', lambda e: e.tensor_tensor(
                        out=P[:, 0:nk * 128], in0=E[:, 0:nk * 128],
                        in1=selT[:, kb0:kb0 + nk, :].rearrange('p a n -> p (a n)'), op=ALU.mult),
                        reads=[('b_E', ei), ('b_selT', 0)], writes=[('b_P', ei)])
                    for j in range(nk):
                        kb = kb0 + j
                        first = (kb == 0)
                        last = (kb == qb)
                        k.op('pe', lambda e: e.matmul(ps_u[0:64, 0:128], lhsT=bv[:, kb, h * 64:(h + 1) * 64],
                                                      rhs=P[:, j * 128:(j + 1) * 128], start=first, stop=last,
                                                      skip_group_check=True),
                             reads=[('b_P', ei), ('b_v', 0)], writes=[('ps', bu)])
                        k.op('pe', lambda e: e.matmul(ps_u[0:64, 128:256], lhsT=c.ones_bf[:, 0:64],
                                                      rhs=P[:, j * 128:(j + 1) * 128], start=False, stop=last,
                                                      skip_group_check=True),
                             reads=[('b_P', ei), ('ones_bf', 0)], writes=[('ps', bu)])
                k.op('dve', lambda e: e.reciprocal(out=rz[:, :], in_=ps_u[0:64, 128:256]),
                     reads=[('ps', bu)], writes=[('b_rz', 0)])
                oi = cn['ob'] % 2
                cn['ob'] += 1
                o_ = ob[oi]
                k.op('dve', lambda e: e.tensor_tensor(out=o_[:, :], in0=ps_u[0:64, 0:128], in1=rz[:, :],
                                                      op=ALU.mult),
                     reads=[('ps', bu), ('b_rz', 0)], writes=[('b_ob', oi)])
                k.dma('sp' if oi == 0 else 'act', d['obr'][1, h, :, qs], o_[:, :], reads=[('b_ob', oi)])
        k.barrier()


def mixer_d(k, c, T, lp):
    nc = k.nc
    NB = T // 128
    d = c.d
    with ExitStack() as st:
        def sb(name, shape, dt_):
            return st.enter_context(_sbt(nc, 'sb_' + name, shape, dt_))
        BT = sb('d_BT', [64, 2, T], BF16)
        CT = sb('d_CT', [64, 2, T], BF16)
        xbar = sb('d_xbar', [128, NB, 256], BF16)
        Btok = sb('d_Btok', [128, NB, 2, 64], BF16)
        dte = sb('d_dte', [128, NB, 4], F32)
        etot = sb('d_etot', [128, NB, 4], F32)
        cw = sb('d_cw', [64, 8, 4], F32)
        cb = sb('d_cb', [64, 8], F32)
        nw = sb('d_nw', [64, 4], F32)
        dvec = sb('d_vec', [128, 12], F32)
        dt_ = sb('d_dt', [128, NB, 4], F32)
        a_tok = sb('d_atok', [128, NB, 4], F32)
        acs = sb('d_acs', [128, NB, 4], F32)
        tot = sb('d_tot', [128, NB, 4], F32)
        pre = sb('d_pre', [128, NB, 4], F32)
        Abc = sb('d_Abc', [128, 4], F32)
        k.dma('sp', cw[:, :, :], lp['d_cw'], writes=[('d_cw', 0)])
        k.dma('act', cb[:, :], lp['d_cb'], writes=[('d_cb', 0)])
        k.dma('pool', nw[:, :], lp['d_nw'], writes=[('d_nw', 0)])
        k.dma('sp', dvec[:, :], lp['d_vec'].partition_broadcast(128), writes=[('d_vec', 0)])
        k.dma('act', dt_[:, :, :], d['ddt'].rearrange('b p c -> p b c'), writes=[('d_dt', 0)])
        k.op('dve', lambda e: e.tensor_tensor(out=dt_[:, :, :], in0=dt_[:, :, :],
                                              in1=dvec[:, 0:4].unsqueeze(1).to_broadcast([128, NB, 4]), op=ALU.add),
             reads=[('d_dt', 0), ('d_vec', 0)], writes=[('d_dt', 0)])
        k.op('act', lambda e: e.activation(out=dt_[:, :, :], in_=dt_[:, :, :], func=AF.Exp),
             reads=[('d_dt', 0)], writes=[('d_dt', 0)])
        k.op('act', lambda e: e.activation(out=dt_[:, :, :], in_=dt_[:, :, :], func=AF.Ln, bias=1.0),
             reads=[('d_dt', 0)], writes=[('d_dt', 0)])
        k.op('act', lambda e: e.activation(out=Abc[:, :], in_=dvec[:, 4:8], func=AF.Exp),
             reads=[('d_vec', 0)], writes=[('d_Abc', 0)])
        k.op('dve', lambda e: e.scalar_tensor_tensor(out=a_tok[:, :, :], in0=dt_[:, :, :], scalar=-1.0,
                                                     in1=Abc[:, :].unsqueeze(1).to_broadcast([128, NB, 4]),
                                                     op0=ALU.mult, op1=ALU.mult),
             reads=[('d_dt', 0), ('d_Abc', 0)], writes=[('d_atok', 0)])
        bank = 0
        ps = c.ps[bank]
        k.op('pe', lambda e: e.matmul(ps[:, 0:NB * 4], lhsT=c.triu[:, :], rhs=a_tok[:, :, :].rearrange('p b c -> p (b c)'),
                                      start=True, stop=True),
             reads=[('d_atok', 0), ('triu', 0)], writes=[('ps', bank)])
        k.op('dve', lambda e: e.tensor_copy(out=acs[:, :, :].rearrange('p b c -> p (b c)'), in_=ps[:, 0:NB * 4]),
             reads=[('ps', bank)], writes=[('d_acs', 0)])
        bank = 1
        ps1 = c.ps[bank]
        k.op('pe', lambda e: e.matmul(ps1[:, 0:NB * 4], lhsT=c.ones_f[:, :], rhs=a_tok[:, :, :].rearrange('p b c -> p (b c)'),
                                      start=True, stop=True),
             reads=[('d_atok', 0), ('ones_f', 0)], writes=[('ps', bank)])
        k.op('dve', lambda e: e.tensor_copy(out=tot[:, :, :].rearrange('p b c -> p (b c)'), in_=ps1[:, 0:NB * 4]),
             reads=[('ps', bank)], writes=[('d_tot', 0)])
        k.op('dve', lambda e: e.tensor_tensor(out=dte[:, :, :], in0=tot[:, :, :], in1=acs[:, :, :], op=ALU.subtract),
             reads=[('d_acs', 0), ('d_tot', 0)], writes=[('d_dte', 0)])
        k.op('act', lambda e: e.activation(out=dte[:, :, :], in_=dte[:, :, :], func=AF.Exp),
             reads=[('d_dte', 0)], writes=[('d_dte', 0)])
        k.op('act', lambda e: e.activation(out=etot[:, :, :], in_=tot[:, :, :], func=AF.Exp),
             reads=[('d_tot', 0)], writes=[('d_etot', 0)])
        with ExitStack() as st2:
            xin = [st2.enter_context(_sbt(nc, 'd_xin%d' % i, [64, T + 3], F32)) for i in range(2)]
            acc = [st2.enter_context(_sbt(nc, 'd_acc%d' % i, [64, T], F32)) for i in range(2)]
            for ch in range(8):
                i = ch % 2
                xi, ac = xin[i], acc[i]
                k.op('pool', lambda e: e.memset(xi[:, 0:3], 0.0), writes=[('d_xin', i)])
                k.dma('sp' if i == 0 else 'act', xi[:, 3:T + 3], d['dxbc'][ch, :, 3:T + 3], writes=[('d_xin', i)])
                k.op('dve', lambda e: e.tensor_scalar(out=ac[:, :], in0=xi[:, 0:T], scalar1=cw[:, ch, 0:1],
                                                      scalar2=cb[:, ch:ch + 1], op0=ALU.mult, op1=ALU.add),
                     reads=[('d_xin', i), ('d_cw', 0), ('d_cb', 0)], writes=[('d_acc', i)])
                for tap in range(1, 4):
                    k.op('dve', lambda e: e.scalar_tensor_tensor(out=ac[:, :], in0=xi[:, tap:T + tap],
                                                                 scalar=cw[:, ch, tap:tap + 1], in1=ac[:, :],
                                                                 op0=ALU.mult, op1=ALU.add),
                         reads=[('d_xin', i), ('d_cw', 0), ('d_acc', i)], writes=[('d_acc', i)])
                if ch < 4:
                    k.op('act', lambda e: e.activation(out=ac[:, :], in_=ac[:, :], func=AF.Silu),
                         reads=[('d_acc', i)], writes=[('d_acc', i)])
                    k.dma('pool', d['dxs'][ch], ac[:, :], reads=[('d_acc', i)])
                    for b in range(NB):
                        bank = (b % 4) + 2
                        psx = c.ps[bank]
                        k.op('pe', lambda e: e.transpose(out=psx[:, 0:64], in_=ac[:, b * 128:(b + 1) * 128],
                                                         identity=c.ident[0:64, 0:64]),
                             reads=[('d_acc', i)], writes=[('ps', bank)])
                        k.op('dve', lambda e: e.tensor_scalar(out=xbar[:, b, ch * 64:(ch + 1) * 64], in0=psx[:, 0:64],
                                                              scalar1=dt_[:, b, ch:ch + 1], scalar2=None, op0=ALU.mult),
                             reads=[('ps', bank), ('d_dt', 0)], writes=[('d_xbar', ch)])
                elif ch < 6:
                    k.op('act', lambda e: e.activation(out=ac[:, :], in_=ac[:, :], func=AF.Silu),
                         reads=[('d_acc', i)], writes=[('d_acc', i)])
                    k.op('pool', lambda e: e.tensor_copy(out=BT[:, ch - 4, :], in_=ac[:, :]),
                         reads=[('d_acc', i)], writes=[('d_BC', ch)])
                    for b in range(NB):
                        bank = (b % 4) + 2
                        psx = c.ps[bank]
                        k.op('pe', lambda e: e.transpose(out=psx[:, 0:64], in_=ac[:, b * 128:(b + 1) * 128],
                                                         identity=c.ident[0:64, 0:64]),
                             reads=[('d_acc', i)], writes=[('ps', bank)])
                        k.op('act', lambda e: e.copy(out=Btok[:, b, ch - 4, :], in_=psx[:, 0:64]),
                             reads=[('ps', bank)], writes=[('d_Btok', ch)])
                else:
                    k.op('act', lambda e: e.activation(out=CT[:, ch - 6, :], in_=ac[:, :], func=AF.Silu),
                         reads=[('d_acc', i)], writes=[('d_BC', ch)])
            k.barrier()
        with ExitStack() as st3:
            def sb3(name, shape, dt2):
                return st3.enter_context(_sbt(nc, 'sb_' + name, shape, dt2))
            arg = [sb3('d_arg%d' % i, [128, 4, 128], F32) for i in range(2)]
            Dm = [sb3('d_Dm%d' % i, [128, 4, 128], F32) for i in range(2)]
            MT = [sb3('d_MT%d' % i, [128, 4, 128], BF16) for i in range(2)]
            ecs = [sb3('d_ecs%d' % i, [64, 4, 128], F32) for i in range(2)]
            Cd = [sb3('d_Cd%d' % i, [64, 4, 128], F32) for i in range(2)]
            Bd = [sb3('d_Bd%d' % i, [128, 4, 64], BF16) for i in range(2)]
            zts = [sb3('d_zt%d' % i, [64, 4, 128], F32) for i in range(2)]
            xsts = [sb3('d_xst%d' % i, [64, 4, 128], F32) for i in range(2)]
            state = sb3('d_state', [64, 4, 64], F32)
            stmp = sb3('d_stmp', [64, 4, 64], F32)
            CTf = sb3('d_CTf', [64, 2, 128], F32)
            y = sb3('d_y', [64, 4, 128], F32)
            ysq = sb3('d_ysq', [64, 4, 128], F32)
            ss = sb3('d_ss', [64, 2, 128], F32)
            obs = [sb3('d_ob%d' % i, [64, 4, 128], BF16) for i in range(2)]
            k.op('dve', lambda e: e.memset(state[:, :, :], 0.0), writes=[('d_state', 0)])
            bcn = [0]

            def nbank():
                b = bcn[0] % 8
                bcn[0] += 1
                return b
            for lb in range(NB):
                ls = slice(lb * 128, (lb + 1) * 128)
                i2 = lb % 2
                zt, xst, ob = zts[i2], xsts[i2], obs[i2]
                k.dma('sp', zt[:, :, :], d['dz'][:, :, ls].rearrange('h p t -> p h t'), writes=[('d_zt', i2)])
                k.dma('act', xst[:, :, :], d['dxs'][:, :, ls].rearrange('h p t -> p h t'), writes=[('d_xst', i2)])
                bank = nbank()
                psb = c.ps[bank]
                for h in range(4):
                    k.op('pe', lambda e: e.matmul(psb[:, h * 128:(h + 1) * 128],
                                                  lhsT=a_tok[:, lb, h:h + 1].to_broadcast([128, 128]),
                                                  rhs=c.triu[:, :], start=True, stop=True),
                         reads=[('d_atok', 0), ('triu', 0)], writes=[('ps', bank)])
                a_, D_, M_, ec_, Cd_, Bd_ = arg[i2], Dm[i2], MT[i2], ecs[i2], Cd[i2], Bd[i2]
                k.op('dve', lambda e: e.tensor_tensor(
                    out=a_[:, :, :], in0=psb[:, :].rearrange('p (h l) -> p h l', h=4),
                    in1=acs[:, lb, :].unsqueeze(2).to_broadcast([128, 4, 128]), op=ALU.subtract),
                    reads=[('ps', bank), ('d_acs', 0)], writes=[('d_arg', i2)])
                k.op('act', lambda e: e.activation(out=ec_[:, :, :],
                                                   in_=psb[0:64, :].rearrange('p (h l) -> p h l', h=4), func=AF.Exp),
                     reads=[('ps', bank)], writes=[('d_ecs', i2)])
                k.op('pool', lambda e: e.tensor_scalar_min(out=a_[:, :, :], in0=a_[:, :, :], scalar1=0.0),
                     reads=[('d_arg', i2)], writes=[('d_arg', i2)])
                k.op('act', lambda e: e.activation(out=D_[:, :, :], in_=a_[:, :, :], func=AF.Exp),
                     reads=[('d_arg', i2)], writes=[('d_Dm', i2)])
                k.op('pool', lambda e: e.tensor_tensor(
                    out=D_[:, :, :], in0=D_[:, :, :],
                    in1=c.triu[:, :].unsqueeze(1).to_broadcast([128, 4, 128]), op=ALU.mult),
                    reads=[('d_Dm', i2), ('triu', 0)], writes=[('d_Dm', i2)])
                bg = nbank()
                ps_g = c.ps[bg]
                for g in range(2):
                    k.op('pe', lambda e: e.matmul(ps_g[:, g * 128:(g + 1) * 128], lhsT=BT[:, g, ls],
                                                  rhs=CT[:, g, ls], start=True, stop=True),
                         reads=[('d_BC', 0)], writes=[('ps', bg)])
                for g in range(2):
                    k.op('dve', lambda e: e.tensor_tensor(
                        out=M_[:, 2 * g:2 * g + 2, :], in0=D_[:, 2 * g:2 * g + 2, :],
                        in1=ps_g[:, g * 128:(g + 1) * 128].unsqueeze(1).to_broadcast([128, 2, 128]),
                        op=ALU.mult),
                        reads=[('d_Dm', i2), ('ps', bg)], writes=[('d_MT', i2)])
                k.op('pool', lambda e: e.tensor_copy(out=CTf[:, :, :], in_=CT[:, :, ls]),
                     reads=[('d_BC', 0)], writes=[('d_CTf', 0)])
                for g in range(2):
                    k.op('pool', lambda e: e.tensor_tensor(
                        out=Cd_[:, 2 * g:2 * g + 2, :], in0=ec_[:, 2 * g:2 * g + 2, :],
                        in1=CTf[:, g, :].unsqueeze(1).to_broadcast([64, 2, 128]), op=ALU.mult),
                        reads=[('d_ecs', i2), ('d_CTf', 0)], writes=[('d_Cd', i2)])
                for g in range(2):
                    k.op('dve', lambda e: e.tensor_tensor(
                        out=Bd_[:, 2 * g:2 * g + 2, :],
                        in0=Btok[:, lb, g, :].unsqueeze(1).to_broadcast([128, 2, 64]),
                        in1=dte[:, lb, 2 * g:2 * g + 2].unsqueeze(2).to_broadcast([128, 2, 64]), op=ALU.mult),
                        reads=[('d_Btok', 0), ('d_dte', 0)], writes=[('d_Bd', i2)])
                bu = nbank()
                ps_y = c.ps[bu]
                for h in range(4):
                    k.op('pe', lambda e: e.matmul(ps_y[0:64, h * 128:(h + 1) * 128],
                                                  lhsT=xbar[:, lb, h * 64:(h + 1) * 64], rhs=M_[:, h, :],
                                                  start=(h == 0), stop=(lb == 0), skip_group_check=True),
                         reads=[('d_MT', i2), ('d_xbar', 0)], writes=[('ps', bu)])
                    if lb > 0:
                        k.op('pe', lambda e: e.matmul(ps_y[0:64, h * 128:(h + 1) * 128],
                                                      lhsT=state[:, h, :], rhs=Cd_[:, h, :],
                                                      start=False, stop=True, skip_group_check=True),
                             reads=[('d_Cd', i2), ('d_state', 0)], writes=[('ps', bu)])
                if lb < NB - 1:
                    bs_ = nbank()
                    ps_s = c.ps[bs_]
                    for h in range(4):
                        k.op('pe', lambda e: e.matmul(ps_s[0:64, h * 64:(h + 1) * 64], lhsT=Bd_[:, h, :],
                                                      rhs=xbar[:, lb, h * 64:(h + 1) * 64], start=True, stop=True),
                             reads=[('d_Bd', i2), ('d_xbar', 0)], writes=[('ps', bs_)])
                    k.op('dve', lambda e: e.tensor_tensor(
                        out=stmp[:, :, :], in0=state[:, :, :],
                        in1=etot[0:64, lb, :].unsqueeze(2).to_broadcast([64, 4, 64]), op=ALU.mult),
                        reads=[('d_state', 0), ('d_etot', 0)], writes=[('d_stmp', 0)])
                    k.op('dve', lambda e: e.tensor_tensor(
                        out=state[:, :, :], in0=stmp[:, :, :],
                        in1=ps_s[0:64, 0:256].rearrange('p (h q) -> p h q', h=4), op=ALU.add),
                        reads=[('d_stmp', 0), ('ps', bs_)], writes=[('d_state', 0)])
                for h in range(4):
                    k.op('dve', lambda e: e.scalar_tensor_tensor(out=y[:, h, :], in0=xst[:, h, :],
                                                                 scalar=dvec[0:64, 8 + h:9 + h],
                                                                 in1=ps_y[0:64, h * 128:(h + 1) * 128],
                                                                 op0=ALU.mult, op1=ALU.add),
                         reads=[('d_xst', i2), ('d_vec', 0), ('ps', bu)], writes=[('d_y', 0)])
                k.op('act', lambda e: e.activation(out=zt[:, :, :], in_=zt[:, :, :], func=AF.Silu),
                     reads=[('d_zt', i2)], writes=[('d_zt', i2)])
                k.op('dve', lambda e: e.tensor_tensor(out=y[:, :, :], in0=y[:, :, :], in1=zt[:, :, :], op=ALU.mult),
                     reads=[('d_y', 0), ('d_zt', i2)], writes=[('d_y', 0)])
                k.op('act', lambda e: e.activation(out=ysq[:, :, :], in_=y[:, :, :], func=AF.Square),
                     reads=[('d_y', 0)], writes=[('d_ysq', 0)])
                bank = nbank()
                psq = c.ps[bank]
                k.op('pe', lambda e: e.matmul(psq[0:64, :], lhsT=c.ones_f[0:64, 0:64],
                                              rhs=ysq[:, :, :].rearrange('p h l -> p (h l)'), start=True, stop=True),
                     reads=[('d_ysq', 0), ('ones_f', 0)], writes=[('ps', bank)])
                k.op('act', lambda e: e.copy(out=ysq[:, :, :].rearrange('p h l -> p (h l)'), in_=psq[0:64, :]),
                     reads=[('ps', bank)], writes=[('d_ysq', 0)])
                psv = ysq[:, :, :].rearrange('p (g a) l -> p g a l', g=2, a=2)
                k.op('dve', lambda e: e.tensor_tensor(out=ss[:, :, :], in0=psv[:, :, 0, :], in1=psv[:, :, 1, :],
                                                      op=ALU.add),
                     reads=[('d_ysq', 0)], writes=[('d_ss', 0)])
                k.op('act', lambda e: e.activation(out=ss[:, :, :], in_=ss[:, :, :], func=AF.Ln, scale=1.0 / 128,
                                                   bias=c.eps5[0:64, 0:1]),
                     reads=[('d_ss', 0), ('eps', 0)], writes=[('d_ss', 0)])
                k.op('act', lambda e: e.activation(out=ss[:, :, :], in_=ss[:, :, :], func=AF.Exp, scale=-0.5),
                     reads=[('d_ss', 0)], writes=[('d_ss', 0)])
                for g in range(2):
                    k.op('dve', lambda e: e.tensor_tensor(
                        out=y[:, 2 * g:2 * g + 2, :], in0=y[:, 2 * g:2 * g + 2, :],
                        in1=ss[:, g, :].unsqueeze(1).to_broadcast([64, 2, 128]), op=ALU.mult),
                        reads=[('d_y', 0), ('d_ss', 0)], writes=[('d_y', 0)])
                k.op('dve', lambda e: e.tensor_tensor(out=ob[:, :, :], in0=y[:, :, :],
                                                      in1=nw[:, :].unsqueeze(2).to_broadcast([64, 4, 128]), op=ALU.mult),
                     reads=[('d_y', 0), ('d_nw', 0)], writes=[('d_ob', i2)])
                k.dma('pool', d['obr'][3, :, :, ls].rearrange('h p t -> p h t'), ob[:, :, :], reads=[('d_ob', i2)])
            k.barrier()


def mixer_c(k, c, T, lp, layer):
    nc = k.nc
    d = c.d
    N = 256
    NCH = N // 64
    NG = T // N
    with ExitStack() as st:
        def sb(name, shape, dt_=F32):
            return st.enter_context(_sbt(nc, 'c_' + name, shape, dt_))
        p64 = sb('p64', [64, 46])
        wa2 = sb('wa2', [64, 256])
        g2 = sb('g2', [64, 256])
        k.dma('sp', p64[:, :], lp['c_p64'], writes=[('c_par', 0)])
        k.dma('act', wa2[:, :], lp['c_wa2'], writes=[('c_par', 1)])
        k.dma('pool', g2[:, :], lp['c_g2'], writes=[('c_par', 2)])
        if layer > 0:
            v1t = sb('v1t', [64, 4, 16])
            v2 = sb('v2', [16, 256])
            k.dma('sp', v1t[:, :, :], lp['c_v1'], writes=[('c_par', 3)])
            k.dma('act', v2[:, :], lp['c_v2'], writes=[('c_par', 4)])
        mu, w0, a0, kkp, kap, rkp, gnw, gnb, v0 = (p64[:, 0:14], p64[:, 14:18], p64[:, 18:22], p64[:, 22:26],
                                                   p64[:, 26:30], p64[:, 30:34], p64[:, 34:38], p64[:, 38:42],
                                                   p64[:, 42:46])
        prm = sb('prm', [64, 8])
        k.barrier()
        k.op('dve', lambda e: e.tensor_scalar(out=prm[:, 0:4], in0=w0, scalar1=-1.0, scalar2=None, op0=ALU.mult),
             writes=[('c_prm', 0)])
        k.op('dve', lambda e: e.tensor_scalar(out=prm[:, 4:8], in0=kap, scalar1=-1.0, scalar2=1.0, op0=ALU.mult,
                                              op1=ALU.add), writes=[('c_prm', 0)])
        k.barrier()
        negw0, omka = prm[:, 0:4], prm[:, 4:8]
        msk = sb('msk', [64, 3, 64])
        k.dma('sp', msk[:, :, :], c.cin['cmask'], writes=[('c_msk', 0)])
        H = sb('H', [64, 4, 64])
        k.op('dve', lambda e: e.memset(H[:, :, :], 0.0), writes=[('c_H', 0)])
        pc = sb('pc', [64, 14, N + 1])
        pcs = sb('pcs', [64, 14, N])
        tmp = sb('tmp', [64, 14, N])
        th = sb('th', [64, N])
        sg = sb('sg', [64, N])
        e2 = sb('e2', [64, 4, N])
        lw = sb('lw', [64, 4, N])
        base = sb('base', [64, 4, NCH])
        P_ = sb('P', [64, 4, N])
        Pm1 = sb('Pm1', [64, 4, N])
        iP = sb('iP', [64, 4, N])
        a_s = sb('a_s', [64, 4, N])
        g_s = sb('g_s', [64, 4, N])
        kk = sb('kk', [64, 4, N])
        t1 = sb('t1', [64, 4, N])
        kmod = sb('kmod', [64, 4, N])
        bon = sb('bon', [64, 4, N])
        ar = sb('ar', [64, 4, NCH, 2, 64])
        bT = sb('bT', [64, 4, N])
        kT = sb('kT', [64, 4, N])
        vT = sb('vT', [64, 4, N])
        vf = sb('vf', [64, 4, N])
        vl = sb('vl', [16, N])
        tok = sb('tok', [64, NCH, 3, 4, 64])
        MA = sb('MA', [64, 4, 2, 64])
        MB = sb('MB', [64, 4, 2, 64])
        XX = [sb('XX%d' % i, [64, 4, 2, 64]) for i in range(2)]
        TT = sb('TT', [64, 4, 64])
        Xs = sb('Xs', [64, 4, 64])
        Us = sb('Us', [64, 4, 64])
        yT = sb('yT', [64, 4, N])
        dd = sb('dd', [64, 4, N])
        ob = sb('ob', [64, 4, N], BF16)
        identb = c.ident[0:64, 0:64].unsqueeze(1).to_broadcast([64, 4, 64])
        ones64 = c.ones_f[0:64, 0:64]
        bc = [0]

        def nbank():
            b = bc[0] % 8
            bc[0] += 1
            return b

        def ph(t_):
            return t_[:, :, :].rearrange('p h n -> p (h n)')

        def bcast4(col):
            return col.unsqueeze(2).to_broadcast([64, 4, N])

        def headsum(src, dst_fn):
            for half in range(2):
                bank = nbank()
                ps = c.ps[bank]
                k.op('pe', lambda e: e.matmul(ps[0:64, :], lhsT=ones64,
                                              rhs=src[:, 2 * half:2 * half + 2, :].rearrange('p h n -> p (h n)'),
                                              start=True, stop=True),
                     reads=[('c_w', id(src))], writes=[('ps', bank)])
                dst_fn(half, ps[0:64, :].rearrange('p (h n) -> p h n', h=2), bank)

        def W(t_):
            return [('c_w', id(t_))]

        for gi in range(NG):
            t0 = gi * N
            ts = slice(t0, t0 + N)
            if gi == 0:
                k.dma('sp', pc[:, :, 1:N + 1], d['cpc'][:, :, 1:N + 1].rearrange('g p t -> p g t'), writes=W(pc))
                k.op('dve', lambda e: e.memset(pc[:, :, 0:1], 0.0), writes=W(pc))
            else:
                k.dma('sp', pc[:, :, :], d['cpc'][:, :, t0:t0 + N + 1].rearrange('g p t -> p g t'), writes=W(pc))
            k.op('dve', lambda e: e.tensor_tensor(out=tmp[:, :, :], in0=pc[:, :, 0:N], in1=pc[:, :, 1:N + 1],
                                                  op=ALU.subtract), reads=W(pc), writes=W(tmp))
            k.op('pool', lambda e: e.tensor_tensor(out=tmp[:, :, :], in0=tmp[:, :, :],
                                                   in1=mu.unsqueeze(2).to_broadcast([64, 14, N]), op=ALU.mult),
                 reads=W(tmp), writes=W(tmp))
            k.op('dve', lambda e: e.tensor_tensor(out=pcs[:, :, :], in0=tmp[:, :, :], in1=pc[:, :, 1:N + 1],
                                                  op=ALU.add), reads=W(tmp) + W(pc), writes=W(pcs))
            r_, k_, v_ = pcs[:, 0:4, :], pcs[:, 4:8, :], pcs[:, 8:12, :]
            k.op('act', lambda e: e.activation(out=th[0:32, :], in_=pcs[0:32, 12, :], func=AF.Tanh),
                 reads=W(pcs), writes=W(th))
            k.op('act', lambda e: e.activation(out=sg[:, :], in_=pcs[:, 13, :], func=AF.Sigmoid),
                 reads=W(pcs), writes=W(sg))
            for half in range(2):
                bank = nbank()
                ps = c.ps[bank]
                for hh in range(2):
                    h = 2 * half + hh
                    k.op('pe', lambda e: e.matmul(ps[0:64, hh * N:(hh + 1) * N], lhsT=wa2[0:32, h * 64:(h + 1) * 64],
                                                  rhs=th[0:32, :], start=True, stop=True),
                         reads=W(th), writes=[('ps', bank)])
                    k.op('act', lambda e: e.activation(out=e2[:, h, :], in_=ps[0:64, hh * N:(hh + 1) * N],
                                                       func=AF.Exp, scale=-1.0, bias=negw0[:, h:h + 1]),
                         reads=[('ps', bank)], writes=W(e2))
            k.op('act', lambda e: e.activation(out=e2[:, :, :], in_=e2[:, :, :], func=AF.Ln, bias=1.0),
                 reads=W(e2), writes=W(e2))
            k.op('act', lambda e: e.activation(out=e2[:, :, :], in_=e2[:, :, :], func=AF.Exp, scale=-1.0,
                                               bias=c.mhalf[0:64, 0:1]),
                 reads=W(e2), writes=W(e2))
            for half in range(2):
                bank = nbank()
                ps = c.ps[bank]
                for hh in range(2):
                    h = 2 * half + hh
                    k.op('pe', lambda e: e.matmul(ps[0:64, hh * N:(hh + 1) * N], lhsT=wa2[32:64, h * 64:(h + 1) * 64],
                                                  rhs=pcs[32:64, 12, :], start=True, stop=True),
                         reads=W(pcs), writes=[('ps', bank)])
                    k.op('act', lambda e: e.activation(out=a_s[:, h, :], in_=ps[0:64, hh * N:(hh + 1) * N],
                                                       func=AF.Sigmoid, bias=a0[:, h:h + 1]),
                         reads=[('ps', bank)], writes=W(a_s))
            for half in range(2):
                bank = nbank()
                ps = c.ps[bank]
                for hh in range(2):
                    h = 2 * half + hh
                    k.op('pe', lambda e: e.matmul(ps[0:64, hh * N:(hh + 1) * N], lhsT=g2[:, h * 64:(h + 1) * 64],
                                                  rhs=sg[:, :], start=True, stop=True),
                         reads=W(sg), writes=[('ps', bank)])
                k.op('act', lambda e: e.copy(out=g_s[:, 2 * half:2 * half + 2, :],
                                             in_=ps[0:64, :].rearrange('p (h n) -> p h n', h=2)),
                     reads=[('ps', bank)], writes=W(g_s))
            if layer == 0:
                k.op('pool', lambda e: e.tensor_copy(out=vT[:, :, :], in_=v_), reads=W(pcs), writes=W(vT))
                k.dma('act', d['vfirst'][:, :, ts].rearrange('h p t -> p h t'), vT[:, :, :], reads=W(vT))
            else:
                k.dma('act', vf[:, :, :], d['vfirst'][:, :, ts].rearrange('h p t -> p h t'), writes=W(vf))
                bank = nbank()
                ps = c.ps[bank]
                for h in range(4):
                    k.op('pe', lambda e: e.matmul(ps[0:16, 0:N], lhsT=v1t[:, h, :], rhs=pcs[:, 8 + h, :],
                                                  start=(h == 0), stop=(h == 3)),
                         reads=W(pcs), writes=[('ps', bank)])
                k.op('act', lambda e: e.copy(out=vl[:, :], in_=ps[0:16, 0:N]), reads=[('ps', bank)], writes=W(vl))
                for half in range(2):
                    bank = nbank()
                    ps = c.ps[bank]
                    for hh in range(2):
                        h = 2 * half + hh
                        k.op('pe', lambda e: e.matmul(ps[0:64, hh * N:(hh + 1) * N], lhsT=v2[0:16, h * 64:(h + 1) * 64],
                                                      rhs=vl[:, :], start=True, stop=True),
                             reads=W(vl), writes=[('ps', bank)])
                        k.op('act', lambda e: e.activation(out=t1[:, h, :], in_=ps[0:64, hh * N:(hh + 1) * N],
                                                           func=AF.Sigmoid, bias=v0[:, h:h + 1]),
                             reads=[('ps', bank)], writes=W(t1))
                k.op('dve', lambda e: e.tensor_tensor(out=vf[:, :, :], in0=vf[:, :, :], in1=v_, op=ALU.subtract),
                     reads=W(vf) + W(pcs), writes=W(vf))
                k.op('dve', lambda e: e.tensor_tensor(out=vf[:, :, :], in0=vf[:, :, :], in1=t1[:, :, :], op=ALU.mult),
                     reads=W(vf) + W(t1), writes=W(vf))
                k.op('dve', lambda e: e.tensor_tensor(out=vT[:, :, :], in0=vf[:, :, :], in1=v_, op=ALU.add),
                     reads=W(vf) + W(pcs), writes=W(vT))
            k.op('dve', lambda e: e.tensor_tensor(out=kk[:, :, :], in0=k_, in1=bcast4(kkp), op=ALU.mult),
                 reads=W(pcs), writes=W(kk))
            k.op('act', lambda e: e.activation(out=t1[:, :, :], in_=kk[:, :, :], func=AF.Square),
                 reads=W(kk), writes=W(t1))

            def kk_norm(half, psv, bank):
                k.op('act', lambda e: e.activation(out=dd[:, 2 * half:2 * half + 2, :], in_=psv, func=AF.Sqrt),
                     reads=[('ps', bank)], writes=W(dd))
            headsum(t1, kk_norm)
            k.op('dve', lambda e: e.tensor_scalar_max(out=dd[:, :, :], in0=dd[:, :, :], scalar1=1e-12),
                 reads=W(dd), writes=W(dd))
            k.op('dve', lambda e: e.reciprocal(out=dd[:, :, :], in_=dd[:, :, :]), reads=W(dd), writes=W(dd))
            k.op('dve', lambda e: e.tensor_tensor(out=kk[:, :, :], in0=kk[:, :, :], in1=dd[:, :, :], op=ALU.mult),
                 reads=W(kk) + W(dd), writes=W(kk))
            k.op('pool', lambda e: e.tensor_tensor(out=t1[:, :, :], in0=a_s[:, :, :], in1=bcast4(kap), op=ALU.mult),
                 reads=W(a_s), writes=W(t1))
            k.op('pool', lambda e: e.tensor_tensor(out=t1[:, :, :], in0=t1[:, :, :], in1=bcast4(omka), op=ALU.add),
                 reads=W(t1), writes=W(t1))
            k.op('dve', lambda e: e.tensor_tensor(out=kmod[:, :, :], in0=k_, in1=t1[:, :, :], op=ALU.mult),
                 reads=W(pcs) + W(t1), writes=W(kmod))
            k.op('dve', lambda e: e.tensor_tensor(out=t1[:, :, :], in0=r_, in1=kmod[:, :, :], op=ALU.mult),
                 reads=W(pcs) + W(kmod), writes=W(t1))
            k.op('pool', lambda e: e.tensor_tensor(out=t1[:, :, :], in0=t1[:, :, :], in1=bcast4(rkp), op=ALU.mult),
                 reads=W(t1), writes=W(t1))

            def bon_fn(half, psv, bank):
                k.op('dve', lambda e: e.tensor_tensor(out=bon[:, 2 * half:2 * half + 2, :], in0=psv,
                                                      in1=vT[:, 2 * half:2 * half + 2, :], op=ALU.mult),
                     reads=[('ps', bank)] + W(vT), writes=W(bon))
            headsum(t1, bon_fn)
            for h in range(4):
                k.op('dve', lambda e: e.tensor_tensor_scan(out=lw[:, h, :], data0=e2[:, h, :], data1=e2[:, h, :],
                                                           initial=0.0, op0=ALU.add, op1=ALU.max),
                     reads=W(e2), writes=W(lw))
            k.op('dve', lambda e: e.memset(base[:, :, 0:1], 0.0), writes=W(base))
            k.op('dve', lambda e: e.tensor_copy(out=base[:, :, 1:NCH], in_=lw[:, :, 63:N - 1:64]),
                 reads=W(lw), writes=W(base))
            lw4 = lw[:, :, :].rearrange('p h (c s) -> p h c s', s=64)
            k.op('dve', lambda e: e.tensor_tensor(out=lw4, in0=lw4,
                                                  in1=base[:, :, :].unsqueeze(3).to_broadcast([64, 4, NCH, 64]),
                                                  op=ALU.subtract),
                 reads=W(lw) + W(base), writes=W(lw))
            k.op('act', lambda e: e.activation(out=P_[:, :, :], in_=lw[:, :, :], func=AF.Exp, scale=-1.0),
                 reads=W(lw), writes=W(P_))
            k.op('act', lambda e: e.activation(out=iP[:, :, :], in_=lw[:, :, :], func=AF.Exp),
                 reads=W(lw), writes=W(iP))
            k.op('dve', lambda e: e.tensor_tensor(out=t1[:, :, :], in0=lw[:, :, :], in1=e2[:, :, :], op=ALU.subtract),
                 reads=W(lw) + W(e2), writes=W(t1))
            k.op('act', lambda e: e.activation(out=Pm1[:, :, :], in_=t1[:, :, :], func=AF.Exp, scale=-1.0),
                 reads=W(t1), writes=W(Pm1))
            ar5 = ar[:, :, :, :, :]
            k.op('dve', lambda e: e.scalar_tensor_tensor(
                out=ar5[:, :, :, 0, :], in0=kk[:, :, :].rearrange('p h (c s) -> p h c s', s=64), scalar=-1.0,
                in1=Pm1[:, :, :].rearrange('p h (c s) -> p h c s', s=64), op0=ALU.mult, op1=ALU.mult),
                reads=W(kk) + W(Pm1), writes=W(ar))
            k.op('dve', lambda e: e.tensor_tensor(
                out=ar5[:, :, :, 1, :], in0=r_.rearrange('p h (c s) -> p h c s', s=64),
                in1=P_[:, :, :].rearrange('p h (c s) -> p h c s', s=64), op=ALU.mult),
                reads=W(pcs) + W(P_), writes=W(ar))
            k.op('dve', lambda e: e.tensor_tensor(out=bT[:, :, :], in0=kk[:, :, :], in1=a_s[:, :, :], op=ALU.mult),
                 reads=W(kk) + W(a_s), writes=W(bT))
            k.op('dve', lambda e: e.tensor_tensor(out=bT[:, :, :], in0=bT[:, :, :], in1=iP[:, :, :], op=ALU.mult),
                 reads=W(bT) + W(iP), writes=W(bT))
            k.op('dve', lambda e: e.tensor_tensor(out=kT[:, :, :], in0=kmod[:, :, :], in1=iP[:, :, :], op=ALU.mult),
                 reads=W(kmod) + W(iP), writes=W(kT))
            for ci in range(NCH):
                cs = slice(ci * 64, (ci + 1) * 64)
                for qi, src in enumerate((bT, kT, vT)):
                    bank = nbank()
                    ps = c.ps[bank]
                    for h in range(4):
                        k.op('pe', lambda e: e.transpose(out=ps[0:64, h * 64:(h + 1) * 64], in_=src[:, h, cs],
                                                         identity=c.ident[0:64, 0:64]),
                             reads=W(src), writes=[('ps', bank)])
                    eng = 'act' if qi % 2 == 0 else 'dve'
                    dst = tok[:, ci, qi, :, :]
                    srcp = ps[0:64, 0:256].rearrange('p (h n) -> p h n', h=4)
                    if eng == 'act':
                        k.op('act', lambda e: e.copy(out=dst, in_=srcp), reads=[('ps', bank)], writes=W(tok))
                    else:
                        k.op('dve', lambda e: e.tensor_copy(out=dst, in_=srcp), reads=[('ps', bank)], writes=W(tok))
            for ci in range(NCH):
                cs = slice(ci * 64, (ci + 1) * 64)
                bA, bB, bC = nbank(), nbank(), nbank()
                psA, psB, psC = c.ps[bA], c.ps[bB], c.ps[bC]
                for h in range(4):
                    arh = ar[:, h, ci, :, :].rearrange('p a s -> p (a s)')
                    k.op('pe', lambda e: e.matmul(psA[0:64, h * 128:(h + 1) * 128], lhsT=bT[:, h, cs], rhs=arh,
                                                  start=True, stop=True),
                         reads=W(bT) + W(ar), writes=[('ps', bA)])
                    k.op('pe', lambda e: e.matmul(psB[0:64, h * 128:(h + 1) * 128], lhsT=kT[:, h, cs], rhs=arh,
                                                  start=True, stop=True),
                         reads=W(kT) + W(ar), writes=[('ps', bB)])
                    k.op('pe', lambda e: e.matmul(psC[0:64, h * 64:(h + 1) * 64], lhsT=ar[:, h, ci, 0, :],
                                                  rhs=bT[:, h, cs], start=True, stop=True),
                         reads=W(bT) + W(ar), writes=[('ps', bC)])
                mUU = msk[:, 0:2, :].unsqueeze(1).to_broadcast([64, 4, 2, 64])
                k.op('dve', lambda e: e.tensor_tensor(out=MA[:, :, :, :],
                                                      in0=psA[0:64, :].rearrange('p (h a s) -> p h a s', h=4, a=2),
                                                      in1=mUU, op=ALU.mult),
                     reads=[('ps', bA), ('c_msk', 0)], writes=W(MA))
                k.op('dve', lambda e: e.tensor_tensor(out=MB[:, :, :, :],
                                                      in0=psB[0:64, :].rearrange('p (h a s) -> p h a s', h=4, a=2),
                                                      in1=mUU, op=ALU.mult),
                     reads=[('ps', bB), ('c_msk', 0)], writes=W(MB))
                X = XX[0]
                k.op('pool', lambda e: e.tensor_copy(out=X[:, :, 0, :], in_=MA[:, :, 0, :]), reads=W(MA), writes=W(X))
                k.op('dve', lambda e: e.tensor_tensor(out=X[:, :, 1, :],
                                                      in0=psC[0:64, 0:256].rearrange('p (h s) -> p h s', h=4),
                                                      in1=msk[:, 2, :].unsqueeze(1).to_broadcast([64, 4, 64]),
                                                      op=ALU.mult),
                     reads=[('ps', bC), ('c_msk', 0)], writes=W(X))
                k.op('pool', lambda e: e.tensor_tensor(out=TT[:, :, :], in0=MA[:, :, 0, :], in1=identb, op=ALU.add),
                     reads=W(MA), writes=W(TT))
                for it_ in range(5):
                    Xo, Xn = XX[it_ % 2], XX[(it_ + 1) % 2]
                    bank = nbank()
                    ps = c.ps[bank]
                    for h in range(4):
                        k.op('pe', lambda e: e.matmul(ps[0:64, h * 128:h * 128 + 64], lhsT=Xo[:, h, 1, :],
                                                      rhs=Xo[:, h, 0, :], start=True, stop=True),
                             reads=W(Xo), writes=[('ps', bank)])
                        k.op('pe', lambda e: e.matmul(ps[0:64, h * 128 + 64:(h + 1) * 128], lhsT=Xo[:, h, 0, :],
                                                      rhs=Xo[:, h, 1, :], start=True, stop=True),
                             reads=W(Xo), writes=[('ps', bank)])
                    k.op('act', lambda e: e.copy(out=Xn[:, :, :, :],
                                                 in_=ps[0:64, :].rearrange('p (h a s) -> p h a s', h=4, a=2)),
                         reads=[('ps', bank)], writes=W(Xn))
                    bank2 = nbank()
                    ps2 = c.ps[bank2]
                    for h in range(4):
                        k.op('pe', lambda e: e.matmul(ps2[0:64, h * 64:(h + 1) * 64], lhsT=Xn[:, h, 1, :],
                                                      rhs=TT[:, h, :], start=True, stop=True),
                             reads=W(Xn) + W(TT), writes=[('ps', bank2)])
                    k.op('dve', lambda e: e.tensor_tensor(out=TT[:, :, :], in0=TT[:, :, :],
                                                          in1=ps2[0:64, 0:256].rearrange('p (h s) -> p h s', h=4),
                                                          op=ALU.add),
                         reads=[('ps', bank2)] + W(TT), writes=W(TT))
                bank = nbank()
                ps = c.ps[bank]
                for h in range(4):
                    k.op('pe', lambda e: e.matmul(ps[0:64, h * 64:(h + 1) * 64], lhsT=ar[:, h, ci, 0, :], rhs=H[:, h, :],
                                                  start=(h == 0), stop=False, skip_group_check=True),
                         reads=W(ar) + [('c_H', 0)], writes=[('ps', bank)])
                    k.op('pe', lambda e: e.matmul(ps[0:64, h * 64:(h + 1) * 64], lhsT=MB[:, h, 0, :],
                                                  rhs=tok[:, ci, 2, h, :], start=False, stop=True,
                                                  skip_group_check=True),
                         reads=W(MB) + W(tok), writes=[('ps', bank)])
                k.op('act', lambda e: e.copy(out=Xs[:, :, :], in_=ps[0:64, 0:256].rearrange('p (h s) -> p h s', h=4)),
                     reads=[('ps', bank)], writes=W(Xs))
                bank = nbank()
                ps = c.ps[bank]
                for h in range(4):
                    k.op('pe', lambda e: e.matmul(ps[0:64, h * 64:(h + 1) * 64], lhsT=TT[:, h, :], rhs=Xs[:, h, :],
                                                  start=True, stop=True),
                         reads=W(TT) + W(Xs), writes=[('ps', bank)])
                k.op('dve', lambda e: e.tensor_copy(out=Us[:, :, :],
                                                    in_=ps[0:64, 0:256].rearrange('p (h s) -> p h s', h=4)),
                     reads=[('ps', bank)], writes=W(Us))
                bank = nbank()
                ps = c.ps[bank]
                for h in range(4):
                    o_ = ps[0:64, h * 64:(h + 1) * 64]
                    k.op('pe', lambda e: e.matmul(o_, lhsT=H[:, h, :], rhs=ar[:, h, ci, 1, :], start=(h == 0),
                                                  stop=False, skip_group_check=True),
                         reads=W(ar) + [('c_H', 0)], writes=[('ps', bank)])
                    k.op('pe', lambda e: e.matmul(o_, lhsT=Us[:, h, :], rhs=MA[:, h, 1, :], start=False, stop=False,
                                                  skip_group_check=True),
                         reads=W(MA) + W(Us), writes=[('ps', bank)])
                    k.op('pe', lambda e: e.matmul(o_, lhsT=tok[:, ci, 2, h, :], rhs=MB[:, h, 1, :], start=False,
                                                  stop=True, skip_group_check=True),
                         reads=W(MB) + W(tok), writes=[('ps', bank)])
                k.op('act', lambda e: e.copy(out=yT[:, :, cs], in_=ps[0:64, 0:256].rearrange('p (h s) -> p h s', h=4)),
                     reads=[('ps', bank)], writes=W(yT))
                bank = nbank()
                ps = c.ps[bank]
                for h in range(4):
                    o_ = ps[0:64, h * 64:(h + 1) * 64]
                    k.op('pe', lambda e: e.matmul(o_, lhsT=tok[:, ci, 0, h, :], rhs=Us[:, h, :], start=(h == 0),
                                                  stop=False, skip_group_check=True),
                         reads=W(tok) + W(Us), writes=[('ps', bank)])
                    k.op('pe', lambda e: e.matmul(o_, lhsT=tok[:, ci, 1, h, :], rhs=tok[:, ci, 2, h, :], start=False,
                                                  stop=True, skip_group_check=True),
                         reads=W(tok), writes=[('ps', bank)])
                k.op('dve', lambda e: e.tensor_tensor(out=H[:, :, :], in0=H[:, :, :],
                                                      in1=ps[0:64, 0:256].rearrange('p (h s) -> p h s', h=4),
                                                      op=ALU.add),
                     reads=[('ps', bank), ('c_H', 0)], writes=[('c_H', 0)])
                pcl = P_[:, :, ci * 64 + 63:ci * 64 + 64].to_broadcast([64, 4, 64])
                k.op('dve', lambda e: e.tensor_tensor(out=H[:, :, :], in0=H[:, :, :], in1=pcl, op=ALU.mult),
                     reads=[('c_H', 0)] + W(P_), writes=[('c_H', 0)])
            def mean_fn(half, psv, bank):
                k.op('dve', lambda e: e.scalar_tensor_tensor(out=dd[:, 2 * half:2 * half + 2, :], in0=psv,
                                                             scalar=-1.0 / 64, in1=yT[:, 2 * half:2 * half + 2, :],
                                                             op0=ALU.mult, op1=ALU.add),
                     reads=[('ps', bank)] + W(yT), writes=W(dd))
            headsum(yT, mean_fn)
            k.op('act', lambda e: e.activation(out=t1[:, :, :], in_=dd[:, :, :], func=AF.Square),
                 reads=W(dd), writes=W(t1))

            def var_fn(half, psv, bank):
                k.op('act', lambda e: e.activation(out=kmod[:, 2 * half:2 * half + 2, :], in_=psv, func=AF.Ln,
                                                   scale=1.0 / 64, bias=c.epsgn[0:64, 0:1]),
                     reads=[('ps', bank)], writes=W(kmod))
            headsum(t1, var_fn)
            k.op('act', lambda e: e.activation(out=kmod[:, :, :], in_=kmod[:, :, :], func=AF.Exp, scale=-0.5),
                 reads=W(kmod), writes=W(kmod))
            k.op('dve', lambda e: e.tensor_tensor(out=dd[:, :, :], in0=dd[:, :, :], in1=kmod[:, :, :], op=ALU.mult),
                 reads=W(dd) + W(kmod), writes=W(dd))
            k.op('pool', lambda e: e.tensor_tensor(out=dd[:, :, :], in0=dd[:, :, :], in1=bcast4(gnw), op=ALU.mult),
                 reads=W(dd), writes=W(dd))
            k.op('pool', lambda e: e.tensor_tensor(out=dd[:, :, :], in0=dd[:, :, :], in1=bcast4(gnb), op=ALU.add),
                 reads=W(dd), writes=W(dd))
            k.op('dve', lambda e: e.tensor_tensor(out=dd[:, :, :], in0=dd[:, :, :], in1=bon[:, :, :], op=ALU.add),
                 reads=W(dd) + W(bon), writes=W(dd))
            k.op('dve', lambda e: e.tensor_tensor(out=ob[:, :, :], in0=dd[:, :, :], in1=g_s[:, :, :], op=ALU.mult),
                 reads=W(dd) + W(g_s), writes=W(ob))
            k.dma('pool', d['obr'][2, :, :, ts].rearrange('h p t -> p h t'), ob[:, :, :], reads=W(ob))
        k.barrier()


ALPHA = (2 * 2) ** 0.25


def ln_block(k, c, src, skey, dst, dkey, gb, gkey, tmp):
    st6, mv = tmp
    for i in range(2):
        k.op('dve', lambda e: e.bn_stats(out=st6[:, i, :], in_=src[:, i * 512:(i + 1) * 512]),
             reads=[skey], writes=[('ln_st', 0)])
    k.op('dve', lambda e: e.bn_aggr(out=mv[:, 0:2], in_=st6[:, :, :].rearrange('p a b -> p (a b)')),
         reads=[('ln_st', 0)], writes=[('ln_mv', 0)])
    k.op('act', lambda e: e.activation(out=mv[:, 2:3], in_=mv[:, 1:2], func=AF.Ln, bias=c.eps5[:, 0:1]),
         reads=[('ln_mv', 0)], writes=[('ln_mv', 1)])
    k.op('act', lambda e: e.activation(out=mv[:, 3:4], in_=mv[:, 2:3], func=AF.Exp, scale=-0.5),
         reads=[('ln_mv', 1)], writes=[('ln_mv', 2)])
    k.op('dve', lambda e: e.tensor_scalar(out=dst, in0=src[:, :], scalar1=mv[:, 0:1], scalar2=mv[:, 3:4],
                                          op0=ALU.subtract, op1=ALU.mult),
         reads=[skey, ('ln_mv', 0), ('ln_mv', 2)], writes=[dkey])
    k.op('pool', lambda e: e.tensor_tensor(out=dst, in0=dst, in1=gb[:, 0, :], op=ALU.mult),
         reads=[dkey, gkey], writes=[dkey])
    k.op('pool', lambda e: e.tensor_tensor(out=dst, in0=dst, in1=gb[:, 1, :], op=ALU.add),
         reads=[dkey, gkey], writes=[dkey])


def phase_merge(k, c, T, lp, x_dram, x1_dram):
    nc = k.nc
    NB = T // 128
    d = c.d
    with ExitStack() as st:
        def sb(name, shape, dt_=F32):
            return st.enter_context(_sbt(nc, 'm_' + name, shape, dt_))
        wb = sb('wb', [64, 16, 1024], BF16)
        wo = sb('wo', [128, 8, 1024], BF16)
        stg = [sb('stg%d' % i, [128, 4096]) for i in range(2)]
        gb = sb('gb', [128, 2, 1024])
        k.dma('sp', gb[:, 0, :], lp['ln1_g'].partition_broadcast(128), writes=[('m_gb', 0)])
        k.dma('act', gb[:, 1, :], lp['ln1_b'].partition_broadcast(128), writes=[('m_gb', 0)])
        for n in range(4):
            s_ = stg[n % 2]
            k.dma('sp' if n % 2 == 0 else 'act', s_[0:64, :].rearrange('p (a c) -> p a c', a=4),
                  lp['w_branch'][n].rearrange('(a p) c -> p a c', p=64), writes=[('m_stg', n % 2)])
            k.op('dve' if n % 2 == 0 else 'pool', lambda e: e.tensor_copy(
                out=wb[:, 4 * n:4 * n + 4, :], in_=s_[0:64, :].rearrange('p (a c) -> p a c', a=4)),
                reads=[('m_stg', n % 2)], writes=[('m_wb', 0)])
        for hf in range(2):
            s_ = stg[hf]
            k.dma('sp' if hf == 0 else 'act', s_[:, :].rearrange('p (a c) -> p a c', a=4),
                  lp['w_out'][hf * 512:(hf + 1) * 512, :].rearrange('(a p) c -> p a c', p=128),
                  writes=[('m_stg', hf)])
            k.op('dve' if hf == 0 else 'pool', lambda e: e.tensor_copy(
                out=wo[:, 4 * hf:4 * hf + 4, :], in_=s_[:, :].rearrange('p (a c) -> p a c', a=4)),
                reads=[('m_stg', hf)], writes=[('m_wo', 0)])
        ob = [sb('ob%d' % i, [64, 16, 128], BF16) for i in range(2)]
        gs = [sb('gs%d' % i, [128, 4096], BF16) for i in range(2)]
        xs = [sb('xs%d' % i, [128, 1024]) for i in range(2)]
        mg = sb('mg', [128, 1024])
        tm = sb('tm', [128, 512])
        mT = sb('mT', [128, 8, 128], BF16)
        h1 = sb('h1', [128, 1024])
        xo = [sb('xo%d' % i, [128, 1024]) for i in range(2)]
        st6 = sb('st6', [128, 2, 6])
        mv = sb('mv', [128, 4])
        bc = [0]

        def nbank():
            b = bc[0] % 8
            bc[0] += 1
            return b
        for b in range(NB):
            i2 = b % 2
            bs = slice(b * 128, (b + 1) * 128)
            k.dma('sp', ob[i2][:, :, :], d['obr'][:, :, :, bs].rearrange('n h p t -> p (n h) t'),
                  writes=[('m_ob', i2)])
            k.dma('act', gs[i2][:, :], d['gsig'][b], writes=[('m_gs', i2)])
            k.dma('pool', xs[i2][:, :], x_dram[bs, :], writes=[('m_xs', i2)])
            for n in range(4):
                for hc in range(2):
                    bank = nbank()
                    ps = c.ps[bank]
                    for h in range(4):
                        k.op('pe', lambda e: e.matmul(ps[:, :], lhsT=ob[i2][:, 4 * n + h, :],
                                                      rhs=wb[:, 4 * n + h, hc * 512:(hc + 1) * 512],
                                                      start=(h == 0), stop=(h == 3)),
                             reads=[('m_ob', i2), ('m_wb', 0)], writes=[('ps', bank)])
                    gsl = gs[i2][:, n * 1024 + hc * 512:n * 1024 + (hc + 1) * 512]
                    msl = mg[:, hc * 512:(hc + 1) * 512]
                    if n == 0:
                        k.op('dve', lambda e: e.tensor_tensor(out=msl, in0=ps[:, :], in1=gsl, op=ALU.mult),
                             reads=[('ps', bank), ('m_gs', i2)], writes=[('m_mg', hc)])
                    else:
                        k.op('dve', lambda e: e.tensor_tensor(out=tm[:, :], in0=ps[:, :], in1=gsl, op=ALU.mult),
                             reads=[('ps', bank), ('m_gs', i2)], writes=[('m_tm', 0)])
                        k.op('pool', lambda e: e.tensor_tensor(out=msl, in0=msl, in1=tm[:, :], op=ALU.add),
                             reads=[('m_tm', 0), ('m_mg', hc)], writes=[('m_mg', hc)])
            for hc in range(2):
                bank = nbank()
                ps = c.ps[bank]
                for j in range(4):
                    ch = hc * 4 + j
                    k.op('pe', lambda e: e.transpose(out=ps[:, j * 128:(j + 1) * 128],
                                                     in_=mg[:, ch * 128:(ch + 1) * 128], identity=c.ident[:, :]),
                         reads=[('m_mg', hc)], writes=[('ps', bank)])
                k.op('act', lambda e: e.copy(out=mT[:, hc * 4:(hc + 1) * 4, :],
                                             in_=ps[:, :].rearrange('p (j n) -> p j n', j=4)),
                     reads=[('ps', bank)], writes=[('m_mT', 0)])
            for hc in range(2):
                bank = nbank()
                ps = c.ps[bank]
                for kc in range(8):
                    k.op('pe', lambda e: e.matmul(ps[:, :], lhsT=mT[:, kc, :], rhs=wo[:, kc, hc * 512:(hc + 1) * 512],
                                                  start=(kc == 0), stop=(kc == 7)),
                         reads=[('m_mT', 0), ('m_wo', 0)], writes=[('ps', bank)])
                k.op('dve', lambda e: e.scalar_tensor_tensor(out=h1[:, hc * 512:(hc + 1) * 512],
                                                             in0=xs[i2][:, hc * 512:(hc + 1) * 512], scalar=ALPHA,
                                                             in1=ps[:, :], op0=ALU.mult, op1=ALU.add),
                     reads=[('ps', bank), ('m_xs', i2)], writes=[('m_h1', 0)])
            ln_block(k, c, h1, ('m_h1', 0), xo[i2][:, :], ('m_xo', i2), gb, ('m_gb', 0), (st6, mv))
            k.dma('sp', x1_dram[bs, :], xo[i2][:, :], reads=[('m_xo', i2)])
        k.barrier()


def phase_moe(k, c, T, lp, x1_dram, out_dram):
    nc = k.nc
    NB = T // 128
    HT = min(T, 1024)
    NH = T // HT
    NBH = HT // 128
    NTG = HT // 512
    with ExitStack() as st:
        def sb(name, shape, dt_=F32):
            return st.enter_context(_sbt(nc, 'e_' + name, shape, dt_))
        gate = sb('gate', [128, NBH, 32])
        xTp = sb('xTp', [128, 8, HT], BF16)
        wrl = WLoader(k, st, 'e_wr', width=36, nbuf=1)
        wr, wrkey = wrl.load(lp['r_w'], 0, 36)
        lg = sb('lg', [128, 36])
        sm = sb('sm', [128, 16])
        w8 = [sb('w8%d' % i, [128, 4, 8]) for i in range(4)]
        oh = sb('oh', [128, 4])
        gb = sb('gb', [128, 2, 1024])
        rb = sb('rb', [128, 36])
        k.dma('sp', gb[:, 0, :], lp['ln2_g'].partition_broadcast(128), writes=[('e_gb', 0)])
        k.dma('act', gb[:, 1, :], lp['ln2_b'].partition_broadcast(128), writes=[('e_gb', 0)])
        k.dma('pool', rb[:, :], lp['r_bias'].partition_broadcast(128), writes=[('e_rb', 0)])
        def router():
            for b in range(NBH):
                bank = b % 8
                ps = c.ps[bank]
                for kc in range(8):
                    k.op('pe', lambda e: e.matmul(ps[:, 0:36], lhsT=xTp[:, kc, b * 128:(b + 1) * 128], rhs=wr[:, kc, 0:36],
                                                  start=(kc == 0), stop=(kc == 7)),
                         reads=[wrkey], writes=[('ps', bank)])
                R_ = [('e_r', 0)]
                k.op('dve', lambda e: e.tensor_tensor(out=lg[:, :], in0=ps[:, 0:36], in1=rb[:, :], op=ALU.add),
                     reads=[('ps', bank), ('e_rb', 0)] + R_, writes=R_)
                le = lg[:, 4:36].rearrange('p (g j) -> p g j', g=4)
                k.op('dve', lambda e: e.tensor_reduce(out=sm[:, 0:1], in_=lg[:, 0:4], axis=AX.X, op=ALU.max),
                     reads=R_, writes=R_)
                k.op('dve', lambda e: e.tensor_scalar(out=oh[:, :], in0=lg[:, 0:4], scalar1=sm[:, 0:1], scalar2=None,
                                                      op0=ALU.is_equal), reads=R_, writes=R_)
                k.op('dve', lambda e: e.tensor_scalar(out=sm[:, 1:2], in0=sm[:, 0:1], scalar1=-1.0, scalar2=None,
                                                      op0=ALU.mult), reads=R_, writes=R_)
                k.op('act', lambda e: e.activation(out=sm[:, 4:8], in_=lg[:, 0:4], func=AF.Exp, bias=sm[:, 1:2],
                                                   accum_out=sm[:, 2:3]), reads=R_, writes=R_)
                k.op('dve', lambda e: e.reciprocal(out=sm[:, 3:4], in_=sm[:, 2:3]), reads=R_, writes=R_)
                k.op('dve', lambda e: e.tensor_reduce(out=sm[:, 8:12], in_=le, axis=AX.X, op=ALU.max),
                     reads=R_, writes=R_)
                m1b = sm[:, 8:12].unsqueeze(2).to_broadcast([128, 4, 8])
                k.op('dve', lambda e: e.tensor_tensor(out=w8[0][:, :, :], in0=le, in1=m1b, op=ALU.is_equal),
                     reads=R_, writes=R_)
                k.op('dve', lambda e: e.scalar_tensor_tensor(out=w8[1][:, :, :].rearrange('p g j -> p (g j)'),
                                                             in0=w8[0][:, :, :].rearrange('p g j -> p (g j)'),
                                                             scalar=-1.0e30, in1=lg[:, 4:36],
                                                             op0=ALU.mult, op1=ALU.add), reads=R_, writes=R_)
                k.op('dve', lambda e: e.tensor_reduce(out=sm[:, 12:16], in_=w8[1][:, :, :], axis=AX.X, op=ALU.max),
                     reads=R_, writes=R_)
                m2b = sm[:, 12:16].unsqueeze(2).to_broadcast([128, 4, 8])
                k.op('dve', lambda e: e.tensor_tensor(out=w8[0][:, :, :], in0=le, in1=m2b, op=ALU.is_ge),
                     reads=R_, writes=R_)
                k.op('dve', lambda e: e.tensor_tensor(out=w8[1][:, :, :], in0=le, in1=m1b, op=ALU.subtract),
                     reads=R_, writes=R_)
                k.op('act', lambda e: e.activation(out=w8[1][:, :, :], in_=w8[1][:, :, :], func=AF.Exp),
                     reads=R_, writes=R_)
                k.op('dve', lambda e: e.tensor_tensor(out=oh[:, :], in0=oh[:, :],
                                                      in1=sm[:, 3:4].to_broadcast([128, 4]), op=ALU.mult),
                     reads=R_, writes=R_)
                k.op('dve', lambda e: e.tensor_tensor(out=sm[:, 4:8], in0=sm[:, 12:16], in1=sm[:, 8:12],
                                                      op=ALU.subtract), reads=R_, writes=R_)
                k.op('act', lambda e: e.activation(out=sm[:, 4:8], in_=sm[:, 4:8], func=AF.Exp), reads=R_, writes=R_)
                k.op('dve', lambda e: e.tensor_scalar(out=sm[:, 4:8], in0=sm[:, 4:8], scalar1=1.0, scalar2=None,
                                                      op0=ALU.add), reads=R_, writes=R_)
                k.op('dve', lambda e: e.reciprocal(out=sm[:, 4:8], in_=sm[:, 4:8]), reads=R_, writes=R_)
                k.op('dve', lambda e: e.tensor_tensor(out=sm[:, 4:8], in0=sm[:, 4:8], in1=oh[:, :], op=ALU.mult),
                     reads=R_, writes=R_)
                k.op('dve', lambda e: e.tensor_tensor(out=w8[0][:, :, :], in0=w8[0][:, :, :], in1=w8[1][:, :, :],
                                                      op=ALU.mult), reads=R_, writes=R_)
                k.op('dve', lambda e: e.tensor_tensor(out=gate[:, b, :].rearrange('p (g j) -> p g j', g=4),
                                                      in0=w8[0][:, :, :],
                                                      in1=sm[:, 4:8].unsqueeze(2).to_broadcast([128, 4, 8]),
                                                      op=ALU.mult), reads=R_, writes=R_ + [('e_gate', 0)])
            k.barrier()
        wstg = [sb('wstg%d' % i, [128, 4096]) for i in range(2)]
        wset = [[sb('wb%d_%d' % (i, j), [128, 4096], BF16) for j in range(3)] for i in range(2)]
        wcnt = [0]

        def wload(src3, seti, j, q):
            si = wcnt[0] % 2
            wcnt[0] += 1
            stg_ = wstg[si]
            a = src3.shape[1]
            k.dma(q, stg_[:, :].rearrange('p (a c) -> p a c', a=a), src3, writes=[('e_wstg', si)])
            k.op('pool', lambda e: e.tensor_copy(out=wset[seti][j][:, :], in_=stg_[:, :]),
                 reads=[('e_wstg', si)], writes=[('e_wset', seti, j)])
        yacc = sb('yacc', [128, NBH, 1024])
        sl = [sb('sl%d' % i, [128, 512]) for i in range(2)]
        hT = [sb('hT%d' % i, [128, 4, 512], BF16) for i in range(2)]
        xs = [sb('xs%d' % i, [128, 1024]) for i in range(2)]
        st6 = sb('st6', [128, 2, 6])
        mv = sb('mv', [128, 4])
        bc = [0]

        def nbank():
            b = bc[0] % 8
            bc[0] += 1
            return b
        for hp in range(NH):
            tb0 = hp * HT
            phase_x(k, c, x1_dram[tb0:tb0 + HT, :], HT, xT=xTp)
            router()
            for ex in range(32):
                seti = (hp * 32 + ex) % 2
                wload(lp['e_gate'][ex].rearrange('(a p) c -> p a c', p=128), seti, 0, 'sp')
                wload(lp['e_up'][ex].rearrange('(a p) c -> p a c', p=128), seti, 1, 'act')
                wload(lp['e_down'][ex].rearrange('(a p) c -> p a c', p=128), seti, 2, 'sp')
                wg = wset[seti][0][:, :].rearrange('p (a c) -> p a c', a=8)
                wu = wset[seti][1][:, :].rearrange('p (a c) -> p a c', a=8)
                wd_b = wset[seti][2][:, :].rearrange('p (a c) -> p a c', a=4)
                wgkey, wukey, wdkey = ('e_wset', seti, 0), ('e_wset', seti, 1), ('e_wset', seti, 2)
                for tg in range(NTG):
                    ts0 = tg * 512
                    hh = hT[(ex * NTG + tg) % 2]
                    hkey = ('e_hT', (ex * NTG + tg) % 2)
                    for cc in range(4):
                        bg, bu = nbank(), nbank()
                        psg, psu = c.ps[bg], c.ps[bu]
                        for kc in range(8):
                            k.op('pe', lambda e: e.matmul(psg[:, :], lhsT=wg[:, kc, cc * 128:(cc + 1) * 128],
                                                          rhs=xTp[:, kc, ts0:ts0 + 512], start=(kc == 0),
                                                          stop=(kc == 7)),
                                 reads=[wgkey], writes=[('ps', bg)])
                        for kc in range(8):
                            k.op('pe', lambda e: e.matmul(psu[:, :], lhsT=wu[:, kc, cc * 128:(cc + 1) * 128],
                                                          rhs=xTp[:, kc, ts0:ts0 + 512], start=(kc == 0),
                                                          stop=(kc == 7)),
                                 reads=[wukey], writes=[('ps', bu)])
                        s_ = sl[cc % 2]
                        k.op('act', lambda e: e.activation(out=s_[:, :], in_=psg[:, :], func=AF.Silu),
                             reads=[('ps', bg)], writes=[('e_sl', cc % 2)])
                        k.op('dve', lambda e: e.tensor_tensor(out=hh[:, cc, :], in0=s_[:, :], in1=psu[:, :],
                                                              op=ALU.mult),
                             reads=[('e_sl', cc % 2), ('ps', bu)], writes=[hkey])
                    for bl in range(4):
                        bloc = tg * 4 + bl
                        bglob = tb0 // 128 + bloc
                        for hc in range(2):
                            bank = nbank()
                            ps = c.ps[bank]
                            for cc in range(4):
                                k.op('pe', lambda e: e.matmul(ps[:, :], lhsT=hh[:, cc, bl * 128:(bl + 1) * 128],
                                                              rhs=wd_b[:, cc, hc * 512:(hc + 1) * 512],
                                                              start=(cc == 0), stop=(cc == 3)),
                                     reads=[hkey, wdkey], writes=[('ps', bank)])
                            ya = yacc[:, bloc, hc * 512:(hc + 1) * 512]
                            if ex == 0:
                                k.op('dve', lambda e: e.tensor_scalar(out=ya, in0=ps[:, :],
                                                                      scalar1=gate[:, bloc, ex:ex + 1], scalar2=None,
                                                                      op0=ALU.mult),
                                     reads=[('ps', bank), ('e_gate', 0)], writes=[('e_y', bloc)])
                            else:
                                k.op('dve', lambda e: e.scalar_tensor_tensor(out=ya, in0=ps[:, :],
                                                                             scalar=gate[:, bloc, ex:ex + 1], in1=ya,
                                                                             op0=ALU.mult, op1=ALU.add),
                                     reads=[('ps', bank), ('e_gate', 0), ('e_y', bloc)], writes=[('e_y', bloc)])
            for bloc in range(NBH):
                bglob = tb0 // 128 + bloc
                i2 = bloc % 2
                bs = slice(bglob * 128, (bglob + 1) * 128)
                k.dma('sp', xs[i2][:, :], x1_dram[bs, :], writes=[('e_xs', i2)])
                k.op('dve', lambda e: e.scalar_tensor_tensor(out=yacc[:, bloc, :], in0=xs[i2][:, :], scalar=ALPHA,
                                                             in1=yacc[:, bloc, :], op0=ALU.mult, op1=ALU.add),
                     reads=[('e_xs', i2), ('e_y', bloc)], writes=[('e_y', bloc)])
                ln_block(k, c, yacc[:, bloc, :], ('e_y', bloc), xs[i2][:, :], ('e_xs', i2), gb, ('e_gb', 0), (st6, mv))
                k.dma('act', out_dram[bs, :], xs[i2][:, :], reads=[('e_xs', i2)])
        k.barrier()


PARAM_SHAPES = None


def pack_params(inp):
    L = inp['w_in'].shape[0]
    f = lambda a: np.ascontiguousarray(np.asarray(a, dtype=np.float32))
    hp = lambda v: v.reshape(4, 64).T
    p = {}
    p['w_in'] = f(inp['w_in'])
    p['w_branch'] = f(inp['w_branch'])
    p['w_out'] = f(inp['w_out'])
    for n in ('ln1_g', 'ln1_b', 'ln2_g', 'ln2_b'):
        p[n] = f(inp[n]).reshape(L, 1, 1024)
    p['r_w'] = f(np.concatenate([inp['r_group'], inp['r_expert']], axis=2))
    p['r_bias'] = f(np.concatenate([inp['r_group_b'], inp['r_expert_b']], axis=1)).reshape(L, 1, 36)
    p['e_gate'] = f(inp['e_gate'])
    p['e_up'] = f(inp['e_up'])
    p['e_down'] = f(inp['e_down'])
    p64 = []
    for l in range(L):
        v0 = inp['c_v0'][max(l - 1, 0)]
        p64.append(np.concatenate([np.asarray(inp['c_mu'][l]).reshape(14, 64).T, hp(np.asarray(inp['c_w0'][l])),
                                   hp(np.asarray(inp['c_a0'][l])), hp(np.asarray(inp['c_kk'][l])),
                                   hp(np.asarray(inp['c_ka'][l])), hp(np.asarray(inp['c_rk'][l]).reshape(-1)),
                                   hp(np.asarray(inp['c_gn_w'][l])), hp(np.asarray(inp['c_gn_b'][l])),
                                   hp(np.asarray(v0))], axis=1))
    p['c_p64'] = f(np.stack(p64))
    p['c_wa2'] = f(np.concatenate([inp['c_w2'], inp['c_a2']], axis=1))
    p['c_g2'] = f(inp['c_g2'])
    p['c_v1'] = f(np.asarray(inp['c_v1']).reshape(L - 1, 4, 64, 16).transpose(0, 2, 1, 3))
    p['c_v2'] = f(inp['c_v2'])
    p['d_cw'] = f(np.asarray(inp['d_conv_w']).transpose(0, 2, 1).reshape(L, 8, 64, 4).transpose(0, 2, 1, 3))
    p['d_cb'] = f(np.asarray(inp['d_conv_b']).reshape(L, 8, 64).transpose(0, 2, 1))
    p['d_nw'] = f(np.asarray(inp['d_norm_w']).reshape(L, 4, 64).transpose(0, 2, 1))
    p['d_vec'] = f(np.concatenate([inp['d_dt_bias'], inp['d_a_log'], inp['d_skip']], axis=1)).reshape(L, 1, 12)
    return p


def build_full(pshapes, T=4096, depth=2, debug=False, phases=None):
    nc = bass.Bass("TRN2", target_bir_lowering=False)
    k = K(nc)
    c = Ctx()
    x = nc.dram_tensor("x", [T, 1024], F32, kind="ExternalInput").ap()
    y = nc.dram_tensor("y", [T, 1024], F32, kind="ExternalOutput").ap()
    P = {n: nc.dram_tensor(n, list(shp), F32, kind="ExternalInput").ap() for n, shp in pshapes.items()}
    c.cin = {n: nc.dram_tensor(n, list(v.shape), F32, kind="ExternalInput").ap() for n, v in make_consts().items()}
    alloc_scratch(nc, c, T, debug=debug)
    kind = 'ExternalOutput' if debug else 'Internal'
    x1 = nc.dram_tensor('x1s', [T, 1024], F32, kind=kind).ap()
    xmid = nc.dram_tensor('xmid', [T, 1024], F32, kind=kind).ap()
    with ExitStack() as st:
        setup_common(nc, k, c, T, st)
        for l in range(depth):
            lp = {n: P[n][l] for n in P if n not in ('c_v1', 'c_v2')}
            if l > 0:
                lp['c_v1'] = P['c_v1'][l - 1]
                lp['c_v2'] = P['c_v2'][l - 1]
            x_in = x if l == 0 else xmid
            x_out = y if l == depth - 1 else xmid
            on = lambda n: phases is None or n in phases
            if on('p'):
                with ExitStack() as st2:
                    c.xT = st2.enter_context(_sbt(nc, 'xT', [128, 8, T], BF16))
                    phase_x(k, c, x_in, T)
                    phase_p(k, c, lp['w_in'], T)
            if on('a'):
                mixer_a(k, c, T)
            if on('b'):
                mixer_b(k, c, T)
            if on('c'):
                mixer_c(k, c, T, lp, l)
            if on('d'):
                mixer_d(k, c, T, lp)
            if on('m'):
                phase_merge(k, c, T, lp, x_in, x1)
            if on('e'):
                phase_moe(k, c, T, lp, x1, x_out)
        k.barrier()
    return nc, k


def kernel(**inputs):
    x = np.asarray(inputs['x'], dtype=np.float32)
    B, T, _ = x.shape
    p = pack_params(inputs)
    pshapes = {n: v.shape for n, v in p.items()}
    nc, _k = build_full(pshapes, T=T, depth=p['w_in'].shape[0])
    cs = make_consts()
    in_maps = []
    for b in range(B):
        m = {'x': np.ascontiguousarray(x[b])}
        m.update(p)
        m.update(cs)
        in_maps.append(m)
    res = run_bass_kernel_spmd(nc, in_maps, core_ids=list(range(B)))
    return np.stack([np.asarray(r['y'], dtype=np.float32) for r in res.results], axis=0)
```

```python
import numpy as np
from contextlib import ExitStack
import concourse.bass as bass
import concourse.mybir as mybir
from concourse.bass_utils import run_bass_kernel_spmd

F32 = mybir.dt.float32
BF16 = mybir.dt.bfloat16
AF = mybir.ActivationFunctionType
ALU = mybir.AluOpType
AX = mybir.AxisListType

ENG = ('pe', 'act', 'dve', 'pool', 'sp')
SAME_ENG_SYNC = True


_UID = [0]


def _sbt(nc, name, shape, dtype):
    _UID[0] += 1
    return nc.sbuf_tensor('%s_u%d' % (name, _UID[0]), shape, dtype)


class K:
    def __init__(self, nc):
        self.nc = nc
        self.eng = {'pe': nc.tensor, 'act': nc.scalar, 'dve': nc.vector,
                    'pool': nc.gpsimd, 'sp': nc.sync}
        self.sem = {e: nc.alloc_semaphore('s_' + e) for e in ENG}
        self.cnt = {e: 0 for e in ENG}
        self.known = {e: {} for e in ENG}
        self.res = {}
        self.NDS = 16
        self.dq = ('sp', 'act', 'pool')
        self.dsem = {q: [nc.alloc_semaphore('d_%s_%d' % (q, i)) for i in range(self.NDS)]
                     for q in self.dq}
        self.dcnt = {q: 0 for q in self.dq}
        self.semobj = {}
        for e in ENG:
            self.semobj['E' + e] = self.sem[e]
        for q in self.dq:
            for i in range(self.NDS):
                self.semobj['D%s%d' % (q, i)] = self.dsem[q][i]
        self.ninst = 0

    def _collect(self, reads, writes):
        need = {}
        for r in reads:
            st = self.res.get(r)
            if st is not None and st[0] is not None:
                k, v = st[0]
                if need.get(k, 0) < v:
                    need[k] = v
        for w in writes:
            st = self.res.get(w)
            if st is not None:
                if st[0] is not None:
                    k, v = st[0]
                    if need.get(k, 0) < v:
                        need[k] = v
                for k, v in st[1].items():
                    if need.get(k, 0) < v:
                        need[k] = v
        return need

    def _wait(self, e, need):
        kn = self.known[e]
        for k, v in need.items():
            if k == 'E' + e and (e == 'pe' or not SAME_ENG_SYNC):
                continue
            if kn.get(k, 0) < v:
                self.eng[e].wait_ge(self.semobj[k], v)
                kn[k] = v
                self.ninst += 1

    def _record(self, ev, reads, writes):
        for w in writes:
            self.res[w] = [ev, {}]
        for r in reads:
            st = self.res.get(r)
            if st is None:
                st = [None, {}]
                self.res[r] = st
            k, v = ev
            if st[1].get(k, 0) < v:
                st[1][k] = v

    def op(self, e, fn, reads=(), writes=()):
        if any(r[0] == 'ps' for r in reads):
            writes = list(writes) + [r for r in reads if r[0] == 'ps']
            reads = [r for r in reads if r[0] != 'ps']
        self._wait(e, self._collect(reads, writes))
        inst = fn(self.eng[e])
        self.cnt[e] += 1
        inst.then_inc(self.sem[e], 1)
        self.ninst += 1
        self._record(('E' + e, self.cnt[e]), reads, writes)

    def dma(self, q, out, in_, reads=(), writes=(), **kw):
        if q == 'act':
            q = 'sp'
        need = self._collect(reads, writes)
        n = self.dcnt[q]
        slot, rnd = n % self.NDS, n // self.NDS
        key = 'D%s%d' % (q, slot)
        if rnd > 0 and need.get(key, 0) < 16 * rnd:
            need[key] = 16 * rnd
        self._wait(q, need)
        inst = self.eng[q].dma_start(out=out, in_=in_, **kw)
        inst.then_inc(self.dsem[q][slot], 16)
        self.dcnt[q] = n + 1
        self.ninst += 1
        self._record((key, 16 * (rnd + 1)), reads, writes)

    def all_events(self):
        need = {}
        for e in ENG:
            if self.cnt[e] > 0:
                need['E' + e] = self.cnt[e]
        for q in self.dq:
            n = self.dcnt[q]
            for slot in range(self.NDS):
                uses = (n - slot + self.NDS - 1) // self.NDS if n > slot else 0
                if uses > 0:
                    need['D%s%d' % (q, slot)] = 16 * uses
        return need

    def barrier(self, engines=ENG):
        need = self.all_events()
        for e in engines:
            self._wait(e, dict(need))
        self.res = {}


D_MODEL = 1024
IN_W = 9160
OFF = dict(a_qkv=0, b_qkv=2304, biq=3072, bik=3328, biw=3392, c_in=3396, d_z=4292, d_xbc=4548,
           d_dt=5060, gates=5064)
A_PAT = ((128, 1), (512, 4), (2048, 16))


class Ctx:
    pass


def phase_x(k, c, x_dram, T, xT=None):
    nc = k.nc
    NB = T // 128
    if xT is None:
        xT = c.xT
    with ExitStack() as st:
        xs = [st.enter_context(_sbt(nc, 'xs%d' % i, [128, 1024], F32)) for i in range(2)]
        for b in range(NB):
            s = xs[b % 2]
            k.dma('sp' if b % 2 == 0 else 'act', s[:, :], x_dram[b * 128:(b + 1) * 128, :],
                  writes=[('xs', b % 2)])
            for half in range(2):
                bank = (2 * b + half) % 8
                ps = c.ps[bank]
                for j in range(4):
                    ch = half * 4 + j
                    k.op('pe', lambda e, ps=ps, j=j, ch=ch, s=s: e.transpose(
                        out=ps[:, j * 128:(j + 1) * 128], in_=s[:, ch * 128:(ch + 1) * 128],
                        identity=c.ident[:, :]),
                        reads=[('xs', b % 2)], writes=[('ps', bank)])
                dst = xT[:, half * 4:(half + 1) * 4, b * 128:(b + 1) * 128]
                src_ = ps[:, :].rearrange('p (j n) -> p j n', j=4)
                if half == 0:
                    k.op('act', lambda e, dst=dst, src_=src_: e.copy(out=dst, in_=src_),
                         reads=[('ps', bank)], writes=[('xT', b)])
                else:
                    k.op('dve', lambda e, dst=dst, src_=src_: e.tensor_copy(out=dst, in_=src_),
                         reads=[('ps', bank)], writes=[('xT', b)])
        k.barrier()


class WLoader:
    def __init__(self, k, st, name, width=512, nbuf=2):
        nc = k.nc
        self.k = k
        self.name = name
        self.nbuf = nbuf
        self.f = [st.enter_context(_sbt(nc, '%s_f%d' % (name, i), [128, 8, width], F32)) for i in range(nbuf)]
        self.b = [st.enter_context(_sbt(nc, '%s_b%d' % (name, i), [128, 8, width], BF16)) for i in range(nbuf)]
        self.n = 0

    def load(self, w_dram, col0, ncols, q='sp', cast='pool'):
        k = self.k
        i = self.n % self.nbuf
        self.n += 1
        src = w_dram[:, col0:col0 + ncols].rearrange('(ko ki) n -> ki ko n', ki=128)
        k.dma(q, self.f[i][:, :, 0:ncols], src, writes=[(self.name + 'f', i)])
        fi, bi = self.f[i], self.b[i]
        if cast == 'pool':
            k.op('pool', lambda e: e.tensor_copy(out=bi[:, :, 0:ncols], in_=fi[:, :, 0:ncols]),
                 reads=[(self.name + 'f', i)], writes=[(self.name + 'b', i)])
        else:
            k.op('dve', lambda e: e.tensor_copy(out=bi[:, :, 0:ncols], in_=fi[:, :, 0:ncols]),
                 reads=[(self.name + 'f', i)], writes=[(self.name + 'b', i)])
        return bi, (self.name + 'b', i)


def phase_p(k, c, w_in, T, only=None):
    nc = k.nc
    NB = T // 128
    NG = T // 512
    d = c.d
    with ExitStack() as st:
        wl = WLoader(k, st, 'wl')
        stg = [st.enter_context(_sbt(nc, 'pstg%d' % i, [128, T], F32)) for i in range(2)]
        stgb = [st.enter_context(_sbt(nc, 'pstgb%d' % i, [128, T], BF16)) for i in range(2)]
        tst = [st.enter_context(_sbt(nc, 'ptst%d' % i, [128, 512], F32)) for i in range(2)]
        tstb = [st.enter_context(_sbt(nc, 'ptstb%d' % i, [128, 512], BF16)) for i in range(2)]
        cnt = {'bank': 0, 'fm': 0, 'tm': 0, 'ev': 0}

        def evac(dst, src, bank, wkey, func=None, scale=1.0):
            cnt['ev'] += 1
            if func is not None or cnt['ev'] % 2 == 0:
                f = func if func is not None else AF.Copy
                k.op('act', lambda e: e.activation(out=dst, in_=src, func=f, scale=scale),
                     reads=[('ps', bank)], writes=[wkey])
            else:
                k.op('dve', lambda e: e.tensor_scalar(out=dst, in0=src, scalar1=float(scale), scalar2=None,
                                                      op0=ALU.mult),
                     reads=[('ps', bank)], writes=[wkey])

        jobs = []

        def fm_group(col0, total, cw, dst_fn, bf, pad=0, scale=1.0):
            for s0 in range(0, total, 512):
                sw = min(512, total - s0)

                def run(wt, wkey, s0=s0, sw=sw):
                    for j0 in range(0, sw, cw):
                        i = cnt['fm'] % 2
                        cnt['fm'] += 1
                        sg = stgb[i] if bf else stg[i]
                        skey = ('pstgb' if bf else 'pstg', i)
                        for g in range(NG):
                            bank = cnt['bank'] % 8
                            cnt['bank'] += 1
                            ps = c.ps[bank]
                            for kk in range(8):
                                k.op('pe', lambda e, ps=ps, kk=kk, j0=j0, g=g: e.matmul(
                                    ps[0:cw, :], lhsT=wt[:, kk, j0:j0 + cw], rhs=c.xT[:, kk, g * 512:(g + 1) * 512],
                                    start=(kk == 0), stop=(kk == 7)),
                                    reads=[wkey, ('xT', 0)], writes=[('ps', bank)])
                            evac(sg[0:cw, g * 512:(g + 1) * 512], ps[0:cw, :], bank, skey, scale=scale)
                        k.dma('sp', dst_fn((s0 + j0) // cw), sg[0:cw, :], reads=[skey])
                jobs.append((col0 + s0, sw, run))

        def tm_group(col0, ncols, dst_fn, bf, func=None, tokens=None, nblk=None):
            def run(wt, wkey):
                for b in range(nblk if nblk is not None else NB):
                    tok = tokens(b) if tokens is not None else slice(b * 128, (b + 1) * 128)
                    bank = cnt['bank'] % 8
                    cnt['bank'] += 1
                    ps = c.ps[bank]
                    for kk in range(8):
                        k.op('pe', lambda e, ps=ps, kk=kk, tok=tok: e.matmul(
                            ps[:, 0:ncols], lhsT=c.xT[:, kk, tok], rhs=wt[:, kk, 0:ncols],
                            start=(kk == 0), stop=(kk == 7)),
                            reads=[wkey, ('xT', 0)], writes=[('ps', bank)])
                    i = cnt['tm'] % 2
                    cnt['tm'] += 1
                    sg = tstb[i] if bf else tst[i]
                    skey = ('ptstb' if bf else 'ptst', i)
                    evac(sg[:, 0:ncols], ps[:, 0:ncols], bank, skey, func=func)
                    k.dma('sp', dst_fn(b), sg[:, 0:ncols], reads=[skey])
            jobs.append((col0, ncols, run))

        def want(n):
            return only is None or n in only

        if want('a'):
            fm_group(OFF['a_qkv'], 768, 64, lambda j: d['aq'][j], True)
            fm_group(OFF['a_qkv'] + 768, 768, 64, lambda j: d['ak'][j], True)
            for g, (win, dil) in enumerate(A_PAT):
                nbc = T // (128 * dil)

                def toks(b, dil=dil, nbc=nbc):
                    r, bi = b // nbc, b % nbc
                    s0 = r + dil * 128 * bi
                    return slice(s0, s0 + dil * 127 + 1, dil)
                tm_group(OFF['a_qkv'] + 1536 + g * 256, 256, lambda b, g=g: d['av'][g, b], True, tokens=toks)
        if want('b'):
            fm_group(OFF['b_qkv'], 256, 64, lambda j: d['bq'][j], True)
            fm_group(OFF['b_qkv'] + 256, 256, 64, lambda j: d['bk'][j], True)
            tm_group(OFF['b_qkv'] + 512, 256, lambda b: d['bv'][b], True)
            fm_group(OFF['biq'], 256, 64, lambda j: d['biq'][j], True)
            fm_group(OFF['bik'], 64, 64, lambda j: d['bik'][j], True)
            tm_group(OFF['biw'], 4, lambda b: d['biw'][b], False)
        if want('c'):
            fm_group(OFF['c_in'], 896, 64, lambda j: d['cpc'][j, :, 1:T + 1], False)
        if want('d'):
            fm_group(OFF['d_z'], 256, 64, lambda j: d['dz'][j], False)
            fm_group(OFF['d_xbc'], 512, 64, lambda j: d['dxbc'][j, :, 3:T + 3], False)
            tm_group(OFF['d_dt'], 4, lambda b: d['ddt'][b], False)
        if want('g'):
            for s in range(8):
                tm_group(OFF['gates'] + s * 512, 512, lambda b, s=s: d['gsig'][b, :, s * 512:(s + 1) * 512], True,
                         func=AF.Sigmoid)
        loaded = {}
        if jobs:
            loaded[0] = wl.load(w_in, jobs[0][0], jobs[0][1])
        for ji, (jc0, jn, run) in enumerate(jobs):
            if ji + 1 < len(jobs):
                loaded[ji + 1] = wl.load(w_in, jobs[ji + 1][0], jobs[ji + 1][1])
            wt, wkey = loaded.pop(ji)
            run(wt, wkey)
        k.barrier()


def alloc_scratch(nc, c, T, debug=False):
    NB = T // 128
    kind = 'ExternalOutput' if debug else 'Internal'
    d = {}

    def dt(name, shape, dtype):
        d[name] = nc.dram_tensor(name, shape, dtype, kind=kind).ap()
    dt('aq', [12, 64, T], BF16)
    dt('ak', [12, 64, T], BF16)
    dt('av', [3, NB, 128, 256], BF16)
    dt('bq', [4, 64, T], BF16)
    dt('bk', [4, 64, T], BF16)
    dt('bv', [NB, 128, 256], BF16)
    dt('biq', [4, 64, T], BF16)
    dt('bik', [1, 64, T], BF16)
    dt('biw', [NB, 128, 4], F32)
    dt('cpc', [14, 64, T + 1], F32)
    dt('dz', [4, 64, T], F32)
    dt('dxbc', [8, 64, T + 3], F32)
    dt('ddt', [NB, 128, 4], F32)
    dt('gsig', [NB, 128, 4096], BF16)
    dt('dxs', [4, 64, T], F32)
    dt('vfirst', [4, 64, T], F32)
    dt('obr', [4, 4, 64, T], BF16)
    c.d = d


def setup_common(nc, k, c, T, st):
    c.ps = [nc.alloc_psum_tensor('ps%d' % i, [128, 512], F32) for i in range(8)]
    c.ident = st.enter_context(_sbt(nc, 'ident_sb', [128, 128], F32))
    k.dma('sp', c.ident[:, :], c.cin['ident'], writes=[('ident', 0)])
    tmp = st.enter_context(_sbt(nc, 'cst_tmp', [128, 256], F32))
    c.maskA = st.enter_context(_sbt(nc, 'maskA_sb', [128, 256], BF16))
    k.dma('sp', tmp[:, :], c.cin['maskA'], writes=[('cst_tmp', 0)])
    k.op('dve', lambda e: e.tensor_copy(out=c.maskA[:, :], in_=tmp[:, :]), reads=[('cst_tmp', 0)],
         writes=[('maskA', 0)])
    c.negmask = st.enter_context(_sbt(nc, 'negmask_sb', [128, 128], F32))
    k.dma('act', c.negmask[:, :], c.cin['negmask'], writes=[('negmask', 0)])
    c.triu = st.enter_context(_sbt(nc, 'triu_sb', [128, 128], F32))
    k.dma('pool', c.triu[:, :], c.cin['triu'], writes=[('triu', 0)])
    c.ones_f = st.enter_context(_sbt(nc, 'ones_f_sb', [128, 128], F32))
    k.dma('sp', c.ones_f[:, :], c.cin['ones_f'], writes=[('ones_f', 0)])
    c.eps5 = st.enter_context(_sbt(nc, 'eps5_sb', [128, 1], F32))
    k.dma('act', c.eps5[:, :], c.cin['eps5'], writes=[('eps', 0)])
    c.mhalf = st.enter_context(_sbt(nc, 'mhalf_sb', [128, 1], F32))
    k.dma('pool', c.mhalf[:, :], c.cin['mhalf'], writes=[('mhalf', 0)])
    c.epsgn = st.enter_context(_sbt(nc, 'epsgn_sb', [128, 1], F32))
    k.dma('sp', c.epsgn[:, :], c.cin['epsgn'], writes=[('epsgn', 0)])
    c.ones_bf = st.enter_context(_sbt(nc, 'ones_bf', [128, 128], BF16))
    k.op('dve', lambda e: e.memset(c.ones_bf[:, :], 1.0), writes=[('ones_bf', 0)])
    k.barrier()


def mixer_a(k, c, T):
    nc = k.nc
    NB = T // 128
    d = c.d
    with ExitStack() as st:
        vall = st.enter_context(_sbt(nc, 'a_v', [128, 3, NB, 256], BF16))
        for g in range(3):
            k.dma(('sp', 'act', 'pool')[g], vall[:, g, :, :], d['av'][g].rearrange('b p c -> p b c'),
                  writes=[('a_v', g)])
        qk = [[st.enter_context(_sbt(nc, 'a_qk%d%d' % (g, s), [64, T], BF16)) for s in range(2)]
              for g in range(3)]
        uz = st.enter_context(_sbt(nc, 'a_uz', [64, 2, T], F32))
        rz = st.enter_context(_sbt(nc, 'a_rz', [64, T], F32))
        ob = st.enter_context(_sbt(nc, 'a_ob', [64, T], BF16))
        Eb = [st.enter_context(_sbt(nc, 'a_E%d' % i, [128, 256], BF16)) for i in range(2)]
        Pb = [st.enter_context(_sbt(nc, 'a_P%d' % i, [128, 256], BF16)) for i in range(2)]
        it = 0
        for h in range(4):
            for g in range(3):
                k.dma('sp', qk[g][0][:, :], d['aq'][g * 4 + h], writes=[('a_q', g)])
                k.dma('act', qk[g][1][:, :], d['ak'][g * 4 + h], writes=[('a_k', g)])
            for g, (win, dil) in enumerate(A_PAT):
                nbc = T // (128 * dil)
                qT, kT = qk[g]
                for blk in range(NB):
                    r, bi = blk // nbc, blk % nbc
                    s0 = r + dil * 128 * bi
                    qs = slice(s0, s0 + dil * 127 + 1, dil)
                    bs, bu = it % 4, 4 + it % 4
                    ps_s, ps_u = c.ps[bs], c.ps[bu]
                    i2 = it % 2
                    it += 1
                    lo = 128 if bi == 0 else 0
                    tiles = ([] if bi == 0 else [(0, blk - 1, s0 - dil * 128)]) + [(1, blk, s0)]
                    for slot, kb, ks0 in tiles:
                        ks = slice(ks0, ks0 + dil * 127 + 1, dil)
                        k.op('pe', lambda e, slot=slot, ks=ks: e.matmul(
                            ps_s[:, slot * 128:(slot + 1) * 128], lhsT=kT[:, ks], rhs=qT[:, qs],
                            start=True, stop=True),
                            reads=[('a_q', g), ('a_k', g)], writes=[('ps', bs)])
                    E, P = Eb[i2], Pb[i2]
                    k.op('act', lambda e: e.activation(out=E[:, lo:256], in_=ps_s[:, lo:256], func=AF.Exp,
                                                       scale=0.125),
                         reads=[('ps', bs)], writes=[('a_E', i2)])
                    k.op('dve', lambda e: e.tensor_tensor(out=P[:, lo:256], in0=E[:, lo:256],
                                                          in1=c.maskA[:, lo:256], op=ALU.mult),
                         reads=[('a_E', i2), ('maskA', 0)], writes=[('a_P', i2)])
                    for ti, (slot, kb, ks0) in enumerate(tiles):
                        first, last = ti == 0, ti == len(tiles) - 1
                        k.op('pe', lambda e, slot=slot, kb=kb, first=first, last=last: e.matmul(
                            ps_u[0:64, 0:128], lhsT=vall[:, g, kb, h * 64:(h + 1) * 64],
                            rhs=P[:, slot * 128:(slot + 1) * 128], start=first, stop=last,
                            skip_group_check=True),
                            reads=[('a_P', i2), ('a_v', g)], writes=[('ps', bu)])
                        k.op('pe', lambda e, slot=slot, first=first, last=last: e.matmul(
                            ps_u[0:64, 128:256], lhsT=c.ones_bf[:, 0:64],
                            rhs=P[:, slot * 128:(slot + 1) * 128], start=False, stop=last,
                            skip_group_check=True),
                            reads=[('a_P', i2), ('ones_bf', 0)], writes=[('ps', bu)])
                    dst = uz[:, :, qs]
                    src = ps_u[0:64, 0:256].rearrange('p (a n) -> p a n', a=2)
                    if g == 0:
                        k.op('act', lambda e, dst=dst, src=src: e.copy(out=dst, in_=src),
                             reads=[('ps', bu)], writes=[('a_uz', 0)])
                    else:
                        k.op('dve', lambda e, dst=dst, src=src: e.tensor_tensor(out=dst, in0=dst, in1=src,
                                                                                op=ALU.add),
                             reads=[('ps', bu), ('a_uz', 0)], writes=[('a_uz', 0)])
            k.op('dve', lambda e: e.reciprocal(out=rz[:, :], in_=uz[:, 1, :]), reads=[('a_uz', 0)],
                 writes=[('a_rz', 0)])
            k.op('dve', lambda e: e.tensor_tensor(out=ob[:, :], in0=uz[:, 0, :], in1=rz[:, :], op=ALU.mult),
                 reads=[('a_uz', 0), ('a_rz', 0)], writes=[('a_ob', 0)])
            k.dma('sp', d['obr'][0, h], ob[:, :], reads=[('a_ob', 0)])
        k.barrier()


def make_consts():
    j = np.arange(128)[:, None]
    i = np.arange(128)[None, :]
    cs = {}
    cs['ident'] = np.eye(128, dtype=np.float32)
    cs['maskA'] = np.concatenate([(j >= i), (j <= i)], axis=1).astype(np.float32)
    cs['triu'] = (j <= i).astype(np.float32)
    cs['ones_f'] = np.ones((128, 128), np.float32)
    cs['eps5'] = np.full((128, 1), 1e-5, np.float32)
    cs['mhalf'] = np.full((128, 1), -0.5, np.float32)
    cs['epsgn'] = np.full((128, 1), 64e-5, np.float32)
    s_ = np.arange(64)[:, None]
    t_ = np.arange(64)[None, :]
    cs['cmask'] = np.stack([(s_ < t_), (s_ <= t_), (s_ > t_)], axis=1).astype(np.float32)
    cs['negmask'] = np.where(i <= j, 0.0, -1.0e30).astype(np.float32)
    return cs


NEG = -1.0e30


def mixer_b(k, c, T):
    nc = k.nc
    NB = T // 128
    d = c.d
    with ExitStack() as st:
        def sb(name, shape, dt_):
            return st.enter_context(_sbt(nc, name, shape, dt_))
        bqs = [sb('b_q%d' % i, [64, 4, 128], BF16) for i in range(2)]
        bk = sb('b_k', [64, 4, T], BF16)
        biqs = [sb('b_iq%d' % i, [64, 4, 128], BF16) for i in range(2)]
        bik = sb('b_ik', [64, T], BF16)
        bv = sb('b_v', [128, NB, 256], BF16)
        biw = sb('b_iw', [128, NB, 4], F32)
        scores = [sb('b_score%d' % i, [128, T], F32) for i in range(2)]
        works = [sb('b_work%d' % i, [128, T], F32) for i in range(2)]
        cums = [sb('b_cum%d' % i, [128, T], F32) for i in range(2)]
        ngts = [sb('b_ngt%d' % i, [128, 2], F32) for i in range(2)]
        selTs = [sb('b_selT%d' % i, [128, NB, 128], BF16) for i in range(2)]
        rt = [sb('b_rt%d' % i, [128, 512], F32) for i in range(2)]
        m8s = [[sb('b_m8%d_%d' % (j, i), [128, 8], F32) for i in range(2)] for j in range(2)]
        Eb = [sb('b_E%d' % i, [128, 512], BF16) for i in range(2)]
        Pb = [sb('b_P%d' % i, [128, 512], BF16) for i in range(2)]
        rz = sb('b_rz', [64, 128], F32)
        ob = [sb('b_ob%d' % i, [64, 128], BF16) for i in range(2)]
        k.dma('act', bk[:, :, :], d['bk'].rearrange('h p t -> p h t'), writes=[('b_k', 0)])
        k.dma('sp', bik[:, :], d['bik'][0], writes=[('b_ik', 0)])
        k.dma('act', bv[:, :, :], d['bv'].rearrange('b p c -> p b c'), writes=[('b_v', 0)])
        k.dma('pool', biw[:, :, :], d['biw'].rearrange('b p c -> p b c'), writes=[('b_iw', 0)])
        cn = {'bank': 0, 'rt': 0, 'ep': 0, 'ob': 0}

        def nbank():
            b = cn['bank'] % 8
            cn['bank'] += 1
            return b
        def stage1(qb):
            par = qb % 2
            score, work, cum, ngt, selT, m8 = scores[par], works[par], cums[par], ngts[par], selTs[par], m8s[par]
            L = 128 * (qb + 1)
            qs = slice(qb * 128, (qb + 1) * 128)
            bq, biq = bqs[qb % 2], biqs[qb % 2]
            k.dma('sp', bq[:, :, :], d['bq'][:, :, qs].rearrange('h p t -> p h t'), writes=[('b_q', qb % 2)])
            k.dma('act', biq[:, :, :], d['biq'][:, :, qs].rearrange('h p t -> p h t'), writes=[('b_iq', qb % 2)])
            for kg in range((L + 511) // 512):
                n = min(512, L - kg * 512)
                seg = slice(kg * 512, kg * 512 + n)
                for ih in range(4):
                    bank = nbank()
                    ps = c.ps[bank]
                    k.op('pe', lambda e: e.matmul(ps[:, 0:n], lhsT=biq[:, ih, :], rhs=bik[:, seg],
                                                  start=True, stop=True),
                         reads=[('b_iq', qb % 2), ('b_ik', 0)], writes=[('ps', bank)])
                    ri = cn['rt'] % 2
                    cn['rt'] += 1
                    r_ = rt[ri]
                    k.op('act', lambda e: e.activation(out=r_[:, 0:n], in_=ps[:, 0:n], func=AF.Relu),
                         reads=[('ps', bank)], writes=[('b_rt', ri)])
                    if ih == 0:
                        k.op('dve', lambda e: e.tensor_scalar(out=score[:, seg], in0=r_[:, 0:n],
                                                              scalar1=biw[:, qb, 0:1], scalar2=None, op0=ALU.mult),
                             reads=[('b_rt', ri), ('b_iw', 0)], writes=[('b_score', par)])
                    else:
                        k.op('dve', lambda e: e.scalar_tensor_tensor(
                            out=score[:, seg], in0=r_[:, 0:n], scalar=biw[:, qb, ih:ih + 1], in1=score[:, seg],
                            op0=ALU.mult, op1=ALU.add),
                            reads=[('b_rt', ri), ('b_iw', 0), ('b_score', par)], writes=[('b_score', par)])
            yield
            k.op('dve', lambda e: e.tensor_tensor(out=score[:, qs], in0=score[:, qs], in1=c.negmask[:, :],
                                                  op=ALU.add),
                 reads=[('b_score', par), ('negmask', 0)], writes=[('b_score', par)])
            if qb >= 2:
                for r in range(32):
                    m = m8[r % 2]
                    src = score if r == 0 else work
                    k.op('dve', lambda e: e.max(out=m[:, :], in_=src[:, 0:L]),
                         reads=[('b_score', par), ('b_work', par)], writes=[('b_m8', par, r % 2)])
                    if r < 31:
                        k.op('dve', lambda e: e.match_replace(out=work[:, 0:L], in_to_replace=m[:, :],
                                                              in_values=src[:, 0:L], imm_value=NEG),
                             reads=[('b_score', par), ('b_m8', par, r % 2)], writes=[('b_work', par)])
                    yield
                thr = m8[1][:, 7:8]
                k.op('dve', lambda e: e.tensor_scalar(out=work[:, 0:L], in0=score[:, 0:L], scalar1=thr,
                                                      scalar2=0.0, op0=ALU.is_gt, op1=ALU.add,
                                                      accum_out=ngt[:, 0:1]),
                     reads=[('b_score', par), ('b_m8', par, 1)], writes=[('b_work', par), ('b_ngt', par, 0)])
                k.op('dve', lambda e: e.tensor_scalar(out=ngt[:, 1:2], in0=ngt[:, 0:1], scalar1=-1.0,
                                                      scalar2=256.0, op0=ALU.mult, op1=ALU.add),
                     reads=[('b_ngt', par, 0)], writes=[('b_ngt', par, 1)])
                k.op('dve', lambda e: e.tensor_scalar(out=work[:, 0:L], in0=score[:, 0:L], scalar1=thr,
                                                      scalar2=None, op0=ALU.is_equal),
                     reads=[('b_score', par), ('b_m8', par, 1), ('b_ngt', par, 0)], writes=[('b_work', par)])
                k.op('dve', lambda e: e.tensor_tensor_scan(out=cum[:, 0:L], data0=work[:, 0:L],
                                                           data1=work[:, 0:L], initial=0.0,
                                                           op0=ALU.add, op1=ALU.max),
                     reads=[('b_work', par)], writes=[('b_cum', par)])
                k.op('dve', lambda e: e.scalar_tensor_tensor(out=cum[:, 0:L], in0=cum[:, 0:L],
                                                             scalar=ngt[:, 1:2], in1=work[:, 0:L],
                                                             op0=ALU.is_le, op1=ALU.mult),
                     reads=[('b_work', par), ('b_cum', par), ('b_ngt', par, 1)], writes=[('b_cum', par)])
                k.op('dve', lambda e: e.scalar_tensor_tensor(out=work[:, 0:L], in0=score[:, 0:L],
                                                             scalar=thr, in1=cum[:, 0:L],
                                                             op0=ALU.is_gt, op1=ALU.add),
                     reads=[('b_score', par), ('b_cum', par), ('b_m8', par, 1)], writes=[('b_work', par)])
            else:
                k.op('dve', lambda e: e.tensor_scalar(out=work[:, 0:L], in0=score[:, 0:L],
                                                      scalar1=-1.0e29, scalar2=None, op0=ALU.is_ge),
                     reads=[('b_score', par)], writes=[('b_work', par)])
            for kb0 in range(0, qb + 1, 4):
                nk = min(4, qb + 1 - kb0)
                bank = nbank()
                ps = c.ps[bank]
                for j in range(nk):
                    kb = kb0 + j
                    k.op('pe', lambda e: e.transpose(out=ps[:, j * 128:(j + 1) * 128],
                                                     in_=work[:, kb * 128:(kb + 1) * 128], identity=c.ident[:, :]),
                         reads=[('b_work', par)], writes=[('ps', bank)])
                k.op('act', lambda e: e.copy(out=selT[:, kb0:kb0 + nk, :],
                                             in_=ps[:, 0:nk * 128].rearrange('p (a n) -> p a n', a=nk)),
                     reads=[('ps', bank)], writes=[('b_selT', par)])
            yield

        def stage2(qb):
            par = qb % 2
            selT = selTs[par]
            qs = slice(qb * 128, (qb + 1) * 128)
            bq = bqs[par]
            for h in range(4):
                bu = nbank()
                ps_u = c.ps[bu]
                for kb0 in range(0, qb + 1, 4):
                    nk = min(4, qb + 1 - kb0)
                    bs = nbank()
                    if bs == bu:
                        bs = nbank()
                    ps_s = c.ps[bs]
                    for j in range(nk):
                        kb = kb0 + j
                        k.op('pe', lambda e: e.matmul(ps_s[:, j * 128:(j + 1) * 128],
                                                      lhsT=bk[:, h, kb * 128:(kb + 1) * 128], rhs=bq[:, h, :],
                                                      start=True, stop=True),
                             reads=[('b_q', qb % 2), ('b_k', 0)], writes=[('ps', bs)])
                    ei = cn['ep'] % 2
                    cn['ep'] += 1
                    E, P = Eb[ei], Pb[ei]
                    k.op('act', lambda e: e.activation(out=E[:, 0:nk * 128], in_=ps_s[:, 0:nk * 128], func=AF.Exp,
                                                       scale=0.125),
                         reads=[('ps', bs)], writes=[('b_E', ei)])
                    k.op('dve', lambda e: e.tensor_tensor(
                        out=P[:, 0:nk * 128], in0=E[:, 0:nk * 128],
                        in1=selT[:, kb0:kb0 + nk, :].rearrange('p a n -> p (a n)'), op=ALU.mult),
                        reads=[('b_E', ei), ('b_selT', par)], writes=[('b_P', ei)])
                    for j in range(nk):
                        kb = kb0 + j
                        first = (kb == 0)
                        last = (kb == qb)
                        k.op('pe', lambda e: e.matmul(ps_u[0:64, 0:128], lhsT=bv[:, kb, h * 64:(h + 1) * 64],
                                                      rhs=P[:, j * 128:(j + 1) * 128], start=first, stop=last,
                                                      skip_group_check=True),
                             reads=[('b_P', ei), ('b_v', 0)], writes=[('ps', bu)])
                        k.op('pe', lambda e: e.matmul(ps_u[0:64, 128:256], lhsT=c.ones_bf[:, 0:64],
                                                      rhs=P[:, j * 128:(j + 1) * 128], start=False, stop=last,
                                                      skip_group_check=True),
                             reads=[('b_P', ei), ('ones_bf', 0)], writes=[('ps', bu)])
                    yield
                k.op('dve', lambda e: e.reciprocal(out=rz[:, :], in_=ps_u[0:64, 128:256]),
                     reads=[('ps', bu)], writes=[('b_rz', 0)])
                oi = cn['ob'] % 2
                cn['ob'] += 1
                o_ = ob[oi]
                k.op('dve', lambda e: e.tensor_tensor(out=o_[:, :], in0=ps_u[0:64, 0:128], in1=rz[:, :],
                                                      op=ALU.mult),
                     reads=[('ps', bu), ('b_rz', 0)], writes=[('b_ob', oi)])
                k.dma('sp' if oi == 0 else 'act', d['obr'][1, h, :, qs], o_[:, :], reads=[('b_ob', oi)])

        def run_interleaved(g1, g2):
            d1, d2 = g1 is None, g2 is None
            while not (d1 and d2):
                if not d1:
                    try:
                        next(g1)
                    except StopIteration:
                        d1 = True
                if not d2:
                    try:
                        next(g2)
                    except StopIteration:
                        d2 = True
        prev = None
        for qb in range(NB):
            run_interleaved(stage1(qb), prev)
            prev = stage2(qb)
        run_interleaved(None, prev)
        k.barrier()


def mixer_d(k, c, T, lp):
    nc = k.nc
    NB = T // 128
    d = c.d
    with ExitStack() as st:
        def sb(name, shape, dt_):
            return st.enter_context(_sbt(nc, 'sb_' + name, shape, dt_))
        BT = sb('d_BT', [64, 2, T], BF16)
        CT = sb('d_CT', [64, 2, T], BF16)
        xbar = sb('d_xbar', [128, NB, 256], BF16)
        Btok = sb('d_Btok', [128, NB, 2, 64], BF16)
        dte = sb('d_dte', [128, NB, 4], F32)
        etot = sb('d_etot', [128, NB, 4], F32)
        cw = sb('d_cw', [64, 8, 4], F32)
        cb = sb('d_cb', [64, 8], F32)
        nw = sb('d_nw', [64, 4], F32)
        dvec = sb('d_vec', [128, 12], F32)
        dt_ = sb('d_dt', [128, NB, 4], F32)
        a_tok = sb('d_atok', [128, NB, 4], F32)
        acs = sb('d_acs', [128, NB, 4], F32)
        tot = sb('d_tot', [128, NB, 4], F32)
        pre = sb('d_pre', [128, NB, 4], F32)
        Abc = sb('d_Abc', [128, 4], F32)
        k.dma('sp', cw[:, :, :], lp['d_cw'], writes=[('d_cw', 0)])
        k.dma('act', cb[:, :], lp['d_cb'], writes=[('d_cb', 0)])
        k.dma('pool', nw[:, :], lp['d_nw'], writes=[('d_nw', 0)])
        k.dma('sp', dvec[:, :], lp['d_vec'].partition_broadcast(128), writes=[('d_vec', 0)])
        k.dma('act', dt_[:, :, :], d['ddt'].rearrange('b p c -> p b c'), writes=[('d_dt', 0)])
        k.op('dve', lambda e: e.tensor_tensor(out=dt_[:, :, :], in0=dt_[:, :, :],
                                              in1=dvec[:, 0:4].unsqueeze(1).to_broadcast([128, NB, 4]), op=ALU.add),
             reads=[('d_dt', 0), ('d_vec', 0)], writes=[('d_dt', 0)])
        k.op('act', lambda e: e.activation(out=dt_[:, :, :], in_=dt_[:, :, :], func=AF.Exp),
             reads=[('d_dt', 0)], writes=[('d_dt', 0)])
        k.op('act', lambda e: e.activation(out=dt_[:, :, :], in_=dt_[:, :, :], func=AF.Ln, bias=1.0),
             reads=[('d_dt', 0)], writes=[('d_dt', 0)])
        k.op('act', lambda e: e.activation(out=Abc[:, :], in_=dvec[:, 4:8], func=AF.Exp),
             reads=[('d_vec', 0)], writes=[('d_Abc', 0)])
        k.op('dve', lambda e: e.scalar_tensor_tensor(out=a_tok[:, :, :], in0=dt_[:, :, :], scalar=-1.0,
                                                     in1=Abc[:, :].unsqueeze(1).to_broadcast([128, NB, 4]),
                                                     op0=ALU.mult, op1=ALU.mult),
             reads=[('d_dt', 0), ('d_Abc', 0)], writes=[('d_atok', 0)])
        bank = 0
        ps = c.ps[bank]
        k.op('pe', lambda e: e.matmul(ps[:, 0:NB * 4], lhsT=c.triu[:, :], rhs=a_tok[:, :, :].rearrange('p b c -> p (b c)'),
                                      start=True, stop=True),
             reads=[('d_atok', 0), ('triu', 0)], writes=[('ps', bank)])
        k.op('dve', lambda e: e.tensor_copy(out=acs[:, :, :].rearrange('p b c -> p (b c)'), in_=ps[:, 0:NB * 4]),
             reads=[('ps', bank)], writes=[('d_acs', 0)])
        bank = 1
        ps1 = c.ps[bank]
        k.op('pe', lambda e: e.matmul(ps1[:, 0:NB * 4], lhsT=c.ones_f[:, :], rhs=a_tok[:, :, :].rearrange('p b c -> p (b c)'),
                                      start=True, stop=True),
             reads=[('d_atok', 0), ('ones_f', 0)], writes=[('ps', bank)])
        k.op('dve', lambda e: e.tensor_copy(out=tot[:, :, :].rearrange('p b c -> p (b c)'), in_=ps1[:, 0:NB * 4]),
             reads=[('ps', bank)], writes=[('d_tot', 0)])
        k.op('dve', lambda e: e.tensor_tensor(out=dte[:, :, :], in0=tot[:, :, :], in1=acs[:, :, :], op=ALU.subtract),
             reads=[('d_acs', 0), ('d_tot', 0)], writes=[('d_dte', 0)])
        k.op('act', lambda e: e.activation(out=dte[:, :, :], in_=dte[:, :, :], func=AF.Exp),
             reads=[('d_dte', 0)], writes=[('d_dte', 0)])
        k.op('act', lambda e: e.activation(out=etot[:, :, :], in_=tot[:, :, :], func=AF.Exp),
             reads=[('d_tot', 0)], writes=[('d_etot', 0)])
        with ExitStack() as st2:
            xin = [st2.enter_context(_sbt(nc, 'd_xin%d' % i, [64, T + 3], F32)) for i in range(2)]
            acc = [st2.enter_context(_sbt(nc, 'd_acc%d' % i, [64, T], F32)) for i in range(2)]
            for ch in range(8):
                i = ch % 2
                xi, ac = xin[i], acc[i]
                k.op('pool', lambda e: e.memset(xi[:, 0:3], 0.0), writes=[('d_xin', i)])
                k.dma('sp' if i == 0 else 'act', xi[:, 3:T + 3], d['dxbc'][ch, :, 3:T + 3], writes=[('d_xin', i)])
                k.op('dve', lambda e: e.tensor_scalar(out=ac[:, :], in0=xi[:, 0:T], scalar1=cw[:, ch, 0:1],
                                                      scalar2=cb[:, ch:ch + 1], op0=ALU.mult, op1=ALU.add),
                     reads=[('d_xin', i), ('d_cw', 0), ('d_cb', 0)], writes=[('d_acc', i)])
                for tap in range(1, 4):
                    k.op('dve', lambda e: e.scalar_tensor_tensor(out=ac[:, :], in0=xi[:, tap:T + tap],
                                                                 scalar=cw[:, ch, tap:tap + 1], in1=ac[:, :],
                                                                 op0=ALU.mult, op1=ALU.add),
                         reads=[('d_xin', i), ('d_cw', 0), ('d_acc', i)], writes=[('d_acc', i)])
                if ch < 4:
                    k.op('act', lambda e: e.activation(out=ac[:, :], in_=ac[:, :], func=AF.Silu),
                         reads=[('d_acc', i)], writes=[('d_acc', i)])
                    k.dma('pool', d['dxs'][ch], ac[:, :], reads=[('d_acc', i)])
                    for b in range(NB):
                        bank = (b % 4) + 2
                        psx = c.ps[bank]
                        k.op('pe', lambda e: e.transpose(out=psx[:, 0:64], in_=ac[:, b * 128:(b + 1) * 128],
                                                         identity=c.ident[0:64, 0:64]),
                             reads=[('d_acc', i)], writes=[('ps', bank)])
                        k.op('dve', lambda e: e.tensor_scalar(out=xbar[:, b, ch * 64:(ch + 1) * 64], in0=psx[:, 0:64],
                                                              scalar1=dt_[:, b, ch:ch + 1], scalar2=None, op0=ALU.mult),
                             reads=[('ps', bank), ('d_dt', 0)], writes=[('d_xbar', ch)])
                elif ch < 6:
                    k.op('act', lambda e: e.activation(out=ac[:, :], in_=ac[:, :], func=AF.Silu),
                         reads=[('d_acc', i)], writes=[('d_acc', i)])
                    k.op('pool', lambda e: e.tensor_copy(out=BT[:, ch - 4, :], in_=ac[:, :]),
                         reads=[('d_acc', i)], writes=[('d_BC', ch)])
                    for b in range(NB):
                        bank = (b % 4) + 2
                        psx = c.ps[bank]
                        k.op('pe', lambda e: e.transpose(out=psx[:, 0:64], in_=ac[:, b * 128:(b + 1) * 128],
                                                         identity=c.ident[0:64, 0:64]),
                             reads=[('d_acc', i)], writes=[('ps', bank)])
                        k.op('act', lambda e: e.copy(out=Btok[:, b, ch - 4, :], in_=psx[:, 0:64]),
                             reads=[('ps', bank)], writes=[('d_Btok', ch)])
                else:
                    k.op('act', lambda e: e.activation(out=CT[:, ch - 6, :], in_=ac[:, :], func=AF.Silu),
                         reads=[('d_acc', i)], writes=[('d_BC', ch)])
            k.barrier()
        with ExitStack() as st3:
            def sb3(name, shape, dt2):
                return st3.enter_context(_sbt(nc, 'sb_' + name, shape, dt2))
            arg = [sb3('d_arg%d' % i, [128, 4, 128], F32) for i in range(2)]
            Dm = [sb3('d_Dm%d' % i, [128, 4, 128], F32) for i in range(2)]
            MT = [sb3('d_MT%d' % i, [128, 4, 128], BF16) for i in range(2)]
            ecs = [sb3('d_ecs%d' % i, [64, 4, 128], F32) for i in range(2)]
            Cd = [sb3('d_Cd%d' % i, [64, 4, 128], F32) for i in range(2)]
            Bd = [sb3('d_Bd%d' % i, [128, 4, 64], BF16) for i in range(2)]
            zts = [sb3('d_zt%d' % i, [64, 4, 128], F32) for i in range(2)]
            xsts = [sb3('d_xst%d' % i, [64, 4, 128], F32) for i in range(2)]
            state = sb3('d_state', [64, 4, 64], F32)
            stmp = sb3('d_stmp', [64, 4, 64], F32)
            CTf = sb3('d_CTf', [64, 2, 128], F32)
            y = sb3('d_y', [64, 4, 128], F32)
            ysq = sb3('d_ysq', [64, 4, 128], F32)
            ss = sb3('d_ss', [64, 2, 128], F32)
            obs = [sb3('d_ob%d' % i, [64, 4, 128], BF16) for i in range(2)]
            k.op('dve', lambda e: e.memset(state[:, :, :], 0.0), writes=[('d_state', 0)])
            bcn = [0]

            def nbank():
                b = bcn[0] % 8
                bcn[0] += 1
                return b
            for lb in range(NB):
                ls = slice(lb * 128, (lb + 1) * 128)
                i2 = lb % 2
                zt, xst, ob = zts[i2], xsts[i2], obs[i2]
                k.dma('sp', zt[:, :, :], d['dz'][:, :, ls].rearrange('h p t -> p h t'), writes=[('d_zt', i2)])
                k.dma('act', xst[:, :, :], d['dxs'][:, :, ls].rearrange('h p t -> p h t'), writes=[('d_xst', i2)])
                bank = nbank()
                psb = c.ps[bank]
                for h in range(4):
                    k.op('pe', lambda e: e.matmul(psb[:, h * 128:(h + 1) * 128],
                                                  lhsT=a_tok[:, lb, h:h + 1].to_broadcast([128, 128]),
                                                  rhs=c.triu[:, :], start=True, stop=True),
                         reads=[('d_atok', 0), ('triu', 0)], writes=[('ps', bank)])
                a_, D_, M_, ec_, Cd_, Bd_ = arg[i2], Dm[i2], MT[i2], ecs[i2], Cd[i2], Bd[i2]
                k.op('dve', lambda e: e.tensor_tensor(
                    out=a_[:, :, :], in0=psb[:, :].rearrange('p (h l) -> p h l', h=4),
                    in1=acs[:, lb, :].unsqueeze(2).to_broadcast([128, 4, 128]), op=ALU.subtract),
                    reads=[('ps', bank), ('d_acs', 0)], writes=[('d_arg', i2)])
                k.op('act', lambda e: e.activation(out=ec_[:, :, :],
                                                   in_=psb[0:64, :].rearrange('p (h l) -> p h l', h=4), func=AF.Exp),
                     reads=[('ps', bank)], writes=[('d_ecs', i2)])
                k.op('pool', lambda e: e.tensor_scalar_min(out=a_[:, :, :], in0=a_[:, :, :], scalar1=0.0),
                     reads=[('d_arg', i2)], writes=[('d_arg', i2)])
                k.op('act', lambda e: e.activation(out=D_[:, :, :], in_=a_[:, :, :], func=AF.Exp),
                     reads=[('d_arg', i2)], writes=[('d_Dm', i2)])
                k.op('pool', lambda e: e.tensor_tensor(
                    out=D_[:, :, :], in0=D_[:, :, :],
                    in1=c.triu[:, :].unsqueeze(1).to_broadcast([128, 4, 128]), op=ALU.mult),
                    reads=[('d_Dm', i2), ('triu', 0)], writes=[('d_Dm', i2)])
                bg = nbank()
                ps_g = c.ps[bg]
                for g in range(2):
                    k.op('pe', lambda e: e.matmul(ps_g[:, g * 128:(g + 1) * 128], lhsT=BT[:, g, ls],
                                                  rhs=CT[:, g, ls], start=True, stop=True),
                         reads=[('d_BC', 0)], writes=[('ps', bg)])
                for g in range(2):
                    k.op('dve', lambda e: e.tensor_tensor(
                        out=M_[:, 2 * g:2 * g + 2, :], in0=D_[:, 2 * g:2 * g + 2, :],
                        in1=ps_g[:, g * 128:(g + 1) * 128].unsqueeze(1).to_broadcast([128, 2, 128]),
                        op=ALU.mult),
                        reads=[('d_Dm', i2), ('ps', bg)], writes=[('d_MT', i2)])
                k.op('pool', lambda e: e.tensor_copy(out=CTf[:, :, :], in_=CT[:, :, ls]),
                     reads=[('d_BC', 0)], writes=[('d_CTf', 0)])
                for g in range(2):
                    k.op('pool', lambda e: e.tensor_tensor(
                        out=Cd_[:, 2 * g:2 * g + 2, :], in0=ec_[:, 2 * g:2 * g + 2, :],
                        in1=CTf[:, g, :].unsqueeze(1).to_broadcast([64, 2, 128]), op=ALU.mult),
                        reads=[('d_ecs', i2), ('d_CTf', 0)], writes=[('d_Cd', i2)])
                for g in range(2):
                    k.op('dve', lambda e: e.tensor_tensor(
                        out=Bd_[:, 2 * g:2 * g + 2, :],
                        in0=Btok[:, lb, g, :].unsqueeze(1).to_broadcast([128, 2, 64]),
                        in1=dte[:, lb, 2 * g:2 * g + 2].unsqueeze(2).to_broadcast([128, 2, 64]), op=ALU.mult),
                        reads=[('d_Btok', 0), ('d_dte', 0)], writes=[('d_Bd', i2)])
                bu = nbank()
                ps_y = c.ps[bu]
                for h in range(4):
                    k.op('pe', lambda e: e.matmul(ps_y[0:64, h * 128:(h + 1) * 128],
                                                  lhsT=xbar[:, lb, h * 64:(h + 1) * 64], rhs=M_[:, h, :],
                                                  start=(h == 0), stop=(lb == 0), skip_group_check=True),
                         reads=[('d_MT', i2), ('d_xbar', 0)], writes=[('ps', bu)])
                    if lb > 0:
                        k.op('pe', lambda e: e.matmul(ps_y[0:64, h * 128:(h + 1) * 128],
                                                      lhsT=state[:, h, :], rhs=Cd_[:, h, :],
                                                      start=False, stop=True, skip_group_check=True),
                             reads=[('d_Cd', i2), ('d_state', 0)], writes=[('ps', bu)])
                if lb < NB - 1:
                    bs_ = nbank()
                    ps_s = c.ps[bs_]
                    for h in range(4):
                        k.op('pe', lambda e: e.matmul(ps_s[0:64, h * 64:(h + 1) * 64], lhsT=Bd_[:, h, :],
                                                      rhs=xbar[:, lb, h * 64:(h + 1) * 64], start=True, stop=True),
                             reads=[('d_Bd', i2), ('d_xbar', 0)], writes=[('ps', bs_)])
                    k.op('dve', lambda e: e.tensor_tensor(
                        out=stmp[:, :, :], in0=state[:, :, :],
                        in1=etot[0:64, lb, :].unsqueeze(2).to_broadcast([64, 4, 64]), op=ALU.mult),
                        reads=[('d_state', 0), ('d_etot', 0)], writes=[('d_stmp', 0)])
                    k.op('dve', lambda e: e.tensor_tensor(
                        out=state[:, :, :], in0=stmp[:, :, :],
                        in1=ps_s[0:64, 0:256].rearrange('p (h q) -> p h q', h=4), op=ALU.add),
                        reads=[('d_stmp', 0), ('ps', bs_)], writes=[('d_state', 0)])
                for h in range(4):
                    k.op('dve', lambda e: e.scalar_tensor_tensor(out=y[:, h, :], in0=xst[:, h, :],
                                                                 scalar=dvec[0:64, 8 + h:9 + h],
                                                                 in1=ps_y[0:64, h * 128:(h + 1) * 128],
                                                                 op0=ALU.mult, op1=ALU.add),
                         reads=[('d_xst', i2), ('d_vec', 0), ('ps', bu)], writes=[('d_y', 0)])
                k.op('act', lambda e: e.activation(out=zt[:, :, :], in_=zt[:, :, :], func=AF.Silu),
                     reads=[('d_zt', i2)], writes=[('d_zt', i2)])
                k.op('dve', lambda e: e.tensor_tensor(out=y[:, :, :], in0=y[:, :, :], in1=zt[:, :, :], op=ALU.mult),
                     reads=[('d_y', 0), ('d_zt', i2)], writes=[('d_y', 0)])
                k.op('act', lambda e: e.activation(out=ysq[:, :, :], in_=y[:, :, :], func=AF.Square),
                     reads=[('d_y', 0)], writes=[('d_ysq', 0)])
                bank = nbank()
                psq = c.ps[bank]
                k.op('pe', lambda e: e.matmul(psq[0:64, :], lhsT=c.ones_f[0:64, 0:64],
                                              rhs=ysq[:, :, :].rearrange('p h l -> p (h l)'), start=True, stop=True),
                     reads=[('d_ysq', 0), ('ones_f', 0)], writes=[('ps', bank)])
                k.op('act', lambda e: e.copy(out=ysq[:, :, :].rearrange('p h l -> p (h l)'), in_=psq[0:64, :]),
                     reads=[('ps', bank)], writes=[('d_ysq', 0)])
                psv = ysq[:, :, :].rearrange('p (g a) l -> p g a l', g=2, a=2)
                k.op('dve', lambda e: e.tensor_tensor(out=ss[:, :, :], in0=psv[:, :, 0, :], in1=psv[:, :, 1, :],
                                                      op=ALU.add),
                     reads=[('d_ysq', 0)], writes=[('d_ss', 0)])
                k.op('act', lambda e: e.activation(out=ss[:, :, :], in_=ss[:, :, :], func=AF.Ln, scale=1.0 / 128,
                                                   bias=c.eps5[0:64, 0:1]),
                     reads=[('d_ss', 0), ('eps', 0)], writes=[('d_ss', 0)])
                k.op('act', lambda e: e.activation(out=ss[:, :, :], in_=ss[:, :, :], func=AF.Exp, scale=-0.5),
                     reads=[('d_ss', 0)], writes=[('d_ss', 0)])
                for g in range(2):
                    k.op('dve', lambda e: e.tensor_tensor(
                        out=y[:, 2 * g:2 * g + 2, :], in0=y[:, 2 * g:2 * g + 2, :],
                        in1=ss[:, g, :].unsqueeze(1).to_broadcast([64, 2, 128]), op=ALU.mult),
                        reads=[('d_y', 0), ('d_ss', 0)], writes=[('d_y', 0)])
                k.op('dve', lambda e: e.tensor_tensor(out=ob[:, :, :], in0=y[:, :, :],
                                                      in1=nw[:, :].unsqueeze(2).to_broadcast([64, 4, 128]), op=ALU.mult),
                     reads=[('d_y', 0), ('d_nw', 0)], writes=[('d_ob', i2)])
                k.dma('pool', d['obr'][3, :, :, ls].rearrange('h p t -> p h t'), ob[:, :, :], reads=[('d_ob', i2)])
            k.barrier()


def mixer_c(k, c, T, lp, layer):
    nc = k.nc
    d = c.d
    N = 256
    NCH = N // 64
    NG = T // N
    with ExitStack() as st:
        def sb(name, shape, dt_=F32):
            return st.enter_context(_sbt(nc, 'c_' + name, shape, dt_))
        p64 = sb('p64', [64, 46])
        wa2 = sb('wa2', [64, 256])
        g2 = sb('g2', [64, 256])
        k.dma('sp', p64[:, :], lp['c_p64'], writes=[('c_par', 0)])
        k.dma('act', wa2[:, :], lp['c_wa2'], writes=[('c_par', 1)])
        k.dma('pool', g2[:, :], lp['c_g2'], writes=[('c_par', 2)])
        if layer > 0:
            v1t = sb('v1t', [64, 4, 16])
            v2 = sb('v2', [16, 256])
            k.dma('sp', v1t[:, :, :], lp['c_v1'], writes=[('c_par', 3)])
            k.dma('act', v2[:, :], lp['c_v2'], writes=[('c_par', 4)])
        mu, w0, a0, kkp, kap, rkp, gnw, gnb, v0 = (p64[:, 0:14], p64[:, 14:18], p64[:, 18:22], p64[:, 22:26],
                                                   p64[:, 26:30], p64[:, 30:34], p64[:, 34:38], p64[:, 38:42],
                                                   p64[:, 42:46])
        prm = sb('prm', [64, 8])
        k.barrier()
        k.op('dve', lambda e: e.tensor_scalar(out=prm[:, 0:4], in0=w0, scalar1=-1.0, scalar2=None, op0=ALU.mult),
             writes=[('c_prm', 0)])
        k.op('dve', lambda e: e.tensor_scalar(out=prm[:, 4:8], in0=kap, scalar1=-1.0, scalar2=1.0, op0=ALU.mult,
                                              op1=ALU.add), writes=[('c_prm', 0)])
        k.barrier()
        negw0, omka = prm[:, 0:4], prm[:, 4:8]
        msk = sb('msk', [64, 3, 64])
        k.dma('sp', msk[:, :, :], c.cin['cmask'], writes=[('c_msk', 0)])
        H = sb('H', [64, 4, 64])
        k.op('dve', lambda e: e.memset(H[:, :, :], 0.0), writes=[('c_H', 0)])
        pc = sb('pc', [64, 14, N + 1])
        pcs = sb('pcs', [64, 14, N])
        tmp = sb('tmp', [64, 14, N])
        th = sb('th', [64, N])
        sg = sb('sg', [64, N])
        e2 = sb('e2', [64, 4, N])
        lw = sb('lw', [64, 4, N])
        base = sb('base', [64, 4, NCH])
        P_ = sb('P', [64, 4, N])
        Pm1 = sb('Pm1', [64, 4, N])
        iP = sb('iP', [64, 4, N])
        a_s = sb('a_s', [64, 4, N])
        g_s = sb('g_s', [64, 4, N])
        kk = sb('kk', [64, 4, N])
        t1 = sb('t1', [64, 4, N])
        kmod = sb('kmod', [64, 4, N])
        bon = sb('bon', [64, 4, N])
        ar = sb('ar', [64, 4, NCH, 2, 64])
        bT = sb('bT', [64, 4, N])
        kT = sb('kT', [64, 4, N])
        vT = sb('vT', [64, 4, N])
        vf = sb('vf', [64, 4, N])
        vl = sb('vl', [16, N])
        tok = sb('tok', [64, NCH, 3, 4, 64])
        MAs = [sb('MA%d' % i, [64, 4, 2, 64]) for i in range(NCH)]
        MBs = [sb('MB%d' % i, [64, 4, 2, 64]) for i in range(NCH)]
        XXs = [[sb('XX%d_%d' % (j, i), [64, 4, 2, 64]) for i in range(2)] for j in range(NCH)]
        TTs = [sb('TT%d' % i, [64, 4, 64]) for i in range(NCH)]
        Xs = sb('Xs', [64, 4, 64])
        Us = sb('Us', [64, 4, 64])
        yT = sb('yT', [64, 4, N])
        dd = sb('dd', [64, 4, N])
        ob = sb('ob', [64, 4, N], BF16)
        identb = c.ident[0:64, 0:64].unsqueeze(1).to_broadcast([64, 4, 64])
        ones64 = c.ones_f[0:64, 0:64]
        bc = [0]

        def nbank():
            b = bc[0] % 8
            bc[0] += 1
            return b

        def ph(t_):
            return t_[:, :, :].rearrange('p h n -> p (h n)')

        def bcast4(col):
            return col.unsqueeze(2).to_broadcast([64, 4, N])

        def headsum(src, dst_fn):
            for half in range(2):
                bank = nbank()
                ps = c.ps[bank]
                k.op('pe', lambda e: e.matmul(ps[0:64, :], lhsT=ones64,
                                              rhs=src[:, 2 * half:2 * half + 2, :].rearrange('p h n -> p (h n)'),
                                              start=True, stop=True),
                     reads=[('c_w', id(src))], writes=[('ps', bank)])
                dst_fn(half, ps[0:64, :].rearrange('p (h n) -> p h n', h=2), bank)

        def W(t_):
            return [('c_w', id(t_))]

        for gi in range(NG):
            t0 = gi * N
            ts = slice(t0, t0 + N)
            if gi == 0:
                k.dma('sp', pc[:, :, 1:N + 1], d['cpc'][:, :, 1:N + 1].rearrange('g p t -> p g t'), writes=W(pc))
                k.op('dve', lambda e: e.memset(pc[:, :, 0:1], 0.0), writes=W(pc))
            else:
                k.dma('sp', pc[:, :, :], d['cpc'][:, :, t0:t0 + N + 1].rearrange('g p t -> p g t'), writes=W(pc))
            k.op('dve', lambda e: e.tensor_tensor(out=tmp[:, :, :], in0=pc[:, :, 0:N], in1=pc[:, :, 1:N + 1],
                                                  op=ALU.subtract), reads=W(pc), writes=W(tmp))
            k.op('pool', lambda e: e.tensor_tensor(out=tmp[:, :, :], in0=tmp[:, :, :],
                                                   in1=mu.unsqueeze(2).to_broadcast([64, 14, N]), op=ALU.mult),
                 reads=W(tmp), writes=W(tmp))
            k.op('dve', lambda e: e.tensor_tensor(out=pcs[:, :, :], in0=tmp[:, :, :], in1=pc[:, :, 1:N + 1],
                                                  op=ALU.add), reads=W(tmp) + W(pc), writes=W(pcs))
            r_, k_, v_ = pcs[:, 0:4, :], pcs[:, 4:8, :], pcs[:, 8:12, :]
            k.op('act', lambda e: e.activation(out=th[0:32, :], in_=pcs[0:32, 12, :], func=AF.Tanh),
                 reads=W(pcs), writes=W(th))
            k.op('act', lambda e: e.activation(out=sg[:, :], in_=pcs[:, 13, :], func=AF.Sigmoid),
                 reads=W(pcs), writes=W(sg))
            for half in range(2):
                bank = nbank()
                ps = c.ps[bank]
                for hh in range(2):
                    h = 2 * half + hh
                    k.op('pe', lambda e: e.matmul(ps[0:64, hh * N:(hh + 1) * N], lhsT=wa2[0:32, h * 64:(h + 1) * 64],
                                                  rhs=th[0:32, :], start=True, stop=True),
                         reads=W(th), writes=[('ps', bank)])
                    k.op('act', lambda e: e.activation(out=e2[:, h, :], in_=ps[0:64, hh * N:(hh + 1) * N],
                                                       func=AF.Exp, scale=-1.0, bias=negw0[:, h:h + 1]),
                         reads=[('ps', bank)], writes=W(e2))
            k.op('act', lambda e: e.activation(out=e2[:, :, :], in_=e2[:, :, :], func=AF.Ln, bias=1.0),
                 reads=W(e2), writes=W(e2))
            k.op('act', lambda e: e.activation(out=e2[:, :, :], in_=e2[:, :, :], func=AF.Exp, scale=-1.0,
                                               bias=c.mhalf[0:64, 0:1]),
                 reads=W(e2), writes=W(e2))
            for half in range(2):
                bank = nbank()
                ps = c.ps[bank]
                for hh in range(2):
                    h = 2 * half + hh
                    k.op('pe', lambda e: e.matmul(ps[0:64, hh * N:(hh + 1) * N], lhsT=wa2[32:64, h * 64:(h + 1) * 64],
                                                  rhs=pcs[32:64, 12, :], start=True, stop=True),
                         reads=W(pcs), writes=[('ps', bank)])
                    k.op('act', lambda e: e.activation(out=a_s[:, h, :], in_=ps[0:64, hh * N:(hh + 1) * N],
                                                       func=AF.Sigmoid, bias=a0[:, h:h + 1]),
                         reads=[('ps', bank)], writes=W(a_s))
            for half in range(2):
                bank = nbank()
                ps = c.ps[bank]
                for hh in range(2):
                    h = 2 * half + hh
                    k.op('pe', lambda e: e.matmul(ps[0:64, hh * N:(hh + 1) * N], lhsT=g2[:, h * 64:(h + 1) * 64],
                                                  rhs=sg[:, :], start=True, stop=True),
                         reads=W(sg), writes=[('ps', bank)])
                k.op('act', lambda e: e.copy(out=g_s[:, 2 * half:2 * half + 2, :],
                                             in_=ps[0:64, :].rearrange('p (h n) -> p h n', h=2)),
                     reads=[('ps', bank)], writes=W(g_s))
            if layer == 0:
                k.op('pool', lambda e: e.tensor_copy(out=vT[:, :, :], in_=v_), reads=W(pcs), writes=W(vT))
                k.dma('act', d['vfirst'][:, :, ts].rearrange('h p t -> p h t'), vT[:, :, :], reads=W(vT))
            else:
                k.dma('act', vf[:, :, :], d['vfirst'][:, :, ts].rearrange('h p t -> p h t'), writes=W(vf))
                bank = nbank()
                ps = c.ps[bank]
                for h in range(4):
                    k.op('pe', lambda e: e.matmul(ps[0:16, 0:N], lhsT=v1t[:, h, :], rhs=pcs[:, 8 + h, :],
                                                  start=(h == 0), stop=(h == 3)),
                         reads=W(pcs), writes=[('ps', bank)])
                k.op('act', lambda e: e.copy(out=vl[:, :], in_=ps[0:16, 0:N]), reads=[('ps', bank)], writes=W(vl))
                for half in range(2):
                    bank = nbank()
                    ps = c.ps[bank]
                    for hh in range(2):
                        h = 2 * half + hh
                        k.op('pe', lambda e: e.matmul(ps[0:64, hh * N:(hh + 1) * N], lhsT=v2[0:16, h * 64:(h + 1) * 64],
                                                      rhs=vl[:, :], start=True, stop=True),
                             reads=W(vl), writes=[('ps', bank)])
                        k.op('act', lambda e: e.activation(out=t1[:, h, :], in_=ps[0:64, hh * N:(hh + 1) * N],
                                                           func=AF.Sigmoid, bias=v0[:, h:h + 1]),
                             reads=[('ps', bank)], writes=W(t1))
                k.op('dve', lambda e: e.tensor_tensor(out=vf[:, :, :], in0=vf[:, :, :], in1=v_, op=ALU.subtract),
                     reads=W(vf) + W(pcs), writes=W(vf))
                k.op('dve', lambda e: e.tensor_tensor(out=vf[:, :, :], in0=vf[:, :, :], in1=t1[:, :, :], op=ALU.mult),
                     reads=W(vf) + W(t1), writes=W(vf))
                k.op('dve', lambda e: e.tensor_tensor(out=vT[:, :, :], in0=vf[:, :, :], in1=v_, op=ALU.add),
                     reads=W(vf) + W(pcs), writes=W(vT))
            k.op('dve', lambda e: e.tensor_tensor(out=kk[:, :, :], in0=k_, in1=bcast4(kkp), op=ALU.mult),
                 reads=W(pcs), writes=W(kk))
            k.op('act', lambda e: e.activation(out=t1[:, :, :], in_=kk[:, :, :], func=AF.Square),
                 reads=W(kk), writes=W(t1))

            def kk_norm(half, psv, bank):
                k.op('act', lambda e: e.activation(out=dd[:, 2 * half:2 * half + 2, :], in_=psv, func=AF.Sqrt),
                     reads=[('ps', bank)], writes=W(dd))
            headsum(t1, kk_norm)
            k.op('dve', lambda e: e.tensor_scalar_max(out=dd[:, :, :], in0=dd[:, :, :], scalar1=1e-12),
                 reads=W(dd), writes=W(dd))
            k.op('dve', lambda e: e.reciprocal(out=dd[:, :, :], in_=dd[:, :, :]), reads=W(dd), writes=W(dd))
            k.op('dve', lambda e: e.tensor_tensor(out=kk[:, :, :], in0=kk[:, :, :], in1=dd[:, :, :], op=ALU.mult),
                 reads=W(kk) + W(dd), writes=W(kk))
            k.op('pool', lambda e: e.tensor_tensor(out=t1[:, :, :], in0=a_s[:, :, :], in1=bcast4(kap), op=ALU.mult),
                 reads=W(a_s), writes=W(t1))
            k.op('pool', lambda e: e.tensor_tensor(out=t1[:, :, :], in0=t1[:, :, :], in1=bcast4(omka), op=ALU.add),
                 reads=W(t1), writes=W(t1))
            k.op('dve', lambda e: e.tensor_tensor(out=kmod[:, :, :], in0=k_, in1=t1[:, :, :], op=ALU.mult),
                 reads=W(pcs) + W(t1), writes=W(kmod))
            k.op('dve', lambda e: e.tensor_tensor(out=t1[:, :, :], in0=r_, in1=kmod[:, :, :], op=ALU.mult),
                 reads=W(pcs) + W(kmod), writes=W(t1))
            k.op('pool', lambda e: e.tensor_tensor(out=t1[:, :, :], in0=t1[:, :, :], in1=bcast4(rkp), op=ALU.mult),
                 reads=W(t1), writes=W(t1))

            def bon_fn(half, psv, bank):
                k.op('dve', lambda e: e.tensor_tensor(out=bon[:, 2 * half:2 * half + 2, :], in0=psv,
                                                      in1=vT[:, 2 * half:2 * half + 2, :], op=ALU.mult),
                     reads=[('ps', bank)] + W(vT), writes=W(bon))
            headsum(t1, bon_fn)
            for h in range(4):
                k.op('dve', lambda e: e.tensor_tensor_scan(out=lw[:, h, :], data0=e2[:, h, :], data1=e2[:, h, :],
                                                           initial=0.0, op0=ALU.add, op1=ALU.max),
                     reads=W(e2), writes=W(lw))
            k.op('dve', lambda e: e.memset(base[:, :, 0:1], 0.0), writes=W(base))
            k.op('dve', lambda e: e.tensor_copy(out=base[:, :, 1:NCH], in_=lw[:, :, 63:N - 1:64]),
                 reads=W(lw), writes=W(base))
            lw4 = lw[:, :, :].rearrange('p h (c s) -> p h c s', s=64)
            k.op('dve', lambda e: e.tensor_tensor(out=lw4, in0=lw4,
                                                  in1=base[:, :, :].unsqueeze(3).to_broadcast([64, 4, NCH, 64]),
                                                  op=ALU.subtract),
                 reads=W(lw) + W(base), writes=W(lw))
            k.op('act', lambda e: e.activation(out=P_[:, :, :], in_=lw[:, :, :], func=AF.Exp, scale=-1.0),
                 reads=W(lw), writes=W(P_))
            k.op('act', lambda e: e.activation(out=iP[:, :, :], in_=lw[:, :, :], func=AF.Exp),
                 reads=W(lw), writes=W(iP))
            k.op('dve', lambda e: e.tensor_tensor(out=t1[:, :, :], in0=lw[:, :, :], in1=e2[:, :, :], op=ALU.subtract),
                 reads=W(lw) + W(e2), writes=W(t1))
            k.op('act', lambda e: e.activation(out=Pm1[:, :, :], in_=t1[:, :, :], func=AF.Exp, scale=-1.0),
                 reads=W(t1), writes=W(Pm1))
            ar5 = ar[:, :, :, :, :]
            k.op('dve', lambda e: e.scalar_tensor_tensor(
                out=ar5[:, :, :, 0, :], in0=kk[:, :, :].rearrange('p h (c s) -> p h c s', s=64), scalar=-1.0,
                in1=Pm1[:, :, :].rearrange('p h (c s) -> p h c s', s=64), op0=ALU.mult, op1=ALU.mult),
                reads=W(kk) + W(Pm1), writes=W(ar))
            k.op('dve', lambda e: e.tensor_tensor(
                out=ar5[:, :, :, 1, :], in0=r_.rearrange('p h (c s) -> p h c s', s=64),
                in1=P_[:, :, :].rearrange('p h (c s) -> p h c s', s=64), op=ALU.mult),
                reads=W(pcs) + W(P_), writes=W(ar))
            k.op('dve', lambda e: e.tensor_tensor(out=bT[:, :, :], in0=kk[:, :, :], in1=a_s[:, :, :], op=ALU.mult),
                 reads=W(kk) + W(a_s), writes=W(bT))
            k.op('dve', lambda e: e.tensor_tensor(out=bT[:, :, :], in0=bT[:, :, :], in1=iP[:, :, :], op=ALU.mult),
                 reads=W(bT) + W(iP), writes=W(bT))
            k.op('dve', lambda e: e.tensor_tensor(out=kT[:, :, :], in0=kmod[:, :, :], in1=iP[:, :, :], op=ALU.mult),
                 reads=W(kmod) + W(iP), writes=W(kT))
            for ci in range(NCH):
                cs = slice(ci * 64, (ci + 1) * 64)
                for qi, src in enumerate((bT, kT, vT)):
                    bank = nbank()
                    ps = c.ps[bank]
                    for h in range(4):
                        k.op('pe', lambda e: e.transpose(out=ps[0:64, h * 64:(h + 1) * 64], in_=src[:, h, cs],
                                                         identity=c.ident[0:64, 0:64]),
                             reads=W(src), writes=[('ps', bank)])
                    eng = 'act' if qi % 2 == 0 else 'dve'
                    dst = tok[:, ci, qi, :, :]
                    srcp = ps[0:64, 0:256].rearrange('p (h n) -> p h n', h=4)
                    if eng == 'act':
                        k.op('act', lambda e: e.copy(out=dst, in_=srcp), reads=[('ps', bank)], writes=W(tok))
                    else:
                        k.op('dve', lambda e: e.tensor_copy(out=dst, in_=srcp), reads=[('ps', bank)], writes=W(tok))
            for ci in range(NCH):
                cs = slice(ci * 64, (ci + 1) * 64)
                MA, MB, XX, TT = MAs[ci], MBs[ci], XXs[ci], TTs[ci]
                bA, bB, bC = nbank(), nbank(), nbank()
                psA, psB, psC = c.ps[bA], c.ps[bB], c.ps[bC]
                for h in range(4):
                    arh = ar[:, h, ci, :, :].rearrange('p a s -> p (a s)')
                    k.op('pe', lambda e: e.matmul(psA[0:64, h * 128:(h + 1) * 128], lhsT=bT[:, h, cs], rhs=arh,
                                                  start=True, stop=True),
                         reads=W(bT) + W(ar), writes=[('ps', bA)])
                    k.op('pe', lambda e: e.matmul(psB[0:64, h * 128:(h + 1) * 128], lhsT=kT[:, h, cs], rhs=arh,
                                                  start=True, stop=True),
                         reads=W(kT) + W(ar), writes=[('ps', bB)])
                    k.op('pe', lambda e: e.matmul(psC[0:64, h * 64:(h + 1) * 64], lhsT=ar[:, h, ci, 0, :],
                                                  rhs=bT[:, h, cs], start=True, stop=True),
                         reads=W(bT) + W(ar), writes=[('ps', bC)])
                mUU = msk[:, 0:2, :].unsqueeze(1).to_broadcast([64, 4, 2, 64])
                k.op('dve', lambda e: e.tensor_tensor(out=MA[:, :, :, :],
                                                      in0=psA[0:64, :].rearrange('p (h a s) -> p h a s', h=4, a=2),
                                                      in1=mUU, op=ALU.mult),
                     reads=[('ps', bA), ('c_msk', 0)], writes=W(MA))
                k.op('dve', lambda e: e.tensor_tensor(out=MB[:, :, :, :],
                                                      in0=psB[0:64, :].rearrange('p (h a s) -> p h a s', h=4, a=2),
                                                      in1=mUU, op=ALU.mult),
                     reads=[('ps', bB), ('c_msk', 0)], writes=W(MB))
                X = XX[0]
                k.op('pool', lambda e: e.tensor_copy(out=X[:, :, 0, :], in_=MA[:, :, 0, :]), reads=W(MA), writes=W(X))
                k.op('dve', lambda e: e.tensor_tensor(out=X[:, :, 1, :],
                                                      in0=psC[0:64, 0:256].rearrange('p (h s) -> p h s', h=4),
                                                      in1=msk[:, 2, :].unsqueeze(1).to_broadcast([64, 4, 64]),
                                                      op=ALU.mult),
                     reads=[('ps', bC), ('c_msk', 0)], writes=W(X))
                k.op('pool', lambda e: e.tensor_tensor(out=TT[:, :, :], in0=MA[:, :, 0, :], in1=identb, op=ALU.add),
                     reads=W(MA), writes=W(TT))
            for it_ in range(5):
                for ci in range(NCH):
                    MA, MB, XX, TT = MAs[ci], MBs[ci], XXs[ci], TTs[ci]
                    Xo, Xn = XX[it_ % 2], XX[(it_ + 1) % 2]
                    bank = nbank()
                    ps = c.ps[bank]
                    for h in range(4):
                        k.op('pe', lambda e: e.matmul(ps[0:64, h * 128:h * 128 + 64], lhsT=Xo[:, h, 1, :],
                                                      rhs=Xo[:, h, 0, :], start=True, stop=True),
                             reads=W(Xo), writes=[('ps', bank)])
                        k.op('pe', lambda e: e.matmul(ps[0:64, h * 128 + 64:(h + 1) * 128], lhsT=Xo[:, h, 0, :],
                                                      rhs=Xo[:, h, 1, :], start=True, stop=True),
                             reads=W(Xo), writes=[('ps', bank)])
                    k.op('act', lambda e: e.copy(out=Xn[:, :, :, :],
                                                 in_=ps[0:64, :].rearrange('p (h a s) -> p h a s', h=4, a=2)),
                         reads=[('ps', bank)], writes=W(Xn))
                for ci in range(NCH):
                    MA, MB, XX, TT = MAs[ci], MBs[ci], XXs[ci], TTs[ci]
                    Xn = XX[(it_ + 1) % 2]
                    bank2 = nbank()
                    ps2 = c.ps[bank2]
                    for h in range(4):
                        k.op('pe', lambda e: e.matmul(ps2[0:64, h * 64:(h + 1) * 64], lhsT=Xn[:, h, 1, :],
                                                      rhs=TT[:, h, :], start=True, stop=True),
                             reads=W(Xn) + W(TT), writes=[('ps', bank2)])
                    k.op('dve', lambda e: e.tensor_tensor(out=TT[:, :, :], in0=TT[:, :, :],
                                                          in1=ps2[0:64, 0:256].rearrange('p (h s) -> p h s', h=4),
                                                          op=ALU.add),
                         reads=[('ps', bank2)] + W(TT), writes=W(TT))
            for ci in range(NCH):
                cs = slice(ci * 64, (ci + 1) * 64)
                MA, MB, XX, TT = MAs[ci], MBs[ci], XXs[ci], TTs[ci]
                bank = nbank()
                ps = c.ps[bank]
                for h in range(4):
                    k.op('pe', lambda e: e.matmul(ps[0:64, h * 64:(h + 1) * 64], lhsT=ar[:, h, ci, 0, :], rhs=H[:, h, :],
                                                  start=(h == 0), stop=False, skip_group_check=True),
                         reads=W(ar) + [('c_H', 0)], writes=[('ps', bank)])
                    k.op('pe', lambda e: e.matmul(ps[0:64, h * 64:(h + 1) * 64], lhsT=MB[:, h, 0, :],
                                                  rhs=tok[:, ci, 2, h, :], start=False, stop=True,
                                                  skip_group_check=True),
                         reads=W(MB) + W(tok), writes=[('ps', bank)])
                k.op('act', lambda e: e.copy(out=Xs[:, :, :], in_=ps[0:64, 0:256].rearrange('p (h s) -> p h s', h=4)),
                     reads=[('ps', bank)], writes=W(Xs))
                bank = nbank()
                ps = c.ps[bank]
                for h in range(4):
                    k.op('pe', lambda e: e.matmul(ps[0:64, h * 64:(h + 1) * 64], lhsT=TT[:, h, :], rhs=Xs[:, h, :],
                                                  start=True, stop=True),
                         reads=W(TT) + W(Xs), writes=[('ps', bank)])
                k.op('dve', lambda e: e.tensor_copy(out=Us[:, :, :],
                                                    in_=ps[0:64, 0:256].rearrange('p (h s) -> p h s', h=4)),
                     reads=[('ps', bank)], writes=W(Us))
                bank = nbank()
                ps = c.ps[bank]
                for h in range(4):
                    o_ = ps[0:64, h * 64:(h + 1) * 64]
                    k.op('pe', lambda e: e.matmul(o_, lhsT=H[:, h, :], rhs=ar[:, h, ci, 1, :], start=(h == 0),
                                                  stop=False, skip_group_check=True),
                         reads=W(ar) + [('c_H', 0)], writes=[('ps', bank)])
                    k.op('pe', lambda e: e.matmul(o_, lhsT=Us[:, h, :], rhs=MA[:, h, 1, :], start=False, stop=False,
                                                  skip_group_check=True),
                         reads=W(MA) + W(Us), writes=[('ps', bank)])
                    k.op('pe', lambda e: e.matmul(o_, lhsT=tok[:, ci, 2, h, :], rhs=MB[:, h, 1, :], start=False,
                                                  stop=True, skip_group_check=True),
                         reads=W(MB) + W(tok), writes=[('ps', bank)])
                k.op('act', lambda e: e.copy(out=yT[:, :, cs], in_=ps[0:64, 0:256].rearrange('p (h s) -> p h s', h=4)),
                     reads=[('ps', bank)], writes=W(yT))
                bank = nbank()
                ps = c.ps[bank]
                for h in range(4):
                    o_ = ps[0:64, h * 64:(h + 1) * 64]
                    k.op('pe', lambda e: e.matmul(o_, lhsT=tok[:, ci, 0, h, :], rhs=Us[:, h, :], start=(h == 0),
                                                  stop=False, skip_group_check=True),
                         reads=W(tok) + W(Us), writes=[('ps', bank)])
                    k.op('pe', lambda e: e.matmul(o_, lhsT=tok[:, ci, 1, h, :], rhs=tok[:, ci, 2, h, :], start=False,
                                                  stop=True, skip_group_check=True),
                         reads=W(tok), writes=[('ps', bank)])
                k.op('dve', lambda e: e.tensor_tensor(out=H[:, :, :], in0=H[:, :, :],
                                                      in1=ps[0:64, 0:256].rearrange('p (h s) -> p h s', h=4),
                                                      op=ALU.add),
                     reads=[('ps', bank), ('c_H', 0)], writes=[('c_H', 0)])
                pcl = P_[:, :, ci * 64 + 63:ci * 64 + 64].to_broadcast([64, 4, 64])
                k.op('dve', lambda e: e.tensor_tensor(out=H[:, :, :], in0=H[:, :, :], in1=pcl, op=ALU.mult),
                     reads=[('c_H', 0)] + W(P_), writes=[('c_H', 0)])
            def mean_fn(half, psv, bank):
                k.op('dve', lambda e: e.scalar_tensor_tensor(out=dd[:, 2 * half:2 * half + 2, :], in0=psv,
                                                             scalar=-1.0 / 64, in1=yT[:, 2 * half:2 * half + 2, :],
                                                             op0=ALU.mult, op1=ALU.add),
                     reads=[('ps', bank)] + W(yT), writes=W(dd))
            headsum(yT, mean_fn)
            k.op('act', lambda e: e.activation(out=t1[:, :, :], in_=dd[:, :, :], func=AF.Square),
                 reads=W(dd), writes=W(t1))

            def var_fn(half, psv, bank):
                k.op('act', lambda e: e.activation(out=kmod[:, 2 * half:2 * half + 2, :], in_=psv, func=AF.Ln,
                                                   scale=1.0 / 64, bias=c.epsgn[0:64, 0:1]),
                     reads=[('ps', bank)], writes=W(kmod))
            headsum(t1, var_fn)
            k.op('act', lambda e: e.activation(out=kmod[:, :, :], in_=kmod[:, :, :], func=AF.Exp, scale=-0.5),
                 reads=W(kmod), writes=W(kmod))
            k.op('dve', lambda e: e.tensor_tensor(out=dd[:, :, :], in0=dd[:, :, :], in1=kmod[:, :, :], op=ALU.mult),
                 reads=W(dd) + W(kmod), writes=W(dd))
            k.op('pool', lambda e: e.tensor_tensor(out=dd[:, :, :], in0=dd[:, :, :], in1=bcast4(gnw), op=ALU.mult),
                 reads=W(dd), writes=W(dd))
            k.op('pool', lambda e: e.tensor_tensor(out=dd[:, :, :], in0=dd[:, :, :], in1=bcast4(gnb), op=ALU.add),
                 reads=W(dd), writes=W(dd))
            k.op('dve', lambda e: e.tensor_tensor(out=dd[:, :, :], in0=dd[:, :, :], in1=bon[:, :, :], op=ALU.add),
                 reads=W(dd) + W(bon), writes=W(dd))
            k.op('dve', lambda e: e.tensor_tensor(out=ob[:, :, :], in0=dd[:, :, :], in1=g_s[:, :, :], op=ALU.mult),
                 reads=W(dd) + W(g_s), writes=W(ob))
            k.dma('pool', d['obr'][2, :, :, ts].rearrange('h p t -> p h t'), ob[:, :, :], reads=W(ob))
        k.barrier()


ALPHA = (2 * 2) ** 0.25


def ln_block(k, c, src, skey, dst, dkey, gb, gkey, tmp):
    st6, mv = tmp
    for i in range(2):
        k.op('dve', lambda e: e.bn_stats(out=st6[:, i, :], in_=src[:, i * 512:(i + 1) * 512]),
             reads=[skey], writes=[('ln_st', 0)])
    k.op('dve', lambda e: e.bn_aggr(out=mv[:, 0:2], in_=st6[:, :, :].rearrange('p a b -> p (a b)')),
         reads=[('ln_st', 0)], writes=[('ln_mv', 0)])
    k.op('act', lambda e: e.activation(out=mv[:, 2:3], in_=mv[:, 1:2], func=AF.Ln, bias=c.eps5[:, 0:1]),
         reads=[('ln_mv', 0)], writes=[('ln_mv', 1)])
    k.op('act', lambda e: e.activation(out=mv[:, 3:4], in_=mv[:, 2:3], func=AF.Exp, scale=-0.5),
         reads=[('ln_mv', 1)], writes=[('ln_mv', 2)])
    k.op('dve', lambda e: e.tensor_scalar(out=dst, in0=src[:, :], scalar1=mv[:, 0:1], scalar2=mv[:, 3:4],
                                          op0=ALU.subtract, op1=ALU.mult),
         reads=[skey, ('ln_mv', 0), ('ln_mv', 2)], writes=[dkey])
    k.op('pool', lambda e: e.tensor_tensor(out=dst, in0=dst, in1=gb[:, 0, :], op=ALU.mult),
         reads=[dkey, gkey], writes=[dkey])
    k.op('pool', lambda e: e.tensor_tensor(out=dst, in0=dst, in1=gb[:, 1, :], op=ALU.add),
         reads=[dkey, gkey], writes=[dkey])


def phase_merge(k, c, T, lp, x_dram, x1_dram):
    nc = k.nc
    NB = T // 128
    d = c.d
    with ExitStack() as st:
        def sb(name, shape, dt_=F32):
            return st.enter_context(_sbt(nc, 'm_' + name, shape, dt_))
        wb = sb('wb', [64, 16, 1024], BF16)
        wo = sb('wo', [128, 8, 1024], BF16)
        stg = [sb('stg%d' % i, [128, 4096]) for i in range(2)]
        gb = sb('gb', [128, 2, 1024])
        k.dma('sp', gb[:, 0, :], lp['ln1_g'].partition_broadcast(128), writes=[('m_gb', 0)])
        k.dma('act', gb[:, 1, :], lp['ln1_b'].partition_broadcast(128), writes=[('m_gb', 0)])
        for n in range(4):
            s_ = stg[n % 2]
            k.dma('sp' if n % 2 == 0 else 'act', s_[0:64, :].rearrange('p (a c) -> p a c', a=4),
                  lp['w_branch'][n].rearrange('(a p) c -> p a c', p=64), writes=[('m_stg', n % 2)])
            k.op('dve' if n % 2 == 0 else 'pool', lambda e: e.tensor_copy(
                out=wb[:, 4 * n:4 * n + 4, :], in_=s_[0:64, :].rearrange('p (a c) -> p a c', a=4)),
                reads=[('m_stg', n % 2)], writes=[('m_wb', 0)])
        for hf in range(2):
            s_ = stg[hf]
            k.dma('sp' if hf == 0 else 'act', s_[:, :].rearrange('p (a c) -> p a c', a=4),
                  lp['w_out'][hf * 512:(hf + 1) * 512, :].rearrange('(a p) c -> p a c', p=128),
                  writes=[('m_stg', hf)])
            k.op('dve' if hf == 0 else 'pool', lambda e: e.tensor_copy(
                out=wo[:, 4 * hf:4 * hf + 4, :], in_=s_[:, :].rearrange('p (a c) -> p a c', a=4)),
                reads=[('m_stg', hf)], writes=[('m_wo', 0)])
        ob = [sb('ob%d' % i, [64, 16, 128], BF16) for i in range(2)]
        gs = [sb('gs%d' % i, [128, 4096], BF16) for i in range(2)]
        xs = [sb('xs%d' % i, [128, 1024]) for i in range(2)]
        mg = sb('mg', [128, 1024])
        tm = sb('tm', [128, 512])
        mT = sb('mT', [128, 8, 128], BF16)
        h1 = sb('h1', [128, 1024])
        xo = [sb('xo%d' % i, [128, 1024]) for i in range(2)]
        st6 = sb('st6', [128, 2, 6])
        mv = sb('mv', [128, 4])
        bc = [0]

        def nbank():
            b = bc[0] % 8
            bc[0] += 1
            return b
        for b in range(NB):
            i2 = b % 2
            bs = slice(b * 128, (b + 1) * 128)
            k.dma('sp', ob[i2][:, :, :], d['obr'][:, :, :, bs].rearrange('n h p t -> p (n h) t'),
                  writes=[('m_ob', i2)])
            k.dma('act', gs[i2][:, :], d['gsig'][b], writes=[('m_gs', i2)])
            k.dma('pool', xs[i2][:, :], x_dram[bs, :], writes=[('m_xs', i2)])
            for n in range(4):
                for hc in range(2):
                    bank = nbank()
                    ps = c.ps[bank]
                    for h in range(4):
                        k.op('pe', lambda e: e.matmul(ps[:, :], lhsT=ob[i2][:, 4 * n + h, :],
                                                      rhs=wb[:, 4 * n + h, hc * 512:(hc + 1) * 512],
                                                      start=(h == 0), stop=(h == 3)),
                             reads=[('m_ob', i2), ('m_wb', 0)], writes=[('ps', bank)])
                    gsl = gs[i2][:, n * 1024 + hc * 512:n * 1024 + (hc + 1) * 512]
                    msl = mg[:, hc * 512:(hc + 1) * 512]
                    if n == 0:
                        k.op('dve', lambda e: e.tensor_tensor(out=msl, in0=ps[:, :], in1=gsl, op=ALU.mult),
                             reads=[('ps', bank), ('m_gs', i2)], writes=[('m_mg', hc)])
                    else:
                        k.op('dve', lambda e: e.tensor_tensor(out=tm[:, :], in0=ps[:, :], in1=gsl, op=ALU.mult),
                             reads=[('ps', bank), ('m_gs', i2)], writes=[('m_tm', 0)])
                        k.op('pool', lambda e: e.tensor_tensor(out=msl, in0=msl, in1=tm[:, :], op=ALU.add),
                             reads=[('m_tm', 0), ('m_mg', hc)], writes=[('m_mg', hc)])
            for hc in range(2):
                bank = nbank()
                ps = c.ps[bank]
                for j in range(4):
                    ch = hc * 4 + j
                    k.op('pe', lambda e: e.transpose(out=ps[:, j * 128:(j + 1) * 128],
                                                     in_=mg[:, ch * 128:(ch + 1) * 128], identity=c.ident[:, :]),
                         reads=[('m_mg', hc)], writes=[('ps', bank)])
                k.op('act', lambda e: e.copy(out=mT[:, hc * 4:(hc + 1) * 4, :],
                                             in_=ps[:, :].rearrange('p (j n) -> p j n', j=4)),
                     reads=[('ps', bank)], writes=[('m_mT', 0)])
            for hc in range(2):
                bank = nbank()
                ps = c.ps[bank]
                for kc in range(8):
                    k.op('pe', lambda e: e.matmul(ps[:, :], lhsT=mT[:, kc, :], rhs=wo[:, kc, hc * 512:(hc + 1) * 512],
                                                  start=(kc == 0), stop=(kc == 7)),
                         reads=[('m_mT', 0), ('m_wo', 0)], writes=[('ps', bank)])
                k.op('dve', lambda e: e.scalar_tensor_tensor(out=h1[:, hc * 512:(hc + 1) * 512],
                                                             in0=xs[i2][:, hc * 512:(hc + 1) * 512], scalar=ALPHA,
                                                             in1=ps[:, :], op0=ALU.mult, op1=ALU.add),
                     reads=[('ps', bank), ('m_xs', i2)], writes=[('m_h1', 0)])
            ln_block(k, c, h1, ('m_h1', 0), xo[i2][:, :], ('m_xo', i2), gb, ('m_gb', 0), (st6, mv))
            k.dma('sp', x1_dram[bs, :], xo[i2][:, :], reads=[('m_xo', i2)])
        k.barrier()


def phase_moe(k, c, T, lp, x1_dram, out_dram):
    nc = k.nc
    NB = T // 128
    HT = min(T, 1024)
    NH = T // HT
    NBH = HT // 128
    NTG = HT // 512
    with ExitStack() as st:
        def sb(name, shape, dt_=F32):
            return st.enter_context(_sbt(nc, 'e_' + name, shape, dt_))
        gate = sb('gate', [128, NBH, 32])
        xTp = sb('xTp', [128, 8, HT], BF16)
        wrl = WLoader(k, st, 'e_wr', width=36, nbuf=1)
        wr, wrkey = wrl.load(lp['r_w'], 0, 36)
        lg = sb('lg', [128, 36])
        sm = sb('sm', [128, 16])
        w8 = [sb('w8%d' % i, [128, 4, 8]) for i in range(4)]
        oh = sb('oh', [128, 4])
        gb = sb('gb', [128, 2, 1024])
        rb = sb('rb', [128, 36])
        k.dma('sp', gb[:, 0, :], lp['ln2_g'].partition_broadcast(128), writes=[('e_gb', 0)])
        k.dma('act', gb[:, 1, :], lp['ln2_b'].partition_broadcast(128), writes=[('e_gb', 0)])
        k.dma('pool', rb[:, :], lp['r_bias'].partition_broadcast(128), writes=[('e_rb', 0)])
        def router():
            for b in range(NBH):
                bank = b % 8
                ps = c.ps[bank]
                for kc in range(8):
                    k.op('pe', lambda e: e.matmul(ps[:, 0:36], lhsT=xTp[:, kc, b * 128:(b + 1) * 128], rhs=wr[:, kc, 0:36],
                                                  start=(kc == 0), stop=(kc == 7)),
                         reads=[wrkey], writes=[('ps', bank)])
                R_ = [('e_r', 0)]
                k.op('dve', lambda e: e.tensor_tensor(out=lg[:, :], in0=ps[:, 0:36], in1=rb[:, :], op=ALU.add),
                     reads=[('ps', bank), ('e_rb', 0)] + R_, writes=R_)
                le = lg[:, 4:36].rearrange('p (g j) -> p g j', g=4)
                k.op('dve', lambda e: e.tensor_reduce(out=sm[:, 0:1], in_=lg[:, 0:4], axis=AX.X, op=ALU.max),
                     reads=R_, writes=R_)
                k.op('dve', lambda e: e.tensor_scalar(out=oh[:, :], in0=lg[:, 0:4], scalar1=sm[:, 0:1], scalar2=None,
                                                      op0=ALU.is_equal), reads=R_, writes=R_)
                k.op('dve', lambda e: e.tensor_scalar(out=sm[:, 1:2], in0=sm[:, 0:1], scalar1=-1.0, scalar2=None,
                                                      op0=ALU.mult), reads=R_, writes=R_)
                k.op('act', lambda e: e.activation(out=sm[:, 4:8], in_=lg[:, 0:4], func=AF.Exp, bias=sm[:, 1:2],
                                                   accum_out=sm[:, 2:3]), reads=R_, writes=R_)
                k.op('dve', lambda e: e.reciprocal(out=sm[:, 3:4], in_=sm[:, 2:3]), reads=R_, writes=R_)
                k.op('dve', lambda e: e.tensor_reduce(out=sm[:, 8:12], in_=le, axis=AX.X, op=ALU.max),
                     reads=R_, writes=R_)
                m1b = sm[:, 8:12].unsqueeze(2).to_broadcast([128, 4, 8])
                k.op('dve', lambda e: e.tensor_tensor(out=w8[0][:, :, :], in0=le, in1=m1b, op=ALU.is_equal),
                     reads=R_, writes=R_)
                k.op('dve', lambda e: e.scalar_tensor_tensor(out=w8[1][:, :, :].rearrange('p g j -> p (g j)'),
                                                             in0=w8[0][:, :, :].rearrange('p g j -> p (g j)'),
                                                             scalar=-1.0e30, in1=lg[:, 4:36],
                                                             op0=ALU.mult, op1=ALU.add), reads=R_, writes=R_)
                k.op('dve', lambda e: e.tensor_reduce(out=sm[:, 12:16], in_=w8[1][:, :, :], axis=AX.X, op=ALU.max),
                     reads=R_, writes=R_)
                m2b = sm[:, 12:16].unsqueeze(2).to_broadcast([128, 4, 8])
                k.op('dve', lambda e: e.tensor_tensor(out=w8[0][:, :, :], in0=le, in1=m2b, op=ALU.is_ge),
                     reads=R_, writes=R_)
                k.op('dve', lambda e: e.tensor_tensor(out=w8[1][:, :, :], in0=le, in1=m1b, op=ALU.subtract),
                     reads=R_, writes=R_)
                k.op('act', lambda e: e.activation(out=w8[1][:, :, :], in_=w8[1][:, :, :], func=AF.Exp),
                     reads=R_, writes=R_)
                k.op('dve', lambda e: e.tensor_tensor(out=oh[:, :], in0=oh[:, :],
                                                      in1=sm[:, 3:4].to_broadcast([128, 4]), op=ALU.mult),
                     reads=R_, writes=R_)
                k.op('dve', lambda e: e.tensor_tensor(out=sm[:, 4:8], in0=sm[:, 12:16], in1=sm[:, 8:12],
                                                      op=ALU.subtract), reads=R_, writes=R_)
                k.op('act', lambda e: e.activation(out=sm[:, 4:8], in_=sm[:, 4:8], func=AF.Exp), reads=R_, writes=R_)
                k.op('dve', lambda e: e.tensor_scalar(out=sm[:, 4:8], in0=sm[:, 4:8], scalar1=1.0, scalar2=None,
                                                      op0=ALU.add), reads=R_, writes=R_)
                k.op('dve', lambda e: e.reciprocal(out=sm[:, 4:8], in_=sm[:, 4:8]), reads=R_, writes=R_)
                k.op('dve', lambda e: e.tensor_tensor(out=sm[:, 4:8], in0=sm[:, 4:8], in1=oh[:, :], op=ALU.mult),
                     reads=R_, writes=R_)
                k.op('dve', lambda e: e.tensor_tensor(out=w8[0][:, :, :], in0=w8[0][:, :, :], in1=w8[1][:, :, :],
                                                      op=ALU.mult), reads=R_, writes=R_)
                k.op('dve', lambda e: e.tensor_tensor(out=gate[:, b, :].rearrange('p (g j) -> p g j', g=4),
                                                      in0=w8[0][:, :, :],
                                                      in1=sm[:, 4:8].unsqueeze(2).to_broadcast([128, 4, 8]),
                                                      op=ALU.mult), reads=R_, writes=R_ + [('e_gate', 0)])
            k.barrier()
        wstg = [sb('wstg%d' % i, [128, 2048]) for i in range(4)]
        wset = [[sb('wb%d_%d' % (i, j), [128, 4096], BF16) for j in range(3)] for i in range(2)]
        wcnt = [0]

        def wload(src3, seti, j, q):
            a = src3.shape[1]
            ah = a // 2
            for hf in range(2):
                si = wcnt[0] % 4
                wcnt[0] += 1
                stg_ = wstg[si]
                k.dma('sp', stg_[:, :].rearrange('p (a c) -> p a c', a=ah), src3[:, hf * ah:(hf + 1) * ah, :],
                      writes=[('e_wstg', si)])
                k.op('pool', lambda e: e.tensor_copy(out=wset[seti][j][:, hf * 2048:(hf + 1) * 2048], in_=stg_[:, :]),
                     reads=[('e_wstg', si)], writes=[('e_wset', seti, j)])
        yacc = sb('yacc', [128, NBH, 1024])
        sl = [sb('sl%d' % i, [128, 512]) for i in range(2)]
        hT = [sb('hT%d' % i, [128, 4, 512], BF16) for i in range(2)]
        xs = [sb('xs%d' % i, [128, 1024]) for i in range(2)]
        st6 = sb('st6', [128, 2, 6])
        mv = sb('mv', [128, 4])
        bc = [0]

        def nbank():
            b = bc[0] % 8
            bc[0] += 1
            return b
        for hp in range(NH):
            tb0 = hp * HT
            phase_x(k, c, x1_dram[tb0:tb0 + HT, :], HT, xT=xTp)
            router()
            for ex in range(32):
                seti = (hp * 32 + ex) % 2
                wload(lp['e_gate'][ex].rearrange('(a p) c -> p a c', p=128), seti, 0, 'sp')
                wload(lp['e_up'][ex].rearrange('(a p) c -> p a c', p=128), seti, 1, 'act')
                wload(lp['e_down'][ex].rearrange('(a p) c -> p a c', p=128), seti, 2, 'sp')
                wg = wset[seti][0][:, :].rearrange('p (a c) -> p a c', a=8)
                wu = wset[seti][1][:, :].rearrange('p (a c) -> p a c', a=8)
                wd_b = wset[seti][2][:, :].rearrange('p (a c) -> p a c', a=4)
                wgkey, wukey, wdkey = ('e_wset', seti, 0), ('e_wset', seti, 1), ('e_wset', seti, 2)
                for tg in range(NTG):
                    ts0 = tg * 512
                    hh = hT[(ex * NTG + tg) % 2]
                    hkey = ('e_hT', (ex * NTG + tg) % 2)
                    for cc in range(4):
                        bg, bu = nbank(), nbank()
                        psg, psu = c.ps[bg], c.ps[bu]
                        for kc in range(8):
                            k.op('pe', lambda e: e.matmul(psg[:, :], lhsT=wg[:, kc, cc * 128:(cc + 1) * 128],
                                                          rhs=xTp[:, kc, ts0:ts0 + 512], start=(kc == 0),
                                                          stop=(kc == 7)),
                                 reads=[wgkey], writes=[('ps', bg)])
                        for kc in range(8):
                            k.op('pe', lambda e: e.matmul(psu[:, :], lhsT=wu[:, kc, cc * 128:(cc + 1) * 128],
                                                          rhs=xTp[:, kc, ts0:ts0 + 512], start=(kc == 0),
                                                          stop=(kc == 7)),
                                 reads=[wukey], writes=[('ps', bu)])
                        s_ = sl[cc % 2]
                        k.op('act', lambda e: e.activation(out=s_[:, :], in_=psg[:, :], func=AF.Silu),
                             reads=[('ps', bg)], writes=[('e_sl', cc % 2)])
                        k.op('dve', lambda e: e.tensor_tensor(out=hh[:, cc, :], in0=s_[:, :], in1=psu[:, :],
                                                              op=ALU.mult),
                             reads=[('e_sl', cc % 2), ('ps', bu)], writes=[hkey])
                    for bl in range(4):
                        bloc = tg * 4 + bl
                        bglob = tb0 // 128 + bloc
                        for hc in range(2):
                            bank = nbank()
                            ps = c.ps[bank]
                            for cc in range(4):
                                k.op('pe', lambda e: e.matmul(ps[:, :], lhsT=hh[:, cc, bl * 128:(bl + 1) * 128],
                                                              rhs=wd_b[:, cc, hc * 512:(hc + 1) * 512],
                                                              start=(cc == 0), stop=(cc == 3)),
                                     reads=[hkey, wdkey], writes=[('ps', bank)])
                            ya = yacc[:, bloc, hc * 512:(hc + 1) * 512]
                            if ex == 0:
                                k.op('dve', lambda e: e.tensor_scalar(out=ya, in0=ps[:, :],
                                                                      scalar1=gate[:, bloc, ex:ex + 1], scalar2=None,
                                                                      op0=ALU.mult),
                                     reads=[('ps', bank), ('e_gate', 0)], writes=[('e_y', bloc)])
                            else:
                                k.op('dve', lambda e: e.scalar_tensor_tensor(out=ya, in0=ps[:, :],
                                                                             scalar=gate[:, bloc, ex:ex + 1], in1=ya,
                                                                             op0=ALU.mult, op1=ALU.add),
                                     reads=[('ps', bank), ('e_gate', 0), ('e_y', bloc)], writes=[('e_y', bloc)])
            for bloc in range(NBH):
                bglob = tb0 // 128 + bloc
                i2 = bloc % 2
                bs = slice(bglob * 128, (bglob + 1) * 128)
                k.dma('sp', xs[i2][:, :], x1_dram[bs, :], writes=[('e_xs', i2)])
                k.op('dve', lambda e: e.scalar_tensor_tensor(out=yacc[:, bloc, :], in0=xs[i2][:, :], scalar=ALPHA,
                                                             in1=yacc[:, bloc, :], op0=ALU.mult, op1=ALU.add),
                     reads=[('e_xs', i2), ('e_y', bloc)], writes=[('e_y', bloc)])
                ln_block(k, c, yacc[:, bloc, :], ('e_y', bloc), xs[i2][:, :], ('e_xs', i2), gb, ('e_gb', 0), (st6, mv))
                k.dma('act', out_dram[bs, :], xs[i2][:, :], reads=[('e_xs', i2)])
        k.barrier()


PARAM_SHAPES = None


def pack_params(inp):
    L = inp['w_in'].shape[0]
    f = lambda a: np.ascontiguousarray(np.asarray(a, dtype=np.float32))
    hp = lambda v: v.reshape(4, 64).T
    p = {}
    p['w_in'] = f(inp['w_in'])
    p['w_branch'] = f(inp['w_branch'])
    p['w_out'] = f(inp['w_out'])
    for n in ('ln1_g', 'ln1_b', 'ln2_g', 'ln2_b'):
        p[n] = f(inp[n]).reshape(L, 1, 1024)
    p['r_w'] = f(np.concatenate([inp['r_group'], inp['r_expert']], axis=2))
    p['r_bias'] = f(np.concatenate([inp['r_group_b'], inp['r_expert_b']], axis=1)).reshape(L, 1, 36)
    p['e_gate'] = f(inp['e_gate'])
    p['e_up'] = f(inp['e_up'])
    p['e_down'] = f(inp['e_down'])
    p64 = []
    for l in range(L):
        v0 = inp['c_v0'][max(l - 1, 0)]
        p64.append(np.concatenate([np.asarray(inp['c_mu'][l]).reshape(14, 64).T, hp(np.asarray(inp['c_w0'][l])),
                                   hp(np.asarray(inp['c_a0'][l])), hp(np.asarray(inp['c_kk'][l])),
                                   hp(np.asarray(inp['c_ka'][l])), hp(np.asarray(inp['c_rk'][l]).reshape(-1)),
                                   hp(np.asarray(inp['c_gn_w'][l])), hp(np.asarray(inp['c_gn_b'][l])),
                                   hp(np.asarray(v0))], axis=1))
    p['c_p64'] = f(np.stack(p64))
    p['c_wa2'] = f(np.concatenate([inp['c_w2'], inp['c_a2']], axis=1))
    p['c_g2'] = f(inp['c_g2'])
    p['c_v1'] = f(np.asarray(inp['c_v1']).reshape(L - 1, 4, 64, 16).transpose(0, 2, 1, 3))
    p['c_v2'] = f(inp['c_v2'])
    p['d_cw'] = f(np.asarray(inp['d_conv_w']).transpose(0, 2, 1).reshape(L, 8, 64, 4).transpose(0, 2, 1, 3))
    p['d_cb'] = f(np.asarray(inp['d_conv_b']).reshape(L, 8, 64).transpose(0, 2, 1))
    p['d_nw'] = f(np.asarray(inp['d_norm_w']).reshape(L, 4, 64).transpose(0, 2, 1))
    p['d_vec'] = f(np.concatenate([inp['d_dt_bias'], inp['d_a_log'], inp['d_skip']], axis=1)).reshape(L, 1, 12)
    return p


def build_full(pshapes, T=4096, depth=2, debug=False, phases=None):
    nc = bass.Bass("TRN2", target_bir_lowering=False)
    k = K(nc)
    c = Ctx()
    x = nc.dram_tensor("x", [T, 1024], F32, kind="ExternalInput").ap()
    y = nc.dram_tensor("y", [T, 1024], F32, kind="ExternalOutput").ap()
    P = {n: nc.dram_tensor(n, list(shp), F32, kind="ExternalInput").ap() for n, shp in pshapes.items()}
    c.cin = {n: nc.dram_tensor(n, list(v.shape), F32, kind="ExternalInput").ap() for n, v in make_consts().items()}
    alloc_scratch(nc, c, T, debug=debug)
    kind = 'ExternalOutput' if debug else 'Internal'
    x1 = nc.dram_tensor('x1s', [T, 1024], F32, kind=kind).ap()
    xmid = nc.dram_tensor('xmid', [T, 1024], F32, kind=kind).ap()
    with ExitStack() as st:
        setup_common(nc, k, c, T, st)
        for l in range(depth):
            lp = {n: P[n][l] for n in P if n not in ('c_v1', 'c_v2')}
            if l > 0:
                lp['c_v1'] = P['c_v1'][l - 1]
                lp['c_v2'] = P['c_v2'][l - 1]
            x_in = x if l == 0 else xmid
            x_out = y if l == depth - 1 else xmid
            on = lambda n: phases is None or n in phases
            if on('p'):
                with ExitStack() as st2:
                    c.xT = st2.enter_context(_sbt(nc, 'xT', [128, 8, T], BF16))
                    phase_x(k, c, x_in, T)
                    phase_p(k, c, lp['w_in'], T)
            if on('a'):
                mixer_a(k, c, T)
            if on('b'):
                mixer_b(k, c, T)
            if on('c'):
                mixer_c(k, c, T, lp, l)
            if on('d'):
                mixer_d(k, c, T, lp)
            if on('m'):
                phase_merge(k, c, T, lp, x_in, x1)
            if on('e'):
                phase_moe(k, c, T, lp, x1, x_out)
        k.barrier()
    return nc, k


def kernel(**inputs):
    x = np.asarray(inputs['x'], dtype=np.float32)
    B, T, _ = x.shape
    p = pack_params(inputs)
    pshapes = {n: v.shape for n, v in p.items()}
    nc, _k = build_full(pshapes, T=T, depth=p['w_in'].shape[0])
    cs = make_consts()
    in_maps = []
    for b in range(B):
        m = {'x': np.ascontiguousarray(x[b])}
        m.update(p)
        m.update(cs)
        in_maps.append(m)
    res = run_bass_kernel_spmd(nc, in_maps, core_ids=list(range(B)))
    return np.stack([np.asarray(r['y'], dtype=np.float32) for r in res.results], axis=0)
```

```python
import numpy as np
from contextlib import ExitStack
import concourse.bass as bass
import concourse.mybir as mybir
from concourse.bass_utils import run_bass_kernel_spmd

F32 = mybir.dt.float32
BF16 = mybir.dt.bfloat16
AF = mybir.ActivationFunctionType
ALU = mybir.AluOpType
AX = mybir.AxisListType

ENG = ('pe', 'act', 'dve', 'pool', 'sp')
SAME_ENG_SYNC = True


_UID = [0]


def _sbt(nc, name, shape, dtype):
    _UID[0] += 1
    return nc.sbuf_tensor('%s_u%d' % (name, _UID[0]), shape, dtype)


class K:
    def __init__(self, nc):
        self.nc = nc
        self.eng = {'pe': nc.tensor, 'act': nc.scalar, 'dve': nc.vector,
                    'pool': nc.gpsimd, 'sp': nc.sync}
        self.sem = {e: nc.alloc_semaphore('s_' + e) for e in ENG}
        self.cnt = {e: 0 for e in ENG}
        self.known = {e: {} for e in ENG}
        self.res = {}
        self.NDS = 16
        self.dq = ('sp', 'act', 'pool')
        self.dsem = {q: [nc.alloc_semaphore('d_%s_%d' % (q, i)) for i in range(self.NDS)]
                     for q in self.dq}
        self.dcnt = {q: 0 for q in self.dq}
        self.semobj = {}
        for e in ENG:
            self.semobj['E' + e] = self.sem[e]
        for q in self.dq:
            for i in range(self.NDS):
                self.semobj['D%s%d' % (q, i)] = self.dsem[q][i]
        self.ninst = 0

    def _collect(self, reads, writes):
        need = {}
        for r in reads:
            st = self.res.get(r)
            if st is not None and st[0] is not None:
                k, v = st[0]
                if need.get(k, 0) < v:
                    need[k] = v
        for w in writes:
            st = self.res.get(w)
            if st is not None:
                if st[0] is not None:
                    k, v = st[0]
                    if need.get(k, 0) < v:
                        need[k] = v
                for k, v in st[1].items():
                    if need.get(k, 0) < v:
                        need[k] = v
        return need

    def _wait(self, e, need):
        kn = self.known[e]
        for k, v in need.items():
            if k == 'E' + e and (e == 'pe' or not SAME_ENG_SYNC):
                continue
            if kn.get(k, 0) < v:
                self.eng[e].wait_ge(self.semobj[k], v)
                kn[k] = v
                self.ninst += 1

    def _record(self, ev, reads, writes):
        for w in writes:
            self.res[w] = [ev, {}]
        for r in reads:
            st = self.res.get(r)
            if st is None:
                st = [None, {}]
                self.res[r] = st
            k, v = ev
            if st[1].get(k, 0) < v:
                st[1][k] = v

    def op(self, e, fn, reads=(), writes=(), inc=True):
        if any(r[0] == 'ps' for r in reads):
            writes = list(writes) + [r for r in reads if r[0] == 'ps']
            reads = [r for r in reads if r[0] != 'ps']
        self._wait(e, self._collect(reads, writes))
        inst = fn(self.eng[e])
        self.ninst += 1
        if inc:
            self.cnt[e] += 1
            inst.then_inc(self.sem[e], 1)
            self._record(('E' + e, self.cnt[e]), reads, writes)
        else:
            self._record(('E' + e, self.cnt[e] + 1), reads, writes)

    def dma(self, q, out, in_, reads=(), writes=(), **kw):
        if q == 'act':
            q = 'sp'
        need = self._collect(reads, writes)
        n = self.dcnt[q]
        slot, rnd = n % self.NDS, n // self.NDS
        key = 'D%s%d' % (q, slot)
        if rnd > 0 and need.get(key, 0) < 16 * rnd:
            need[key] = 16 * rnd
        self._wait(q, need)
        inst = self.eng[q].dma_start(out=out, in_=in_, **kw)
        inst.then_inc(self.dsem[q][slot], 16)
        self.dcnt[q] = n + 1
        self.ninst += 1
        self._record((key, 16 * (rnd + 1)), reads, writes)

    def all_events(self):
        need = {}
        for e in ENG:
            if self.cnt[e] > 0:
                need['E' + e] = self.cnt[e]
        for q in self.dq:
            n = self.dcnt[q]
            for slot in range(self.NDS):
                uses = (n - slot + self.NDS - 1) // self.NDS if n > slot else 0
                if uses > 0:
                    need['D%s%d' % (q, slot)] = 16 * uses
        return need

    def barrier(self, engines=ENG):
        need = self.all_events()
        for e in engines:
            self._wait(e, dict(need))
        self.res = {}


D_MODEL = 1024
IN_W = 9160
OFF = dict(a_qkv=0, b_qkv=2304, biq=3072, bik=3328, biw=3392, c_in=3396, d_z=4292, d_xbc=4548,
           d_dt=5060, gates=5064)
A_PAT = ((128, 1), (512, 4), (2048, 16))


class Ctx:
    pass


def phase_x(k, c, x_dram, T, xT=None):
    nc = k.nc
    NB = T // 128
    if xT is None:
        xT = c.xT
    with ExitStack() as st:
        xs = [st.enter_context(_sbt(nc, 'xs%d' % i, [128, 1024], F32)) for i in range(2)]
        for b in range(NB):
            s = xs[b % 2]
            k.dma('sp' if b % 2 == 0 else 'act', s[:, :], x_dram[b * 128:(b + 1) * 128, :],
                  writes=[('xs', b % 2)])
            for half in range(2):
                bank = (2 * b + half) % 8
                ps = c.ps[bank]
                for j in range(4):
                    ch = half * 4 + j
                    k.op('pe', lambda e, ps=ps, j=j, ch=ch, s=s: e.transpose(
                        out=ps[:, j * 128:(j + 1) * 128], in_=s[:, ch * 128:(ch + 1) * 128],
                        identity=c.ident[:, :]),
                        reads=[('xs', b % 2)], writes=[('ps', bank)])
                dst = xT[:, half * 4:(half + 1) * 4, b * 128:(b + 1) * 128]
                src_ = ps[:, :].rearrange('p (j n) -> p j n', j=4)
                if half == 0:
                    k.op('act', lambda e, dst=dst, src_=src_: e.copy(out=dst, in_=src_),
                         reads=[('ps', bank)], writes=[('xT', b)])
                else:
                    k.op('dve', lambda e, dst=dst, src_=src_: e.tensor_copy(out=dst, in_=src_),
                         reads=[('ps', bank)], writes=[('xT', b)])
        k.barrier()


class WLoader:
    def __init__(self, k, st, name, width=512, nbuf=2):
        nc = k.nc
        self.k = k
        self.name = name
        self.nbuf = nbuf
        self.f = [st.enter_context(_sbt(nc, '%s_f%d' % (name, i), [128, 8, width], F32)) for i in range(nbuf)]
        self.b = [st.enter_context(_sbt(nc, '%s_b%d' % (name, i), [128, 8, width], BF16)) for i in range(nbuf)]
        self.n = 0

    def load(self, w_dram, col0, ncols, q='sp', cast='pool'):
        k = self.k
        i = self.n % self.nbuf
        self.n += 1
        src = w_dram[:, col0:col0 + ncols].rearrange('(ko ki) n -> ki ko n', ki=128)
        k.dma(q, self.f[i][:, :, 0:ncols], src, writes=[(self.name + 'f', i)])
        fi, bi = self.f[i], self.b[i]
        if cast == 'pool':
            k.op('pool', lambda e: e.tensor_copy(out=bi[:, :, 0:ncols], in_=fi[:, :, 0:ncols]),
                 reads=[(self.name + 'f', i)], writes=[(self.name + 'b', i)])
        else:
            k.op('dve', lambda e: e.tensor_copy(out=bi[:, :, 0:ncols], in_=fi[:, :, 0:ncols]),
                 reads=[(self.name + 'f', i)], writes=[(self.name + 'b', i)])
        return bi, (self.name + 'b', i)


def phase_p(k, c, w_in, T, only=None):
    nc = k.nc
    NB = T // 128
    NG = T // 512
    d = c.d
    with ExitStack() as st:
        wl = WLoader(k, st, 'wl')
        stg = [st.enter_context(_sbt(nc, 'pstg%d' % i, [128, T], F32)) for i in range(2)]
        stgb = [st.enter_context(_sbt(nc, 'pstgb%d' % i, [128, T], BF16)) for i in range(2)]
        tst = [st.enter_context(_sbt(nc, 'ptst%d' % i, [128, 512], F32)) for i in range(2)]
        tstb = [st.enter_context(_sbt(nc, 'ptstb%d' % i, [128, 512], BF16)) for i in range(2)]
        cnt = {'bank': 0, 'fm': 0, 'tm': 0, 'ev': 0}

        def evac(dst, src, bank, wkey, func=None, scale=1.0):
            cnt['ev'] += 1
            if func is not None or cnt['ev'] % 2 == 0:
                f = func if func is not None else AF.Copy
                k.op('act', lambda e: e.activation(out=dst, in_=src, func=f, scale=scale),
                     reads=[('ps', bank)], writes=[wkey])
            else:
                k.op('dve', lambda e: e.tensor_scalar(out=dst, in0=src, scalar1=float(scale), scalar2=None,
                                                      op0=ALU.mult),
                     reads=[('ps', bank)], writes=[wkey])

        jobs = []

        def fm_group(col0, total, cw, dst_fn, bf, pad=0, scale=1.0):
            for s0 in range(0, total, 512):
                sw = min(512, total - s0)

                def run(wt, wkey, s0=s0, sw=sw):
                    for j0 in range(0, sw, cw):
                        i = cnt['fm'] % 2
                        cnt['fm'] += 1
                        sg = stgb[i] if bf else stg[i]
                        skey = ('pstgb' if bf else 'pstg', i)
                        for g in range(NG):
                            bank = cnt['bank'] % 8
                            cnt['bank'] += 1
                            ps = c.ps[bank]
                            for kk in range(8):
                                k.op('pe', lambda e, ps=ps, kk=kk, j0=j0, g=g: e.matmul(
                                    ps[0:cw, :], lhsT=wt[:, kk, j0:j0 + cw], rhs=c.xT[:, kk, g * 512:(g + 1) * 512],
                                    start=(kk == 0), stop=(kk == 7)),
                                    reads=[wkey, ('xT', 0)], writes=[('ps', bank)], inc=(kk == 7))
                            evac(sg[0:cw, g * 512:(g + 1) * 512], ps[0:cw, :], bank, skey, scale=scale)
                        k.dma('sp', dst_fn((s0 + j0) // cw), sg[0:cw, :], reads=[skey])
                jobs.append((col0 + s0, sw, run))

        def tm_group(col0, ncols, dst_fn, bf, func=None, tokens=None, nblk=None):
            def run(wt, wkey):
                for b in range(nblk if nblk is not None else NB):
                    tok = tokens(b) if tokens is not None else slice(b * 128, (b + 1) * 128)
                    bank = cnt['bank'] % 8
                    cnt['bank'] += 1
                    ps = c.ps[bank]
                    for kk in range(8):
                        k.op('pe', lambda e, ps=ps, kk=kk, tok=tok: e.matmul(
                            ps[:, 0:ncols], lhsT=c.xT[:, kk, tok], rhs=wt[:, kk, 0:ncols],
                            start=(kk == 0), stop=(kk == 7)),
                            reads=[wkey, ('xT', 0)], writes=[('ps', bank)], inc=(kk == 7))
                    i = cnt['tm'] % 2
                    cnt['tm'] += 1
                    sg = tstb[i] if bf else tst[i]
                    skey = ('ptstb' if bf else 'ptst', i)
                    evac(sg[:, 0:ncols], ps[:, 0:ncols], bank, skey, func=func)
                    k.dma('sp', dst_fn(b), sg[:, 0:ncols], reads=[skey])
            jobs.append((col0, ncols, run))

        def want(n):
            return only is None or n in only

        if want('a'):
            fm_group(OFF['a_qkv'], 768, 64, lambda j: d['aq'][j], True)
            fm_group(OFF['a_qkv'] + 768, 768, 64, lambda j: d['ak'][j], True)
            for g, (win, dil) in enumerate(A_PAT):
                nbc = T // (128 * dil)

                def toks(b, dil=dil, nbc=nbc):
                    r, bi = b // nbc, b % nbc
                    s0 = r + dil * 128 * bi
                    return slice(s0, s0 + dil * 127 + 1, dil)
                tm_group(OFF['a_qkv'] + 1536 + g * 256, 256, lambda b, g=g: d['av'][g, b], True, tokens=toks)
        if want('b'):
            fm_group(OFF['b_qkv'], 256, 64, lambda j: d['bq'][j], True)
            fm_group(OFF['b_qkv'] + 256, 256, 64, lambda j: d['bk'][j], True)
            tm_group(OFF['b_qkv'] + 512, 256, lambda b: d['bv'][b], True)
            fm_group(OFF['biq'], 256, 64, lambda j: d['biq'][j], True)
            fm_group(OFF['bik'], 64, 64, lambda j: d['bik'][j], True)
            tm_group(OFF['biw'], 4, lambda b: d['biw'][b], False)
        if want('c'):
            fm_group(OFF['c_in'], 896, 64, lambda j: d['cpc'][j, :, 1:T + 1], False)
        if want('d'):
            fm_group(OFF['d_z'], 256, 64, lambda j: d['dz'][j], False)
            fm_group(OFF['d_xbc'], 512, 64, lambda j: d['dxbc'][j, :, 3:T + 3], False)
            tm_group(OFF['d_dt'], 4, lambda b: d['ddt'][b], False)
        if want('g'):
            for s in range(8):
                tm_group(OFF['gates'] + s * 512, 512, lambda b, s=s: d['gsig'][b, :, s * 512:(s + 1) * 512], True,
                         func=AF.Sigmoid)
        loaded = {}
        if jobs:
            loaded[0] = wl.load(w_in, jobs[0][0], jobs[0][1])
        for ji, (jc0, jn, run) in enumerate(jobs):
            if ji + 1 < len(jobs):
                loaded[ji + 1] = wl.load(w_in, jobs[ji + 1][0], jobs[ji + 1][1])
            wt, wkey = loaded.pop(ji)
            run(wt, wkey)
        k.barrier()


def alloc_scratch(nc, c, T, debug=False):
    NB = T // 128
    kind = 'ExternalOutput' if debug else 'Internal'
    d = {}

    def dt(name, shape, dtype):
        d[name] = nc.dram_tensor(name, shape, dtype, kind=kind).ap()
    dt('aq', [12, 64, T], BF16)
    dt('ak', [12, 64, T], BF16)
    dt('av', [3, NB, 128, 256], BF16)
    dt('bq', [4, 64, T], BF16)
    dt('bk', [4, 64, T], BF16)
    dt('bv', [NB, 128, 256], BF16)
    dt('biq', [4, 64, T], BF16)
    dt('bik', [1, 64, T], BF16)
    dt('biw', [NB, 128, 4], F32)
    dt('cpc', [14, 64, T + 1], F32)
    dt('dz', [4, 64, T], F32)
    dt('dxbc', [8, 64, T + 3], F32)
    dt('ddt', [NB, 128, 4], F32)
    dt('gsig', [NB, 128, 4096], BF16)
    dt('dxs', [4, 64, T], F32)
    dt('vfirst', [4, 64, T], F32)
    dt('obr', [4, 4, 64, T], BF16)
    c.d = d


def setup_common(nc, k, c, T, st):
    c.ps = [nc.alloc_psum_tensor('ps%d' % i, [128, 512], F32) for i in range(8)]
    c.ident = st.enter_context(_sbt(nc, 'ident_sb', [128, 128], F32))
    k.dma('sp', c.ident[:, :], c.cin['ident'], writes=[('ident', 0)])
    tmp = st.enter_context(_sbt(nc, 'cst_tmp', [128, 256], F32))
    c.maskA = st.enter_context(_sbt(nc, 'maskA_sb', [128, 256], BF16))
    k.dma('sp', tmp[:, :], c.cin['maskA'], writes=[('cst_tmp', 0)])
    k.op('dve', lambda e: e.tensor_copy(out=c.maskA[:, :], in_=tmp[:, :]), reads=[('cst_tmp', 0)],
         writes=[('maskA', 0)])
    c.negmask = st.enter_context(_sbt(nc, 'negmask_sb', [128, 128], F32))
    k.dma('act', c.negmask[:, :], c.cin['negmask'], writes=[('negmask', 0)])
    c.triu = st.enter_context(_sbt(nc, 'triu_sb', [128, 128], F32))
    k.dma('pool', c.triu[:, :], c.cin['triu'], writes=[('triu', 0)])
    c.ones_f = st.enter_context(_sbt(nc, 'ones_f_sb', [128, 128], F32))
    k.dma('sp', c.ones_f[:, :], c.cin['ones_f'], writes=[('ones_f', 0)])
    c.eps5 = st.enter_context(_sbt(nc, 'eps5_sb', [128, 1], F32))
    k.dma('act', c.eps5[:, :], c.cin['eps5'], writes=[('eps', 0)])
    c.mhalf = st.enter_context(_sbt(nc, 'mhalf_sb', [128, 1], F32))
    k.dma('pool', c.mhalf[:, :], c.cin['mhalf'], writes=[('mhalf', 0)])
    c.epsgn = st.enter_context(_sbt(nc, 'epsgn_sb', [128, 1], F32))
    k.dma('sp', c.epsgn[:, :], c.cin['epsgn'], writes=[('epsgn', 0)])
    c.ones_bf = st.enter_context(_sbt(nc, 'ones_bf', [128, 128], BF16))
    k.op('dve', lambda e: e.memset(c.ones_bf[:, :], 1.0), writes=[('ones_bf', 0)])
    k.barrier()


def mixer_a(k, c, T):
    nc = k.nc
    NB = T // 128
    d = c.d
    with ExitStack() as st:
        vall = st.enter_context(_sbt(nc, 'a_v', [128, 3, NB, 256], BF16))
        for g in range(3):
            k.dma(('sp', 'act', 'pool')[g], vall[:, g, :, :], d['av'][g].rearrange('b p c -> p b c'),
                  writes=[('a_v', g)])
        qk = [[st.enter_context(_sbt(nc, 'a_qk%d%d' % (g, s), [64, T], BF16)) for s in range(2)]
              for g in range(3)]
        uz = st.enter_context(_sbt(nc, 'a_uz', [64, 2, T], F32))
        rz = st.enter_context(_sbt(nc, 'a_rz', [64, T], F32))
        ob = st.enter_context(_sbt(nc, 'a_ob', [64, T], BF16))
        Eb = [st.enter_context(_sbt(nc, 'a_E%d' % i, [128, 256], BF16)) for i in range(2)]
        Pb = [st.enter_context(_sbt(nc, 'a_P%d' % i, [128, 256], BF16)) for i in range(2)]
        it = 0
        for h in range(4):
            for g in range(3):
                k.dma('sp', qk[g][0][:, :], d['aq'][g * 4 + h], writes=[('a_q', g)])
                k.dma('act', qk[g][1][:, :], d['ak'][g * 4 + h], writes=[('a_k', g)])
            for g, (win, dil) in enumerate(A_PAT):
                nbc = T // (128 * dil)
                qT, kT = qk[g]
                for blk in range(NB):
                    r, bi = blk // nbc, blk % nbc
                    s0 = r + dil * 128 * bi
                    qs = slice(s0, s0 + dil * 127 + 1, dil)
                    bs, bu = it % 4, 4 + it % 4
                    ps_s, ps_u = c.ps[bs], c.ps[bu]
                    i2 = it % 2
                    it += 1
                    lo = 128 if bi == 0 else 0
                    tiles = ([] if bi == 0 else [(0, blk - 1, s0 - dil * 128)]) + [(1, blk, s0)]
                    for slot, kb, ks0 in tiles:
                        ks = slice(ks0, ks0 + dil * 127 + 1, dil)
                        k.op('pe', lambda e, slot=slot, ks=ks: e.matmul(
                            ps_s[:, slot * 128:(slot + 1) * 128], lhsT=kT[:, ks], rhs=qT[:, qs],
                            start=True, stop=True),
                            reads=[('a_q', g), ('a_k', g)], writes=[('ps', bs)])
                    E, P = Eb[i2], Pb[i2]
                    k.op('act', lambda e: e.activation(out=E[:, lo:256], in_=ps_s[:, lo:256], func=AF.Exp,
                                                       scale=0.125),
                         reads=[('ps', bs)], writes=[('a_E', i2)])
                    k.op('dve', lambda e: e.tensor_tensor(out=P[:, lo:256], in0=E[:, lo:256],
                                                          in1=c.maskA[:, lo:256], op=ALU.mult),
                         reads=[('a_E', i2), ('maskA', 0)], writes=[('a_P', i2)])
                    for ti, (slot, kb, ks0) in enumerate(tiles):
                        first, last = ti == 0, ti == len(tiles) - 1
                        k.op('pe', lambda e, slot=slot, kb=kb, first=first, last=last: e.matmul(
                            ps_u[0:64, 0:128], lhsT=vall[:, g, kb, h * 64:(h + 1) * 64],
                            rhs=P[:, slot * 128:(slot + 1) * 128], start=first, stop=last,
                            skip_group_check=True),
                            reads=[('a_P', i2), ('a_v', g)], writes=[('ps', bu)])
                        k.op('pe', lambda e, slot=slot, first=first, last=last: e.matmul(
                            ps_u[0:64, 128:256], lhsT=c.ones_bf[:, 0:64],
                            rhs=P[:, slot * 128:(slot + 1) * 128], start=False, stop=last,
                            skip_group_check=True),
                            reads=[('a_P', i2), ('ones_bf', 0)], writes=[('ps', bu)])
                    dst = uz[:, :, qs]
                    src = ps_u[0:64, 0:256].rearrange('p (a n) -> p a n', a=2)
                    if g == 0:
                        k.op('act', lambda e, dst=dst, src=src: e.copy(out=dst, in_=src),
                             reads=[('ps', bu)], writes=[('a_uz', 0)])
                    else:
                        k.op('dve', lambda e, dst=dst, src=src: e.tensor_tensor(out=dst, in0=dst, in1=src,
                                                                                op=ALU.add),
                             reads=[('ps', bu), ('a_uz', 0)], writes=[('a_uz', 0)])
            k.op('dve', lambda e: e.reciprocal(out=rz[:, :], in_=uz[:, 1, :]), reads=[('a_uz', 0)],
                 writes=[('a_rz', 0)])
            k.op('dve', lambda e: e.tensor_tensor(out=ob[:, :], in0=uz[:, 0, :], in1=rz[:, :], op=ALU.mult),
                 reads=[('a_uz', 0), ('a_rz', 0)], writes=[('a_ob', 0)])
            k.dma('sp', d['obr'][0, h], ob[:, :], reads=[('a_ob', 0)])
        k.barrier()


def make_consts():
    j = np.arange(128)[:, None]
    i = np.arange(128)[None, :]
    cs = {}
    cs['ident'] = np.eye(128, dtype=np.float32)
    cs['maskA'] = np.concatenate([(j >= i), (j <= i)], axis=1).astype(np.float32)
    cs['triu'] = (j <= i).astype(np.float32)
    cs['ones_f'] = np.ones((128, 128), np.float32)
    cs['eps5'] = np.full((128, 1), 1e-5, np.float32)
    cs['mhalf'] = np.full((128, 1), -0.5, np.float32)
    cs['epsgn'] = np.full((128, 1), 64e-5, np.float32)
    s_ = np.arange(64)[:, None]
    t_ = np.arange(64)[None, :]
    cs['cmask'] = np.stack([(s_ < t_), (s_ <= t_), (s_ > t_)], axis=1).astype(np.float32)
    cs['negmask'] = np.where(i <= j, 0.0, -1.0e30).astype(np.float32)
    return cs


NEG = -1.0e30


def mixer_b(k, c, T):
    nc = k.nc
    NB = T // 128
    d = c.d
    with ExitStack() as st:
        def sb(name, shape, dt_):
            return st.enter_context(_sbt(nc, name, shape, dt_))
        bqs = [sb('b_q%d' % i, [64, 4, 128], BF16) for i in range(2)]
        bk = sb('b_k', [64, 4, T], BF16)
        biqs = [sb('b_iq%d' % i, [64, 4, 128], BF16) for i in range(2)]
        bik = sb('b_ik', [64, T], BF16)
        bv = sb('b_v', [128, NB, 256], BF16)
        biw = sb('b_iw', [128, NB, 4], F32)
        scores = [sb('b_score%d' % i, [128, T], F32) for i in range(2)]
        works = [sb('b_work%d' % i, [128, T], F32) for i in range(2)]
        cums = [sb('b_cum%d' % i, [128, T], F32) for i in range(2)]
        ngts = [sb('b_ngt%d' % i, [128, 2], F32) for i in range(2)]
        selTs = [sb('b_selT%d' % i, [128, NB, 128], BF16) for i in range(2)]
        rt = [sb('b_rt%d' % i, [128, 512], F32) for i in range(2)]
        m8s = [[sb('b_m8%d_%d' % (j, i), [128, 8], F32) for i in range(2)] for j in range(2)]
        Eb = [sb('b_E%d' % i, [128, 512], BF16) for i in range(2)]
        Pb = [sb('b_P%d' % i, [128, 512], BF16) for i in range(2)]
        rz = sb('b_rz', [64, 128], F32)
        ob = [sb('b_ob%d' % i, [64, 128], BF16) for i in range(2)]
        k.dma('act', bk[:, :, :], d['bk'].rearrange('h p t -> p h t'), writes=[('b_k', 0)])
        k.dma('sp', bik[:, :], d['bik'][0], writes=[('b_ik', 0)])
        k.dma('act', bv[:, :, :], d['bv'].rearrange('b p c -> p b c'), writes=[('b_v', 0)])
        k.dma('pool', biw[:, :, :], d['biw'].rearrange('b p c -> p b c'), writes=[('b_iw', 0)])
        cn = {'bank': 0, 'rt': 0, 'ep': 0, 'ob': 0}

        def nbank():
            b = cn['bank'] % 8
            cn['bank'] += 1
            return b
        def stage1(qb):
            par = qb % 2
            score, work, cum, ngt, selT, m8 = scores[par], works[par], cums[par], ngts[par], selTs[par], m8s[par]
            L = 128 * (qb + 1)
            qs = slice(qb * 128, (qb + 1) * 128)
            bq, biq = bqs[qb % 2], biqs[qb % 2]
            k.dma('sp', bq[:, :, :], d['bq'][:, :, qs].rearrange('h p t -> p h t'), writes=[('b_q', qb % 2)])
            k.dma('act', biq[:, :, :], d['biq'][:, :, qs].rearrange('h p t -> p h t'), writes=[('b_iq', qb % 2)])
            for kg in range((L + 511) // 512):
                n = min(512, L - kg * 512)
                seg = slice(kg * 512, kg * 512 + n)
                for ih in range(4):
                    bank = nbank()
                    ps = c.ps[bank]
                    k.op('pe', lambda e: e.matmul(ps[:, 0:n], lhsT=biq[:, ih, :], rhs=bik[:, seg],
                                                  start=True, stop=True),
                         reads=[('b_iq', qb % 2), ('b_ik', 0)], writes=[('ps', bank)])
                    ri = cn['rt'] % 2
                    cn['rt'] += 1
                    r_ = rt[ri]
                    k.op('act', lambda e: e.activation(out=r_[:, 0:n], in_=ps[:, 0:n], func=AF.Relu),
                         reads=[('ps', bank)], writes=[('b_rt', ri)])
                    if ih == 0:
                        k.op('dve', lambda e: e.tensor_scalar(out=score[:, seg], in0=r_[:, 0:n],
                                                              scalar1=biw[:, qb, 0:1], scalar2=None, op0=ALU.mult),
                             reads=[('b_rt', ri), ('b_iw', 0)], writes=[('b_score', par)])
                    else:
                        k.op('dve', lambda e: e.scalar_tensor_tensor(
                            out=score[:, seg], in0=r_[:, 0:n], scalar=biw[:, qb, ih:ih + 1], in1=score[:, seg],
                            op0=ALU.mult, op1=ALU.add),
                            reads=[('b_rt', ri), ('b_iw', 0), ('b_score', par)], writes=[('b_score', par)])
            yield
            k.op('dve', lambda e: e.tensor_tensor(out=score[:, qs], in0=score[:, qs], in1=c.negmask[:, :],
                                                  op=ALU.add),
                 reads=[('b_score', par), ('negmask', 0)], writes=[('b_score', par)])
            if qb >= 2:
                for r in range(32):
                    m = m8[r % 2]
                    src = score if r == 0 else work
                    k.op('dve', lambda e: e.max(out=m[:, :], in_=src[:, 0:L]),
                         reads=[('b_score', par), ('b_work', par)], writes=[('b_m8', par, r % 2)])
                    if r < 31:
                        k.op('dve', lambda e: e.match_replace(out=work[:, 0:L], in_to_replace=m[:, :],
                                                              in_values=src[:, 0:L], imm_value=NEG),
                             reads=[('b_score', par), ('b_m8', par, r % 2)], writes=[('b_work', par)])
                    yield
                thr = m8[1][:, 7:8]
                k.op('dve', lambda e: e.tensor_scalar(out=work[:, 0:L], in0=score[:, 0:L], scalar1=thr,
                                                      scalar2=0.0, op0=ALU.is_gt, op1=ALU.add,
                                                      accum_out=ngt[:, 0:1]),
                     reads=[('b_score', par), ('b_m8', par, 1)], writes=[('b_work', par), ('b_ngt', par, 0)])
                k.op('dve', lambda e: e.tensor_scalar(out=ngt[:, 1:2], in0=ngt[:, 0:1], scalar1=-1.0,
                                                      scalar2=256.0, op0=ALU.mult, op1=ALU.add),
                     reads=[('b_ngt', par, 0)], writes=[('b_ngt', par, 1)])
                k.op('dve', lambda e: e.tensor_scalar(out=work[:, 0:L], in0=score[:, 0:L], scalar1=thr,
                                                      scalar2=None, op0=ALU.is_equal),
                     reads=[('b_score', par), ('b_m8', par, 1), ('b_ngt', par, 0)], writes=[('b_work', par)])
                k.op('dve', lambda e: e.tensor_tensor_scan(out=cum[:, 0:L], data0=work[:, 0:L],
                                                           data1=work[:, 0:L], initial=0.0,
                                                           op0=ALU.add, op1=ALU.max),
                     reads=[('b_work', par)], writes=[('b_cum', par)])
                k.op('dve', lambda e: e.scalar_tensor_tensor(out=cum[:, 0:L], in0=cum[:, 0:L],
                                                             scalar=ngt[:, 1:2], in1=work[:, 0:L],
                                                             op0=ALU.is_le, op1=ALU.mult),
                     reads=[('b_work', par), ('b_cum', par), ('b_ngt', par, 1)], writes=[('b_cum', par)])
                k.op('dve', lambda e: e.scalar_tensor_tensor(out=work[:, 0:L], in0=score[:, 0:L],
                                                             scalar=thr, in1=cum[:, 0:L],
                                                             op0=ALU.is_gt, op1=ALU.add),
                     reads=[('b_score', par), ('b_cum', par), ('b_m8', par, 1)], writes=[('b_work', par)])
            else:
                k.op('dve', lambda e: e.tensor_scalar(out=work[:, 0:L], in0=score[:, 0:L],
                                                      scalar1=-1.0e29, scalar2=None, op0=ALU.is_ge),
                     reads=[('b_score', par)], writes=[('b_work', par)])
            for kb0 in range(0, qb + 1, 4):
                nk = min(4, qb + 1 - kb0)
                bank = nbank()
                ps = c.ps[bank]
                for j in range(nk):
                    kb = kb0 + j
                    k.op('pe', lambda e: e.transpose(out=ps[:, j * 128:(j + 1) * 128],
                                                     in_=work[:, kb * 128:(kb + 1) * 128], identity=c.ident[:, :]),
                         reads=[('b_work', par)], writes=[('ps', bank)])
                k.op('act', lambda e: e.copy(out=selT[:, kb0:kb0 + nk, :],
                                             in_=ps[:, 0:nk * 128].rearrange('p (a n) -> p a n', a=nk)),
                     reads=[('ps', bank)], writes=[('b_selT', par)])
            yield

        def stage2(qb):
            par = qb % 2
            selT = selTs[par]
            qs = slice(qb * 128, (qb + 1) * 128)
            bq = bqs[par]
            for h in range(4):
                bu = nbank()
                ps_u = c.ps[bu]
                for kb0 in range(0, qb + 1, 4):
                    nk = min(4, qb + 1 - kb0)
                    bs = nbank()
                    if bs == bu:
                        bs = nbank()
                    ps_s = c.ps[bs]
                    for j in range(nk):
                        kb = kb0 + j
                        k.op('pe', lambda e: e.matmul(ps_s[:, j * 128:(j + 1) * 128],
                                                      lhsT=bk[:, h, kb * 128:(kb + 1) * 128], rhs=bq[:, h, :],
                                                      start=True, stop=True),
                             reads=[('b_q', qb % 2), ('b_k', 0)], writes=[('ps', bs)])
                    ei = cn['ep'] % 2
                    cn['ep'] += 1
                    E, P = Eb[ei], Pb[ei]
                    k.op('act', lambda e: e.activation(out=E[:, 0:nk * 128], in_=ps_s[:, 0:nk * 128], func=AF.Exp,
                                                       scale=0.125),
                         reads=[('ps', bs)], writes=[('b_E', ei)])
                    k.op('dve', lambda e: e.tensor_tensor(
                        out=P[:, 0:nk * 128], in0=E[:, 0:nk * 128],
                        in1=selT[:, kb0:kb0 + nk, :].rearrange('p a n -> p (a n)'), op=ALU.mult),
                        reads=[('b_E', ei), ('b_selT', par)], writes=[('b_P', ei)])
                    for j in range(nk):
                        kb = kb0 + j
                        first = (kb == 0)
                        last = (kb == qb)
                        k.op('pe', lambda e: e.matmul(ps_u[0:64, 0:128], lhsT=bv[:, kb, h * 64:(h + 1) * 64],
                                                      rhs=P[:, j * 128:(j + 1) * 128], start=first, stop=last,
                                                      skip_group_check=True),
                             reads=[('b_P', ei), ('b_v', 0)], writes=[('ps', bu)])
                        k.op('pe', lambda e: e.matmul(ps_u[0:64, 128:256], lhsT=c.ones_bf[:, 0:64],
                                                      rhs=P[:, j * 128:(j + 1) * 128], start=False, stop=last,
                                                      skip_group_check=True),
                             reads=[('b_P', ei), ('ones_bf', 0)], writes=[('ps', bu)])
                    yield
                k.op('dve', lambda e: e.reciprocal(out=rz[:, :], in_=ps_u[0:64, 128:256]),
                     reads=[('ps', bu)], writes=[('b_rz', 0)])
                oi = cn['ob'] % 2
                cn['ob'] += 1
                o_ = ob[oi]
                k.op('dve', lambda e: e.tensor_tensor(out=o_[:, :], in0=ps_u[0:64, 0:128], in1=rz[:, :],
                                                      op=ALU.mult),
                     reads=[('ps', bu), ('b_rz', 0)], writes=[('b_ob', oi)])
                k.dma('sp' if oi == 0 else 'act', d['obr'][1, h, :, qs], o_[:, :], reads=[('b_ob', oi)])

        def run_interleaved(g1, g2):
            d1, d2 = g1 is None, g2 is None
            while not (d1 and d2):
                if not d1:
                    try:
                        next(g1)
                    except StopIteration:
                        d1 = True
                if not d2:
                    try:
                        next(g2)
                    except StopIteration:
                        d2 = True
        prev = None
        for qb in range(NB):
            run_interleaved(stage1(qb), prev)
            prev = stage2(qb)
        run_interleaved(None, prev)
        k.barrier()


def mixer_d(k, c, T, lp):
    nc = k.nc
    NB = T // 128
    d = c.d
    with ExitStack() as st:
        def sb(name, shape, dt_):
            return st.enter_context(_sbt(nc, 'sb_' + name, shape, dt_))
        BT = sb('d_BT', [64, 2, T], BF16)
        CT = sb('d_CT', [64, 2, T], BF16)
        xbar = sb('d_xbar', [128, NB, 256], BF16)
        Btok = sb('d_Btok', [128, NB, 2, 64], BF16)
        dte = sb('d_dte', [128, NB, 4], F32)
        etot = sb('d_etot', [128, NB, 4], F32)
        cw = sb('d_cw', [64, 8, 4], F32)
        cb = sb('d_cb', [64, 8], F32)
        nw = sb('d_nw', [64, 4], F32)
        dvec = sb('d_vec', [128, 12], F32)
        dt_ = sb('d_dt', [128, NB, 4], F32)
        a_tok = sb('d_atok', [128, NB, 4], F32)
        acs = sb('d_acs', [128, NB, 4], F32)
        tot = sb('d_tot', [128, NB, 4], F32)
        pre = sb('d_pre', [128, NB, 4], F32)
        Abc = sb('d_Abc', [128, 4], F32)
        k.dma('sp', cw[:, :, :], lp['d_cw'], writes=[('d_cw', 0)])
        k.dma('act', cb[:, :], lp['d_cb'], writes=[('d_cb', 0)])
        k.dma('pool', nw[:, :], lp['d_nw'], writes=[('d_nw', 0)])
        k.dma('sp', dvec[:, :], lp['d_vec'].partition_broadcast(128), writes=[('d_vec', 0)])
        k.dma('act', dt_[:, :, :], d['ddt'].rearrange('b p c -> p b c'), writes=[('d_dt', 0)])
        k.op('dve', lambda e: e.tensor_tensor(out=dt_[:, :, :], in0=dt_[:, :, :],
                                              in1=dvec[:, 0:4].unsqueeze(1).to_broadcast([128, NB, 4]), op=ALU.add),
             reads=[('d_dt', 0), ('d_vec', 0)], writes=[('d_dt', 0)])
        k.op('act', lambda e: e.activation(out=dt_[:, :, :], in_=dt_[:, :, :], func=AF.Exp),
             reads=[('d_dt', 0)], writes=[('d_dt', 0)])
        k.op('act', lambda e: e.activation(out=dt_[:, :, :], in_=dt_[:, :, :], func=AF.Ln, bias=1.0),
             reads=[('d_dt', 0)], writes=[('d_dt', 0)])
        k.op('act', lambda e: e.activation(out=Abc[:, :], in_=dvec[:, 4:8], func=AF.Exp),
             reads=[('d_vec', 0)], writes=[('d_Abc', 0)])
        k.op('dve', lambda e: e.scalar_tensor_tensor(out=a_tok[:, :, :], in0=dt_[:, :, :], scalar=-1.0,
                                                     in1=Abc[:, :].unsqueeze(1).to_broadcast([128, NB, 4]),
                                                     op0=ALU.mult, op1=ALU.mult),
             reads=[('d_dt', 0), ('d_Abc', 0)], writes=[('d_atok', 0)])
        bank = 0
        ps = c.ps[bank]
        k.op('pe', lambda e: e.matmul(ps[:, 0:NB * 4], lhsT=c.triu[:, :], rhs=a_tok[:, :, :].rearrange('p b c -> p (b c)'),
                                      start=True, stop=True),
             reads=[('d_atok', 0), ('triu', 0)], writes=[('ps', bank)])
        k.op('dve', lambda e: e.tensor_copy(out=acs[:, :, :].rearrange('p b c -> p (b c)'), in_=ps[:, 0:NB * 4]),
             reads=[('ps', bank)], writes=[('d_acs', 0)])
        bank = 1
        ps1 = c.ps[bank]
        k.op('pe', lambda e: e.matmul(ps1[:, 0:NB * 4], lhsT=c.ones_f[:, :], rhs=a_tok[:, :, :].rearrange('p b c -> p (b c)'),
                                      start=True, stop=True),
             reads=[('d_atok', 0), ('ones_f', 0)], writes=[('ps', bank)])
        k.op('dve', lambda e: e.tensor_copy(out=tot[:, :, :].rearrange('p b c -> p (b c)'), in_=ps1[:, 0:NB * 4]),
             reads=[('ps', bank)], writes=[('d_tot', 0)])
        k.op('dve', lambda e: e.tensor_tensor(out=dte[:, :, :], in0=tot[:, :, :], in1=acs[:, :, :], op=ALU.subtract),
             reads=[('d_acs', 0), ('d_tot', 0)], writes=[('d_dte', 0)])
        k.op('act', lambda e: e.activation(out=dte[:, :, :], in_=dte[:, :, :], func=AF.Exp),
             reads=[('d_dte', 0)], writes=[('d_dte', 0)])
        k.op('act', lambda e: e.activation(out=etot[:, :, :], in_=tot[:, :, :], func=AF.Exp),
             reads=[('d_tot', 0)], writes=[('d_etot', 0)])
        with ExitStack() as st2:
            xin = [st2.enter_context(_sbt(nc, 'd_xin%d' % i, [64, T + 3], F32)) for i in range(2)]
            acc = [st2.enter_context(_sbt(nc, 'd_acc%d' % i, [64, T], F32)) for i in range(2)]
            for ch in range(8):
                i = ch % 2
                xi, ac = xin[i], acc[i]
                k.op('pool', lambda e: e.memset(xi[:, 0:3], 0.0), writes=[('d_xin', i)])
                k.dma('sp' if i == 0 else 'act', xi[:, 3:T + 3], d['dxbc'][ch, :, 3:T + 3], writes=[('d_xin', i)])
                k.op('dve', lambda e: e.tensor_scalar(out=ac[:, :], in0=xi[:, 0:T], scalar1=cw[:, ch, 0:1],
                                                      scalar2=cb[:, ch:ch + 1], op0=ALU.mult, op1=ALU.add),
                     reads=[('d_xin', i), ('d_cw', 0), ('d_cb', 0)], writes=[('d_acc', i)])
                for tap in range(1, 4):
                    k.op('dve', lambda e: e.scalar_tensor_tensor(out=ac[:, :], in0=xi[:, tap:T + tap],
                                                                 scalar=cw[:, ch, tap:tap + 1], in1=ac[:, :],
                                                                 op0=ALU.mult, op1=ALU.add),
                         reads=[('d_xin', i), ('d_cw', 0), ('d_acc', i)], writes=[('d_acc', i)])
                if ch < 4:
                    k.op('act', lambda e: e.activation(out=ac[:, :], in_=ac[:, :], func=AF.Silu),
                         reads=[('d_acc', i)], writes=[('d_acc', i)])
                    k.dma('pool', d['dxs'][ch], ac[:, :], reads=[('d_acc', i)])
                    for b in range(NB):
                        bank = (b % 4) + 2
                        psx = c.ps[bank]
                        k.op('pe', lambda e: e.transpose(out=psx[:, 0:64], in_=ac[:, b * 128:(b + 1) * 128],
                                                         identity=c.ident[0:64, 0:64]),
                             reads=[('d_acc', i)], writes=[('ps', bank)])
                        k.op('dve', lambda e: e.tensor_scalar(out=xbar[:, b, ch * 64:(ch + 1) * 64], in0=psx[:, 0:64],
                                                              scalar1=dt_[:, b, ch:ch + 1], scalar2=None, op0=ALU.mult),
                             reads=[('ps', bank), ('d_dt', 0)], writes=[('d_xbar', ch)])
                elif ch < 6:
                    k.op('act', lambda e: e.activation(out=ac[:, :], in_=ac[:, :], func=AF.Silu),
                         reads=[('d_acc', i)], writes=[('d_acc', i)])
                    k.op('pool', lambda e: e.tensor_copy(out=BT[:, ch - 4, :], in_=ac[:, :]),
                         reads=[('d_acc', i)], writes=[('d_BC', ch)])
                    for b in range(NB):
                        bank = (b % 4) + 2
                        psx = c.ps[bank]
                        k.op('pe', lambda e: e.transpose(out=psx[:, 0:64], in_=ac[:, b * 128:(b + 1) * 128],
                                                         identity=c.ident[0:64, 0:64]),
                             reads=[('d_acc', i)], writes=[('ps', bank)])
                        k.op('act', lambda e: e.copy(out=Btok[:, b, ch - 4, :], in_=psx[:, 0:64]),
                             reads=[('ps', bank)], writes=[('d_Btok', ch)])
                else:
                    k.op('act', lambda e: e.activation(out=CT[:, ch - 6, :], in_=ac[:, :], func=AF.Silu),
                         reads=[('d_acc', i)], writes=[('d_BC', ch)])
            k.barrier()
        with ExitStack() as st3:
            def sb3(name, shape, dt2):
                return st3.enter_context(_sbt(nc, 'sb_' + name, shape, dt2))
            arg = [sb3('d_arg%d' % i, [128, 4, 128], F32) for i in range(2)]
            Dm = [sb3('d_Dm%d' % i, [128, 4, 128], F32) for i in range(2)]
            MT = [sb3('d_MT%d' % i, [128, 4, 128], BF16) for i in range(2)]
            ecs = [sb3('d_ecs%d' % i, [64, 4, 128], F32) for i in range(2)]
            Cd = [sb3('d_Cd%d' % i, [64, 4, 128], F32) for i in range(2)]
            Bd = [sb3('d_Bd%d' % i, [128, 4, 64], BF16) for i in range(2)]
            zts = [sb3('d_zt%d' % i, [64, 4, 128], F32) for i in range(2)]
            xsts = [sb3('d_xst%d' % i, [64, 4, 128], F32) for i in range(2)]
            state = sb3('d_state', [64, 4, 64], F32)
            stmp = sb3('d_stmp', [64, 4, 64], F32)
            CTf = sb3('d_CTf', [64, 2, 128], F32)
            y = sb3('d_y', [64, 4, 128], F32)
            ysq = sb3('d_ysq', [64, 4, 128], F32)
            ss = sb3('d_ss', [64, 2, 128], F32)
            obs = [sb3('d_ob%d' % i, [64, 4, 128], BF16) for i in range(2)]
            k.op('dve', lambda e: e.memset(state[:, :, :], 0.0), writes=[('d_state', 0)])
            bcn = [0]

            def nbank():
                b = bcn[0] % 8
                bcn[0] += 1
                return b
            for lb in range(NB):
                ls = slice(lb * 128, (lb + 1) * 128)
                i2 = lb % 2
                zt, xst, ob = zts[i2], xsts[i2], obs[i2]
                k.dma('sp', zt[:, :, :], d['dz'][:, :, ls].rearrange('h p t -> p h t'), writes=[('d_zt', i2)])
                k.dma('act', xst[:, :, :], d['dxs'][:, :, ls].rearrange('h p t -> p h t'), writes=[('d_xst', i2)])
                bank = nbank()
                psb = c.ps[bank]
                for h in range(4):
                    k.op('pe', lambda e: e.matmul(psb[:, h * 128:(h + 1) * 128],
                                                  lhsT=a_tok[:, lb, h:h + 1].to_broadcast([128, 128]),
                                                  rhs=c.triu[:, :], start=True, stop=True),
                         reads=[('d_atok', 0), ('triu', 0)], writes=[('ps', bank)])
                a_, D_, M_, ec_, Cd_, Bd_ = arg[i2], Dm[i2], MT[i2], ecs[i2], Cd[i2], Bd[i2]
                k.op('dve', lambda e: e.tensor_tensor(
                    out=a_[:, :, :], in0=psb[:, :].rearrange('p (h l) -> p h l', h=4),
                    in1=acs[:, lb, :].unsqueeze(2).to_broadcast([128, 4, 128]), op=ALU.subtract),
                    reads=[('ps', bank), ('d_acs', 0)], writes=[('d_arg', i2)])
                k.op('act', lambda e: e.activation(out=ec_[:, :, :],
                                                   in_=psb[0:64, :].rearrange('p (h l) -> p h l', h=4), func=AF.Exp),
                     reads=[('ps', bank)], writes=[('d_ecs', i2)])
                k.op('pool', lambda e: e.tensor_scalar_min(out=a_[:, :, :], in0=a_[:, :, :], scalar1=0.0),
                     reads=[('d_arg', i2)], writes=[('d_arg', i2)])
                k.op('act', lambda e: e.activation(out=D_[:, :, :], in_=a_[:, :, :], func=AF.Exp),
                     reads=[('d_arg', i2)], writes=[('d_Dm', i2)])
                k.op('pool', lambda e: e.tensor_tensor(
                    out=D_[:, :, :], in0=D_[:, :, :],
                    in1=c.triu[:, :].unsqueeze(1).to_broadcast([128, 4, 128]), op=ALU.mult),
                    reads=[('d_Dm', i2), ('triu', 0)], writes=[('d_Dm', i2)])
                bg = nbank()
                ps_g = c.ps[bg]
                for g in range(2):
                    k.op('pe', lambda e: e.matmul(ps_g[:, g * 128:(g + 1) * 128], lhsT=BT[:, g, ls],
                                                  rhs=CT[:, g, ls], start=True, stop=True),
                         reads=[('d_BC', 0)], writes=[('ps', bg)])
                for g in range(2):
                    k.op('dve', lambda e: e.tensor_tensor(
                        out=M_[:, 2 * g:2 * g + 2, :], in0=D_[:, 2 * g:2 * g + 2, :],
                        in1=ps_g[:, g * 128:(g + 1) * 128].unsqueeze(1).to_broadcast([128, 2, 128]),
                        op=ALU.mult),
                        reads=[('d_Dm', i2), ('ps', bg)], writes=[('d_MT', i2)])
                k.op('pool', lambda e: e.tensor_copy(out=CTf[:, :, :], in_=CT[:, :, ls]),
                     reads=[('d_BC', 0)], writes=[('d_CTf', 0)])
                for g in range(2):
                    k.op('pool', lambda e: e.tensor_tensor(
                        out=Cd_[:, 2 * g:2 * g + 2, :], in0=ec_[:, 2 * g:2 * g + 2, :],
                        in1=CTf[:, g, :].unsqueeze(1).to_broadcast([64, 2, 128]), op=ALU.mult),
                        reads=[('d_ecs', i2), ('d_CTf', 0)], writes=[('d_Cd', i2)])
                for g in range(2):
                    k.op('dve', lambda e: e.tensor_tensor(
                        out=Bd_[:, 2 * g:2 * g + 2, :],
                        in0=Btok[:, lb, g, :].unsqueeze(1).to_broadcast([128, 2, 64]),
                        in1=dte[:, lb, 2 * g:2 * g + 2].unsqueeze(2).to_broadcast([128, 2, 64]), op=ALU.mult),
                        reads=[('d_Btok', 0), ('d_dte', 0)], writes=[('d_Bd', i2)])
                bu = nbank()
                ps_y = c.ps[bu]
                for h in range(4):
                    k.op('pe', lambda e: e.matmul(ps_y[0:64, h * 128:(h + 1) * 128],
                                                  lhsT=xbar[:, lb, h * 64:(h + 1) * 64], rhs=M_[:, h, :],
                                                  start=(h == 0), stop=(lb == 0), skip_group_check=True),
                         reads=[('d_MT', i2), ('d_xbar', 0)], writes=[('ps', bu)])
                    if lb > 0:
                        k.op('pe', lambda e: e.matmul(ps_y[0:64, h * 128:(h + 1) * 128],
                                                      lhsT=state[:, h, :], rhs=Cd_[:, h, :],
                                                      start=False, stop=True, skip_group_check=True),
                             reads=[('d_Cd', i2), ('d_state', 0)], writes=[('ps', bu)])
                if lb < NB - 1:
                    bs_ = nbank()
                    ps_s = c.ps[bs_]
                    for h in range(4):
                        k.op('pe', lambda e: e.matmul(ps_s[0:64, h * 64:(h + 1) * 64], lhsT=Bd_[:, h, :],
                                                      rhs=xbar[:, lb, h * 64:(h + 1) * 64], start=True, stop=True),
                             reads=[('d_Bd', i2), ('d_xbar', 0)], writes=[('ps', bs_)])
                    k.op('dve', lambda e: e.tensor_tensor(
                        out=stmp[:, :, :], in0=state[:, :, :],
                        in1=etot[0:64, lb, :].unsqueeze(2).to_broadcast([64, 4, 64]), op=ALU.mult),
                        reads=[('d_state', 0), ('d_etot', 0)], writes=[('d_stmp', 0)])
                    k.op('dve', lambda e: e.tensor_tensor(
                        out=state[:, :, :], in0=stmp[:, :, :],
                        in1=ps_s[0:64, 0:256].rearrange('p (h q) -> p h q', h=4), op=ALU.add),
                        reads=[('d_stmp', 0), ('ps', bs_)], writes=[('d_state', 0)])
                for h in range(4):
                    k.op('dve', lambda e: e.scalar_tensor_tensor(out=y[:, h, :], in0=xst[:, h, :],
                                                                 scalar=dvec[0:64, 8 + h:9 + h],
                                                                 in1=ps_y[0:64, h * 128:(h + 1) * 128],
                                                                 op0=ALU.mult, op1=ALU.add),
                         reads=[('d_xst', i2), ('d_vec', 0), ('ps', bu)], writes=[('d_y', 0)])
                k.op('act', lambda e: e.activation(out=zt[:, :, :], in_=zt[:, :, :], func=AF.Silu),
                     reads=[('d_zt', i2)], writes=[('d_zt', i2)])
                k.op('dve', lambda e: e.tensor_tensor(out=y[:, :, :], in0=y[:, :, :], in1=zt[:, :, :], op=ALU.mult),
                     reads=[('d_y', 0), ('d_zt', i2)], writes=[('d_y', 0)])
                k.op('act', lambda e: e.activation(out=ysq[:, :, :], in_=y[:, :, :], func=AF.Square),
                     reads=[('d_y', 0)], writes=[('d_ysq', 0)])
                bank = nbank()
                psq = c.ps[bank]
                k.op('pe', lambda e: e.matmul(psq[0:64, :], lhsT=c.ones_f[0:64, 0:64],
                                              rhs=ysq[:, :, :].rearrange('p h l -> p (h l)'), start=True, stop=True),
                     reads=[('d_ysq', 0), ('ones_f', 0)], writes=[('ps', bank)])
                k.op('act', lambda e: e.copy(out=ysq[:, :, :].rearrange('p h l -> p (h l)'), in_=psq[0:64, :]),
                     reads=[('ps', bank)], writes=[('d_ysq', 0)])
                psv = ysq[:, :, :].rearrange('p (g a) l -> p g a l', g=2, a=2)
                k.op('dve', lambda e: e.tensor_tensor(out=ss[:, :, :], in0=psv[:, :, 0, :], in1=psv[:, :, 1, :],
                                                      op=ALU.add),
                     reads=[('d_ysq', 0)], writes=[('d_ss', 0)])
                k.op('act', lambda e: e.activation(out=ss[:, :, :], in_=ss[:, :, :], func=AF.Ln, scale=1.0 / 128,
                                                   bias=c.eps5[0:64, 0:1]),
                     reads=[('d_ss', 0), ('eps', 0)], writes=[('d_ss', 0)])
                k.op('act', lambda e: e.activation(out=ss[:, :, :], in_=ss[:, :, :], func=AF.Exp, scale=-0.5),
                     reads=[('d_ss', 0)], writes=[('d_ss', 0)])
                for g in range(2):
                    k.op('dve', lambda e: e.tensor_tensor(
                        out=y[:, 2 * g:2 * g + 2, :], in0=y[:, 2 * g:2 * g + 2, :],
                        in1=ss[:, g, :].unsqueeze(1).to_broadcast([64, 2, 128]), op=ALU.mult),
                        reads=[('d_y', 0), ('d_ss', 0)], writes=[('d_y', 0)])
                k.op('dve', lambda e: e.tensor_tensor(out=ob[:, :, :], in0=y[:, :, :],
                                                      in1=nw[:, :].unsqueeze(2).to_broadcast([64, 4, 128]), op=ALU.mult),
                     reads=[('d_y', 0), ('d_nw', 0)], writes=[('d_ob', i2)])
                k.dma('pool', d['obr'][3, :, :, ls].rearrange('h p t -> p h t'), ob[:, :, :], reads=[('d_ob', i2)])
            k.barrier()


def mixer_c(k, c, T, lp, layer):
    nc = k.nc
    d = c.d
    N = 256
    NCH = N // 64
    NG = T // N
    with ExitStack() as st:
        def sb(name, shape, dt_=F32):
            return st.enter_context(_sbt(nc, 'c_' + name, shape, dt_))
        p64 = sb('p64', [64, 46])
        wa2 = sb('wa2', [64, 256])
        g2 = sb('g2', [64, 256])
        k.dma('sp', p64[:, :], lp['c_p64'], writes=[('c_par', 0)])
        k.dma('act', wa2[:, :], lp['c_wa2'], writes=[('c_par', 1)])
        k.dma('pool', g2[:, :], lp['c_g2'], writes=[('c_par', 2)])
        if layer > 0:
            v1t = sb('v1t', [64, 4, 16])
            v2 = sb('v2', [16, 256])
            k.dma('sp', v1t[:, :, :], lp['c_v1'], writes=[('c_par', 3)])
            k.dma('act', v2[:, :], lp['c_v2'], writes=[('c_par', 4)])
        mu, w0, a0, kkp, kap, rkp, gnw, gnb, v0 = (p64[:, 0:14], p64[:, 14:18], p64[:, 18:22], p64[:, 22:26],
                                                   p64[:, 26:30], p64[:, 30:34], p64[:, 34:38], p64[:, 38:42],
                                                   p64[:, 42:46])
        prm = sb('prm', [64, 8])
        k.barrier()
        k.op('dve', lambda e: e.tensor_scalar(out=prm[:, 0:4], in0=w0, scalar1=-1.0, scalar2=None, op0=ALU.mult),
             writes=[('c_prm', 0)])
        k.op('dve', lambda e: e.tensor_scalar(out=prm[:, 4:8], in0=kap, scalar1=-1.0, scalar2=1.0, op0=ALU.mult,
                                              op1=ALU.add), writes=[('c_prm', 0)])
        k.barrier()
        negw0, omka = prm[:, 0:4], prm[:, 4:8]
        msk = sb('msk', [64, 3, 64])
        k.dma('sp', msk[:, :, :], c.cin['cmask'], writes=[('c_msk', 0)])
        H = sb('H', [64, 4, 64])
        k.op('dve', lambda e: e.memset(H[:, :, :], 0.0), writes=[('c_H', 0)])
        pc = sb('pc', [64, 14, N + 1])
        pcs = sb('pcs', [64, 14, N])
        tmp = sb('tmp', [64, 14, N])
        th = sb('th', [64, N])
        sg = sb('sg', [64, N])
        e2 = sb('e2', [64, 4, N])
        lw = sb('lw', [64, 4, N])
        base = sb('base', [64, 4, NCH])
        P_ = sb('P', [64, 4, N])
        Pm1 = sb('Pm1', [64, 4, N])
        iP = sb('iP', [64, 4, N])
        a_s = sb('a_s', [64, 4, N])
        g_s = sb('g_s', [64, 4, N])
        kk = sb('kk', [64, 4, N])
        t1 = sb('t1', [64, 4, N])
        kmod = sb('kmod', [64, 4, N])
        bon = sb('bon', [64, 4, N])
        ar = sb('ar', [64, 4, NCH, 2, 64])
        bT = sb('bT', [64, 4, N])
        kT = sb('kT', [64, 4, N])
        vT = sb('vT', [64, 4, N])
        vf = sb('vf', [64, 4, N])
        vl = sb('vl', [16, N])
        tok = sb('tok', [64, NCH, 3, 4, 64])
        MAs = [sb('MA%d' % i, [64, 4, 2, 64]) for i in range(NCH)]
        MBs = [sb('MB%d' % i, [64, 4, 2, 64]) for i in range(NCH)]
        XXs = [[sb('XX%d_%d' % (j, i), [64, 4, 2, 64]) for i in range(2)] for j in range(NCH)]
        TTs = [sb('TT%d' % i, [64, 4, 64]) for i in range(NCH)]
        Xs = sb('Xs', [64, 4, 64])
        Us = sb('Us', [64, 4, 64])
        yT = sb('yT', [64, 4, N])
        dd = sb('dd', [64, 4, N])
        ob = sb('ob', [64, 4, N], BF16)
        identb = c.ident[0:64, 0:64].unsqueeze(1).to_broadcast([64, 4, 64])
        ones64 = c.ones_f[0:64, 0:64]
        bc = [0]

        def nbank():
            b = bc[0] % 8
            bc[0] += 1
            return b

        def ph(t_):
            return t_[:, :, :].rearrange('p h n -> p (h n)')

        def bcast4(col):
            return col.unsqueeze(2).to_broadcast([64, 4, N])

        def headsum(src, dst_fn):
            for half in range(2):
                bank = nbank()
                ps = c.ps[bank]
                k.op('pe', lambda e: e.matmul(ps[0:64, :], lhsT=ones64,
                                              rhs=src[:, 2 * half:2 * half + 2, :].rearrange('p h n -> p (h n)'),
                                              start=True, stop=True),
                     reads=[('c_w', id(src))], writes=[('ps', bank)])
                dst_fn(half, ps[0:64, :].rearrange('p (h n) -> p h n', h=2), bank)

        def W(t_):
            return [('c_w', id(t_))]

        for gi in range(NG):
            t0 = gi * N
            ts = slice(t0, t0 + N)
            if gi == 0:
                k.dma('sp', pc[:, :, 1:N + 1], d['cpc'][:, :, 1:N + 1].rearrange('g p t -> p g t'), writes=W(pc))
                k.op('dve', lambda e: e.memset(pc[:, :, 0:1], 0.0), writes=W(pc))
            else:
                k.dma('sp', pc[:, :, :], d['cpc'][:, :, t0:t0 + N + 1].rearrange('g p t -> p g t'), writes=W(pc))
            k.op('dve', lambda e: e.tensor_tensor(out=tmp[:, :, :], in0=pc[:, :, 0:N], in1=pc[:, :, 1:N + 1],
                                                  op=ALU.subtract), reads=W(pc), writes=W(tmp))
            k.op('pool', lambda e: e.tensor_tensor(out=tmp[:, :, :], in0=tmp[:, :, :],
                                                   in1=mu.unsqueeze(2).to_broadcast([64, 14, N]), op=ALU.mult),
                 reads=W(tmp), writes=W(tmp))
            k.op('dve', lambda e: e.tensor_tensor(out=pcs[:, :, :], in0=tmp[:, :, :], in1=pc[:, :, 1:N + 1],
                                                  op=ALU.add), reads=W(tmp) + W(pc), writes=W(pcs))
            r_, k_, v_ = pcs[:, 0:4, :], pcs[:, 4:8, :], pcs[:, 8:12, :]
            k.op('act', lambda e: e.activation(out=th[0:32, :], in_=pcs[0:32, 12, :], func=AF.Tanh),
                 reads=W(pcs), writes=W(th))
            k.op('act', lambda e: e.activation(out=sg[:, :], in_=pcs[:, 13, :], func=AF.Sigmoid),
                 reads=W(pcs), writes=W(sg))
            for half in range(2):
                bank = nbank()
                ps = c.ps[bank]
                for hh in range(2):
                    h = 2 * half + hh
                    k.op('pe', lambda e: e.matmul(ps[0:64, hh * N:(hh + 1) * N], lhsT=wa2[0:32, h * 64:(h + 1) * 64],
                                                  rhs=th[0:32, :], start=True, stop=True),
                         reads=W(th), writes=[('ps', bank)])
                    k.op('act', lambda e: e.activation(out=e2[:, h, :], in_=ps[0:64, hh * N:(hh + 1) * N],
                                                       func=AF.Exp, scale=-1.0, bias=negw0[:, h:h + 1]),
                         reads=[('ps', bank)], writes=W(e2))
            k.op('act', lambda e: e.activation(out=e2[:, :, :], in_=e2[:, :, :], func=AF.Ln, bias=1.0),
                 reads=W(e2), writes=W(e2))
            k.op('act', lambda e: e.activation(out=e2[:, :, :], in_=e2[:, :, :], func=AF.Exp, scale=-1.0,
                                               bias=c.mhalf[0:64, 0:1]),
                 reads=W(e2), writes=W(e2))
            for half in range(2):
                bank = nbank()
                ps = c.ps[bank]
                for hh in range(2):
                    h = 2 * half + hh
                    k.op('pe', lambda e: e.matmul(ps[0:64, hh * N:(hh + 1) * N], lhsT=wa2[32:64, h * 64:(h + 1) * 64],
                                                  rhs=pcs[32:64, 12, :], start=True, stop=True),
                         reads=W(pcs), writes=[('ps', bank)])
                    k.op('act', lambda e: e.activation(out=a_s[:, h, :], in_=ps[0:64, hh * N:(hh + 1) * N],
                                                       func=AF.Sigmoid, bias=a0[:, h:h + 1]),
                         reads=[('ps', bank)], writes=W(a_s))
            for half in range(2):
                bank = nbank()
                ps = c.ps[bank]
                for hh in range(2):
                    h = 2 * half + hh
                    k.op('pe', lambda e: e.matmul(ps[0:64, hh * N:(hh + 1) * N], lhsT=g2[:, h * 64:(h + 1) * 64],
                                                  rhs=sg[:, :], start=True, stop=True),
                         reads=W(sg), writes=[('ps', bank)])
                k.op('act', lambda e: e.copy(out=g_s[:, 2 * half:2 * half + 2, :],
                                             in_=ps[0:64, :].rearrange('p (h n) -> p h n', h=2)),
                     reads=[('ps', bank)], writes=W(g_s))
            if layer == 0:
                k.op('pool', lambda e: e.tensor_copy(out=vT[:, :, :], in_=v_), reads=W(pcs), writes=W(vT))
                k.dma('act', d['vfirst'][:, :, ts].rearrange('h p t -> p h t'), vT[:, :, :], reads=W(vT))
            else:
                k.dma('act', vf[:, :, :], d['vfirst'][:, :, ts].rearrange('h p t -> p h t'), writes=W(vf))
                bank = nbank()
                ps = c.ps[bank]
                for h in range(4):
                    k.op('pe', lambda e: e.matmul(ps[0:16, 0:N], lhsT=v1t[:, h, :], rhs=pcs[:, 8 + h, :],
                                                  start=(h == 0), stop=(h == 3)),
                         reads=W(pcs), writes=[('ps', bank)])
                k.op('act', lambda e: e.copy(out=vl[:, :], in_=ps[0:16, 0:N]), reads=[('ps', bank)], writes=W(vl))
                for half in range(2):
                    bank = nbank()
                    ps = c.ps[bank]
                    for hh in range(2):
                        h = 2 * half + hh
                        k.op('pe', lambda e: e.matmul(ps[0:64, hh * N:(hh + 1) * N], lhsT=v2[0:16, h * 64:(h + 1) * 64],
                                                      rhs=vl[:, :], start=True, stop=True),
                             reads=W(vl), writes=[('ps', bank)])
                        k.op('act', lambda e: e.activation(out=t1[:, h, :], in_=ps[0:64, hh * N:(hh + 1) * N],
                                                           func=AF.Sigmoid, bias=v0[:, h:h + 1]),
                             reads=[('ps', bank)], writes=W(t1))
                k.op('dve', lambda e: e.tensor_tensor(out=vf[:, :, :], in0=vf[:, :, :], in1=v_, op=ALU.subtract),
                     reads=W(vf) + W(pcs), writes=W(vf))
                k.op('dve', lambda e: e.tensor_tensor(out=vf[:, :, :], in0=vf[:, :, :], in1=t1[:, :, :], op=ALU.mult),
                     reads=W(vf) + W(t1), writes=W(vf))
                k.op('dve', lambda e: e.tensor_tensor(out=vT[:, :, :], in0=vf[:, :, :], in1=v_, op=ALU.add),
                     reads=W(vf) + W(pcs), writes=W(vT))
            k.op('dve', lambda e: e.tensor_tensor(out=kk[:, :, :], in0=k_, in1=bcast4(kkp), op=ALU.mult),
                 reads=W(pcs), writes=W(kk))
            k.op('act', lambda e: e.activation(out=t1[:, :, :], in_=kk[:, :, :], func=AF.Square),
                 reads=W(kk), writes=W(t1))

            def kk_norm(half, psv, bank):
                k.op('act', lambda e: e.activation(out=dd[:, 2 * half:2 * half + 2, :], in_=psv, func=AF.Sqrt),
                     reads=[('ps', bank)], writes=W(dd))
            headsum(t1, kk_norm)
            k.op('dve', lambda e: e.tensor_scalar_max(out=dd[:, :, :], in0=dd[:, :, :], scalar1=1e-12),
                 reads=W(dd), writes=W(dd))
            k.op('dve', lambda e: e.reciprocal(out=dd[:, :, :], in_=dd[:, :, :]), reads=W(dd), writes=W(dd))
            k.op('dve', lambda e: e.tensor_tensor(out=kk[:, :, :], in0=kk[:, :, :], in1=dd[:, :, :], op=ALU.mult),
                 reads=W(kk) + W(dd), writes=W(kk))
            k.op('pool', lambda e: e.tensor_tensor(out=t1[:, :, :], in0=a_s[:, :, :], in1=bcast4(kap), op=ALU.mult),
                 reads=W(a_s), writes=W(t1))
            k.op('pool', lambda e: e.tensor_tensor(out=t1[:, :, :], in0=t1[:, :, :], in1=bcast4(omka), op=ALU.add),
                 reads=W(t1), writes=W(t1))
            k.op('dve', lambda e: e.tensor_tensor(out=kmod[:, :, :], in0=k_, in1=t1[:, :, :], op=ALU.mult),
                 reads=W(pcs) + W(t1), writes=W(kmod))
            k.op('dve', lambda e: e.tensor_tensor(out=t1[:, :, :], in0=r_, in1=kmod[:, :, :], op=ALU.mult),
                 reads=W(pcs) + W(kmod), writes=W(t1))
            k.op('pool', lambda e: e.tensor_tensor(out=t1[:, :, :], in0=t1[:, :, :], in1=bcast4(rkp), op=ALU.mult),
                 reads=W(t1), writes=W(t1))

            def bon_fn(half, psv, bank):
                k.op('dve', lambda e: e.tensor_tensor(out=bon[:, 2 * half:2 * half + 2, :], in0=psv,
                                                      in1=vT[:, 2 * half:2 * half + 2, :], op=ALU.mult),
                     reads=[('ps', bank)] + W(vT), writes=W(bon))
            headsum(t1, bon_fn)
            for h in range(4):
                k.op('dve', lambda e: e.tensor_tensor_scan(out=lw[:, h, :], data0=e2[:, h, :], data1=e2[:, h, :],
                                                           initial=0.0, op0=ALU.add, op1=ALU.max),
                     reads=W(e2), writes=W(lw))
            k.op('dve', lambda e: e.memset(base[:, :, 0:1], 0.0), writes=W(base))
            k.op('dve', lambda e: e.tensor_copy(out=base[:, :, 1:NCH], in_=lw[:, :, 63:N - 1:64]),
                 reads=W(lw), writes=W(base))
            lw4 = lw[:, :, :].rearrange('p h (c s) -> p h c s', s=64)
            k.op('dve', lambda e: e.tensor_tensor(out=lw4, in0=lw4,
                                                  in1=base[:, :, :].unsqueeze(3).to_broadcast([64, 4, NCH, 64]),
                                                  op=ALU.subtract),
                 reads=W(lw) + W(base), writes=W(lw))
            k.op('act', lambda e: e.activation(out=P_[:, :, :], in_=lw[:, :, :], func=AF.Exp, scale=-1.0),
                 reads=W(lw), writes=W(P_))
            k.op('act', lambda e: e.activation(out=iP[:, :, :], in_=lw[:, :, :], func=AF.Exp),
                 reads=W(lw), writes=W(iP))
            k.op('dve', lambda e: e.tensor_tensor(out=t1[:, :, :], in0=lw[:, :, :], in1=e2[:, :, :], op=ALU.subtract),
                 reads=W(lw) + W(e2), writes=W(t1))
            k.op('act', lambda e: e.activation(out=Pm1[:, :, :], in_=t1[:, :, :], func=AF.Exp, scale=-1.0),
                 reads=W(t1), writes=W(Pm1))
            ar5 = ar[:, :, :, :, :]
            k.op('dve', lambda e: e.scalar_tensor_tensor(
                out=ar5[:, :, :, 0, :], in0=kk[:, :, :].rearrange('p h (c s) -> p h c s', s=64), scalar=-1.0,
                in1=Pm1[:, :, :].rearrange('p h (c s) -> p h c s', s=64), op0=ALU.mult, op1=ALU.mult),
                reads=W(kk) + W(Pm1), writes=W(ar))
            k.op('dve', lambda e: e.tensor_tensor(
                out=ar5[:, :, :, 1, :], in0=r_.rearrange('p h (c s) -> p h c s', s=64),
                in1=P_[:, :, :].rearrange('p h (c s) -> p h c s', s=64), op=ALU.mult),
                reads=W(pcs) + W(P_), writes=W(ar))
            k.op('dve', lambda e: e.tensor_tensor(out=bT[:, :, :], in0=kk[:, :, :], in1=a_s[:, :, :], op=ALU.mult),
                 reads=W(kk) + W(a_s), writes=W(bT))
            k.op('dve', lambda e: e.tensor_tensor(out=bT[:, :, :], in0=bT[:, :, :], in1=iP[:, :, :], op=ALU.mult),
                 reads=W(bT) + W(iP), writes=W(bT))
            k.op('dve', lambda e: e.tensor_tensor(out=kT[:, :, :], in0=kmod[:, :, :], in1=iP[:, :, :], op=ALU.mult),
                 reads=W(kmod) + W(iP), writes=W(kT))
            for ci in range(NCH):
                cs = slice(ci * 64, (ci + 1) * 64)
                for qi, src in enumerate((bT, kT, vT)):
                    bank = nbank()
                    ps = c.ps[bank]
                    for h in range(4):
                        k.op('pe', lambda e: e.transpose(out=ps[0:64, h * 64:(h + 1) * 64], in_=src[:, h, cs],
                                                         identity=c.ident[0:64, 0:64]),
                             reads=W(src), writes=[('ps', bank)])
                    eng = 'act' if qi % 2 == 0 else 'dve'
                    dst = tok[:, ci, qi, :, :]
                    srcp = ps[0:64, 0:256].rearrange('p (h n) -> p h n', h=4)
                    if eng == 'act':
                        k.op('act', lambda e: e.copy(out=dst, in_=srcp), reads=[('ps', bank)], writes=W(tok))
                    else:
                        k.op('dve', lambda e: e.tensor_copy(out=dst, in_=srcp), reads=[('ps', bank)], writes=W(tok))
            for ci in range(NCH):
                cs = slice(ci * 64, (ci + 1) * 64)
                MA, MB, XX, TT = MAs[ci], MBs[ci], XXs[ci], TTs[ci]
                bA, bB, bC = nbank(), nbank(), nbank()
                psA, psB, psC = c.ps[bA], c.ps[bB], c.ps[bC]
                for h in range(4):
                    arh = ar[:, h, ci, :, :].rearrange('p a s -> p (a s)')
                    k.op('pe', lambda e: e.matmul(psA[0:64, h * 128:(h + 1) * 128], lhsT=bT[:, h, cs], rhs=arh,
                                                  start=True, stop=True),
                         reads=W(bT) + W(ar), writes=[('ps', bA)])
                    k.op('pe', lambda e: e.matmul(psB[0:64, h * 128:(h + 1) * 128], lhsT=kT[:, h, cs], rhs=arh,
                                                  start=True, stop=True),
                         reads=W(kT) + W(ar), writes=[('ps', bB)])
                    k.op('pe', lambda e: e.matmul(psC[0:64, h * 64:(h + 1) * 64], lhsT=ar[:, h, ci, 0, :],
                                                  rhs=bT[:, h, cs], start=True, stop=True),
                         reads=W(bT) + W(ar), writes=[('ps', bC)])
                mUU = msk[:, 0:2, :].unsqueeze(1).to_broadcast([64, 4, 2, 64])
                k.op('dve', lambda e: e.tensor_tensor(out=MA[:, :, :, :],
                                                      in0=psA[0:64, :].rearrange('p (h a s) -> p h a s', h=4, a=2),
                                                      in1=mUU, op=ALU.mult),
                     reads=[('ps', bA), ('c_msk', 0)], writes=W(MA))
                k.op('dve', lambda e: e.tensor_tensor(out=MB[:, :, :, :],
                                                      in0=psB[0:64, :].rearrange('p (h a s) -> p h a s', h=4, a=2),
                                                      in1=mUU, op=ALU.mult),
                     reads=[('ps', bB), ('c_msk', 0)], writes=W(MB))
                X = XX[0]
                k.op('pool', lambda e: e.tensor_copy(out=X[:, :, 0, :], in_=MA[:, :, 0, :]), reads=W(MA), writes=W(X))
                k.op('dve', lambda e: e.tensor_tensor(out=X[:, :, 1, :],
                                                      in0=psC[0:64, 0:256].rearrange('p (h s) -> p h s', h=4),
                                                      in1=msk[:, 2, :].unsqueeze(1).to_broadcast([64, 4, 64]),
                                                      op=ALU.mult),
                     reads=[('ps', bC), ('c_msk', 0)], writes=W(X))
                k.op('pool', lambda e: e.tensor_tensor(out=TT[:, :, :], in0=MA[:, :, 0, :], in1=identb, op=ALU.add),
                     reads=W(MA), writes=W(TT))
            for it_ in range(5):
                for ci in range(NCH):
                    MA, MB, XX, TT = MAs[ci], MBs[ci], XXs[ci], TTs[ci]
                    Xo, Xn = XX[it_ % 2], XX[(it_ + 1) % 2]
                    bank = nbank()
                    ps = c.ps[bank]
                    for h in range(4):
                        k.op('pe', lambda e: e.matmul(ps[0:64, h * 128:h * 128 + 64], lhsT=Xo[:, h, 1, :],
                                                      rhs=Xo[:, h, 0, :], start=True, stop=True),
                             reads=W(Xo), writes=[('ps', bank)])
                        k.op('pe', lambda e: e.matmul(ps[0:64, h * 128 + 64:(h + 1) * 128], lhsT=Xo[:, h, 0, :],
                                                      rhs=Xo[:, h, 1, :], start=True, stop=True),
                             reads=W(Xo), writes=[('ps', bank)])
                    k.op('act', lambda e: e.copy(out=Xn[:, :, :, :],
                                                 in_=ps[0:64, :].rearrange('p (h a s) -> p h a s', h=4, a=2)),
                         reads=[('ps', bank)], writes=W(Xn))
                for ci in range(NCH):
                    MA, MB, XX, TT = MAs[ci], MBs[ci], XXs[ci], TTs[ci]
                    Xn = XX[(it_ + 1) % 2]
                    bank2 = nbank()
                    ps2 = c.ps[bank2]
                    for h in range(4):
                        k.op('pe', lambda e: e.matmul(ps2[0:64, h * 64:(h + 1) * 64], lhsT=Xn[:, h, 1, :],
                                                      rhs=TT[:, h, :], start=True, stop=True),
                             reads=W(Xn) + W(TT), writes=[('ps', bank2)])
                    k.op('dve', lambda e: e.tensor_tensor(out=TT[:, :, :], in0=TT[:, :, :],
                                                          in1=ps2[0:64, 0:256].rearrange('p (h s) -> p h s', h=4),
                                                          op=ALU.add),
                         reads=[('ps', bank2)] + W(TT), writes=W(TT))
            for ci in range(NCH):
                cs = slice(ci * 64, (ci + 1) * 64)
                MA, MB, XX, TT = MAs[ci], MBs[ci], XXs[ci], TTs[ci]
                bank = nbank()
                ps = c.ps[bank]
                for h in range(4):
                    k.op('pe', lambda e: e.matmul(ps[0:64, h * 64:(h + 1) * 64], lhsT=ar[:, h, ci, 0, :], rhs=H[:, h, :],
                                                  start=(h == 0), stop=False, skip_group_check=True),
                         reads=W(ar) + [('c_H', 0)], writes=[('ps', bank)])
                    k.op('pe', lambda e: e.matmul(ps[0:64, h * 64:(h + 1) * 64], lhsT=MB[:, h, 0, :],
                                                  rhs=tok[:, ci, 2, h, :], start=False, stop=True,
                                                  skip_group_check=True),
                         reads=W(MB) + W(tok), writes=[('ps', bank)])
                k.op('act', lambda e: e.copy(out=Xs[:, :, :], in_=ps[0:64, 0:256].rearrange('p (h s) -> p h s', h=4)),
                     reads=[('ps', bank)], writes=W(Xs))
                bank = nbank()
                ps = c.ps[bank]
                for h in range(4):
                    k.op('pe', lambda e: e.matmul(ps[0:64, h * 64:(h + 1) * 64], lhsT=TT[:, h, :], rhs=Xs[:, h, :],
                                                  start=True, stop=True),
                         reads=W(TT) + W(Xs), writes=[('ps', bank)])
                k.op('dve', lambda e: e.tensor_copy(out=Us[:, :, :],
                                                    in_=ps[0:64, 0:256].rearrange('p (h s) -> p h s', h=4)),
                     reads=[('ps', bank)], writes=W(Us))
                bank = nbank()
                ps = c.ps[bank]
                for h in range(4):
                    o_ = ps[0:64, h * 64:(h + 1) * 64]
                    k.op('pe', lambda e: e.matmul(o_, lhsT=H[:, h, :], rhs=ar[:, h, ci, 1, :], start=(h == 0),
                                                  stop=False, skip_group_check=True),
                         reads=W(ar) + [('c_H', 0)], writes=[('ps', bank)])
                    k.op('pe', lambda e: e.matmul(o_, lhsT=Us[:, h, :], rhs=MA[:, h, 1, :], start=False, stop=False,
                                                  skip_group_check=True),
                         reads=W(MA) + W(Us), writes=[('ps', bank)])
                    k.op('pe', lambda e: e.matmul(o_, lhsT=tok[:, ci, 2, h, :], rhs=MB[:, h, 1, :], start=False,
                                                  stop=True, skip_group_check=True),
                         reads=W(MB) + W(tok), writes=[('ps', bank)])
                k.op('act', lambda e: e.copy(out=yT[:, :, cs], in_=ps[0:64, 0:256].rearrange('p (h s) -> p h s', h=4)),
                     reads=[('ps', bank)], writes=W(yT))
                bank = nbank()
                ps = c.ps[bank]
                for h in range(4):
                    o_ = ps[0:64, h * 64:(h + 1) * 64]
                    k.op('pe', lambda e: e.matmul(o_, lhsT=tok[:, ci, 0, h, :], rhs=Us[:, h, :], start=(h == 0),
                                                  stop=False, skip_group_check=True),
                         reads=W(tok) + W(Us), writes=[('ps', bank)])
                    k.op('pe', lambda e: e.matmul(o_, lhsT=tok[:, ci, 1, h, :], rhs=tok[:, ci, 2, h, :], start=False,
                                                  stop=True, skip_group_check=True),
                         reads=W(tok), writes=[('ps', bank)])
                k.op('dve', lambda e: e.tensor_tensor(out=H[:, :, :], in0=H[:, :, :],
                                                      in1=ps[0:64, 0:256].rearrange('p (h s) -> p h s', h=4),
                                                      op=ALU.add),
                     reads=[('ps', bank), ('c_H', 0)], writes=[('c_H', 0)])
                pcl = P_[:, :, ci * 64 + 63:ci * 64 + 64].to_broadcast([64, 4, 64])
                k.op('dve', lambda e: e.tensor_tensor(out=H[:, :, :], in0=H[:, :, :], in1=pcl, op=ALU.mult),
                     reads=[('c_H', 0)] + W(P_), writes=[('c_H', 0)])
            def mean_fn(half, psv, bank):
                k.op('dve', lambda e: e.scalar_tensor_tensor(out=dd[:, 2 * half:2 * half + 2, :], in0=psv,
                                                             scalar=-1.0 / 64, in1=yT[:, 2 * half:2 * half + 2, :],
                                                             op0=ALU.mult, op1=ALU.add),
                     reads=[('ps', bank)] + W(yT), writes=W(dd))
            headsum(yT, mean_fn)
            k.op('act', lambda e: e.activation(out=t1[:, :, :], in_=dd[:, :, :], func=AF.Square),
                 reads=W(dd), writes=W(t1))

            def var_fn(half, psv, bank):
                k.op('act', lambda e: e.activation(out=kmod[:, 2 * half:2 * half + 2, :], in_=psv, func=AF.Ln,
                                                   scale=1.0 / 64, bias=c.epsgn[0:64, 0:1]),
                     reads=[('ps', bank)], writes=W(kmod))
            headsum(t1, var_fn)
            k.op('act', lambda e: e.activation(out=kmod[:, :, :], in_=kmod[:, :, :], func=AF.Exp, scale=-0.5),
                 reads=W(kmod), writes=W(kmod))
            k.op('dve', lambda e: e.tensor_tensor(out=dd[:, :, :], in0=dd[:, :, :], in1=kmod[:, :, :], op=ALU.mult),
                 reads=W(dd) + W(kmod), writes=W(dd))
            k.op('pool', lambda e: e.tensor_tensor(out=dd[:, :, :], in0=dd[:, :, :], in1=bcast4(gnw), op=ALU.mult),
                 reads=W(dd), writes=W(dd))
            k.op('pool', lambda e: e.tensor_tensor(out=dd[:, :, :], in0=dd[:, :, :], in1=bcast4(gnb), op=ALU.add),
                 reads=W(dd), writes=W(dd))
            k.op('dve', lambda e: e.tensor_tensor(out=dd[:, :, :], in0=dd[:, :, :], in1=bon[:, :, :], op=ALU.add),
                 reads=W(dd) + W(bon), writes=W(dd))
            k.op('dve', lambda e: e.tensor_tensor(out=ob[:, :, :], in0=dd[:, :, :], in1=g_s[:, :, :], op=ALU.mult),
                 reads=W(dd) + W(g_s), writes=W(ob))
            k.dma('pool', d['obr'][2, :, :, ts].rearrange('h p t -> p h t'), ob[:, :, :], reads=W(ob))
        k.barrier()


ALPHA = (2 * 2) ** 0.25


def ln_block(k, c, src, skey, dst, dkey, gb, gkey, tmp):
    st6, mv = tmp
    for i in range(2):
        k.op('dve', lambda e: e.bn_stats(out=st6[:, i, :], in_=src[:, i * 512:(i + 1) * 512]),
             reads=[skey], writes=[('ln_st', 0)])
    k.op('dve', lambda e: e.bn_aggr(out=mv[:, 0:2], in_=st6[:, :, :].rearrange('p a b -> p (a b)')),
         reads=[('ln_st', 0)], writes=[('ln_mv', 0)])
    k.op('act', lambda e: e.activation(out=mv[:, 2:3], in_=mv[:, 1:2], func=AF.Ln, bias=c.eps5[:, 0:1]),
         reads=[('ln_mv', 0)], writes=[('ln_mv', 1)])
    k.op('act', lambda e: e.activation(out=mv[:, 3:4], in_=mv[:, 2:3], func=AF.Exp, scale=-0.5),
         reads=[('ln_mv', 1)], writes=[('ln_mv', 2)])
    k.op('dve', lambda e: e.tensor_scalar(out=dst, in0=src[:, :], scalar1=mv[:, 0:1], scalar2=mv[:, 3:4],
                                          op0=ALU.subtract, op1=ALU.mult),
         reads=[skey, ('ln_mv', 0), ('ln_mv', 2)], writes=[dkey])
    k.op('pool', lambda e: e.tensor_tensor(out=dst, in0=dst, in1=gb[:, 0, :], op=ALU.mult),
         reads=[dkey, gkey], writes=[dkey])
    k.op('pool', lambda e: e.tensor_tensor(out=dst, in0=dst, in1=gb[:, 1, :], op=ALU.add),
         reads=[dkey, gkey], writes=[dkey])


def phase_merge(k, c, T, lp, x_dram, x1_dram):
    nc = k.nc
    NB = T // 128
    d = c.d
    with ExitStack() as st:
        def sb(name, shape, dt_=F32):
            return st.enter_context(_sbt(nc, 'm_' + name, shape, dt_))
        wb = sb('wb', [64, 16, 1024], BF16)
        wo = sb('wo', [128, 8, 1024], BF16)
        stg = [sb('stg%d' % i, [128, 4096]) for i in range(2)]
        gb = sb('gb', [128, 2, 1024])
        k.dma('sp', gb[:, 0, :], lp['ln1_g'].partition_broadcast(128), writes=[('m_gb', 0)])
        k.dma('act', gb[:, 1, :], lp['ln1_b'].partition_broadcast(128), writes=[('m_gb', 0)])
        for n in range(4):
            s_ = stg[n % 2]
            k.dma('sp' if n % 2 == 0 else 'act', s_[0:64, :].rearrange('p (a c) -> p a c', a=4),
                  lp['w_branch'][n].rearrange('(a p) c -> p a c', p=64), writes=[('m_stg', n % 2)])
            k.op('dve' if n % 2 == 0 else 'pool', lambda e: e.tensor_copy(
                out=wb[:, 4 * n:4 * n + 4, :], in_=s_[0:64, :].rearrange('p (a c) -> p a c', a=4)),
                reads=[('m_stg', n % 2)], writes=[('m_wb', 0)])
        for hf in range(2):
            s_ = stg[hf]
            k.dma('sp' if hf == 0 else 'act', s_[:, :].rearrange('p (a c) -> p a c', a=4),
                  lp['w_out'][hf * 512:(hf + 1) * 512, :].rearrange('(a p) c -> p a c', p=128),
                  writes=[('m_stg', hf)])
            k.op('dve' if hf == 0 else 'pool', lambda e: e.tensor_copy(
                out=wo[:, 4 * hf:4 * hf + 4, :], in_=s_[:, :].rearrange('p (a c) -> p a c', a=4)),
                reads=[('m_stg', hf)], writes=[('m_wo', 0)])
        ob = [sb('ob%d' % i, [64, 16, 128], BF16) for i in range(2)]
        gs = [sb('gs%d' % i, [128, 4096], BF16) for i in range(2)]
        xs = [sb('xs%d' % i, [128, 1024]) for i in range(2)]
        mg = sb('mg', [128, 1024])
        tm = sb('tm', [128, 512])
        mT = sb('mT', [128, 8, 128], BF16)
        h1 = sb('h1', [128, 1024])
        xo = [sb('xo%d' % i, [128, 1024]) for i in range(2)]
        st6 = sb('st6', [128, 2, 6])
        mv = sb('mv', [128, 4])
        bc = [0]

        def nbank():
            b = bc[0] % 8
            bc[0] += 1
            return b
        for b in range(NB):
            i2 = b % 2
            bs = slice(b * 128, (b + 1) * 128)
            k.dma('sp', ob[i2][:, :, :], d['obr'][:, :, :, bs].rearrange('n h p t -> p (n h) t'),
                  writes=[('m_ob', i2)])
            k.dma('act', gs[i2][:, :], d['gsig'][b], writes=[('m_gs', i2)])
            k.dma('pool', xs[i2][:, :], x_dram[bs, :], writes=[('m_xs', i2)])
            for n in range(4):
                for hc in range(2):
                    bank = nbank()
                    ps = c.ps[bank]
                    for h in range(4):
                        k.op('pe', lambda e: e.matmul(ps[:, :], lhsT=ob[i2][:, 4 * n + h, :],
                                                      rhs=wb[:, 4 * n + h, hc * 512:(hc + 1) * 512],
                                                      start=(h == 0), stop=(h == 3)),
                             reads=[('m_ob', i2), ('m_wb', 0)], writes=[('ps', bank)], inc=(h == 3))
                    gsl = gs[i2][:, n * 1024 + hc * 512:n * 1024 + (hc + 1) * 512]
                    msl = mg[:, hc * 512:(hc + 1) * 512]
                    if n == 0:
                        k.op('dve', lambda e: e.tensor_tensor(out=msl, in0=ps[:, :], in1=gsl, op=ALU.mult),
                             reads=[('ps', bank), ('m_gs', i2)], writes=[('m_mg', hc)])
                    else:
                        k.op('dve', lambda e: e.tensor_tensor(out=tm[:, :], in0=ps[:, :], in1=gsl, op=ALU.mult),
                             reads=[('ps', bank), ('m_gs', i2)], writes=[('m_tm', 0)])
                        k.op('pool', lambda e: e.tensor_tensor(out=msl, in0=msl, in1=tm[:, :], op=ALU.add),
                             reads=[('m_tm', 0), ('m_mg', hc)], writes=[('m_mg', hc)])
            for hc in range(2):
                bank = nbank()
                ps = c.ps[bank]
                for j in range(4):
                    ch = hc * 4 + j
                    k.op('pe', lambda e: e.transpose(out=ps[:, j * 128:(j + 1) * 128],
                                                     in_=mg[:, ch * 128:(ch + 1) * 128], identity=c.ident[:, :]),
                         reads=[('m_mg', hc)], writes=[('ps', bank)])
                k.op('act', lambda e: e.copy(out=mT[:, hc * 4:(hc + 1) * 4, :],
                                             in_=ps[:, :].rearrange('p (j n) -> p j n', j=4)),
                     reads=[('ps', bank)], writes=[('m_mT', 0)])
            for hc in range(2):
                bank = nbank()
                ps = c.ps[bank]
                for kc in range(8):
                    k.op('pe', lambda e: e.matmul(ps[:, :], lhsT=mT[:, kc, :], rhs=wo[:, kc, hc * 512:(hc + 1) * 512],
                                                  start=(kc == 0), stop=(kc == 7)),
                         reads=[('m_mT', 0), ('m_wo', 0)], writes=[('ps', bank)], inc=(kc == 7))
                k.op('dve', lambda e: e.scalar_tensor_tensor(out=h1[:, hc * 512:(hc + 1) * 512],
                                                             in0=xs[i2][:, hc * 512:(hc + 1) * 512], scalar=ALPHA,
                                                             in1=ps[:, :], op0=ALU.mult, op1=ALU.add),
                     reads=[('ps', bank), ('m_xs', i2)], writes=[('m_h1', 0)])
            ln_block(k, c, h1, ('m_h1', 0), xo[i2][:, :], ('m_xo', i2), gb, ('m_gb', 0), (st6, mv))
            k.dma('sp', x1_dram[bs, :], xo[i2][:, :], reads=[('m_xo', i2)])
        k.barrier()


def phase_moe(k, c, T, lp, x1_dram, out_dram):
    nc = k.nc
    NB = T // 128
    HT = min(T, 1024)
    NH = T // HT
    NBH = HT // 128
    NTG = HT // 512
    with ExitStack() as st:
        def sb(name, shape, dt_=F32):
            return st.enter_context(_sbt(nc, 'e_' + name, shape, dt_))
        gate = sb('gate', [128, NBH, 32])
        xTp = sb('xTp', [128, 8, HT], BF16)
        wrl = WLoader(k, st, 'e_wr', width=36, nbuf=1)
        wr, wrkey = wrl.load(lp['r_w'], 0, 36)
        lg = sb('lg', [128, 36])
        sm = sb('sm', [128, 16])
        w8 = [sb('w8%d' % i, [128, 4, 8]) for i in range(4)]
        oh = sb('oh', [128, 4])
        gb = sb('gb', [128, 2, 1024])
        rb = sb('rb', [128, 36])
        k.dma('sp', gb[:, 0, :], lp['ln2_g'].partition_broadcast(128), writes=[('e_gb', 0)])
        k.dma('act', gb[:, 1, :], lp['ln2_b'].partition_broadcast(128), writes=[('e_gb', 0)])
        k.dma('pool', rb[:, :], lp['r_bias'].partition_broadcast(128), writes=[('e_rb', 0)])
        def router():
            for b in range(NBH):
                bank = b % 8
                ps = c.ps[bank]
                for kc in range(8):
                    k.op('pe', lambda e: e.matmul(ps[:, 0:36], lhsT=xTp[:, kc, b * 128:(b + 1) * 128], rhs=wr[:, kc, 0:36],
                                                  start=(kc == 0), stop=(kc == 7)),
                         reads=[wrkey], writes=[('ps', bank)])
                R_ = [('e_r', 0)]
                k.op('dve', lambda e: e.tensor_tensor(out=lg[:, :], in0=ps[:, 0:36], in1=rb[:, :], op=ALU.add),
                     reads=[('ps', bank), ('e_rb', 0)] + R_, writes=R_)
                le = lg[:, 4:36].rearrange('p (g j) -> p g j', g=4)
                k.op('dve', lambda e: e.tensor_reduce(out=sm[:, 0:1], in_=lg[:, 0:4], axis=AX.X, op=ALU.max),
                     reads=R_, writes=R_)
                k.op('dve', lambda e: e.tensor_scalar(out=oh[:, :], in0=lg[:, 0:4], scalar1=sm[:, 0:1], scalar2=None,
                                                      op0=ALU.is_equal), reads=R_, writes=R_)
                k.op('dve', lambda e: e.tensor_scalar(out=sm[:, 1:2], in0=sm[:, 0:1], scalar1=-1.0, scalar2=None,
                                                      op0=ALU.mult), reads=R_, writes=R_)
                k.op('act', lambda e: e.activation(out=sm[:, 4:8], in_=lg[:, 0:4], func=AF.Exp, bias=sm[:, 1:2],
                                                   accum_out=sm[:, 2:3]), reads=R_, writes=R_)
                k.op('dve', lambda e: e.reciprocal(out=sm[:, 3:4], in_=sm[:, 2:3]), reads=R_, writes=R_)
                k.op('dve', lambda e: e.tensor_reduce(out=sm[:, 8:12], in_=le, axis=AX.X, op=ALU.max),
                     reads=R_, writes=R_)
                m1b = sm[:, 8:12].unsqueeze(2).to_broadcast([128, 4, 8])
                k.op('dve', lambda e: e.tensor_tensor(out=w8[0][:, :, :], in0=le, in1=m1b, op=ALU.is_equal),
                     reads=R_, writes=R_)
                k.op('dve', lambda e: e.scalar_tensor_tensor(out=w8[1][:, :, :].rearrange('p g j -> p (g j)'),
                                                             in0=w8[0][:, :, :].rearrange('p g j -> p (g j)'),
                                                             scalar=-1.0e30, in1=lg[:, 4:36],
                                                             op0=ALU.mult, op1=ALU.add), reads=R_, writes=R_)
                k.op('dve', lambda e: e.tensor_reduce(out=sm[:, 12:16], in_=w8[1][:, :, :], axis=AX.X, op=ALU.max),
                     reads=R_, writes=R_)
                m2b = sm[:, 12:16].unsqueeze(2).to_broadcast([128, 4, 8])
                k.op('dve', lambda e: e.tensor_tensor(out=w8[0][:, :, :], in0=le, in1=m2b, op=ALU.is_ge),
                     reads=R_, writes=R_)
                k.op('dve', lambda e: e.tensor_tensor(out=w8[1][:, :, :], in0=le, in1=m1b, op=ALU.subtract),
                     reads=R_, writes=R_)
                k.op('act', lambda e: e.activation(out=w8[1][:, :, :], in_=w8[1][:, :, :], func=AF.Exp),
                     reads=R_, writes=R_)
                k.op('dve', lambda e: e.tensor_tensor(out=oh[:, :], in0=oh[:, :],
                                                      in1=sm[:, 3:4].to_broadcast([128, 4]), op=ALU.mult),
                     reads=R_, writes=R_)
                k.op('dve', lambda e: e.tensor_tensor(out=sm[:, 4:8], in0=sm[:, 12:16], in1=sm[:, 8:12],
                                                      op=ALU.subtract), reads=R_, writes=R_)
                k.op('act', lambda e: e.activation(out=sm[:, 4:8], in_=sm[:, 4:8], func=AF.Exp), reads=R_, writes=R_)
                k.op('dve', lambda e: e.tensor_scalar(out=sm[:, 4:8], in0=sm[:, 4:8], scalar1=1.0, scalar2=None,
                                                      op0=ALU.add), reads=R_, writes=R_)
                k.op('dve', lambda e: e.reciprocal(out=sm[:, 4:8], in_=sm[:, 4:8]), reads=R_, writes=R_)
                k.op('dve', lambda e: e.tensor_tensor(out=sm[:, 4:8], in0=sm[:, 4:8], in1=oh[:, :], op=ALU.mult),
                     reads=R_, writes=R_)
                k.op('dve', lambda e: e.tensor_tensor(out=w8[0][:, :, :], in0=w8[0][:, :, :], in1=w8[1][:, :, :],
                                                      op=ALU.mult), reads=R_, writes=R_)
                k.op('dve', lambda e: e.tensor_tensor(out=gate[:, b, :].rearrange('p (g j) -> p g j', g=4),
                                                      in0=w8[0][:, :, :],
                                                      in1=sm[:, 4:8].unsqueeze(2).to_broadcast([128, 4, 8]),
                                                      op=ALU.mult), reads=R_, writes=R_ + [('e_gate', 0)])
            k.barrier()
        wstg = [sb('wstg%d' % i, [128, 2048]) for i in range(4)]
        wset = [[sb('wb%d_%d' % (i, j), [128, 4096], BF16) for j in range(3)] for i in range(2)]
        wcnt = [0]

        def wload(src3, seti, j, q):
            a = src3.shape[1]
            ah = a // 2
            for hf in range(2):
                si = wcnt[0] % 4
                wcnt[0] += 1
                stg_ = wstg[si]
                k.dma('sp', stg_[:, :].rearrange('p (a c) -> p a c', a=ah), src3[:, hf * ah:(hf + 1) * ah, :],
                      writes=[('e_wstg', si)])
                k.op('pool', lambda e: e.tensor_copy(out=wset[seti][j][:, hf * 2048:(hf + 1) * 2048], in_=stg_[:, :]),
                     reads=[('e_wstg', si)], writes=[('e_wset', seti, j)])
        yacc = sb('yacc', [128, NBH, 1024])
        sl = [sb('sl%d' % i, [128, 512]) for i in range(2)]
        hT = [sb('hT%d' % i, [128, 4, 512], BF16) for i in range(2)]
        xs = [sb('xs%d' % i, [128, 1024]) for i in range(2)]
        st6 = sb('st6', [128, 2, 6])
        mv = sb('mv', [128, 4])
        bc = [0]

        def nbank():
            b = bc[0] % 8
            bc[0] += 1
            return b
        for hp in range(NH):
            tb0 = hp * HT
            phase_x(k, c, x1_dram[tb0:tb0 + HT, :], HT, xT=xTp)
            router()
            for ex in range(32):
                seti = (hp * 32 + ex) % 2
                wload(lp['e_gate'][ex].rearrange('(a p) c -> p a c', p=128), seti, 0, 'sp')
                wload(lp['e_up'][ex].rearrange('(a p) c -> p a c', p=128), seti, 1, 'act')
                wload(lp['e_down'][ex].rearrange('(a p) c -> p a c', p=128), seti, 2, 'sp')
                wg = wset[seti][0][:, :].rearrange('p (a c) -> p a c', a=8)
                wu = wset[seti][1][:, :].rearrange('p (a c) -> p a c', a=8)
                wd_b = wset[seti][2][:, :].rearrange('p (a c) -> p a c', a=4)
                wgkey, wukey, wdkey = ('e_wset', seti, 0), ('e_wset', seti, 1), ('e_wset', seti, 2)
                for tg in range(NTG):
                    ts0 = tg * 512
                    hh = hT[(ex * NTG + tg) % 2]
                    hkey = ('e_hT', (ex * NTG + tg) % 2)
                    for cc in range(4):
                        bg, bu = nbank(), nbank()
                        psg, psu = c.ps[bg], c.ps[bu]
                        for kc in range(8):
                            k.op('pe', lambda e: e.matmul(psg[:, :], lhsT=wg[:, kc, cc * 128:(cc + 1) * 128],
                                                          rhs=xTp[:, kc, ts0:ts0 + 512], start=(kc == 0),
                                                          stop=(kc == 7)),
                                 reads=[wgkey], writes=[('ps', bg)], inc=(kc == 7))
                        for kc in range(8):
                            k.op('pe', lambda e: e.matmul(psu[:, :], lhsT=wu[:, kc, cc * 128:(cc + 1) * 128],
                                                          rhs=xTp[:, kc, ts0:ts0 + 512], start=(kc == 0),
                                                          stop=(kc == 7)),
                                 reads=[wukey], writes=[('ps', bu)], inc=(kc == 7))
                        s_ = sl[cc % 2]
                        k.op('act', lambda e: e.activation(out=s_[:, :], in_=psg[:, :], func=AF.Silu),
                             reads=[('ps', bg)], writes=[('e_sl', cc % 2)])
                        k.op('dve', lambda e: e.tensor_tensor(out=hh[:, cc, :], in0=s_[:, :], in1=psu[:, :],
                                                              op=ALU.mult),
                             reads=[('e_sl', cc % 2), ('ps', bu)], writes=[hkey])
                    for bl in range(4):
                        bloc = tg * 4 + bl
                        bglob = tb0 // 128 + bloc
                        for hc in range(2):
                            bank = nbank()
                            ps = c.ps[bank]
                            for cc in range(4):
                                k.op('pe', lambda e: e.matmul(ps[:, :], lhsT=hh[:, cc, bl * 128:(bl + 1) * 128],
                                                              rhs=wd_b[:, cc, hc * 512:(hc + 1) * 512],
                                                              start=(cc == 0), stop=(cc == 3)),
                                     reads=[hkey, wdkey], writes=[('ps', bank)], inc=(cc == 3))
                            ya = yacc[:, bloc, hc * 512:(hc + 1) * 512]
                            if ex == 0:
                                k.op('dve', lambda e: e.tensor_scalar(out=ya, in0=ps[:, :],
                                                                      scalar1=gate[:, bloc, ex:ex + 1], scalar2=None,
                                                                      op0=ALU.mult),
                                     reads=[('ps', bank), ('e_gate', 0)], writes=[('e_y', bloc)])
                            else:
                                k.op('dve', lambda e: e.scalar_tensor_tensor(out=ya, in0=ps[:, :],
                                                                             scalar=gate[:, bloc, ex:ex + 1], in1=ya,
                                                                             op0=ALU.mult, op1=ALU.add),
                                     reads=[('ps', bank), ('e_gate', 0), ('e_y', bloc)], writes=[('e_y', bloc)])
            for bloc in range(NBH):
                bglob = tb0 // 128 + bloc
                i2 = bloc % 2
                bs = slice(bglob * 128, (bglob + 1) * 128)
                k.dma('sp', xs[i2][:, :], x1_dram[bs, :], writes=[('e_xs', i2)])
                k.op('dve', lambda e: e.scalar_tensor_tensor(out=yacc[:, bloc, :], in0=xs[i2][:, :], scalar=ALPHA,
                                                             in1=yacc[:, bloc, :], op0=ALU.mult, op1=ALU.add),
                     reads=[('e_xs', i2), ('e_y', bloc)], writes=[('e_y', bloc)])
                ln_block(k, c, yacc[:, bloc, :], ('e_y', bloc), xs[i2][:, :], ('e_xs', i2), gb, ('e_gb', 0), (st6, mv))
                k.dma('act', out_dram[bs, :], xs[i2][:, :], reads=[('e_xs', i2)])
        k.barrier()


PARAM_SHAPES = None


def pack_params(inp):
    L = inp['w_in'].shape[0]
    f = lambda a: np.ascontiguousarray(np.asarray(a, dtype=np.float32))
    hp = lambda v: v.reshape(4, 64).T
    p = {}
    p['w_in'] = f(inp['w_in'])
    p['w_branch'] = f(inp['w_branch'])
    p['w_out'] = f(inp['w_out'])
    for n in ('ln1_g', 'ln1_b', 'ln2_g', 'ln2_b'):
        p[n] = f(inp[n]).reshape(L, 1, 1024)
    p['r_w'] = f(np.concatenate([inp['r_group'], inp['r_expert']], axis=2))
    p['r_bias'] = f(np.concatenate([inp['r_group_b'], inp['r_expert_b']], axis=1)).reshape(L, 1, 36)
    p['e_gate'] = f(inp['e_gate'])
    p['e_up'] = f(inp['e_up'])
    p['e_down'] = f(inp['e_down'])
    p64 = []
    for l in range(L):
        v0 = inp['c_v0'][max(l - 1, 0)]
        p64.append(np.concatenate([np.asarray(inp['c_mu'][l]).reshape(14, 64).T, hp(np.asarray(inp['c_w0'][l])),
                                   hp(np.asarray(inp['c_a0'][l])), hp(np.asarray(inp['c_kk'][l])),
                                   hp(np.asarray(inp['c_ka'][l])), hp(np.asarray(inp['c_rk'][l]).reshape(-1)),
                                   hp(np.asarray(inp['c_gn_w'][l])), hp(np.asarray(inp['c_gn_b'][l])),
                                   hp(np.asarray(v0))], axis=1))
    p['c_p64'] = f(np.stack(p64))
    p['c_wa2'] = f(np.concatenate([inp['c_w2'], inp['c_a2']], axis=1))
    p['c_g2'] = f(inp['c_g2'])
    p['c_v1'] = f(np.asarray(inp['c_v1']).reshape(L - 1, 4, 64, 16).transpose(0, 2, 1, 3))
    p['c_v2'] = f(inp['c_v2'])
    p['d_cw'] = f(np.asarray(inp['d_conv_w']).transpose(0, 2, 1).reshape(L, 8, 64, 4).transpose(0, 2, 1, 3))
    p['d_cb'] = f(np.asarray(inp['d_conv_b']).reshape(L, 8, 64).transpose(0, 2, 1))
    p['d_nw'] = f(np.asarray(inp['d_norm_w']).reshape(L, 4, 64).transpose(0, 2, 1))
    p['d_vec'] = f(np.concatenate([inp['d_dt_bias'], inp['d_a_log'], inp['d_skip']], axis=1)).reshape(L, 1, 12)
    return p


def build_full(pshapes, T=4096, depth=2, debug=False, phases=None):
    nc = bass.Bass("TRN2", target_bir_lowering=False)
    k = K(nc)
    c = Ctx()
    x = nc.dram_tensor("x", [T, 1024], F32, kind="ExternalInput").ap()
    y = nc.dram_tensor("y", [T, 1024], F32, kind="ExternalOutput").ap()
    P = {n: nc.dram_tensor(n, list(shp), F32, kind="ExternalInput").ap() for n, shp in pshapes.items()}
    c.cin = {n: nc.dram_tensor(n, list(v.shape), F32, kind="ExternalInput").ap() for n, v in make_consts().items()}
    alloc_scratch(nc, c, T, debug=debug)
    kind = 'ExternalOutput' if debug else 'Internal'
    x1 = nc.dram_tensor('x1s', [T, 1024], F32, kind=kind).ap()
    xmid = nc.dram_tensor('xmid', [T, 1024], F32, kind=kind).ap()
    with ExitStack() as st:
        setup_common(nc, k, c, T, st)
        for l in range(depth):
            lp = {n: P[n][l] for n in P if n not in ('c_v1', 'c_v2')}
            if l > 0:
                lp['c_v1'] = P['c_v1'][l - 1]
                lp['c_v2'] = P['c_v2'][l - 1]
            x_in = x if l == 0 else xmid
            x_out = y if l == depth - 1 else xmid
            on = lambda n: phases is None or n in phases
            if on('p'):
                with ExitStack() as st2:
                    c.xT = st2.enter_context(_sbt(nc, 'xT', [128, 8, T], BF16))
                    phase_x(k, c, x_in, T)
                    phase_p(k, c, lp['w_in'], T)
            if on('a'):
                mixer_a(k, c, T)
            if on('b'):
                mixer_b(k, c, T)
            if on('c'):
                mixer_c(k, c, T, lp, l)
            if on('d'):
                mixer_d(k, c, T, lp)
            if on('m'):
                phase_merge(k, c, T, lp, x_in, x1)
            if on('e'):
                phase_moe(k, c, T, lp, x1, x_out)
        k.barrier()
    return nc, k


def kernel(**inputs):
    x = np.asarray(inputs['x'], dtype=np.float32)
    B, T, _ = x.shape
    p = pack_params(inputs)
    pshapes = {n: v.shape for n, v in p.items()}
    nc, _k = build_full(pshapes, T=T, depth=p['w_in'].shape[0])
    cs = make_consts()
    in_maps = []
    for b in range(B):
        m = {'x': np.ascontiguousarray(x[b])}
        m.update(p)
        m.update(cs)
        in_maps.append(m)
    res = run_bass_kernel_spmd(nc, in_maps, core_ids=list(range(B)))
    return np.stack([np.asarray(r['y'], dtype=np.float32) for r in res.results], axis=0)
```
